# Optimizing a Trainium2 kernel written in Bass

```python
import math
import jax, jax.numpy as jnp
from jax import lax
import numpy as np

D_MODEL = 2048
BATCH = 4
SEQ = 2048
DEPTH = 1

RMS_EPS = 1e-6
GLA_HEADS = 4
GLA_DK = D_MODEL // 2 // GLA_HEADS
GLA_DV = D_MODEL // GLA_HEADS
GLA_QK = GLA_HEADS * GLA_DK
GLA_V = GLA_HEADS * GLA_DV
GLA_GATE_RANK = 16
GLA_TAU = 16.0
GLA_CHUNK = 64
DSA_HEADS = 16
DSA_HEAD_DIM = 128
DSA_W = DSA_HEADS * DSA_HEAD_DIM
IDX_HEADS = 8
IDX_DIM = 64
IDX_TOPK_MAX = 256
DSA_QBLOCK = 64
ROPE_THETA = 500000.0
ROPE_FRACTION = 4
D_FF = -(-8 * D_MODEL // (3 * 256)) * 256
N_MOD = 6

IN_SPLITS = (GLA_QK, GLA_QK, GLA_V, GLA_V, GLA_GATE_RANK,
             DSA_W, DSA_W, DSA_W,
             IDX_HEADS * IDX_DIM, IDX_DIM, IDX_HEADS,
             D_MODEL, D_MODEL)
IN_WIDTH = sum(IN_SPLITS)

kernel_name = "hybrid_gla_dsa_gated_merge_adaln"


def rms_norm(x, gain):
    xf = x.astype(jnp.float32)
    y = xf * lax.rsqrt(jnp.mean(xf * xf, axis=-1, keepdims=True) + RMS_EPS)
    return (y * gain.astype(jnp.float32)).astype(x.dtype)


def partial_rope(x, positions):
    d = x.shape[-1]
    rot = d // ROPE_FRACTION
    half = rot // 2
    inv_freq = jnp.power(ROPE_THETA, -jnp.arange(0, rot, 2, dtype=jnp.float32) / rot)
    ang = positions.astype(jnp.float32)[..., None] * inv_freq
    cos = jnp.cos(ang)[:, :, None, :]
    sin = jnp.sin(ang)[:, :, None, :]
    xf = x.astype(jnp.float32)
    x1, x2, rest = xf[..., :half], xf[..., half:rot], xf[..., rot:]
    out = jnp.concatenate([x1 * cos - x2 * sin, x1 * sin + x2 * cos, rest], axis=-1)
    return out.astype(x.dtype)


def gla_chunked(q, k, v, log_g):
    B, S, H, dk = q.shape
    dv = v.shape[-1]
    C = GLA_CHUNK
    N = S // C

    def chunks(a):
        return jnp.moveaxis(a.astype(jnp.float32).reshape(B, N, C, H, a.shape[-1]), 1, 0)

    qc, kc, vc = chunks(q), chunks(k), chunks(v)
    bc = jnp.cumsum(chunks(log_g), axis=2)
    causal = jnp.tril(jnp.ones((C, C), dtype=bool))[None, :, :, None, None]

    def step(state, inp):
        q_, k_, v_, b_ = inp
        diff = b_[:, :, None] - b_[:, None, :]
        decay = jnp.exp(jnp.where(causal, diff, -jnp.inf))
        attn = jnp.einsum('bihd,bjhd,bijhd->bijh', q_, k_, decay)
        o = (jnp.einsum('bijh,bjhv->bihv', attn, v_)
             + jnp.einsum('bihd,bhdv->bihv', q_ * jnp.exp(b_), state))
        b_last = b_[:, -1]
        k_dec = k_ * jnp.exp(b_last[:, None] - b_)
        state = state * jnp.exp(b_last)[..., None] + jnp.einsum('bjhd,bjhv->bhdv', k_dec, v_)
        return state, o

    state0 = jnp.zeros((B, H, dk, dv), jnp.float32)
    _, o = lax.scan(step, state0, (qc, kc, vc, bc))
    return jnp.moveaxis(o, 0, 1).reshape(B, S, H, dv)


def dsa_attention(q, k, v, iq, ik, iw):
    B, S, H, Dh = q.shape
    topk = min(IDX_TOPK_MAX, S // 4)
    QB = DSA_QBLOCK
    nblk = S // QB
    key_pos = jnp.arange(S)

    def blocks(a):
        return jnp.moveaxis(a.reshape((B, nblk, QB) + a.shape[2:]), 1, 0)

    ik32 = ik.astype(jnp.float32)

    def one_block(inp):
        qb, iqb, iwb, start = inp
        qpos = start + jnp.arange(QB)
        dots = jnp.einsum('bqhd,bsd->bqhs', iqb.astype(jnp.float32), ik32) * (IDX_DIM ** -0.5)
        score = jnp.einsum('bqh,bqhs->bqs', iwb.astype(jnp.float32), jax.nn.relu(dots))
        admissible = key_pos[None, :] <= qpos[:, None]
        score = jnp.where(admissible[None], score, -jnp.inf)
        _, idx = lax.top_k(score, topk)
        kg = jax.vmap(lambda kk, ii: kk[ii])(k, idx)
        vg = jax.vmap(lambda vv, ii: vv[ii])(v, idx)
        valid = idx <= qpos[None, :, None]
        logits = jnp.einsum('bqhd,bqkhd->bqhk', qb, kg).astype(jnp.float32) * (Dh ** -0.5)
        logits = jnp.where(valid[:, :, None, :], logits, -jnp.inf)
        p = jax.nn.softmax(logits, axis=-1)
        return jnp.einsum('bqhk,bqkhd->bqhd', p.astype(vg.dtype), vg)

    starts = jnp.arange(nblk) * QB
    out = lax.map(one_block, (blocks(q), blocks(iq), blocks(iw), starts))
    return jnp.moveaxis(out, 0, 1).reshape(B, S, H, Dh)


def hybrid_mixer(h, positions, w_in, gla_gate_up, gla_gate_bias, gla_norm_gain,
                 w_branch_gla, w_branch_dsa, w_merge_out):
    B, S, _ = h.shape
    proj = h @ w_in
    offsets = [int(o) for o in np.cumsum(IN_SPLITS)[:-1]]
    (g_q, g_k, g_v, g_r, g_lr, d_q, d_k, d_v, i_q, i_k, i_w,
     gate_a, gate_b) = jnp.split(proj, offsets, axis=-1)

    q_a = g_q.reshape(B, S, GLA_HEADS, GLA_DK) * (GLA_DK ** -0.5)
    k_a = g_k.reshape(B, S, GLA_HEADS, GLA_DK)
    v_a = g_v.reshape(B, S, GLA_HEADS, GLA_DV)
    log_g = jax.nn.log_sigmoid((g_lr @ gla_gate_up + gla_gate_bias).astype(jnp.float32)) / GLA_TAU
    o_a = gla_chunked(q_a, k_a, v_a, log_g.reshape(B, S, GLA_HEADS, GLA_DK))
    o_a = o_a * lax.rsqrt(jnp.mean(o_a * o_a, axis=-1, keepdims=True) + RMS_EPS)
    o_a = o_a * gla_norm_gain.astype(jnp.float32).reshape(GLA_HEADS, GLA_DV)
    o_a = (o_a.reshape(B, S, GLA_V) * jax.nn.silu(g_r.astype(jnp.float32))).astype(h.dtype)
    y_a = o_a @ w_branch_gla

    q_b = partial_rope(d_q.reshape(B, S, DSA_HEADS, DSA_HEAD_DIM), positions)
    k_b = partial_rope(d_k.reshape(B, S, DSA_HEADS, DSA_HEAD_DIM), positions)
    v_b = d_v.reshape(B, S, DSA_HEADS, DSA_HEAD_DIM)
    iq = partial_rope(i_q.reshape(B, S, IDX_HEADS, IDX_DIM), positions)
    ik = partial_rope(i_k[:, :, None, :], positions)[:, :, 0, :]
    iw = i_w * (IDX_HEADS ** -0.5)
    o_b = dsa_attention(q_b, k_b, v_b, iq, ik, iw).reshape(B, S, DSA_W)
    y_b = o_b @ w_branch_dsa

    merged = jax.nn.sigmoid(gate_a) * y_a + jax.nn.sigmoid(gate_b) * y_b
    return merged @ w_merge_out


def swiglu(h, w_gate_up, w_down):
    g, u = jnp.split(h @ w_gate_up, 2, axis=-1)
    return (jax.nn.silu(g) * u) @ w_down


def setup_inputs(seed: int = 0) -> dict:
    key = jax.random.key(seed)
    ks = jax.random.split(key, 17)
    f32 = jnp.float32

    def dense(k, shape, fan_in):
        return jax.random.normal(k, shape, f32) * (fan_in ** -0.5)

    x = jax.random.normal(ks[0], (BATCH, SEQ, D_MODEL), f32)
    c = jax.random.normal(ks[1], (BATCH, D_MODEL), f32)
    positions = jnp.broadcast_to(jnp.arange(SEQ, dtype=jnp.int32)[None, :], (BATCH, SEQ))
    norm1_gain = 1.0 + 0.02 * jax.random.normal(ks[2], (DEPTH, D_MODEL), f32)
    norm2_gain = 1.0 + 0.02 * jax.random.normal(ks[3], (DEPTH, D_MODEL), f32)
    w_ada = dense(ks[4], (DEPTH, D_MODEL, N_MOD * D_MODEL), D_MODEL)
    b_ada = 0.02 * jax.random.normal(ks[5], (DEPTH, N_MOD * D_MODEL), f32)
    w_in = dense(ks[6], (DEPTH, D_MODEL, IN_WIDTH), D_MODEL)
    gla_gate_up = dense(ks[7], (DEPTH, GLA_GATE_RANK, GLA_QK), GLA_GATE_RANK)
    gla_gate_bias = 0.1 * jax.random.normal(ks[8], (DEPTH, GLA_QK), f32)
    gla_norm_gain = 1.0 + 0.02 * jax.random.normal(ks[9], (DEPTH, GLA_V), f32)
    w_branch_gla = dense(ks[10], (DEPTH, GLA_V, D_MODEL), GLA_V)
    w_branch_dsa = dense(ks[11], (DEPTH, DSA_W, D_MODEL), DSA_W)
    w_merge_out = dense(ks[12], (DEPTH, D_MODEL, D_MODEL), D_MODEL)
    w_ffn_gate_up = dense(ks[13], (DEPTH, D_MODEL, 2 * D_FF), D_MODEL)
    w_ffn_down = dense(ks[14], (DEPTH, D_FF, D_MODEL), D_FF)
    final_norm_gain = 1.0 + 0.02 * jax.random.normal(ks[15], (D_MODEL,), f32)
    return {"x": x, "c": c, "positions": positions,
            "norm1_gain": norm1_gain, "norm2_gain": norm2_gain,
            "w_ada": w_ada, "b_ada": b_ada, "w_in": w_in,
            "gla_gate_up": gla_gate_up, "gla_gate_bias": gla_gate_bias,
            "gla_norm_gain": gla_norm_gain, "w_branch_gla": w_branch_gla,
            "w_branch_dsa": w_branch_dsa, "w_merge_out": w_merge_out,
            "w_ffn_gate_up": w_ffn_gate_up, "w_ffn_down": w_ffn_down,
            "final_norm_gain": final_norm_gain}


def reference(x, c, positions, norm1_gain, norm2_gain, w_ada, b_ada, w_in,
              gla_gate_up, gla_gate_bias, gla_norm_gain, w_branch_gla, w_branch_dsa,
              w_merge_out, w_ffn_gate_up, w_ffn_down, final_norm_gain):
    for layer in range(DEPTH):
        mod = jax.nn.silu(c) @ w_ada[layer] + b_ada[layer]
        sh1, sc1, g1, sh2, sc2, g2 = [m[:, None, :] for m in jnp.split(mod, N_MOD, axis=-1)]
        h = rms_norm(x, norm1_gain[layer]) * (1.0 + sc1) + sh1
        x = x + g1 * hybrid_mixer(h, positions, w_in[layer], gla_gate_up[layer],
                                  gla_gate_bias[layer], gla_norm_gain[layer],
                                  w_branch_gla[layer], w_branch_dsa[layer], w_merge_out[layer])
        h = rms_norm(x, norm2_gain[layer]) * (1.0 + sc2) + sh2
        x = x + g2 * swiglu(h, w_ffn_gate_up[layer], w_ffn_down[layer])
    return rms_norm(x, final_norm_gain)
```

```python
import math
from contextlib import ExitStack

import numpy as np
import concourse.bass as bass
import concourse.mybir as mybir
from concourse.bass_utils import run_bass_kernel_spmd

F32 = mybir.dt.float32
BF16 = mybir.dt.bfloat16
I32 = mybir.dt.int32
AF = mybir.ActivationFunctionType
ALU = mybir.AluOpType
AX = mybir.AxisListType

D = 2048
SEQ = 2048
NB = 4
TOWN = 1024
TALL = 2048
NT = 16
KC = 16
DFF = 5632
EPS = 1e-6
NEG = -1.0e30
TOPK = 256
NBISECT = 22

O_GQ, O_GK, O_GV, O_GR, O_GLR = 0, 1024, 2048, 4096, 6144
O_DQ, O_DK, O_DV = 6160, 8208, 10256
O_IQ, O_IK, O_IW = 12304, 12816, 12880
O_GA, O_GB = 12888, 14936
IN_W = 16984


class Sched:
    def __init__(self, nc, es, ndma=32):
        self.nc = nc
        self.eng = {'pe': nc.tensor, 'act': nc.scalar, 'dve': nc.vector, 'pool': nc.gpsimd, 'sp': nc.sync}
        self.semobj = {}
        for e in ['pe', 'act', 'dve', 'pool']:
            self.semobj[e] = es.enter_context(nc.semaphore('s_' + e))
        self.ndma = ndma
        for i in range(ndma):
            self.semobj[('d', i)] = es.enter_context(nc.semaphore('sd%d' % i))
        self.cnt = {k: 0 for k in self.semobj}
        self.seen = {e: {} for e in self.eng}
        self.lastw = {}
        self.readers = {}
        self.dma_rr = 0
        self.nwait = 0

    def _wait(self, e, k, v):
        if k == e and e == 'pe':
            return
        if self.seen[e].get(k, 0) >= v:
            return
        self.eng[e].wait_ge(self.semobj[k], v)
        self.seen[e][k] = v
        self.nwait += 1

    def _deps(self, e, r, w):
        for key in r:
            for k, v in self.lastw.get(key, {}).items():
                self._wait(e, k, v)
        for key in w:
            for k, v in self.lastw.get(key, {}).items():
                self._wait(e, k, v)
            for k, v in self.readers.get(key, {}).items():
                self._wait(e, k, v)

    def _record(self, ev, r, w):
        k, v = ev
        for key in r:
            d = self.readers.setdefault(key, {})
            d[k] = max(d.get(k, 0), v)
        for key in w:
            self.lastw[key] = {k: v}
            self.readers[key] = {}

    def op(self, e, fn, r=(), w=()):
        ex = [k for k in r if isinstance(k, str) and k[:2] in ('mm', 'tp', 'ax')]
        if ex:
            w = list(w) + [k for k in ex if k not in w]
        self._deps(e, r, w)
        ins = fn(self.eng[e])
        self.cnt[e] += 1
        ins.then_inc(self.semobj[e], 1)
        self._record((e, self.cnt[e]), r, w)

    def dma(self, q, out, in_, r=(), w=(), **kw):
        slot = ('d', self.dma_rr % self.ndma)
        self.dma_rr += 1
        if self.cnt[slot] > 0:
            self._wait(q, slot, self.cnt[slot])
        self._deps(q, r, w)
        ins = self.eng[q].dma_start(out=out, in_=in_, **kw)
        self.cnt[slot] += 16
        ins.then_inc(self.semobj[slot], 16)
        self._record((slot, self.cnt[slot]), r, w)

    def barrier(self):
        for e in self.eng:
            for k in self.semobj:
                if self.cnt[k] > 0:
                    self._wait(e, k, self.cnt[k])
        self.lastw = {}
        self.readers = {}


def build_program(dbg=(), stop=None):
    nc = bass.Bass("TRN2", target_bir_lowering=False)
    import os
    stop = stop or os.environ.get('KSTOP')

    def din(name, shape, dt=F32):
        return nc.dram_tensor(name, list(shape), dt, kind="ExternalInput").ap()

    def dscr(name, shape, dt=BF16):
        kind = "ExternalOutput" if name in dbg else "Internal"
        return nc.dram_tensor(name, list(shape), dt, kind=kind).ap()

    xs = din("xs", [TALL, D])
    cfm = din("cfm", [128, KC])
    posi = din("posi", [128, NT], I32)
    w_ada = din("w_ada", [D, 6 * D])
    b_ada = din("b_ada", [1, 6 * D])
    w_in = din("w_in", [D, IN_W])
    gate_up = din("gate_up", [16, 1024])
    gate_bias = din("gate_bias", [1, 1024])
    gla_gain = din("gla_gain", [1, D])
    w_ba = din("w_ba", [D, D])
    w_bd = din("w_bd", [D, D])
    w_mo = din("w_mo", [D, D])
    w_gu = din("w_gu", [D, 2 * DFF])
    w_dn = din("w_dn", [DFF, D])
    n1g = din("n1g", [128, KC])
    n2g = din("n2g", [128, KC])
    fng = din("fng", [1, D])
    cst = din("cst", [128, 1024])
    out = nc.dram_tensor("out", [TOWN, D], F32, kind="ExternalOutput").ap()

    modrow_d = dscr("modrow_d", [1, 6 * D], F32)
    GQ = dscr("GQ", [TOWN, 1024])
    GK = dscr("GK", [TALL, 1024])
    GV = dscr("GV", [TALL, 2048])
    GR = dscr("GR", [TOWN, 2048])
    DQ = dscr("DQ", [TOWN, 2048])
    DK = dscr("DK", [TALL, 2048])
    DV = dscr("DV", [TALL, 2048])
    IQ = dscr("IQ", [TOWN, 512])
    IKW = dscr("IKW", [TALL, 72], F32)
    GA = dscr("GA", [TOWN, 2048])
    GB = dscr("GB", [TOWN, 2048])
    OA = dscr("OA", [TOWN, 2048])
    MG = dscr("MG", [TOWN, 2048])
    OBT = dscr("OBT", [128, KC, TOWN]) if "OBT" in dbg else None
    MTD = dscr("MTD", [128, NT, TOWN]) if "MTD" in dbg else None

    with ExitStack() as es:
        S = Sched(nc, es)

        def sb(stack, name, shape, dt):
            return stack.enter_context(nc.sbuf_tensor(name, list(shape), dt))

        mm = [es.enter_context(nc.psum_tensor("mm%d" % i, [128, 512], F32)) for i in range(4)]
        tp = [es.enter_context(nc.psum_tensor("tp%d" % i, [128, 1024], BF16)) for i in range(2)]
        ax = [es.enter_context(nc.psum_tensor("ax%d" % i, [128, 512], F32)) for i in range(2)]
        rr = {'mm': 0, 'tp': 0, 'stg': 0, 'wb': 0}

        def next_mm():
            i = rr['mm'] % 4
            rr['mm'] += 1
            return mm[i], 'mm%d' % i

        def next_tp():
            i = rr['tp'] % 2
            rr['tp'] += 1
            return tp[i], 'tp%d' % i

        cst_t = sb(es, "cst_t", [128, 1024], F32)
        S.dma('sp', cst_t[:], cst, w=['cst'])
        identf = cst_t[:, 0:128]
        triu = cst_t[:, 128:256]
        cmask = cst_t[:, 256:384]
        ctxflag = cst_t[:, 384:385]
        ctxneg = cst_t[:, 385:386]
        invf_d = cst_t[:, 400:416]
        invf_i = cst_t[:, 416:424]
        ident = sb(es, "ident", [128, 128], BF16)
        ones_bf = sb(es, "ones_bf", [128, 128], BF16)
        S.op('dve', lambda v: v.tensor_copy(out=ident[:], in_=identf), r=['cst'], w=['ident'])
        S.op('dve', lambda v: v.memset(ones_bf[:], 1.0), w=['ones_bf'])
        modfm = sb(es, "modfm", [128, 96], F32)
        A1 = sb(es, "A1", [128, KC], F32)
        A2 = sb(es, "A2", [128, KC], F32)
        n1g_t = sb(es, "n1g_t", [128, KC], F32)
        n2g_t = sb(es, "n2g_t", [128, KC], F32)
        S.dma('sp', n1g_t[:], n1g, w=['n1g'])
        S.dma('sp', n2g_t[:], n2g, w=['n2g'])
        wbuf = [sb(es, "wbuf%d" % i, [128, KC, 512], BF16) for i in range(2)]
        stg = [sb(es, "stg%d" % i, [128, 512], BF16) for i in range(4)]
        small = sb(es, "small", [128, 64], F32)
        junk = sb(es, "junk", [128, 2048], BF16)
        glrT = sb(es, "glrT", [32, TALL], F32)

        def next_stg():
            i = rr['stg'] % 4
            rr['stg'] += 1
            return stg[i], 'stg%d' % i

        def load_w(W, r0, nk, c0, nb, q='pool'):
            i = rr['wb'] % 2
            rr['wb'] += 1
            key = 'wbuf%d' % i
            src = W[r0:r0 + nk * 128, c0:c0 + nb].rearrange("(kc p) n -> p kc n", p=128)
            S.dma(q, wbuf[i][:, 0:nk, 0:nb], src, w=[key])
            return wbuf[i], key

        def linear(actT, akey, W, c0, nb, tts, evac, r0=0, nk=KC, m=128):
            wb, wkey = load_w(W, r0, nk, c0, nb)
            for tt in tts:
                ps, pkey = next_mm()
                for kc in range(nk):
                    S.op('pe', lambda p: p.matmul(ps[0:m, 0:nb], lhsT=actT(kc, tt), rhs=wb[:, kc, 0:nb],
                                                  start=(kc == 0), stop=(kc == nk - 1)),
                         r=[akey, wkey], w=[pkey])
                evac(tt, ps, pkey)

        def rstd_from_ss(ss_ap, n, key):
            S.op('dve', lambda v: v.tensor_scalar(out=ss_ap, in0=ss_ap, scalar1=1.0 / n, scalar2=EPS,
                                                  op0=ALU.mult, op1=ALU.add), r=[key], w=[key])
            S.op('act', lambda a: a.activation(out=ss_ap, in_=ss_ap, func=AF.Sqrt), r=[key], w=[key])
            S.op('dve', lambda v: v.reciprocal(out=ss_ap, in_=ss_ap), r=[key], w=[key])

        def to_feature_major(src_tile, skey, nchunk, dst_fn, dkey, evac_eng_fn):
            for c0 in range(0, nchunk, 8):
                n = min(8, nchunk - c0)
                tps, tkey = next_tp()
                for c in range(n):
                    S.op('pe', lambda p: p.transpose(out=tps[:, c * 128:(c + 1) * 128],
                                                     in_=src_tile[:, (c0 + c) * 128:(c0 + c + 1) * 128],
                                                     identity=ident[:]),
                         r=[skey, 'ident'], w=[tkey])
                evac_eng_fn(c0, n, tps, tkey)

        with ExitStack() as pa:
            c_t = sb(pa, "c_t", [128, KC], F32)
            sT = sb(pa, "sT", [128, KC], BF16)
            brow = sb(pa, "brow", [1, 6 * D], F32)
            mrow = sb(pa, "mrow", [1, 6 * D], F32)
            S.dma('sp', c_t[:], cfm, w=['c_t'])
            S.dma('sp', brow[:], b_ada, w=['brow'])
            S.op('act', lambda a: a.activation(out=sT[:], in_=c_t[:], func=AF.Silu), r=['c_t'], w=['sT'])

            def evac_mod(cb):
                def f(tt, ps, pkey):
                    S.op('dve', lambda v: v.tensor_tensor(out=mrow[0:1, cb * 512:(cb + 1) * 512], in0=ps[0:1, :],
                                                          in1=brow[0:1, cb * 512:(cb + 1) * 512], op=ALU.add),
                         r=[pkey, 'brow'], w=['mrow'])
                return f
            for cb in range(24):
                linear(lambda kc, tt: sT[:, kc:kc + 1], 'sT', w_ada, cb * 512, 512, [0], evac_mod(cb), m=1)
            S.dma('sp', modrow_d, mrow[:], r=['mrow'], w=['modrow_d'])
            with nc.allow_non_contiguous_dma(reason="one-time 48KB relayout of the modulation vector"):
                S.dma('sp', modfm[:], modrow_d[0, :].rearrange("(j p) -> p j", p=128), r=['modrow_d'], w=['modfm'])
            S.op('dve', lambda v: v.scalar_tensor_tensor(out=A1[:], in0=modfm[:, 16:32], scalar=1.0, in1=n1g_t[:],
                                                         op0=ALU.add, op1=ALU.mult), r=['modfm', 'n1g'], w=['A1'])
            S.op('dve', lambda v: v.scalar_tensor_tensor(out=A2[:], in0=modfm[:, 64:80], scalar=1.0, in1=n2g_t[:],
                                                         op0=ALU.add, op1=ALU.mult), r=['modfm', 'n2g'], w=['A2'])
            S.barrier()
        if stop == 'A':
            return nc
        sh1 = modfm[:, 0:16]
        sh2 = modfm[:, 48:64]

        def norm_to_fm(x_tile, xkey, dstT, dkey, tcol, A, sh, akeys, xn, xnkey, ss_ap):
            S.op('act', lambda a: a.activation(out=junk[:], in_=x_tile, func=AF.Square, accum_out=ss_ap),
                 r=[xkey], w=['junk', 'small'])
            rstd_from_ss(ss_ap, D, 'small')
            S.op('dve', lambda v: v.tensor_scalar(out=xn[:], in0=x_tile, scalar1=ss_ap, scalar2=None, op0=ALU.mult),
                 r=[xkey, 'small'], w=[xnkey])

            def ev(c0, n, tps, tkey):
                for c in range(n):
                    kc = c0 + c
                    S.op('act', lambda a: a.activation(out=dstT[:, kc, tcol:tcol + 128], in_=tps[:, c * 128:(c + 1) * 128],
                                                       func=AF.Identity, scale=A[:, kc:kc + 1], bias=sh[:, kc:kc + 1]),
                         r=[tkey] + akeys, w=[dkey])
            to_feature_major(xn, xnkey, KC, None, dkey, ev)

        with ExitStack() as pbc:
            hT = sb(pbc, "hT", [128, KC, TALL], BF16)
            with ExitStack() as pb:
                xt = [sb(pb, "xt%d" % i, [128, D], F32) for i in range(2)]
                xn = [sb(pb, "xn%d" % i, [128, D], BF16) for i in range(2)]
                for tt in range(NT):
                    i = tt % 2
                    S.dma('sp' if i == 0 else 'act', xt[i][:], xs[tt * 128:(tt + 1) * 128, :], w=['xt%d' % i])
                    norm_to_fm(xt[i][:], 'xt%d' % i, hT, 'hT', tt * 128, A1, sh1, ['A1', 'modfm'],
                               xn[i], 'xn%d' % i, small[:, i:i + 1])
                S.barrier()

            with ExitStack() as pc:
                posf = sb(pc, "posf", [128, NT], F32)
                pos_i = sb(pc, "pos_i", [128, NT], I32)
                ang = sb(pc, "ang", [128, NT, 16], F32)
                kf = sb(pc, "kf", [128, NT, 16], F32)
                ki = sb(pc, "ki", [128, NT, 16], I32)
                kf2 = sb(pc, "kf2", [128, NT, 16], F32)
                sinD = sb(pc, "sinD", [128, NT, 1, 16], F32)
                cosD = sb(pc, "cosD", [128, NT, 1, 16], F32)
                sinI = sb(pc, "sinI", [128, NT, 1, 8], F32)
                cosI = sb(pc, "cosI", [128, NT, 1, 8], F32)
                ggain = sb(pc, "ggain", [128, D], F32)
                rt = [sb(pc, "rt%d" % i, [128, 4, 16], F32) for i in range(4)]
                f32stg = sb(pc, "f32stg", [128, 512], F32)
                f32stg2 = sb(pc, "f32stg2", [128, 72], F32)
                S.dma('sp', pos_i[:], posi, w=['pos_i'])
                S.dma('act', ggain[:], gla_gain[0, :].partition_broadcast(128), w=['ggain'])
                S.op('dve', lambda v: v.tensor_copy(out=posf[:], in_=pos_i[:]), r=['pos_i'], w=['posf'])
                TWO_PI = 2.0 * math.pi

                def make_tables(invf, nj, sin_t, cos_t, key):
                    for tt in range(NT):
                        S.op('dve', lambda v: v.tensor_scalar(out=ang[:, tt, 0:nj], in0=invf, scalar1=posf[:, tt:tt + 1],
                                                              scalar2=None, op0=ALU.mult), r=['cst', 'posf', 'ang'], w=['ang'])
                    a = ang[:, :, 0:nj]
                    kk = kf[:, :, 0:nj]
                    mm_ = kf2[:, :, 0:nj]
                    S.op('dve', lambda v: v.tensor_scalar(out=kk, in0=a, scalar1=1.0 / TWO_PI, scalar2=None,
                                                          op0=ALU.mult), r=['ang'], w=['kf'])
                    S.op('dve', lambda v: v.tensor_copy(out=ki[:, :, 0:nj], in_=kk), r=['kf'], w=['ki'])
                    S.op('dve', lambda v: v.tensor_copy(out=kk, in_=ki[:, :, 0:nj]), r=['ki'], w=['kf'])
                    S.op('dve', lambda v: v.scalar_tensor_tensor(out=a, in0=kk, scalar=-TWO_PI, in1=a,
                                                                 op0=ALU.mult, op1=ALU.add), r=['kf', 'ang'], w=['ang'])
                    for shift, dst in ((0.0, sin_t), (math.pi / 2, cos_t)):
                        S.op('dve', lambda v: v.tensor_scalar(out=kk, in0=a, scalar1=shift, scalar2=None,
                                                              op0=ALU.add), r=['ang', 'kf'], w=['kf'])
                        for cmp, bound, sgn in ((ALU.is_gt, math.pi, -1.0), (ALU.is_lt, -math.pi, 1.0)):
                            S.op('dve', lambda v: v.tensor_scalar(out=mm_, in0=kk, scalar1=bound, scalar2=sgn * TWO_PI,
                                                                  op0=cmp, op1=ALU.mult), r=['kf'], w=['kf2'])
                            S.op('dve', lambda v: v.tensor_tensor(out=kk, in0=kk, in1=mm_, op=ALU.add),
                                 r=['kf', 'kf2'], w=['kf'])
                        S.op('act', lambda a_: a_.activation(out=dst[:, :, 0, :], in_=kk, func=AF.Sin),
                             r=['kf'], w=[key])
                make_tables(invf_d, 16, sinD, cosD, 'tabD')
                make_tables(invf_i, 8, sinI, cosI, 'tabI')
                S.op('dve', lambda v: v.memset(glrT[:, :], 1.0), w=['glrT'])
                wb, wkey = load_w(w_in, 0, KC, O_GLR, 16)
                for tg in range(4):
                    ps, pkey = next_mm()
                    for kc in range(KC):
                        S.op('pe', lambda p: p.matmul(ps[0:16, :], lhsT=wb[:, kc, 0:16], rhs=hT[:, kc, tg * 512:(tg + 1) * 512],
                                                      start=(kc == 0), stop=(kc == KC - 1)), r=['hT', wkey], w=[pkey])
                    S.op('act', lambda a: a.activation(out=glrT[0:16, tg * 512:(tg + 1) * 512], in_=ps[0:16, :], func=AF.Identity),
                         r=[pkey], w=['glrT'])

                if stop == 'C1':
                    S.barrier()
                    return nc
                own = list(range(8, 16))
                allt = list(range(NT))
                hact = lambda kc, tt: hT[:, kc, tt * 128:(tt + 1) * 128]

                def store(dst, own_only, c0, nb):
                    def f(tt, ps, pkey):
                        st, skey = next_stg()
                        S.op('act', lambda a: a.activation(out=st[:, 0:nb], in_=ps[:, 0:nb], func=AF.Identity), r=[pkey], w=[skey])
                        row = (tt - 8 if own_only else tt) * 128
                        S.dma('sp', dst[row:row + 128, c0:c0 + nb], st[:, 0:nb], r=[skey], w=[(id(dst), tt)])
                    return f

                def store_act(dst, c0, nb, func, mul=None):
                    def f(tt, ps, pkey):
                        st, skey = next_stg()
                        if mul is None:
                            S.op('act', lambda a: a.activation(out=st[:, 0:nb], in_=ps[:, 0:nb], func=func), r=[pkey], w=[skey])
                        else:
                            S.op('act', lambda a: a.activation(out=f32stg[:, 0:nb], in_=ps[:, 0:nb], func=func), r=[pkey], w=['f32stg'])
                            S.op('dve', lambda v: v.tensor_tensor(out=st[:, 0:nb], in0=f32stg[:, 0:nb], in1=mul[:, c0:c0 + nb],
                                                                  op=ALU.mult), r=['f32stg', 'ggain'], w=[skey])
                        row = (tt - 8) * 128
                        S.dma('sp', dst[row:row + 128, c0:c0 + nb], st[:, 0:nb], r=[skey], w=[(id(dst), tt)])
                    return f

                def rope_ops(x1, x2, o1, o2, cs, sn, pkey, skey, tkey, shape):
                    t = [rt[i][:].rearrange("p a b -> p (a b)")[:, 0:shape[0] * shape[1]].rearrange("p (a b) -> p a b", b=shape[1])
                         for i in range(4)]
                    S.op('dve', lambda v: v.tensor_tensor(out=t[0], in0=x1, in1=cs, op=ALU.mult), r=[pkey, tkey], w=['rt0'])
                    S.op('dve', lambda v: v.tensor_tensor(out=t[1], in0=x2, in1=sn, op=ALU.mult), r=[pkey, tkey], w=['rt1'])
                    S.op('dve', lambda v: v.tensor_tensor(out=o1, in0=t[0], in1=t[1], op=ALU.subtract), r=['rt0', 'rt1'], w=[skey])
                    S.op('dve', lambda v: v.tensor_tensor(out=t[2], in0=x1, in1=sn, op=ALU.mult), r=[pkey, tkey], w=['rt2'])
                    S.op('dve', lambda v: v.tensor_tensor(out=t[3], in0=x2, in1=cs, op=ALU.mult), r=[pkey, tkey], w=['rt3'])
                    S.op('dve', lambda v: v.tensor_tensor(out=o2, in0=t[2], in1=t[3], op=ALU.add), r=['rt2', 'rt3'], w=[skey])

                def store_rope_d(dst, own_only, c0):
                    def f(tt, ps, pkey):
                        st, skey = next_stg()
                        S.op('act', lambda a: a.activation(out=st[:, :], in_=ps[:, :], func=AF.Identity), r=[pkey], w=[skey])
                        pv = ps[:, :].rearrange("p (h d) -> p h d", d=128)
                        sv = st[:, :].rearrange("p (h d) -> p h d", d=128)
                        rope_ops(pv[:, :, 0:16], pv[:, :, 16:32], sv[:, :, 0:16], sv[:, :, 16:32],
                                 cosD[:, tt, :, :].to_broadcast([128, 4, 16]), sinD[:, tt, :, :].to_broadcast([128, 4, 16]), pkey, skey, 'tabD', (4, 16))
                        row = (tt - 8 if own_only else tt) * 128
                        S.dma('sp', dst[row:row + 128, c0:c0 + 512], st[:, :], r=[skey], w=[(id(dst), tt)])
                    return f

                def store_iq(tt, ps, pkey):
                    st, skey = next_stg()
                    S.op('act', lambda a: a.activation(out=st[:, :], in_=ps[:, :], func=AF.Identity), r=[pkey], w=[skey])
                    pv = ps[:, :].rearrange("p (h d) -> p h d", d=64)
                    sv = st[:, :].rearrange("p (h d) -> p h d", d=64)
                    rope_ops(pv[:, :, 0:8], pv[:, :, 8:16], sv[:, :, 0:8], sv[:, :, 8:16],
                             cosI[:, tt, :, :].to_broadcast([128, 8, 8]), sinI[:, tt, :, :].to_broadcast([128, 8, 8]), pkey, skey, 'tabI', (8, 8))
                    row = (tt - 8) * 128
                    S.dma('sp', IQ[row:row + 128, :], st[:, :], r=[skey], w=[('IQ', tt)])

                def store_ikw(tt, ps, pkey):
                    S.op('act', lambda a: a.activation(out=f32stg2[:, :], in_=ps[:, 0:72], func=AF.Identity), r=[pkey], w=['f32stg2'])
                    rope_ops(ps[:, 0:8].rearrange("p (a b) -> p a b", a=1), ps[:, 8:16].rearrange("p (a b) -> p a b", a=1),
                             f32stg2[:, 0:8].rearrange("p (a b) -> p a b", a=1), f32stg2[:, 8:16].rearrange("p (a b) -> p a b", a=1),
                             cosI[:, tt, :, :], sinI[:, tt, :, :], pkey, 'f32stg2', 'tabI', (1, 8))
                    S.dma('sp', IKW[tt * 128:(tt + 1) * 128, :], f32stg2[:, :], r=['f32stg2'], w=[('IKW', tt)])

                for cb in range(2):
                    linear(hact, 'hT', w_in, O_GQ + cb * 512, 512, own, store(GQ, True, cb * 512, 512))
                if stop == 'C2':
                    S.barrier()
                    return nc
                for cb in range(2):
                    linear(hact, 'hT', w_in, O_GK + cb * 512, 512, allt, store(GK, False, cb * 512, 512))
                for cb in range(4):
                    linear(hact, 'hT', w_in, O_GV + cb * 512, 512, allt, store(GV, False, cb * 512, 512))
                for cb in range(4):
                    linear(hact, 'hT', w_in, O_GR + cb * 512, 512, own, store_act(GR, cb * 512, 512, AF.Silu, mul=ggain))
                if stop == 'C3':
                    S.barrier()
                    return nc
                for cb in range(4):
                    linear(hact, 'hT', w_in, O_DQ + cb * 512, 512, own, store_rope_d(DQ, True, cb * 512))
                if stop == 'C4':
                    S.barrier()
                    return nc
                for cb in range(4):
                    linear(hact, 'hT', w_in, O_DK + cb * 512, 512, allt, store_rope_d(DK, False, cb * 512))
                for cb in range(4):
                    linear(hact, 'hT', w_in, O_DV + cb * 512, 512, allt, store(DV, False, cb * 512, 512))
                if stop == 'C5':
                    S.barrier()
                    return nc
                linear(hact, 'hT', w_in, O_IQ, 512, own, store_iq)
                if stop == 'C6':
                    S.barrier()
                    return nc
                linear(hact, 'hT', w_in, O_IK, 72, allt, store_ikw)
                for cb in range(4):
                    linear(hact, 'hT', w_in, O_GA + cb * 512, 512, own, store_act(GA, cb * 512, 512, AF.Sigmoid))
                for cb in range(4):
                    linear(hact, 'hT', w_in, O_GB + cb * 512, 512, own, store_act(GB, cb * 512, 512, AF.Sigmoid))
                S.barrier()
                if stop == 'C':
                    return nc
        with ExitStack() as pd:
            gu_aug = sb(pd, "gu_aug", [32, 1024], F32)
            S.dma('sp', gu_aug[0:16, :], gate_up, w=['gu_aug'])
            S.dma('sp', gu_aug[16:17, :], gate_bias, w=['gu_aug'])
            Sst = sb(pd, "Sst", [128, 2, 512], F32)
            Sbf = sb(pd, "Sbf", [128, 2, 512], BF16)
            kt_ = [sb(pd, "kt%d" % i, [128, 256], BF16) for i in range(2)]
            vt_ = [sb(pd, "vt%d" % i, [128, 512], BF16) for i in range(2)]
            qt_ = [sb(pd, "qt%d" % i, [128, 256], BF16) for i in range(2)]
            gs_ = [sb(pd, "gs%d" % i, [128, 512], BF16) for i in range(2)]
            sp_ = sb(pd, "sp_", [128, 256], F32)
            Epos = sb(pd, "Epos", [128, 256], F32)
            Eneg = sb(pd, "Eneg", [128, 256], F32)
            Etok = sb(pd, "Etok", [128, 256], F32)
            ktok = sb(pd, "ktok", [128, 256], BF16)
            kT_ = sb(pd, "kT_", [128, 256], BF16)
            qT_ = sb(pd, "qT_", [128, 256], BF16)
            attnT = sb(pd, "attnT", [128, 128], BF16)
            oa_ = [sb(pd, "oa%d" % i, [128, 512], BF16) for i in range(2)]
            for h in range(4):
                S.op('dve', lambda v: v.memset(Sst[:], 0.0), w=['Sst'])
                S.op('dve', lambda v: v.memset(Sbf[:], 0.0), w=['Sbf'])
                for n in range(NT):
                    i = n % 2
                    ownt = n >= 8
                    r0 = n * 128
                    S.dma('sp', kt_[i][:], GK[r0:r0 + 128, h * 256:(h + 1) * 256], w=['kt%d' % i])
                    S.dma('act', vt_[i][:], GV[r0:r0 + 128, h * 512:(h + 1) * 512], w=['vt%d' % i])
                    if ownt:
                        q0 = (n - 8) * 128
                        S.dma('sp', qt_[i][:], GQ[q0:q0 + 128, h * 256:(h + 1) * 256], w=['qt%d' % i])
                        S.dma('act', gs_[i][:], GR[q0:q0 + 128, h * 512:(h + 1) * 512], w=['gs%d' % i])
                    ps, pk = next_mm()
                    S.op('pe', lambda p: p.matmul(ps[:, 0:256], lhsT=glrT[0:17, r0:r0 + 128], rhs=gu_aug[0:17, h * 256:(h + 1) * 256],
                                                  start=True, stop=True), r=['glrT', 'gu_aug'], w=[pk])
                    S.op('act', lambda a: a.activation(out=sp_[:], in_=ps[:, 0:256], func=AF.Exp, scale=-1.0), r=[pk], w=['sp_'])
                    S.op('act', lambda a: a.activation(out=sp_[:], in_=sp_[:], func=AF.Ln, bias=1.0), r=['sp_'], w=['sp_'])
                    ps2, pk2 = next_mm()
                    for cc in range(2):
                        S.op('pe', lambda p: p.matmul(ps2[:, cc * 128:(cc + 1) * 128], lhsT=sp_[:, cc * 128:(cc + 1) * 128], rhs=triu,
                                                      start=True, stop=True), r=['sp_', 'cst'], w=[pk2])
                    ps3, pk3 = next_mm()
                    S.op('pe', lambda p: p.matmul(ps3[:, 0:256], lhsT=triu, rhs=sp_[:], start=True, stop=True), r=['sp_', 'cst'], w=[pk3])
                    S.op('act', lambda a: a.activation(out=Epos[:], in_=ps2[:, 0:256], func=AF.Exp, scale=-1.0 / 16), r=[pk2], w=['Epos'])
                    S.op('act', lambda a: a.activation(out=Eneg[:], in_=ps2[:, 0:256], func=AF.Exp, scale=1.0 / 16), r=[pk2], w=['Eneg'])
                    S.op('act', lambda a: a.activation(out=Etok[:], in_=ps3[:, 0:256], func=AF.Exp, scale=1.0 / 16), r=[pk3], w=['Etok'])
                    tps, tk = next_tp()
                    for cc in range(2):
                        S.op('pe', lambda p: p.transpose(out=tps[:, cc * 128:(cc + 1) * 128], in_=kt_[i][:, cc * 128:(cc + 1) * 128],
                                                         identity=ident[:]), r=['kt%d' % i, 'ident'], w=[tk])
                    if ownt:
                        for cc in range(2):
                            S.op('pe', lambda p: p.transpose(out=tps[:, 256 + cc * 128:256 + (cc + 1) * 128],
                                                             in_=qt_[i][:, cc * 128:(cc + 1) * 128], identity=ident[:]),
                                 r=['qt%d' % i, 'ident'], w=[tk])
                    S.op('dve', lambda v: v.tensor_tensor(out=kT_[:], in0=tps[:, 0:256], in1=Eneg[:], op=ALU.mult),
                         r=[tk, 'Eneg'], w=['kT_'])
                    S.op('pool', lambda g: g.tensor_tensor(out=ktok[:], in0=kt_[i][:], in1=Etok[:], op=ALU.mult),
                         r=['kt%d' % i, 'Etok'], w=['ktok'])
                    if ownt:
                        S.op('dve', lambda v: v.scalar_tensor_tensor(out=qT_[:], in0=tps[:, 256:512], scalar=1.0 / 16, in1=Epos[:],
                                                                     op0=ALU.mult, op1=ALU.mult), r=[tk, 'Epos'], w=['qT_'])
                        psA, pkA = next_mm()
                        for cc in range(2):
                            S.op('pe', lambda p: p.matmul(psA[:, 0:128], lhsT=kT_[:, cc * 128:(cc + 1) * 128],
                                                          rhs=qT_[:, cc * 128:(cc + 1) * 128], start=(cc == 0), stop=(cc == 1)),
                                 r=['kT_', 'qT_'], w=[pkA])
                        S.op('dve', lambda v: v.tensor_tensor(out=attnT[:], in0=psA[:, 0:128], in1=triu, op=ALU.mult),
                             r=[pkA, 'cst'], w=['attnT'])
                        psO, pkO = next_mm()
                        S.op('pe', lambda p: p.matmul(psO[:, :], lhsT=attnT[:], rhs=vt_[i][:], start=True, stop=False),
                             r=['attnT', 'vt%d' % i], w=[pkO])
                        for cc in range(2):
                            S.op('pe', lambda p: p.matmul(psO[:, :], lhsT=qT_[:, cc * 128:(cc + 1) * 128], rhs=Sbf[:, cc, :],
                                                          start=False, stop=(cc == 1)), r=['qT_', 'Sbf'], w=[pkO])
                        ssc = small[:, 4 + i:5 + i]
                        S.op('act', lambda a: a.activation(out=junk[:, 0:512], in_=psO[:, :], func=AF.Square, accum_out=ssc),
                             r=[pkO], w=['junk', 'small'])
                        rstd_from_ss(ssc, 512, 'small')
                        S.op('dve', lambda v: v.scalar_tensor_tensor(out=oa_[i][:], in0=psO[:, :], scalar=ssc, in1=gs_[i][:],
                                                                     op0=ALU.mult, op1=ALU.mult),
                             r=[pkO, 'small', 'gs%d' % i], w=['oa%d' % i])
                        S.dma('sp', OA[q0:q0 + 128, h * 512:(h + 1) * 512], oa_[i][:], r=['oa%d' % i], w=[('OA', n)])
                    for cc in range(2):
                        psU, pkU = next_mm()
                        S.op('pe', lambda p: p.matmul(psU[:, :], lhsT=ktok[:, cc * 128:(cc + 1) * 128], rhs=vt_[i][:],
                                                      start=True, stop=True), r=['ktok', 'vt%d' % i], w=[pkU])
                        S.op('dve', lambda v: v.tensor_tensor(out=Sst[:, cc, :], in0=psU[:, :], in1=Sst[:, cc, :], op=ALU.add),
                             r=[pkU, 'Sst'], w=['Sst'])
                        S.op('dve', lambda v: v.tensor_scalar(out=Sst[:, cc, :], in0=Sst[:, cc, :],
                                                              scalar1=Epos[:, cc * 128 + 127:cc * 128 + 128], scalar2=None, op0=ALU.mult),
                             r=['Sst', 'Epos'], w=['Sst'])
                        if n == 7:
                            S.op('dve', lambda v: v.tensor_scalar(out=Sst[:, cc, :], in0=Sst[:, cc, :], scalar1=ctxflag, scalar2=None,
                                                                  op0=ALU.mult), r=['Sst', 'cst'], w=['Sst'])
                        S.op('act', lambda a: a.activation(out=Sbf[:, cc, :], in_=Sst[:, cc, :], func=AF.Identity), r=['Sst'], w=['Sbf'])
            S.barrier()
            if stop == 'D':
                return nc

        with ExitStack() as pefg:
            o_bT = sb(pefg, "o_bT", [128, KC, TOWN], BF16)
            with ExitStack() as pef:
                maskT = sb(pef, "maskT", [128, NT, TOWN], BF16)
                with ExitStack() as pe_:
                    ikT2 = sb(pe_, "ikT2", [128, TALL], BF16)
                    ikf = sb(pe_, "ikf", [128, 72], F32)
                    ikd = sb(pe_, "ikd", [128, 128], BF16)
                    iqs = sb(pe_, "iqs", [128, 512], BF16)
                    iqT = sb(pe_, "iqT", [128, 4, 128], BF16)
                    iwp = sb(pe_, "iwp", [128, 8], F32)
                    score = sb(pe_, "score", [128, TALL], F32)
                    relu_t = [sb(pe_, "relu%d" % i, [128, 512], F32) for i in range(2)]
                    mask_tm = sb(pe_, "mask_tm", [128, TALL], BF16)
                    S.op('pool', lambda g: g.tensor_copy(out=maskT[:, :, 0:512], in_=junk[:, 0:1].to_broadcast([128, NT, 512])) if False
                         else g.tensor_scalar(out=maskT[:, :, :], in0=maskT[:, :, :], scalar1=0.0, scalar2=None, op0=ALU.mult),
                         w=['maskT'])
                    for kt in range(NT):
                        S.dma('sp', ikf[:], IKW[kt * 128:(kt + 1) * 128, :], w=['ikf'])
                        S.op('dve', lambda v: v.tensor_copy(out=ikd[:, 0:64], in_=ikf[:, 0:64]), r=['ikf'], w=['ikd'])
                        S.op('dve', lambda v: v.tensor_copy(out=ikd[:, 64:128], in_=ikf[:, 0:64]), r=['ikf'], w=['ikd'])
                        tps, tk = next_tp()
                        S.op('pe', lambda p: p.transpose(out=tps[:, 0:128], in_=ikd[:], identity=ident[:]), r=['ikd', 'ident'], w=[tk])
                        S.op('act', lambda a: a.activation(out=ikT2[:, kt * 128:(kt + 1) * 128], in_=tps[:, 0:128], func=AF.Identity),
                             r=[tk], w=['ikT2'])
                    lo, hw, mid, cntv, gev, am = [small[:, 8 + j:9 + j] for j in range(6)]
                    for qi in range(8):
                        tt = 8 + qi
                        nk = 1024 + 128 * (qi + 1)
                        S.dma('sp', iqs[:], IQ[qi * 128:(qi + 1) * 128, :], w=['iqs'])
                        S.dma('act', ikf[:], IKW[tt * 128:(tt + 1) * 128, :], w=['ikf'])
                        tps, tk = next_tp()
                        for c in range(4):
                            S.op('pe', lambda p: p.transpose(out=tps[:, c * 128:(c + 1) * 128], in_=iqs[:, c * 128:(c + 1) * 128],
                                                             identity=ident[:]), r=['iqs', 'ident'], w=[tk])
                        S.op('act', lambda a: a.activation(out=iqT[:].rearrange("p a b -> p (a b)"), in_=tps[:, 0:512], func=AF.Identity),
                             r=[tk], w=['iqT'])
                        S.op('dve', lambda v: v.tensor_scalar(out=iwp[:], in0=ikf[:, 64:72], scalar1=float(8 ** -0.5 * 64 ** -0.5),
                                                              scalar2=None, op0=ALU.mult), r=['ikf'], w=['iwp'])
                        ng = (nk + 511) // 512
                        for g in range(ng):
                            wd_ = min(512, nk - g * 512)
                            for hh in range(8):
                                ps, pk = next_mm()
                                pb_ = (hh % 2) * 64
                                S.op('pe', lambda p: p.matmul(ps[:, 0:wd_], lhsT=iqT[pb_:pb_ + 64, hh // 2, :],
                                                              rhs=ikT2[pb_:pb_ + 64, g * 512:g * 512 + wd_], start=True, stop=True),
                                     r=['iqT', 'ikT2'], w=[pk])
                                rl = relu_t[hh % 2]
                                rk = 'relu%d' % (hh % 2)
                                S.op('act', lambda a: a.activation(out=rl[:, 0:wd_], in_=ps[:, 0:wd_], func=AF.Relu), r=[pk], w=[rk])
                                sc_ = score[:, g * 512:g * 512 + wd_]
                                if hh == 0:
                                    S.op('dve', lambda v: v.tensor_scalar(out=sc_, in0=rl[:, 0:wd_], scalar1=iwp[:, 0:1], scalar2=None,
                                                                          op0=ALU.mult), r=[rk, 'iwp'], w=['score'])
                                else:
                                    S.op('dve', lambda v: v.scalar_tensor_tensor(out=sc_, in0=rl[:, 0:wd_], scalar=iwp[:, hh:hh + 1],
                                                                                 in1=sc_, op0=ALU.mult, op1=ALU.add),
                                         r=[rk, 'iwp', 'score'], w=['score'])
                        S.op('dve', lambda v: v.tensor_reduce(out=am, in_=score[:, 0:nk], axis=AX.X, op=ALU.max,
                                                              apply_absolute_value=True), r=['score'], w=['small'])
                        S.op('dve', lambda v: v.tensor_scalar(out=score[:, 0:1024], in0=score[:, 0:1024], scalar1=ctxneg, scalar2=None,
                                                              op0=ALU.add), r=['score', 'cst'], w=['score'])
                        S.op('dve', lambda v: v.tensor_tensor(out=score[:, nk - 128:nk], in0=score[:, nk - 128:nk], in1=cmask, op=ALU.add),
                             r=['score', 'cst'], w=['score'])
                        S.op('dve', lambda v: v.tensor_scalar(out=hw, in0=am, scalar1=1.0001, scalar2=1e-20, op0=ALU.mult, op1=ALU.add),
                             r=['small'], w=['small'])
                        S.op('dve', lambda v: v.tensor_scalar(out=lo, in0=hw, scalar1=-1.0, scalar2=None, op0=ALU.mult),
                             r=['small'], w=['small'])
                        for it in range(NBISECT):
                            S.op('dve', lambda v: v.tensor_tensor(out=mid, in0=lo, in1=hw, op=ALU.add), r=['small'], w=['small'])
                            S.op('dve', lambda v: v.tensor_scalar(out=junk[:, 0:nk], in0=score[:, 0:nk], scalar1=mid, scalar2=None,
                                                                  op0=ALU.is_ge, op1=ALU.add, accum_out=cntv),
                                 r=['score', 'small'], w=['junk', 'small'])
                            S.op('dve', lambda v: v.tensor_scalar(out=gev, in0=cntv, scalar1=TOPK - 0.5, scalar2=None, op0=ALU.is_ge),
                                 r=['small'], w=['small'])
                            S.op('dve', lambda v: v.scalar_tensor_tensor(out=lo, in0=hw, scalar=gev, in1=lo, op0=ALU.mult, op1=ALU.add),
                                 r=['small'], w=['small'])
                            S.op('dve', lambda v: v.tensor_scalar(out=hw, in0=hw, scalar1=0.5, scalar2=None, op0=ALU.mult),
                                 r=['small'], w=['small'])
                        S.op('dve', lambda v: v.tensor_scalar(out=mask_tm[:, 0:nk], in0=score[:, 0:nk], scalar1=lo, scalar2=None,
                                                              op0=ALU.is_ge), r=['score', 'small'], w=['mask_tm'])
                        nkb = nk // 128
                        for c0 in range(0, nkb, 8):
                            n_ = min(8, nkb - c0)
                            tps, tk = next_tp()
                            for c in range(n_):
                                S.op('pe', lambda p: p.transpose(out=tps[:, c * 128:(c + 1) * 128],
                                                                 in_=mask_tm[:, (c0 + c) * 128:(c0 + c + 1) * 128], identity=ident[:]),
                                     r=['mask_tm', 'ident'], w=[tk])
                            S.op('act', lambda a: a.activation(out=maskT[:, c0:c0 + n_, qi * 128:(qi + 1) * 128],
                                                               in_=tps[:, 0:n_ * 128].rearrange("p (a b) -> p a b", b=128),
                                                               func=AF.Identity), r=[tk], w=['maskT'])
                    S.barrier()
                    if MTD is not None:
                        S.dma('sp', MTD, maskT[:], r=['maskT'], w=['MTD'])
                        S.barrier()
                    if stop == 'E':
                        return nc

                with ExitStack() as pf:
                    kTg = sb(pf, "kTg", [128, 4, TALL], BF16)
                    vg = sb(pf, "vg", [128, NT, 512], BF16)
                    qTg = sb(pf, "qTg", [128, 4, TOWN], BF16)
                    ldt = [sb(pf, "ldt%d" % i, [128, 512], BF16) for i in range(2)]
                    pt_ = [sb(pf, "pt%d" % i, [128, 512], BF16) for i in range(3)]
                    pm_ = [sb(pf, "pm%d" % i, [128, 512], BF16) for i in range(3)]
                    lnd = sb(pf, "lnd", [128, 512], F32)
                    rden = sb(pf, "rden", [128, 512], F32)
                    cnt_p = 0
                    for hg in range(4):
                        S.dma('act', vg[:], DV[:, hg * 512:(hg + 1) * 512].rearrange("(kt p) c -> p kt c", p=128), w=['vg'])
                        for kt in range(NT + 8):
                            i = kt % 2
                            if kt < NT:
                                S.dma('sp', ldt[i][:], DK[kt * 128:(kt + 1) * 128, hg * 512:(hg + 1) * 512], w=['ldt%d' % i])
                                dst = kTg[:, :, kt * 128:(kt + 1) * 128]
                                dk_ = 'kTg'
                            else:
                                qi = kt - NT
                                S.dma('sp', ldt[i][:], DQ[qi * 128:(qi + 1) * 128, hg * 512:(hg + 1) * 512], w=['ldt%d' % i])
                                dst = qTg[:, :, qi * 128:(qi + 1) * 128]
                                dk_ = 'qTg'
                            tps, tk = next_tp()
                            for c in range(4):
                                S.op('pe', lambda p: p.transpose(out=tps[:, c * 128:(c + 1) * 128], in_=ldt[i][:, c * 128:(c + 1) * 128],
                                                                 identity=ident[:]), r=['ldt%d' % i, 'ident'], w=[tk])
                            S.op('act' if kt % 2 == 0 else 'dve',
                                 (lambda a: a.activation(out=dst, in_=tps[:, 0:512].rearrange("p (a b) -> p a b", b=128), func=AF.Identity))
                                 if kt % 2 == 0 else
                                 (lambda v: v.tensor_copy(out=dst, in_=tps[:, 0:512].rearrange("p (a b) -> p a b", b=128))),
                                 r=[tk], w=[dk_])
                        for hh in range(4):
                            h = hg * 4 + hh
                            for qg in range(2):
                                nkb = 8 + 4 * (qg + 1)
                                for kb in range(nkb):
                                    lps, lk = next_mm()
                                    S.op('pe', lambda p: p.matmul(lps[:, :], lhsT=kTg[:, hh, kb * 128:(kb + 1) * 128],
                                                                  rhs=qTg[:, hh, qg * 512:(qg + 1) * 512], start=True, stop=True),
                                         r=['kTg', 'qTg'], w=[lk])
                                    j = cnt_p % 3
                                    cnt_p += 1
                                    S.op('act', lambda a: a.activation(out=pt_[j][:], in_=lps[:, :], func=AF.Exp, scale=float(128 ** -0.5)),
                                         r=[lk], w=['pt%d' % j])
                                    S.op('dve', lambda v: v.tensor_tensor(out=pm_[j][:], in0=pt_[j][:], in1=maskT[:, kb, qg * 512:(qg + 1) * 512],
                                                                          op=ALU.mult), r=['pt%d' % j, 'maskT'], w=['pm%d' % j])
                                    S.op('pe', lambda p: p.matmul(ax[0][:, :], lhsT=vg[:, kb, hh * 128:(hh + 1) * 128], rhs=pm_[j][:],
                                                                  start=(kb == 0), stop=(kb == nkb - 1)), r=['vg', 'pm%d' % j], w=['ax0'])
                                    S.op('pe', lambda p: p.matmul(ax[1][:, :], lhsT=ones_bf[:], rhs=pm_[j][:],
                                                                  start=(kb == 0), stop=(kb == nkb - 1)), r=['ones_bf', 'pm%d' % j], w=['ax1'])
                                S.op('act', lambda a: a.activation(out=lnd[:], in_=ax[1][:, :], func=AF.Ln), r=['ax1'], w=['lnd'])
                                S.op('act', lambda a: a.activation(out=rden[:], in_=lnd[:], func=AF.Exp, scale=-1.0), r=['lnd'], w=['rden'])
                                S.op('dve', lambda v: v.tensor_tensor(out=o_bT[:, h, qg * 512:(qg + 1) * 512], in0=ax[0][:, :], in1=rden[:],
                                                                      op=ALU.mult), r=['ax0', 'rden'], w=['o_bT'])
                    S.barrier()
                    if OBT is not None:
                        S.dma('sp', OBT, o_bT[:], r=['o_bT'], w=['OBT'])
                        S.barrier()
                    if stop == 'F':
                        return nc

            with ExitStack() as pg1:
                o_aT = sb(pg1, "o_aT", [128, KC, TOWN], BF16)
                oat = [sb(pg1, "oat%d" % i, [128, D], BF16) for i in range(2)]
                gat = [sb(pg1, "gat%d" % i, [128, 512], BF16) for i in range(2)]
                gbt = [sb(pg1, "gbt%d" % i, [128, 512], BF16) for i in range(2)]
                m1 = sb(pg1, "m1", [128, 512], F32)
                m2 = sb(pg1, "m2", [128, 512], F32)
                mgs = [sb(pg1, "mgs%d" % i, [128, 512], BF16) for i in range(2)]
                for qi in range(8):
                    i = qi % 2
                    S.dma('sp', oat[i][:], OA[qi * 128:(qi + 1) * 128, :], w=['oat%d' % i])

                    def ev(c0, n, tps, tkey, qi=qi):
                        S.op('act', lambda a: a.activation(out=o_aT[:, c0:c0 + n, qi * 128:(qi + 1) * 128],
                                                           in_=tps[:, 0:n * 128].rearrange("p (a b) -> p a b", b=128),
                                                           func=AF.Identity), r=[tkey], w=['o_aT'])
                    to_feature_major(oat[i], 'oat%d' % i, KC, None, 'o_aT', ev)
                for cb in range(4):
                    wa, wak = load_w(w_ba, 0, KC, cb * 512, 512)
                    wd2, wdk = load_w(w_bd, 0, KC, cb * 512, 512)
                    for qi in range(8):
                        i = qi % 2
                        S.dma('sp', gat[i][:], GA[qi * 128:(qi + 1) * 128, cb * 512:(cb + 1) * 512], w=['gat%d' % i])
                        S.dma('act', gbt[i][:], GB[qi * 128:(qi + 1) * 128, cb * 512:(cb + 1) * 512], w=['gbt%d' % i])
                        psa, pka = next_mm()
                        for kc in range(KC):
                            S.op('pe', lambda p: p.matmul(psa[:, :], lhsT=o_aT[:, kc, qi * 128:(qi + 1) * 128], rhs=wa[:, kc, :],
                                                          start=(kc == 0), stop=(kc == KC - 1)), r=['o_aT', wak], w=[pka])
                        psb, pkb = next_mm()
                        for kc in range(KC):
                            S.op('pe', lambda p: p.matmul(psb[:, :], lhsT=o_bT[:, kc, qi * 128:(qi + 1) * 128], rhs=wd2[:, kc, :],
                                                          start=(kc == 0), stop=(kc == KC - 1)), r=['o_bT', wdk], w=[pkb])
                        S.op('dve', lambda v: v.tensor_tensor(out=m1[:], in0=psa[:, :], in1=gat[i][:], op=ALU.mult),
                             r=[pka, 'gat%d' % i], w=['m1'])
                        S.op('dve', lambda v: v.tensor_tensor(out=m2[:], in0=psb[:, :], in1=gbt[i][:], op=ALU.mult),
                             r=[pkb, 'gbt%d' % i], w=['m2'])
                        S.op('pool', lambda g: g.tensor_tensor(out=mgs[i][:], in0=m1[:], in1=m2[:], op=ALU.add),
                             r=['m1', 'm2'], w=['mgs%d' % i])
                        S.dma('sp', MG[qi * 128:(qi + 1) * 128, cb * 512:(cb + 1) * 512], mgs[i][:], r=['mgs%d' % i], w=[('MG', qi)])
                S.barrier()

        with ExitStack() as px:
            x1 = sb(px, "x1", [128, 8, D], F32)
            rowb = sb(px, "rowb", [128, D], F32)
            with ExitStack() as pg2:
                mergedT = sb(pg2, "mergedT", [128, KC, TOWN], BF16)
                mgt = [sb(pg2, "mgt%d" % i, [128, D], BF16) for i in range(2)]
                xres = [sb(pg2, "xres%d" % i, [128, 512], F32) for i in range(2)]
                tmpf = sb(pg2, "tmpf", [128, 512], F32)
                S.dma('act', rowb[:], modrow_d[0, 2 * D:3 * D].partition_broadcast(128), w=['rowb'])
                for qi in range(8):
                    i = qi % 2
                    S.dma('sp', mgt[i][:], MG[qi * 128:(qi + 1) * 128, :], w=['mgt%d' % i])

                    def ev2(c0, n, tps, tkey, qi=qi):
                        S.op('act', lambda a: a.activation(out=mergedT[:, c0:c0 + n, qi * 128:(qi + 1) * 128],
                                                           in_=tps[:, 0:n * 128].rearrange("p (a b) -> p a b", b=128),
                                                           func=AF.Identity), r=[tkey], w=['mergedT'])
                    to_feature_major(mgt[i], 'mgt%d' % i, KC, None, 'mergedT', ev2)

                def evac_mo(cb):
                    def f(tt, ps, pkey):
                        i = tt % 2
                        S.dma('sp', xres[i][:], xs[TOWN + tt * 128:TOWN + (tt + 1) * 128, cb * 512:(cb + 1) * 512], w=['xres%d' % i])
                        S.op('dve', lambda v: v.tensor_tensor(out=tmpf[:], in0=ps[:, :], in1=rowb[:, cb * 512:(cb + 1) * 512], op=ALU.mult),
                             r=[pkey, 'rowb'], w=['tmpf'])
                        S.op('dve', lambda v: v.tensor_tensor(out=x1[:, tt, cb * 512:(cb + 1) * 512], in0=tmpf[:], in1=xres[i][:], op=ALU.add),
                             r=['tmpf', 'xres%d' % i], w=[('x1', tt)])
                    return f
                for cb in range(4):
                    linear(lambda kc, tt: mergedT[:, kc, tt * 128:(tt + 1) * 128], 'mergedT', w_mo, cb * 512, 512, list(range(8)), evac_mo(cb))
                S.barrier()

            with ExitStack() as ph:
                h2T = sb(ph, "h2T", [128, KC, TOWN], BF16)
                xn2 = [sb(ph, "xn2_%d" % i, [128, D], BF16) for i in range(2)]
                actT = sb(ph, "actT", [128, 11, TOWN], BF16)
                sg = [sb(ph, "sg%d" % i, [128, 512], F32) for i in range(2)]
                tmp2 = sb(ph, "tmp2", [128, 512], F32)
                S.dma('act', rowb[:], modrow_d[0, 5 * D:6 * D].partition_broadcast(128), w=['rowb'])
                for qi in range(8):
                    i = qi % 2
                    norm_to_fm(x1[:, qi, :], ('x1', qi), h2T, 'h2T', qi * 128, A2, sh2, ['A2', 'modfm'],
                               xn2[i], 'xn2_%d' % i, small[:, 16 + i:17 + i])
                cnt_s = 0
                for fb in range(4):
                    for fc in range(11):
                        f0 = fb * 1408 + fc * 128
                        wg, wgk = load_w(w_gu, 0, KC, f0, 128)
                        wu, wuk = load_w(w_gu, 0, KC, DFF + f0, 128)
                        for tg in range(2):
                            psg, pkg = next_mm()
                            for kc in range(KC):
                                S.op('pe', lambda p: p.matmul(psg[:, :], lhsT=wg[:, kc, 0:128], rhs=h2T[:, kc, tg * 512:(tg + 1) * 512],
                                                              start=(kc == 0), stop=(kc == KC - 1)), r=['h2T', wgk], w=[pkg])
                            psu, pku = next_mm()
                            for kc in range(KC):
                                S.op('pe', lambda p: p.matmul(psu[:, :], lhsT=wu[:, kc, 0:128], rhs=h2T[:, kc, tg * 512:(tg + 1) * 512],
                                                              start=(kc == 0), stop=(kc == KC - 1)), r=['h2T', wuk], w=[pku])
                            j = cnt_s % 2
                            cnt_s += 1
                            S.op('act', lambda a: a.activation(out=sg[j][:], in_=psg[:, :], func=AF.Silu), r=[pkg], w=['sg%d' % j])
                            S.op('dve', lambda v: v.tensor_tensor(out=actT[:, fc, tg * 512:(tg + 1) * 512], in0=psu[:, :], in1=sg[j][:],
                                                                  op=ALU.mult), r=[pku, 'sg%d' % j], w=['actT'])

                    def evac_dn(cb):
                        def f(tt, ps, pkey):
                            S.op('dve', lambda v: v.tensor_tensor(out=tmp2[:], in0=ps[:, :], in1=rowb[:, cb * 512:(cb + 1) * 512], op=ALU.mult),
                                 r=[pkey, 'rowb'], w=['tmp2'])
                            S.op('pool', lambda g: g.tensor_tensor(out=x1[:, tt, cb * 512:(cb + 1) * 512], in0=x1[:, tt, cb * 512:(cb + 1) * 512],
                                                                   in1=tmp2[:], op=ALU.add), r=['tmp2', ('x1', tt)], w=[('x1', tt)])
                        return f
                    for cb in range(4):
                        linear(lambda kc, tt: actT[:, kc, tt * 128:(tt + 1) * 128], 'actT', w_dn, cb * 512, 512, list(range(8)),
                               evac_dn(cb), r0=fb * 1408, nk=11)
                S.barrier()

            with ExitStack() as pi_:
                ot = [sb(pi_, "ot%d" % i, [128, D], F32) for i in range(2)]
                S.dma('act', rowb[:], fng[0, :].partition_broadcast(128), w=['rowb'])
                for qi in range(8):
                    i = qi % 2
                    ssc = small[:, 20 + i:21 + i]
                    S.op('act', lambda a: a.activation(out=junk[:], in_=x1[:, qi, :], func=AF.Square, accum_out=ssc),
                         r=[('x1', qi)], w=['junk', 'small'])
                    rstd_from_ss(ssc, D, 'small')
                    S.op('dve', lambda v: v.scalar_tensor_tensor(out=ot[i][:], in0=x1[:, qi, :], scalar=ssc, in1=rowb[:],
                                                                 op0=ALU.mult, op1=ALU.mult), r=[('x1', qi), 'small', 'rowb'], w=['ot%d' % i])
                    S.dma('sp', out[qi * 128:(qi + 1) * 128, :], ot[i][:], r=['ot%d' % i], w=[('out', qi)])
                S.barrier()
        S.barrier()
    return nc


def _consts(half):
    c = np.zeros((128, 1024), np.float32)
    c[:, 0:128] = np.eye(128, dtype=np.float32)
    j = np.arange(128)
    c[:, 128:256] = (j[:, None] <= j[None, :]).astype(np.float32)
    c[:, 256:384] = np.where(j[None, :] <= j[:, None], 0.0, NEG)
    c[:, 384] = 1.0 if half == 1 else 0.0
    c[:, 385] = 0.0 if half == 1 else NEG
    theta = np.float32(500000.0)
    c[:, 400:416] = np.power(theta, -np.arange(0, 32, 2, dtype=np.float32) / np.float32(32))[None, :]
    c[:, 416:424] = np.power(theta, -np.arange(0, 16, 2, dtype=np.float32) / np.float32(16))[None, :]
    return c


def prep_inputs(inputs, cores=None):
    f = lambda a: np.ascontiguousarray(np.asarray(a))
    x = f(inputs["x"]); c = f(inputs["c"]); pos = f(inputs["positions"]).astype(np.int32)
    shared = {
        "w_ada": f(inputs["w_ada"])[0], "b_ada": f(inputs["b_ada"])[0][None, :], "w_in": f(inputs["w_in"])[0],
        "gate_up": f(inputs["gla_gate_up"])[0], "gate_bias": f(inputs["gla_gate_bias"])[0][None, :],
        "gla_gain": f(inputs["gla_norm_gain"])[0][None, :],
        "w_ba": f(inputs["w_branch_gla"])[0], "w_bd": f(inputs["w_branch_dsa"])[0], "w_mo": f(inputs["w_merge_out"])[0],
        "w_gu": f(inputs["w_ffn_gate_up"])[0], "w_dn": f(inputs["w_ffn_down"])[0],
        "n1g": np.ascontiguousarray(f(inputs["norm1_gain"])[0].reshape(16, 128).T),
        "n2g": np.ascontiguousarray(f(inputs["norm2_gain"])[0].reshape(16, 128).T),
        "fng": f(inputs["final_norm_gain"])[None, :],
    }
    maps = []
    for core in (range(8) if cores is None else cores):
        b, half = core // 2, core % 2
        if half == 1:
            xs = x[b]
            p = pos[b]
        else:
            xs = np.concatenate([np.zeros((TOWN, D), np.float32), x[b, :TOWN]], axis=0)
            p = np.concatenate([pos[b, :TOWN], pos[b, :TOWN]])
        m = dict(shared)
        m["xs"] = np.ascontiguousarray(xs)
        m["cfm"] = np.ascontiguousarray(c[b].reshape(16, 128).T)
        m["posi"] = np.ascontiguousarray(p.reshape(16, 128).T)
        m["cst"] = _consts(half)
        maps.append(m)
    return maps


_NC = None


def kernel(**inputs):
    global _NC
    if _NC is None:
        _NC = build_program()
    maps = prep_inputs(inputs)
    res = run_bass_kernel_spmd(_NC, maps, core_ids=list(range(8)))
    outp = np.zeros((NB, SEQ, D), np.float32)
    for core in range(8):
        b, half = core // 2, core % 2
        outp[b, half * TOWN:(half + 1) * TOWN] = res.results[core]["out"]
    return outp
```

```python
import math
from contextlib import ExitStack

import numpy as np
import concourse.bass as bass
import concourse.mybir as mybir
from concourse.bass_utils import run_bass_kernel_spmd

F32 = mybir.dt.float32
BF16 = mybir.dt.bfloat16
I32 = mybir.dt.int32
AF = mybir.ActivationFunctionType
ALU = mybir.AluOpType
AX = mybir.AxisListType

D = 2048
SEQ = 2048
NB = 4
TOWN = 1024
TALL = 2048
NT = 16
KC = 16
DFF = 5632
EPS = 1e-6
NEG = -1.0e30
TOPK = 256
NBISECT = 22

O_GQ, O_GK, O_GV, O_GR, O_GLR = 0, 1024, 2048, 4096, 6144
O_DQ, O_DK, O_DV = 6160, 8208, 10256
O_IQ, O_IK, O_IW = 12304, 12816, 12880
O_GA, O_GB = 12888, 14936
IN_W = 16984


class Sched:
    def __init__(self, nc, es, ndma=24):
        self.nc = nc
        self.eng = {'pe': nc.tensor, 'act': nc.scalar, 'dve': nc.vector, 'pool': nc.gpsimd, 'sp': nc.sync}
        self.semobj = {}
        for e in ['pe', 'act', 'dve', 'pool']:
            self.semobj[e] = es.enter_context(nc.semaphore('s_' + e))
        self.ndma = ndma
        for i in range(ndma):
            self.semobj[('d', i)] = es.enter_context(nc.semaphore('sd%d' % i))
            self.semobj[('g', i)] = es.enter_context(nc.semaphore('sg%d' % i))
        self.dma_rr_g = 0
        self.cnt = {k: 0 for k in self.semobj}
        self.seen = {e: {} for e in self.eng}
        self.lastw = {}
        self.readers = {}
        self.dma_rr = 0
        self.nwait = 0

    def _wait(self, e, k, v):
        if k == e and e == 'pe':
            return
        if self.seen[e].get(k, 0) >= v:
            return
        self.eng[e].wait_ge(self.semobj[k], v)
        self.seen[e][k] = v
        self.nwait += 1

    def _deps(self, e, r, w):
        for key in r:
            for k, v in self.lastw.get(key, {}).items():
                self._wait(e, k, v)
        for key in w:
            for k, v in self.lastw.get(key, {}).items():
                self._wait(e, k, v)
            for k, v in self.readers.get(key, {}).items():
                self._wait(e, k, v)

    def _record(self, ev, r, w):
        k, v = ev
        for key in r:
            d = self.readers.setdefault(key, {})
            d[k] = max(d.get(k, 0), v)
        for key in w:
            self.lastw[key] = {k: v}
            self.readers[key] = {}

    def op(self, e, fn, r=(), w=()):
        ex = [k for k in r if isinstance(k, str) and k[:2] in ('mm', 'tp', 'ax')]
        if ex:
            w = list(w) + [k for k in ex if k not in w]
        self._deps(e, r, w)
        ins = fn(self.eng[e])
        self.cnt[e] += 1
        ins.then_inc(self.semobj[e], 1)
        self._record((e, self.cnt[e]), r, w)

    def dma(self, q, out, in_, r=(), w=(), **kw):
        if q == 'pool':
            slot = ('g', self.dma_rr_g % self.ndma)
            self.dma_rr_g += 1
        else:
            slot = ('d', self.dma_rr % self.ndma)
            self.dma_rr += 1
        if self.cnt[slot] > 0:
            self._wait(q, slot, self.cnt[slot])
        self._deps(q, r, w)
        ins = self.eng[q].dma_start(out=out, in_=in_, **kw)
        self.cnt[slot] += 16
        ins.then_inc(self.semobj[slot], 16)
        self._record((slot, self.cnt[slot]), r, w)

    def barrier(self):
        for e in self.eng:
            for k in self.semobj:
                if self.cnt[k] > 0:
                    self._wait(e, k, self.cnt[k])
        self.lastw = {}
        self.readers = {}


def build_program(dbg=(), stop=None):
    nc = bass.Bass("TRN2", target_bir_lowering=False)
    import os
    stop = stop or os.environ.get('KSTOP')

    def din(name, shape, dt=F32):
        return nc.dram_tensor(name, list(shape), dt, kind="ExternalInput").ap()

    def dscr(name, shape, dt=BF16):
        kind = "ExternalOutput" if name in dbg else "Internal"
        return nc.dram_tensor(name, list(shape), dt, kind=kind).ap()

    xs = din("xs", [TALL, D])
    cfm = din("cfm", [128, KC])
    posi = din("posi", [128, NT], I32)
    w_ada = din("w_ada", [D, 6 * D])
    b_ada = din("b_ada", [1, 6 * D])
    w_in = din("w_in", [D, IN_W])
    gate_up = din("gate_up", [16, 1024])
    gate_bias = din("gate_bias", [1, 1024])
    gla_gain = din("gla_gain", [1, D])
    w_ba = din("w_ba", [D, D])
    w_bd = din("w_bd", [D, D])
    w_mo = din("w_mo", [D, D])
    w_gu = din("w_gu", [D, 2 * DFF])
    w_dn = din("w_dn", [DFF, D])
    n1g = din("n1g", [128, KC])
    n2g = din("n2g", [128, KC])
    fng = din("fng", [1, D])
    cst = din("cst", [128, 1024])
    out = nc.dram_tensor("out", [TOWN, D], F32, kind="ExternalOutput").ap()

    modrow_d = dscr("modrow_d", [1, 6 * D], F32)
    GQ = dscr("GQ", [TOWN, 1024])
    GK = dscr("GK", [TALL, 1024])
    GV = dscr("GV", [TALL, 2048])
    GR = dscr("GR", [TOWN, 2048])
    DQ = dscr("DQ", [TOWN, 2048])
    DK = dscr("DK", [TALL, 2048])
    DV = dscr("DV", [TALL, 2048])
    IQ = dscr("IQ", [TOWN, 512])
    IKW = dscr("IKW", [TALL, 72], F32)
    GA = dscr("GA", [TOWN, 2048])
    GB = dscr("GB", [TOWN, 2048])
    OA = dscr("OA", [TOWN, 2048])
    MG = dscr("MG", [TOWN, 2048])
    OBT = dscr("OBT", [128, KC, TOWN]) if "OBT" in dbg else None
    MTD = dscr("MTD", [128, NT, TOWN]) if "MTD" in dbg else None

    with ExitStack() as es:
        S = Sched(nc, es)

        def sb(stack, name, shape, dt):
            return stack.enter_context(nc.sbuf_tensor(name, list(shape), dt))

        mm, tp, ax = [], [], []
        pstack = [None]
        rr = {'mm': 0, 'tp': 0, 'stg': 0, 'wb': 0}

        def alloc_psum(nm, nt, na):
            if pstack[0] is not None:
                pstack[0].close()
            st = ExitStack()
            pstack[0] = st
            rr['pgen'] = rr.get('pgen', 0) + 1
            g = rr['pgen']
            mm[:] = [st.enter_context(nc.psum_tensor("mm%d_%d" % (i, g), [128, 512], F32)) for i in range(nm)]
            tp[:] = [st.enter_context(nc.psum_tensor("tp%d_%d" % (i, g), [128, 1024], BF16)) for i in range(nt)]
            ax[:] = [st.enter_context(nc.psum_tensor("ax%d_%d" % (i, g), [128, 512], F32)) for i in range(na)]

        alloc_psum(4, 2, 2)

        def next_mm():
            i = rr['mm'] % len(mm)
            rr['mm'] += 1
            return mm[i], 'mm%d' % i

        def next_tp():
            i = rr['tp'] % len(tp)
            rr['tp'] += 1
            return tp[i], 'tp%d' % i

        cst_t = sb(es, "cst_t", [128, 1024], F32)
        S.dma('sp', cst_t[:], cst, w=['cst'])
        identf = cst_t[:, 0:128]
        triu = cst_t[:, 128:256]
        cmask = cst_t[:, 256:384]
        ctxflag = cst_t[:, 384:385]
        ctxneg = cst_t[:, 385:386]
        invf_d = cst_t[:, 400:416]
        invf_i = cst_t[:, 416:424]
        ident = sb(es, "ident", [128, 128], BF16)
        ones_bf = sb(es, "ones_bf", [128, 128], BF16)
        S.op('dve', lambda v: v.tensor_copy(out=ident[:], in_=identf), r=['cst'], w=['ident'])
        S.op('dve', lambda v: v.memset(ones_bf[:], 1.0), w=['ones_bf'])
        modfm = sb(es, "modfm", [128, 96], F32)
        A1 = sb(es, "A1", [128, KC], F32)
        A2 = sb(es, "A2", [128, KC], F32)
        n1g_t = sb(es, "n1g_t", [128, KC], F32)
        n2g_t = sb(es, "n2g_t", [128, KC], F32)
        S.dma('sp', n1g_t[:], n1g, w=['n1g'])
        S.dma('sp', n2g_t[:], n2g, w=['n2g'])
        wbuf = []

        def alloc_wbuf(stack, n, nk=KC, nb=512):
            rr['wgen'] = rr.get('wgen', 0) + 1
            wbuf[:] = [stack.enter_context(nc.sbuf_tensor("wbuf%d_%d" % (i, rr['wgen']), [128, nk, nb], BF16)) for i in range(n)]
        stg = [sb(es, "stg%d" % i, [128, 512], BF16) for i in range(4)]
        small = sb(es, "small", [128, 64], F32)
        junk = sb(es, "junk", [128, 2048], BF16)
        glrT = sb(es, "glrT", [32, TALL], F32)

        def next_stg():
            i = rr['stg'] % 4
            rr['stg'] += 1
            return stg[i], 'stg%d' % i

        def load_w(W, r0, nk, c0, nb, q='pool'):
            i = rr['wb'] % len(wbuf)
            rr['wb'] += 1
            key = 'wbuf%d' % i
            src = W[r0:r0 + nk * 128, c0:c0 + nb].rearrange("(kc p) n -> p kc n", p=128)
            S.dma(q, wbuf[i][:, 0:nk, 0:nb], src, w=[key])
            return wbuf[i], key

        def linear(actT, akey, W, c0, nb, tts, evac, r0=0, nk=KC, m=128):
            wb, wkey = load_w(W, r0, nk, c0, nb)
            for tt in tts:
                ps, pkey = next_mm()
                for kc in range(nk):
                    S.op('pe', lambda p: p.matmul(ps[0:m, 0:nb], lhsT=actT(kc, tt), rhs=wb[:, kc, 0:nb],
                                                  start=(kc == 0), stop=(kc == nk - 1)),
                         r=[akey, wkey], w=[pkey])
                evac(tt, ps, pkey)

        def rstd_from_ss(ss_ap, n, key):
            S.op('dve', lambda v: v.tensor_scalar(out=ss_ap, in0=ss_ap, scalar1=1.0 / n, scalar2=EPS,
                                                  op0=ALU.mult, op1=ALU.add), r=[key], w=[key])
            S.op('act', lambda a: a.activation(out=ss_ap, in_=ss_ap, func=AF.Sqrt), r=[key], w=[key])
            S.op('dve', lambda v: v.reciprocal(out=ss_ap, in_=ss_ap), r=[key], w=[key])

        def to_feature_major(src_tile, skey, nchunk, dst_fn, dkey, evac_eng_fn):
            for c0 in range(0, nchunk, 8):
                n = min(8, nchunk - c0)
                tps, tkey = next_tp()
                for c in range(n):
                    S.op('pe', lambda p: p.transpose(out=tps[:, c * 128:(c + 1) * 128],
                                                     in_=src_tile[:, (c0 + c) * 128:(c0 + c + 1) * 128],
                                                     identity=ident[:]),
                         r=[skey, 'ident'], w=[tkey])
                evac_eng_fn(c0, n, tps, tkey)

        with ExitStack() as pa:
            alloc_wbuf(pa, 2)
            c_t = sb(pa, "c_t", [128, KC], F32)
            sT = sb(pa, "sT", [128, KC], BF16)
            brow = sb(pa, "brow", [1, 6 * D], F32)
            mrow = sb(pa, "mrow", [1, 6 * D], F32)
            S.dma('sp', c_t[:], cfm, w=['c_t'])
            S.dma('sp', brow[:], b_ada, w=['brow'])
            S.op('act', lambda a: a.activation(out=sT[:], in_=c_t[:], func=AF.Silu), r=['c_t'], w=['sT'])

            def evac_mod(cb):
                def f(tt, ps, pkey):
                    S.op('dve', lambda v: v.tensor_tensor(out=mrow[0:1, cb * 512:(cb + 1) * 512], in0=ps[0:1, :],
                                                          in1=brow[0:1, cb * 512:(cb + 1) * 512], op=ALU.add),
                         r=[pkey, 'brow'], w=['mrow'])
                return f
            for cb in range(24):
                linear(lambda kc, tt: sT[:, kc:kc + 1], 'sT', w_ada, cb * 512, 512, [0], evac_mod(cb), m=1)
            S.dma('sp', modrow_d, mrow[:], r=['mrow'], w=['modrow_d'])
            with nc.allow_non_contiguous_dma(reason="one-time 48KB relayout of the modulation vector"):
                S.dma('sp', modfm[:], modrow_d[0, :].rearrange("(j p) -> p j", p=128), r=['modrow_d'], w=['modfm'])
            S.op('dve', lambda v: v.scalar_tensor_tensor(out=A1[:], in0=modfm[:, 16:32], scalar=1.0, in1=n1g_t[:],
                                                         op0=ALU.add, op1=ALU.mult), r=['modfm', 'n1g'], w=['A1'])
            S.op('dve', lambda v: v.scalar_tensor_tensor(out=A2[:], in0=modfm[:, 64:80], scalar=1.0, in1=n2g_t[:],
                                                         op0=ALU.add, op1=ALU.mult), r=['modfm', 'n2g'], w=['A2'])
            S.barrier()
        if stop == 'A':
            return nc
        sh1 = modfm[:, 0:16]
        sh2 = modfm[:, 48:64]

        def norm_to_fm(x_tile, xkey, dstT, dkey, tcol, A, sh, akeys, xn, xnkey, ss_ap):
            S.op('act', lambda a: a.activation(out=junk[:], in_=x_tile, func=AF.Square, accum_out=ss_ap),
                 r=[xkey], w=['junk', 'small'])
            rstd_from_ss(ss_ap, D, 'small')
            S.op('dve', lambda v: v.tensor_scalar(out=xn[:], in0=x_tile, scalar1=ss_ap, scalar2=None, op0=ALU.mult),
                 r=[xkey, 'small'], w=[xnkey])

            def ev(c0, n, tps, tkey):
                for c in range(n):
                    kc = c0 + c
                    S.op('act', lambda a: a.activation(out=dstT[:, kc, tcol:tcol + 128], in_=tps[:, c * 128:(c + 1) * 128],
                                                       func=AF.Identity, scale=A[:, kc:kc + 1], bias=sh[:, kc:kc + 1]),
                         r=[tkey] + akeys, w=[dkey])
            to_feature_major(xn, xnkey, KC, None, dkey, ev)

        with ExitStack() as pbc:
            hT = sb(pbc, "hT", [128, KC, TALL], BF16)
            with ExitStack() as pb:
                xt = [sb(pb, "xt%d" % i, [128, D], F32) for i in range(2)]
                xn = [sb(pb, "xn%d" % i, [128, D], BF16) for i in range(2)]
                for tt in range(NT):
                    i = tt % 2
                    S.dma('sp' if i == 0 else 'act', xt[i][:], xs[tt * 128:(tt + 1) * 128, :], w=['xt%d' % i])
                    norm_to_fm(xt[i][:], 'xt%d' % i, hT, 'hT', tt * 128, A1, sh1, ['A1', 'modfm'],
                               xn[i], 'xn%d' % i, small[:, i:i + 1])
                S.barrier()

            with ExitStack() as pc:
                alloc_wbuf(pc, 2)
                posf = sb(pc, "posf", [128, NT], F32)
                pos_i = sb(pc, "pos_i", [128, NT], I32)
                ang = sb(pc, "ang", [128, NT, 16], F32)
                kf = sb(pc, "kf", [128, NT, 16], F32)
                ki = sb(pc, "ki", [128, NT, 16], I32)
                kf2 = sb(pc, "kf2", [128, NT, 16], F32)
                sinD = sb(pc, "sinD", [128, NT, 1, 16], F32)
                cosD = sb(pc, "cosD", [128, NT, 1, 16], F32)
                sinI = sb(pc, "sinI", [128, NT, 1, 8], F32)
                cosI = sb(pc, "cosI", [128, NT, 1, 8], F32)
                ggain = sb(pc, "ggain", [128, D], F32)
                rt = [sb(pc, "rt%d" % i, [128, 4, 16], F32) for i in range(4)]
                f32stg = sb(pc, "f32stg", [128, 512], F32)
                f32stg2 = sb(pc, "f32stg2", [128, 72], F32)
                S.dma('sp', pos_i[:], posi, w=['pos_i'])
                S.dma('act', ggain[:], gla_gain[0, :].partition_broadcast(128), w=['ggain'])
                S.op('dve', lambda v: v.tensor_copy(out=posf[:], in_=pos_i[:]), r=['pos_i'], w=['posf'])
                TWO_PI = 2.0 * math.pi

                def make_tables(invf, nj, sin_t, cos_t, key):
                    for tt in range(NT):
                        S.op('dve', lambda v: v.tensor_scalar(out=ang[:, tt, 0:nj], in0=invf, scalar1=posf[:, tt:tt + 1],
                                                              scalar2=None, op0=ALU.mult), r=['cst', 'posf', 'ang'], w=['ang'])
                    a = ang[:, :, 0:nj]
                    kk = kf[:, :, 0:nj]
                    mm_ = kf2[:, :, 0:nj]
                    S.op('dve', lambda v: v.tensor_scalar(out=kk, in0=a, scalar1=1.0 / TWO_PI, scalar2=None,
                                                          op0=ALU.mult), r=['ang'], w=['kf'])
                    S.op('dve', lambda v: v.tensor_copy(out=ki[:, :, 0:nj], in_=kk), r=['kf'], w=['ki'])
                    S.op('dve', lambda v: v.tensor_copy(out=kk, in_=ki[:, :, 0:nj]), r=['ki'], w=['kf'])
                    S.op('dve', lambda v: v.scalar_tensor_tensor(out=a, in0=kk, scalar=-TWO_PI, in1=a,
                                                                 op0=ALU.mult, op1=ALU.add), r=['kf', 'ang'], w=['ang'])
                    for shift, dst in ((0.0, sin_t), (math.pi / 2, cos_t)):
                        S.op('dve', lambda v: v.tensor_scalar(out=kk, in0=a, scalar1=shift, scalar2=None,
                                                              op0=ALU.add), r=['ang', 'kf'], w=['kf'])
                        for cmp, bound, sgn in ((ALU.is_gt, math.pi, -1.0), (ALU.is_lt, -math.pi, 1.0)):
                            S.op('dve', lambda v: v.tensor_scalar(out=mm_, in0=kk, scalar1=bound, scalar2=sgn * TWO_PI,
                                                                  op0=cmp, op1=ALU.mult), r=['kf'], w=['kf2'])
                            S.op('dve', lambda v: v.tensor_tensor(out=kk, in0=kk, in1=mm_, op=ALU.add),
                                 r=['kf', 'kf2'], w=['kf'])
                        S.op('act', lambda a_: a_.activation(out=dst[:, :, 0, :], in_=kk, func=AF.Sin),
                             r=['kf'], w=[key])
                make_tables(invf_d, 16, sinD, cosD, 'tabD')
                make_tables(invf_i, 8, sinI, cosI, 'tabI')
                S.op('dve', lambda v: v.memset(glrT[:, :], 1.0), w=['glrT'])
                wb, wkey = load_w(w_in, 0, KC, O_GLR, 16)
                for tg in range(4):
                    ps, pkey = next_mm()
                    for kc in range(KC):
                        S.op('pe', lambda p: p.matmul(ps[0:16, :], lhsT=wb[:, kc, 0:16], rhs=hT[:, kc, tg * 512:(tg + 1) * 512],
                                                      start=(kc == 0), stop=(kc == KC - 1)), r=['hT', wkey], w=[pkey])
                    S.op('act', lambda a: a.activation(out=glrT[0:16, tg * 512:(tg + 1) * 512], in_=ps[0:16, :], func=AF.Identity),
                         r=[pkey], w=['glrT'])

                if stop == 'C1':
                    S.barrier()
                    return nc
                own = list(range(8, 16))
                allt = list(range(NT))
                hact = lambda kc, tt: hT[:, kc, tt * 128:(tt + 1) * 128]

                def store(dst, own_only, c0, nb):
                    def f(tt, ps, pkey):
                        st, skey = next_stg()
                        S.op('act', lambda a: a.activation(out=st[:, 0:nb], in_=ps[:, 0:nb], func=AF.Identity), r=[pkey], w=[skey])
                        row = (tt - 8 if own_only else tt) * 128
                        S.dma('sp', dst[row:row + 128, c0:c0 + nb], st[:, 0:nb], r=[skey], w=[(id(dst), tt)])
                    return f

                def store_act(dst, c0, nb, func, mul=None):
                    def f(tt, ps, pkey):
                        st, skey = next_stg()
                        if mul is None:
                            S.op('act', lambda a: a.activation(out=st[:, 0:nb], in_=ps[:, 0:nb], func=func), r=[pkey], w=[skey])
                        else:
                            S.op('act', lambda a: a.activation(out=f32stg[:, 0:nb], in_=ps[:, 0:nb], func=func), r=[pkey], w=['f32stg'])
                            S.op('dve', lambda v: v.tensor_tensor(out=st[:, 0:nb], in0=f32stg[:, 0:nb], in1=mul[:, c0:c0 + nb],
                                                                  op=ALU.mult), r=['f32stg', 'ggain'], w=[skey])
                        row = (tt - 8) * 128
                        S.dma('sp', dst[row:row + 128, c0:c0 + nb], st[:, 0:nb], r=[skey], w=[(id(dst), tt)])
                    return f

                def rope_ops(x1, x2, o1, o2, cs, sn, pkey, skey, tkey, shape):
                    t = [rt[i][:].rearrange("p a b -> p (a b)")[:, 0:shape[0] * shape[1]].rearrange("p (a b) -> p a b", b=shape[1])
                         for i in range(4)]
                    S.op('dve', lambda v: v.tensor_tensor(out=t[0], in0=x1, in1=cs, op=ALU.mult), r=[pkey, tkey], w=['rt0'])
                    S.op('dve', lambda v: v.tensor_tensor(out=t[1], in0=x2, in1=sn, op=ALU.mult), r=[pkey, tkey], w=['rt1'])
                    S.op('dve', lambda v: v.tensor_tensor(out=o1, in0=t[0], in1=t[1], op=ALU.subtract), r=['rt0', 'rt1'], w=[skey])
                    S.op('dve', lambda v: v.tensor_tensor(out=t[2], in0=x1, in1=sn, op=ALU.mult), r=[pkey, tkey], w=['rt2'])
                    S.op('dve', lambda v: v.tensor_tensor(out=t[3], in0=x2, in1=cs, op=ALU.mult), r=[pkey, tkey], w=['rt3'])
                    S.op('dve', lambda v: v.tensor_tensor(out=o2, in0=t[2], in1=t[3], op=ALU.add), r=['rt2', 'rt3'], w=[skey])

                def store_rope_d(dst, own_only, c0):
                    def f(tt, ps, pkey):
                        st, skey = next_stg()
                        S.op('act', lambda a: a.activation(out=st[:, :], in_=ps[:, :], func=AF.Identity), r=[pkey], w=[skey])
                        pv = ps[:, :].rearrange("p (h d) -> p h d", d=128)
                        sv = st[:, :].rearrange("p (h d) -> p h d", d=128)
                        rope_ops(pv[:, :, 0:16], pv[:, :, 16:32], sv[:, :, 0:16], sv[:, :, 16:32],
                                 cosD[:, tt, :, :].to_broadcast([128, 4, 16]), sinD[:, tt, :, :].to_broadcast([128, 4, 16]), pkey, skey, 'tabD', (4, 16))
                        row = (tt - 8 if own_only else tt) * 128
                        S.dma('sp', dst[row:row + 128, c0:c0 + 512], st[:, :], r=[skey], w=[(id(dst), tt)])
                    return f

                def store_iq(tt, ps, pkey):
                    st, skey = next_stg()
                    S.op('act', lambda a: a.activation(out=st[:, :], in_=ps[:, :], func=AF.Identity), r=[pkey], w=[skey])
                    pv = ps[:, :].rearrange("p (h d) -> p h d", d=64)
                    sv = st[:, :].rearrange("p (h d) -> p h d", d=64)
                    rope_ops(pv[:, :, 0:8], pv[:, :, 8:16], sv[:, :, 0:8], sv[:, :, 8:16],
                             cosI[:, tt, :, :].to_broadcast([128, 8, 8]), sinI[:, tt, :, :].to_broadcast([128, 8, 8]), pkey, skey, 'tabI', (8, 8))
                    row = (tt - 8) * 128
                    S.dma('sp', IQ[row:row + 128, :], st[:, :], r=[skey], w=[('IQ', tt)])

                def store_ikw(tt, ps, pkey):
                    S.op('act', lambda a: a.activation(out=f32stg2[:, :], in_=ps[:, 0:72], func=AF.Identity), r=[pkey], w=['f32stg2'])
                    rope_ops(ps[:, 0:8].rearrange("p (a b) -> p a b", a=1), ps[:, 8:16].rearrange("p (a b) -> p a b", a=1),
                             f32stg2[:, 0:8].rearrange("p (a b) -> p a b", a=1), f32stg2[:, 8:16].rearrange("p (a b) -> p a b", a=1),
                             cosI[:, tt, :, :], sinI[:, tt, :, :], pkey, 'f32stg2', 'tabI', (1, 8))
                    S.dma('sp', IKW[tt * 128:(tt + 1) * 128, :], f32stg2[:, :], r=['f32stg2'], w=[('IKW', tt)])

                for cb in range(2):
                    linear(hact, 'hT', w_in, O_GQ + cb * 512, 512, own, store(GQ, True, cb * 512, 512))
                if stop == 'C2':
                    S.barrier()
                    return nc
                for cb in range(2):
                    linear(hact, 'hT', w_in, O_GK + cb * 512, 512, allt, store(GK, False, cb * 512, 512))
                for cb in range(4):
                    linear(hact, 'hT', w_in, O_GV + cb * 512, 512, allt, store(GV, False, cb * 512, 512))
                for cb in range(4):
                    linear(hact, 'hT', w_in, O_GR + cb * 512, 512, own, store_act(GR, cb * 512, 512, AF.Silu, mul=ggain))
                if stop == 'C3':
                    S.barrier()
                    return nc
                for cb in range(4):
                    linear(hact, 'hT', w_in, O_DQ + cb * 512, 512, own, store_rope_d(DQ, True, cb * 512))
                if stop == 'C4':
                    S.barrier()
                    return nc
                for cb in range(4):
                    linear(hact, 'hT', w_in, O_DK + cb * 512, 512, allt, store_rope_d(DK, False, cb * 512))
                for cb in range(4):
                    linear(hact, 'hT', w_in, O_DV + cb * 512, 512, allt, store(DV, False, cb * 512, 512))
                if stop == 'C5':
                    S.barrier()
                    return nc
                linear(hact, 'hT', w_in, O_IQ, 512, own, store_iq)
                if stop == 'C6':
                    S.barrier()
                    return nc
                linear(hact, 'hT', w_in, O_IK, 72, allt, store_ikw)
                for cb in range(4):
                    linear(hact, 'hT', w_in, O_GA + cb * 512, 512, own, store_act(GA, cb * 512, 512, AF.Sigmoid))
                for cb in range(4):
                    linear(hact, 'hT', w_in, O_GB + cb * 512, 512, own, store_act(GB, cb * 512, 512, AF.Sigmoid))
                S.barrier()
                if stop == 'C':
                    return nc
        with ExitStack() as pd:
            gu_aug = sb(pd, "gu_aug", [32, 1024], F32)
            S.dma('sp', gu_aug[0:16, :], gate_up, w=['gu_aug'])
            S.dma('sp', gu_aug[16:17, :], gate_bias, w=['gu_aug'])
            Sst = sb(pd, "Sst", [128, 2, 512], F32)
            Sbf = sb(pd, "Sbf", [128, 2, 512], BF16)
            kt_ = [sb(pd, "kt%d" % i, [128, 256], BF16) for i in range(2)]
            vt_ = [sb(pd, "vt%d" % i, [128, 512], BF16) for i in range(2)]
            qt_ = [sb(pd, "qt%d" % i, [128, 256], BF16) for i in range(2)]
            gs_ = [sb(pd, "gs%d" % i, [128, 512], BF16) for i in range(2)]
            sp_ = sb(pd, "sp_", [128, 256], F32)
            Epos = sb(pd, "Epos", [128, 256], F32)
            Eneg = sb(pd, "Eneg", [128, 256], F32)
            Etok = sb(pd, "Etok", [128, 256], F32)
            ktok = sb(pd, "ktok", [128, 256], BF16)
            kT_ = sb(pd, "kT_", [128, 256], BF16)
            qT_ = sb(pd, "qT_", [128, 256], BF16)
            attnT = sb(pd, "attnT", [128, 128], BF16)
            oa_ = [sb(pd, "oa%d" % i, [128, 512], BF16) for i in range(2)]
            for h in range(4):
                S.op('dve', lambda v: v.memset(Sst[:], 0.0), w=['Sst'])
                S.op('dve', lambda v: v.memset(Sbf[:], 0.0), w=['Sbf'])
                for n in range(NT):
                    i = n % 2
                    ownt = n >= 8
                    r0 = n * 128
                    S.dma('sp', kt_[i][:], GK[r0:r0 + 128, h * 256:(h + 1) * 256], w=['kt%d' % i])
                    S.dma('act', vt_[i][:], GV[r0:r0 + 128, h * 512:(h + 1) * 512], w=['vt%d' % i])
                    if ownt:
                        q0 = (n - 8) * 128
                        S.dma('sp', qt_[i][:], GQ[q0:q0 + 128, h * 256:(h + 1) * 256], w=['qt%d' % i])
                        S.dma('act', gs_[i][:], GR[q0:q0 + 128, h * 512:(h + 1) * 512], w=['gs%d' % i])
                    ps, pk = next_mm()
                    S.op('pe', lambda p: p.matmul(ps[:, 0:256], lhsT=glrT[0:17, r0:r0 + 128], rhs=gu_aug[0:17, h * 256:(h + 1) * 256],
                                                  start=True, stop=True), r=['glrT', 'gu_aug'], w=[pk])
                    S.op('act', lambda a: a.activation(out=sp_[:], in_=ps[:, 0:256], func=AF.Exp, scale=-1.0), r=[pk], w=['sp_'])
                    S.op('act', lambda a: a.activation(out=sp_[:], in_=sp_[:], func=AF.Ln, bias=1.0), r=['sp_'], w=['sp_'])
                    ps2, pk2 = next_mm()
                    for cc in range(2):
                        S.op('pe', lambda p: p.matmul(ps2[:, cc * 128:(cc + 1) * 128], lhsT=sp_[:, cc * 128:(cc + 1) * 128], rhs=triu,
                                                      start=True, stop=True), r=['sp_', 'cst'], w=[pk2])
                    ps3, pk3 = next_mm()
                    S.op('pe', lambda p: p.matmul(ps3[:, 0:256], lhsT=triu, rhs=sp_[:], start=True, stop=True), r=['sp_', 'cst'], w=[pk3])
                    S.op('act', lambda a: a.activation(out=Epos[:], in_=ps2[:, 0:256], func=AF.Exp, scale=-1.0 / 16), r=[pk2], w=['Epos'])
                    S.op('act', lambda a: a.activation(out=Eneg[:], in_=ps2[:, 0:256], func=AF.Exp, scale=1.0 / 16), r=[pk2], w=['Eneg'])
                    S.op('act', lambda a: a.activation(out=Etok[:], in_=ps3[:, 0:256], func=AF.Exp, scale=1.0 / 16), r=[pk3], w=['Etok'])
                    tps, tk = next_tp()
                    for cc in range(2):
                        S.op('pe', lambda p: p.transpose(out=tps[:, cc * 128:(cc + 1) * 128], in_=kt_[i][:, cc * 128:(cc + 1) * 128],
                                                         identity=ident[:]), r=['kt%d' % i, 'ident'], w=[tk])
                    if ownt:
                        for cc in range(2):
                            S.op('pe', lambda p: p.transpose(out=tps[:, 256 + cc * 128:256 + (cc + 1) * 128],
                                                             in_=qt_[i][:, cc * 128:(cc + 1) * 128], identity=ident[:]),
                                 r=['qt%d' % i, 'ident'], w=[tk])
                    S.op('dve', lambda v: v.tensor_tensor(out=kT_[:], in0=tps[:, 0:256], in1=Eneg[:], op=ALU.mult),
                         r=[tk, 'Eneg'], w=['kT_'])
                    S.op('pool', lambda g: g.tensor_tensor(out=ktok[:], in0=kt_[i][:], in1=Etok[:], op=ALU.mult),
                         r=['kt%d' % i, 'Etok'], w=['ktok'])
                    if ownt:
                        S.op('dve', lambda v: v.scalar_tensor_tensor(out=qT_[:], in0=tps[:, 256:512], scalar=1.0 / 16, in1=Epos[:],
                                                                     op0=ALU.mult, op1=ALU.mult), r=[tk, 'Epos'], w=['qT_'])
                        psA, pkA = next_mm()
                        for cc in range(2):
                            S.op('pe', lambda p: p.matmul(psA[:, 0:128], lhsT=kT_[:, cc * 128:(cc + 1) * 128],
                                                          rhs=qT_[:, cc * 128:(cc + 1) * 128], start=(cc == 0), stop=(cc == 1)),
                                 r=['kT_', 'qT_'], w=[pkA])
                        S.op('dve', lambda v: v.tensor_tensor(out=attnT[:], in0=psA[:, 0:128], in1=triu, op=ALU.mult),
                             r=[pkA, 'cst'], w=['attnT'])
                        psO, pkO = next_mm()
                        S.op('pe', lambda p: p.matmul(psO[:, :], lhsT=attnT[:], rhs=vt_[i][:], start=True, stop=False),
                             r=['attnT', 'vt%d' % i], w=[pkO])
                        for cc in range(2):
                            S.op('pe', lambda p: p.matmul(psO[:, :], lhsT=qT_[:, cc * 128:(cc + 1) * 128], rhs=Sbf[:, cc, :],
                                                          start=False, stop=(cc == 1)), r=['qT_', 'Sbf'], w=[pkO])
                        ssc = small[:, 4 + i:5 + i]
                        S.op('act', lambda a: a.activation(out=junk[:, 0:512], in_=psO[:, :], func=AF.Square, accum_out=ssc),
                             r=[pkO], w=['junk', 'small'])
                        rstd_from_ss(ssc, 512, 'small')
                        S.op('dve', lambda v: v.scalar_tensor_tensor(out=oa_[i][:], in0=psO[:, :], scalar=ssc, in1=gs_[i][:],
                                                                     op0=ALU.mult, op1=ALU.mult),
                             r=[pkO, 'small', 'gs%d' % i], w=['oa%d' % i])
                        S.dma('sp', OA[q0:q0 + 128, h * 512:(h + 1) * 512], oa_[i][:], r=['oa%d' % i], w=[('OA', n)])
                    for cc in range(2):
                        psU, pkU = next_mm()
                        S.op('pe', lambda p: p.matmul(psU[:, :], lhsT=ktok[:, cc * 128:(cc + 1) * 128], rhs=vt_[i][:],
                                                      start=True, stop=True), r=['ktok', 'vt%d' % i], w=[pkU])
                        S.op('dve', lambda v: v.tensor_tensor(out=Sst[:, cc, :], in0=psU[:, :], in1=Sst[:, cc, :], op=ALU.add),
                             r=[pkU, 'Sst'], w=['Sst'])
                        S.op('dve', lambda v: v.tensor_scalar(out=Sst[:, cc, :], in0=Sst[:, cc, :],
                                                              scalar1=Epos[:, cc * 128 + 127:cc * 128 + 128], scalar2=None, op0=ALU.mult),
                             r=['Sst', 'Epos'], w=['Sst'])
                        if n == 7:
                            S.op('dve', lambda v: v.tensor_scalar(out=Sst[:, cc, :], in0=Sst[:, cc, :], scalar1=ctxflag, scalar2=None,
                                                                  op0=ALU.mult), r=['Sst', 'cst'], w=['Sst'])
                        S.op('act', lambda a: a.activation(out=Sbf[:, cc, :], in_=Sst[:, cc, :], func=AF.Identity), r=['Sst'], w=['Sbf'])
            S.barrier()
            if stop == 'D':
                return nc

        with ExitStack() as pefg:
            o_bT = sb(pefg, "o_bT", [128, KC, TOWN], BF16)
            with ExitStack() as pef:
                maskT = sb(pef, "maskT", [128, NT, TOWN], BF16)
                with ExitStack() as pe_:
                    ikT2 = sb(pe_, "ikT2", [128, TALL], BF16)
                    ikf = sb(pe_, "ikf", [128, 72], F32)
                    ikd = sb(pe_, "ikd", [128, 128], BF16)
                    iqs = sb(pe_, "iqs", [128, 512], BF16)
                    iqT = sb(pe_, "iqT", [128, 4, 128], BF16)
                    iwp = sb(pe_, "iwp", [128, 8], F32)
                    score = sb(pe_, "score", [128, TALL], F32)
                    relu_t = [sb(pe_, "relu%d" % i, [128, 512], F32) for i in range(2)]
                    mask_tm = sb(pe_, "mask_tm", [128, TALL], BF16)
                    S.op('pool', lambda g: g.tensor_copy(out=maskT[:, :, 0:512], in_=junk[:, 0:1].to_broadcast([128, NT, 512])) if False
                         else g.tensor_scalar(out=maskT[:, :, :], in0=maskT[:, :, :], scalar1=0.0, scalar2=None, op0=ALU.mult),
                         w=['maskT'])
                    for kt in range(NT):
                        S.dma('sp', ikf[:], IKW[kt * 128:(kt + 1) * 128, :], w=['ikf'])
                        S.op('dve', lambda v: v.tensor_copy(out=ikd[:, 0:64], in_=ikf[:, 0:64]), r=['ikf'], w=['ikd'])
                        S.op('dve', lambda v: v.tensor_copy(out=ikd[:, 64:128], in_=ikf[:, 0:64]), r=['ikf'], w=['ikd'])
                        tps, tk = next_tp()
                        S.op('pe', lambda p: p.transpose(out=tps[:, 0:128], in_=ikd[:], identity=ident[:]), r=['ikd', 'ident'], w=[tk])
                        S.op('act', lambda a: a.activation(out=ikT2[:, kt * 128:(kt + 1) * 128], in_=tps[:, 0:128], func=AF.Identity),
                             r=[tk], w=['ikT2'])
                    lo, hw, mid, cntv, gev, am = [small[:, 8 + j:9 + j] for j in range(6)]
                    for qi in range(8):
                        tt = 8 + qi
                        nk = 1024 + 128 * (qi + 1)
                        S.dma('sp', iqs[:], IQ[qi * 128:(qi + 1) * 128, :], w=['iqs'])
                        S.dma('act', ikf[:], IKW[tt * 128:(tt + 1) * 128, :], w=['ikf'])
                        tps, tk = next_tp()
                        for c in range(4):
                            S.op('pe', lambda p: p.transpose(out=tps[:, c * 128:(c + 1) * 128], in_=iqs[:, c * 128:(c + 1) * 128],
                                                             identity=ident[:]), r=['iqs', 'ident'], w=[tk])
                        S.op('act', lambda a: a.activation(out=iqT[:].rearrange("p a b -> p (a b)"), in_=tps[:, 0:512], func=AF.Identity),
                             r=[tk], w=['iqT'])
                        S.op('dve', lambda v: v.tensor_scalar(out=iwp[:], in0=ikf[:, 64:72], scalar1=float(8 ** -0.5 * 64 ** -0.5),
                                                              scalar2=None, op0=ALU.mult), r=['ikf'], w=['iwp'])
                        ng = (nk + 511) // 512
                        for g in range(ng):
                            wd_ = min(512, nk - g * 512)
                            for hh in range(8):
                                ps, pk = next_mm()
                                pb_ = (hh % 2) * 64
                                S.op('pe', lambda p: p.matmul(ps[:, 0:wd_], lhsT=iqT[pb_:pb_ + 64, hh // 2, :],
                                                              rhs=ikT2[pb_:pb_ + 64, g * 512:g * 512 + wd_], start=True, stop=True),
                                     r=['iqT', 'ikT2'], w=[pk])
                                rl = relu_t[hh % 2]
                                rk = 'relu%d' % (hh % 2)
                                S.op('act', lambda a: a.activation(out=rl[:, 0:wd_], in_=ps[:, 0:wd_], func=AF.Relu), r=[pk], w=[rk])
                                sc_ = score[:, g * 512:g * 512 + wd_]
                                if hh == 0:
                                    S.op('dve', lambda v: v.tensor_scalar(out=sc_, in0=rl[:, 0:wd_], scalar1=iwp[:, 0:1], scalar2=None,
                                                                          op0=ALU.mult), r=[rk, 'iwp'], w=['score'])
                                else:
                                    S.op('dve', lambda v: v.scalar_tensor_tensor(out=sc_, in0=rl[:, 0:wd_], scalar=iwp[:, hh:hh + 1],
                                                                                 in1=sc_, op0=ALU.mult, op1=ALU.add),
                                         r=[rk, 'iwp', 'score'], w=['score'])
                        S.op('dve', lambda v: v.tensor_reduce(out=am, in_=score[:, 0:nk], axis=AX.X, op=ALU.max,
                                                              apply_absolute_value=True), r=['score'], w=['small'])
                        S.op('dve', lambda v: v.tensor_scalar(out=score[:, 0:1024], in0=score[:, 0:1024], scalar1=ctxneg, scalar2=None,
                                                              op0=ALU.add), r=['score', 'cst'], w=['score'])
                        S.op('dve', lambda v: v.tensor_tensor(out=score[:, nk - 128:nk], in0=score[:, nk - 128:nk], in1=cmask, op=ALU.add),
                             r=['score', 'cst'], w=['score'])
                        S.op('dve', lambda v: v.tensor_scalar(out=hw, in0=am, scalar1=1.0001, scalar2=1e-20, op0=ALU.mult, op1=ALU.add),
                             r=['small'], w=['small'])
                        S.op('dve', lambda v: v.tensor_scalar(out=lo, in0=hw, scalar1=-1.0, scalar2=None, op0=ALU.mult),
                             r=['small'], w=['small'])
                        for it in range(NBISECT):
                            S.op('dve', lambda v: v.tensor_tensor(out=mid, in0=lo, in1=hw, op=ALU.add), r=['small'], w=['small'])
                            S.op('dve', lambda v: v.tensor_scalar(out=junk[:, 0:nk], in0=score[:, 0:nk], scalar1=mid, scalar2=None,
                                                                  op0=ALU.is_ge, op1=ALU.add, accum_out=cntv),
                                 r=['score', 'small'], w=['junk', 'small'])
                            S.op('dve', lambda v: v.tensor_scalar(out=gev, in0=cntv, scalar1=TOPK - 0.5, scalar2=None, op0=ALU.is_ge),
                                 r=['small'], w=['small'])
                            S.op('dve', lambda v: v.scalar_tensor_tensor(out=lo, in0=hw, scalar=gev, in1=lo, op0=ALU.mult, op1=ALU.add),
                                 r=['small'], w=['small'])
                            S.op('dve', lambda v: v.tensor_scalar(out=hw, in0=hw, scalar1=0.5, scalar2=None, op0=ALU.mult),
                                 r=['small'], w=['small'])
                        S.op('dve', lambda v: v.tensor_scalar(out=mask_tm[:, 0:nk], in0=score[:, 0:nk], scalar1=lo, scalar2=None,
                                                              op0=ALU.is_ge), r=['score', 'small'], w=['mask_tm'])
                        nkb = nk // 128
                        for c0 in range(0, nkb, 8):
                            n_ = min(8, nkb - c0)
                            tps, tk = next_tp()
                            for c in range(n_):
                                S.op('pe', lambda p: p.transpose(out=tps[:, c * 128:(c + 1) * 128],
                                                                 in_=mask_tm[:, (c0 + c) * 128:(c0 + c + 1) * 128], identity=ident[:]),
                                     r=['mask_tm', 'ident'], w=[tk])
                            S.op('act', lambda a: a.activation(out=maskT[:, c0:c0 + n_, qi * 128:(qi + 1) * 128],
                                                               in_=tps[:, 0:n_ * 128].rearrange("p (a b) -> p a b", b=128),
                                                               func=AF.Identity), r=[tk], w=['maskT'])
                    S.barrier()
                    if MTD is not None:
                        S.dma('sp', MTD, maskT[:], r=['maskT'], w=['MTD'])
                        S.barrier()
                    if stop == 'E':
                        return nc

                with ExitStack() as pf:
                    kTg = sb(pf, "kTg", [128, 4, TALL], BF16)
                    vg = sb(pf, "vg", [128, NT, 512], BF16)
                    qTg = sb(pf, "qTg", [128, 4, TOWN], BF16)
                    ldt = [sb(pf, "ldt%d" % i, [128, 512], BF16) for i in range(2)]
                    pt_ = [sb(pf, "pt%d" % i, [128, 512], BF16) for i in range(4)]
                    pm_ = [sb(pf, "pm%d" % i, [128, 512], BF16) for i in range(4)]
                    alloc_psum(3, 1, 4)
                    lnd = sb(pf, "lnd", [128, 512], F32)
                    rden = sb(pf, "rden", [128, 512], F32)
                    for hg in range(4):
                        S.dma('act', vg[:], DV[:, hg * 512:(hg + 1) * 512].rearrange("(kt p) c -> p kt c", p=128), w=['vg'])
                        for kt in range(NT + 8):
                            i = kt % 2
                            if kt < NT:
                                S.dma('sp', ldt[i][:], DK[kt * 128:(kt + 1) * 128, hg * 512:(hg + 1) * 512], w=['ldt%d' % i])
                                dst = kTg[:, :, kt * 128:(kt + 1) * 128]
                                dk_ = 'kTg'
                            else:
                                qi = kt - NT
                                S.dma('sp', ldt[i][:], DQ[qi * 128:(qi + 1) * 128, hg * 512:(hg + 1) * 512], w=['ldt%d' % i])
                                dst = qTg[:, :, qi * 128:(qi + 1) * 128]
                                dk_ = 'qTg'
                            tps, tk = next_tp()
                            for c in range(4):
                                S.op('pe', lambda p: p.transpose(out=tps[:, c * 128:(c + 1) * 128], in_=ldt[i][:, c * 128:(c + 1) * 128],
                                                                 identity=ident[:]), r=['ldt%d' % i, 'ident'], w=[tk])
                            S.op('act' if kt % 2 == 0 else 'dve',
                                 (lambda a: a.activation(out=dst, in_=tps[:, 0:512].rearrange("p (a b) -> p a b", b=128), func=AF.Identity))
                                 if kt % 2 == 0 else
                                 (lambda v: v.tensor_copy(out=dst, in_=tps[:, 0:512].rearrange("p (a b) -> p a b", b=128))),
                                 r=[tk], w=[dk_])
                        steps = [(hh, qg, kb) for hh in range(4) for qg in range(2) for kb in range(8 + 4 * (qg + 1))]
                        LA = 2
                        slots = {}

                        def qk_stage(idx):
                            hh, qg, kb = steps[idx]
                            lps, lk = next_mm()
                            S.op('pe', lambda p: p.matmul(lps[:, :], lhsT=kTg[:, hh, kb * 128:(kb + 1) * 128],
                                                          rhs=qTg[:, hh, qg * 512:(qg + 1) * 512], start=True, stop=True),
                                 r=['kTg', 'qTg'], w=[lk])
                            j = idx % 4
                            slots[idx] = j
                            S.op('act', lambda a: a.activation(out=pt_[j][:], in_=lps[:, :], func=AF.Exp, scale=float(128 ** -0.5)),
                                 r=[lk], w=['pt%d' % j])
                            S.op('dve', lambda v: v.tensor_tensor(out=pm_[j][:], in0=pt_[j][:], in1=maskT[:, kb, qg * 512:(qg + 1) * 512],
                                                                  op=ALU.mult), r=['pt%d' % j, 'maskT'], w=['pm%d' % j])

                        def pv_stage(idx):
                            hh, qg, kb = steps[idx]
                            nkb = 8 + 4 * (qg + 1)
                            j = slots.pop(idx)
                            pr = (hh * 2 + qg) % 2
                            aO, aD = ax[2 * pr], ax[2 * pr + 1]
                            kO, kD = 'ax%d' % (2 * pr), 'ax%d' % (2 * pr + 1)
                            S.op('pe', lambda p: p.matmul(aO[:, :], lhsT=vg[:, kb, hh * 128:(hh + 1) * 128], rhs=pm_[j][:],
                                                          start=(kb == 0), stop=(kb == nkb - 1)), r=['vg', 'pm%d' % j], w=[kO])
                            S.op('pe', lambda p: p.matmul(aD[:, :], lhsT=ones_bf[:], rhs=pm_[j][:],
                                                          start=(kb == 0), stop=(kb == nkb - 1)), r=['ones_bf', 'pm%d' % j], w=[kD])
                            if kb == nkb - 1:
                                h = hg * 4 + hh
                                S.op('act', lambda a: a.activation(out=lnd[:], in_=aD[:, :], func=AF.Ln), r=[kD], w=['lnd'])
                                S.op('act', lambda a: a.activation(out=rden[:], in_=lnd[:], func=AF.Exp, scale=-1.0), r=['lnd'], w=['rden'])
                                S.op('dve', lambda v: v.tensor_tensor(out=o_bT[:, h, qg * 512:(qg + 1) * 512], in0=aO[:, :], in1=rden[:],
                                                                      op=ALU.mult), r=[kO, 'rden'], w=['o_bT'])

                        for idx in range(len(steps) + LA):
                            if idx < len(steps):
                                qk_stage(idx)
                            if idx - LA >= 0:
                                pv_stage(idx - LA)
                    S.barrier()
                    if OBT is not None:
                        S.dma('sp', OBT, o_bT[:], r=['o_bT'], w=['OBT'])
                        S.barrier()
                    if stop == 'F':
                        return nc
                    alloc_psum(4, 2, 2)

            with ExitStack() as pg1:
                o_aT = sb(pg1, "o_aT", [128, KC, TOWN], BF16)
                oat = [sb(pg1, "oat%d" % i, [128, D], BF16) for i in range(2)]
                gat = [sb(pg1, "gat%d" % i, [128, 512], BF16) for i in range(2)]
                gbt = [sb(pg1, "gbt%d" % i, [128, 512], BF16) for i in range(2)]
                m1 = sb(pg1, "m1", [128, 512], F32)
                m2 = sb(pg1, "m2", [128, 512], F32)
                mgs = [sb(pg1, "mgs%d" % i, [128, 512], BF16) for i in range(2)]
                alloc_wbuf(pg1, 4)
                for qi in range(8):
                    i = qi % 2
                    S.dma('sp', oat[i][:], OA[qi * 128:(qi + 1) * 128, :], w=['oat%d' % i])

                    def ev(c0, n, tps, tkey, qi=qi):
                        S.op('act', lambda a: a.activation(out=o_aT[:, c0:c0 + n, qi * 128:(qi + 1) * 128],
                                                           in_=tps[:, 0:n * 128].rearrange("p (a b) -> p a b", b=128),
                                                           func=AF.Identity), r=[tkey], w=['o_aT'])
                    to_feature_major(oat[i], 'oat%d' % i, KC, None, 'o_aT', ev)
                for cb in range(4):
                    wa, wak = load_w(w_ba, 0, KC, cb * 512, 512)
                    wd2, wdk = load_w(w_bd, 0, KC, cb * 512, 512)
                    for qi in range(8):
                        i = qi % 2
                        S.dma('sp', gat[i][:], GA[qi * 128:(qi + 1) * 128, cb * 512:(cb + 1) * 512], w=['gat%d' % i])
                        S.dma('act', gbt[i][:], GB[qi * 128:(qi + 1) * 128, cb * 512:(cb + 1) * 512], w=['gbt%d' % i])
                        psa, pka = next_mm()
                        for kc in range(KC):
                            S.op('pe', lambda p: p.matmul(psa[:, :], lhsT=o_aT[:, kc, qi * 128:(qi + 1) * 128], rhs=wa[:, kc, :],
                                                          start=(kc == 0), stop=(kc == KC - 1)), r=['o_aT', wak], w=[pka])
                        psb, pkb = next_mm()
                        for kc in range(KC):
                            S.op('pe', lambda p: p.matmul(psb[:, :], lhsT=o_bT[:, kc, qi * 128:(qi + 1) * 128], rhs=wd2[:, kc, :],
                                                          start=(kc == 0), stop=(kc == KC - 1)), r=['o_bT', wdk], w=[pkb])
                        S.op('dve', lambda v: v.tensor_tensor(out=m1[:], in0=psa[:, :], in1=gat[i][:], op=ALU.mult),
                             r=[pka, 'gat%d' % i], w=['m1'])
                        S.op('dve', lambda v: v.tensor_tensor(out=m2[:], in0=psb[:, :], in1=gbt[i][:], op=ALU.mult),
                             r=[pkb, 'gbt%d' % i], w=['m2'])
                        S.op('pool', lambda g: g.tensor_tensor(out=mgs[i][:], in0=m1[:], in1=m2[:], op=ALU.add),
                             r=['m1', 'm2'], w=['mgs%d' % i])
                        S.dma('sp', MG[qi * 128:(qi + 1) * 128, cb * 512:(cb + 1) * 512], mgs[i][:], r=['mgs%d' % i], w=[('MG', qi)])
                S.barrier()

        with ExitStack() as px:
            x1 = sb(px, "x1", [128, 8, D], F32)
            rowb = sb(px, "rowb", [128, D], F32)
            with ExitStack() as pg2:
                alloc_wbuf(pg2, 2)
                mergedT = sb(pg2, "mergedT", [128, KC, TOWN], BF16)
                mgt = [sb(pg2, "mgt%d" % i, [128, D], BF16) for i in range(2)]
                xres = [sb(pg2, "xres%d" % i, [128, 512], F32) for i in range(2)]
                tmpf = sb(pg2, "tmpf", [128, 512], F32)
                S.dma('act', rowb[:], modrow_d[0, 2 * D:3 * D].partition_broadcast(128), w=['rowb'])
                for qi in range(8):
                    i = qi % 2
                    S.dma('sp', mgt[i][:], MG[qi * 128:(qi + 1) * 128, :], w=['mgt%d' % i])

                    def ev2(c0, n, tps, tkey, qi=qi):
                        S.op('act', lambda a: a.activation(out=mergedT[:, c0:c0 + n, qi * 128:(qi + 1) * 128],
                                                           in_=tps[:, 0:n * 128].rearrange("p (a b) -> p a b", b=128),
                                                           func=AF.Identity), r=[tkey], w=['mergedT'])
                    to_feature_major(mgt[i], 'mgt%d' % i, KC, None, 'mergedT', ev2)

                def evac_mo(cb):
                    def f(tt, ps, pkey):
                        i = tt % 2
                        S.dma('sp', xres[i][:], xs[TOWN + tt * 128:TOWN + (tt + 1) * 128, cb * 512:(cb + 1) * 512], w=['xres%d' % i])
                        S.op('dve', lambda v: v.tensor_tensor(out=tmpf[:], in0=ps[:, :], in1=rowb[:, cb * 512:(cb + 1) * 512], op=ALU.mult),
                             r=[pkey, 'rowb'], w=['tmpf'])
                        S.op('dve', lambda v: v.tensor_tensor(out=x1[:, tt, cb * 512:(cb + 1) * 512], in0=tmpf[:], in1=xres[i][:], op=ALU.add),
                             r=['tmpf', 'xres%d' % i], w=[('x1', tt)])
                    return f
                for cb in range(4):
                    linear(lambda kc, tt: mergedT[:, kc, tt * 128:(tt + 1) * 128], 'mergedT', w_mo, cb * 512, 512, list(range(8)), evac_mo(cb))
                S.barrier()

            with ExitStack() as ph:
                alloc_wbuf(ph, 2, 11, 512)
                h2T = sb(ph, "h2T", [128, KC, TOWN], BF16)
                xn2 = [sb(ph, "xn2_%d" % i, [128, D], BF16) for i in range(2)]
                actT = sb(ph, "actT", [128, 11, TOWN], BF16)
                sg = [sb(ph, "sg%d" % i, [128, 512], F32) for i in range(2)]
                tmp2 = sb(ph, "tmp2", [128, 512], F32)
                gub = [sb(ph, "gub%d" % i, [128, KC, 128], BF16) for i in range(6)]
                rr['gu'] = 0

                def load_gu(c0):
                    i = rr['gu'] % 6
                    rr['gu'] += 1
                    S.dma('pool', gub[i][:], w_gu[:, c0:c0 + 128].rearrange("(kc p) n -> p kc n", p=128), w=['gub%d' % i])
                    return gub[i], 'gub%d' % i
                S.dma('act', rowb[:], modrow_d[0, 5 * D:6 * D].partition_broadcast(128), w=['rowb'])
                for qi in range(8):
                    i = qi % 2
                    norm_to_fm(x1[:, qi, :], ('x1', qi), h2T, 'h2T', qi * 128, A2, sh2, ['A2', 'modfm'],
                               xn2[i], 'xn2_%d' % i, small[:, 16 + i:17 + i])
                cnt_s = 0
                for fb in range(4):
                    for fc in range(11):
                        f0 = fb * 1408 + fc * 128
                        wg, wgk = load_gu(f0)
                        wu, wuk = load_gu(DFF + f0)
                        for tg in range(2):
                            psg, pkg = next_mm()
                            for kc in range(KC):
                                S.op('pe', lambda p: p.matmul(psg[:, :], lhsT=wg[:, kc, 0:128], rhs=h2T[:, kc, tg * 512:(tg + 1) * 512],
                                                              start=(kc == 0), stop=(kc == KC - 1)), r=['h2T', wgk], w=[pkg])
                            psu, pku = next_mm()
                            for kc in range(KC):
                                S.op('pe', lambda p: p.matmul(psu[:, :], lhsT=wu[:, kc, 0:128], rhs=h2T[:, kc, tg * 512:(tg + 1) * 512],
                                                              start=(kc == 0), stop=(kc == KC - 1)), r=['h2T', wuk], w=[pku])
                            j = cnt_s % 2
                            cnt_s += 1
                            S.op('act', lambda a: a.activation(out=sg[j][:], in_=psg[:, :], func=AF.Silu), r=[pkg], w=['sg%d' % j])
                            S.op('dve', lambda v: v.tensor_tensor(out=actT[:, fc, tg * 512:(tg + 1) * 512], in0=psu[:, :], in1=sg[j][:],
                                                                  op=ALU.mult), r=[pku, 'sg%d' % j], w=['actT'])

                    def evac_dn(cb):
                        def f(tt, ps, pkey):
                            S.op('dve', lambda v: v.tensor_tensor(out=tmp2[:], in0=ps[:, :], in1=rowb[:, cb * 512:(cb + 1) * 512], op=ALU.mult),
                                 r=[pkey, 'rowb'], w=['tmp2'])
                            S.op('pool', lambda g: g.tensor_tensor(out=x1[:, tt, cb * 512:(cb + 1) * 512], in0=x1[:, tt, cb * 512:(cb + 1) * 512],
                                                                   in1=tmp2[:], op=ALU.add), r=['tmp2', ('x1', tt)], w=[('x1', tt)])
                        return f
                    for cb in range(4):
                        linear(lambda kc, tt: actT[:, kc, tt * 128:(tt + 1) * 128], 'actT', w_dn, cb * 512, 512, list(range(8)),
                               evac_dn(cb), r0=fb * 1408, nk=11)
                S.barrier()

            with ExitStack() as pi_:
                ot = [sb(pi_, "ot%d" % i, [128, D], F32) for i in range(2)]
                S.dma('act', rowb[:], fng[0, :].partition_broadcast(128), w=['rowb'])
                for qi in range(8):
                    i = qi % 2
                    ssc = small[:, 20 + i:21 + i]
                    S.op('act', lambda a: a.activation(out=junk[:], in_=x1[:, qi, :], func=AF.Square, accum_out=ssc),
                         r=[('x1', qi)], w=['junk', 'small'])
                    rstd_from_ss(ssc, D, 'small')
                    S.op('dve', lambda v: v.scalar_tensor_tensor(out=ot[i][:], in0=x1[:, qi, :], scalar=ssc, in1=rowb[:],
                                                                 op0=ALU.mult, op1=ALU.mult), r=[('x1', qi), 'small', 'rowb'], w=['ot%d' % i])
                    S.dma('sp', out[qi * 128:(qi + 1) * 128, :], ot[i][:], r=['ot%d' % i], w=[('out', qi)])
                S.barrier()
        S.barrier()
    return nc


def _consts(half):
    c = np.zeros((128, 1024), np.float32)
    c[:, 0:128] = np.eye(128, dtype=np.float32)
    j = np.arange(128)
    c[:, 128:256] = (j[:, None] <= j[None, :]).astype(np.float32)
    c[:, 256:384] = np.where(j[None, :] <= j[:, None], 0.0, NEG)
    c[:, 384] = 1.0 if half == 1 else 0.0
    c[:, 385] = 0.0 if half == 1 else NEG
    theta = np.float32(500000.0)
    c[:, 400:416] = np.power(theta, -np.arange(0, 32, 2, dtype=np.float32) / np.float32(32))[None, :]
    c[:, 416:424] = np.power(theta, -np.arange(0, 16, 2, dtype=np.float32) / np.float32(16))[None, :]
    return c


def prep_inputs(inputs, cores=None):
    f = lambda a: np.ascontiguousarray(np.asarray(a))
    x = f(inputs["x"]); c = f(inputs["c"]); pos = f(inputs["positions"]).astype(np.int32)
    shared = {
        "w_ada": f(inputs["w_ada"])[0], "b_ada": f(inputs["b_ada"])[0][None, :], "w_in": f(inputs["w_in"])[0],
        "gate_up": f(inputs["gla_gate_up"])[0], "gate_bias": f(inputs["gla_gate_bias"])[0][None, :],
        "gla_gain": f(inputs["gla_norm_gain"])[0][None, :],
        "w_ba": f(inputs["w_branch_gla"])[0], "w_bd": f(inputs["w_branch_dsa"])[0], "w_mo": f(inputs["w_merge_out"])[0],
        "w_gu": f(inputs["w_ffn_gate_up"])[0], "w_dn": f(inputs["w_ffn_down"])[0],
        "n1g": np.ascontiguousarray(f(inputs["norm1_gain"])[0].reshape(16, 128).T),
        "n2g": np.ascontiguousarray(f(inputs["norm2_gain"])[0].reshape(16, 128).T),
        "fng": f(inputs["final_norm_gain"])[None, :],
    }
    maps = []
    for core in (range(8) if cores is None else cores):
        b, half = core // 2, core % 2
        if half == 1:
            xs = x[b]
            p = pos[b]
        else:
            xs = np.concatenate([np.zeros((TOWN, D), np.float32), x[b, :TOWN]], axis=0)
            p = np.concatenate([pos[b, :TOWN], pos[b, :TOWN]])
        m = dict(shared)
        m["xs"] = np.ascontiguousarray(xs)
        m["cfm"] = np.ascontiguousarray(c[b].reshape(16, 128).T)
        m["posi"] = np.ascontiguousarray(p.reshape(16, 128).T)
        m["cst"] = _consts(half)
        maps.append(m)
    return maps


_NC = None


def kernel(**inputs):
    global _NC
    if _NC is None:
        _NC = build_program()
    maps = prep_inputs(inputs)
    res = run_bass_kernel_spmd(_NC, maps, core_ids=list(range(8)))
    outp = np.zeros((NB, SEQ, D), np.float32)
    for core in range(8):
        b, half = core // 2, core % 2
        outp[b, half * TOWN:(half + 1) * TOWN] = res.results[core]["out"]
    return outp
```

```python
import math
from contextlib import ExitStack

import numpy as np
import concourse.bass as bass
import concourse.mybir as mybir
from concourse.bass_utils import run_bass_kernel_spmd

F32 = mybir.dt.float32
BF16 = mybir.dt.bfloat16
I32 = mybir.dt.int32
AF = mybir.ActivationFunctionType
ALU = mybir.AluOpType
AX = mybir.AxisListType

D = 2048
SEQ = 2048
NB = 4
TOWN = 1024
TALL = 2048
NT = 16
KC = 16
DFF = 5632
EPS = 1e-6
NEG = -1.0e30
TOPK = 256
NBISECT = 22

O_GQ, O_GK, O_GV, O_GR, O_GLR = 0, 1024, 2048, 4096, 6144
O_DQ, O_DK, O_DV = 6160, 8208, 10256
O_IQ, O_IK, O_IW = 12304, 12816, 12880
O_GA, O_GB = 12888, 14936
IN_W = 16984


class Sched:
    def __init__(self, nc, es, ndma=24):
        self.nc = nc
        self.eng = {'pe': nc.tensor, 'act': nc.scalar, 'dve': nc.vector, 'pool': nc.gpsimd, 'sp': nc.sync}
        self.semobj = {}
        for e in ['pe', 'act', 'dve', 'pool']:
            self.semobj[e] = es.enter_context(nc.semaphore('s_' + e))
        self.ndma = ndma
        for i in range(ndma):
            self.semobj[('d', i)] = es.enter_context(nc.semaphore('sd%d' % i))
            self.semobj[('g', i)] = es.enter_context(nc.semaphore('sg%d' % i))
        self.dma_rr_g = 0
        self.cnt = {k: 0 for k in self.semobj}
        self.seen = {e: {} for e in self.eng}
        self.lastw = {}
        self.readers = {}
        self.dma_rr = 0
        self.nwait = 0

    def _wait(self, e, k, v):
        if k == e and e == 'pe':
            return
        if self.seen[e].get(k, 0) >= v:
            return
        self.eng[e].wait_ge(self.semobj[k], v)
        self.seen[e][k] = v
        self.nwait += 1

    def _deps(self, e, r, w):
        for key in r:
            for k, v in self.lastw.get(key, {}).items():
                self._wait(e, k, v)
        for key in w:
            for k, v in self.lastw.get(key, {}).items():
                self._wait(e, k, v)
            for k, v in self.readers.get(key, {}).items():
                self._wait(e, k, v)

    def _record(self, ev, r, w):
        k, v = ev
        for key in r:
            d = self.readers.setdefault(key, {})
            d[k] = max(d.get(k, 0), v)
        for key in w:
            self.lastw[key] = {k: v}
            self.readers[key] = {}

    def op(self, e, fn, r=(), w=()):
        ex = [k for k in r if isinstance(k, str) and k[:2] in ('mm', 'tp', 'ax')]
        if ex:
            w = list(w) + [k for k in ex if k not in w]
        self._deps(e, r, w)
        ins = fn(self.eng[e])
        self.cnt[e] += 1
        ins.then_inc(self.semobj[e], 1)
        self._record((e, self.cnt[e]), r, w)

    def dma(self, q, out, in_, r=(), w=(), **kw):
        if q == 'pool':
            slot = ('g', self.dma_rr_g % self.ndma)
            self.dma_rr_g += 1
        else:
            slot = ('d', self.dma_rr % self.ndma)
            self.dma_rr += 1
        if self.cnt[slot] > 0:
            self._wait(q, slot, self.cnt[slot])
        self._deps(q, r, w)
        ins = self.eng[q].dma_start(out=out, in_=in_, **kw)
        self.cnt[slot] += 16
        ins.then_inc(self.semobj[slot], 16)
        self._record((slot, self.cnt[slot]), r, w)

    def barrier(self):
        for e in self.eng:
            for k in self.semobj:
                if self.cnt[k] > 0:
                    self._wait(e, k, self.cnt[k])
        self.lastw = {}
        self.readers = {}


def build_program(dbg=(), stop=None):
    nc = bass.Bass("TRN2", target_bir_lowering=False)
    import os
    stop = stop or os.environ.get('KSTOP')

    def din(name, shape, dt=F32):
        return nc.dram_tensor(name, list(shape), dt, kind="ExternalInput").ap()

    def dscr(name, shape, dt=BF16):
        kind = "ExternalOutput" if name in dbg else "Internal"
        return nc.dram_tensor(name, list(shape), dt, kind=kind).ap()

    xs = din("xs", [TALL, D])
    cfm = din("cfm", [128, KC])
    posi = din("posi", [128, NT], I32)
    w_ada = din("w_ada", [D, 6 * D])
    b_ada = din("b_ada", [1, 6 * D])
    w_in = din("w_in", [D, IN_W])
    gate_up = din("gate_up", [16, 1024])
    gate_bias = din("gate_bias", [1, 1024])
    gla_gain = din("gla_gain", [1, D])
    w_ba = din("w_ba", [D, D])
    w_bd = din("w_bd", [D, D])
    w_mo = din("w_mo", [D, D])
    w_gu = din("w_gu", [D, 2 * DFF])
    w_dn = din("w_dn", [DFF, D])
    n1g = din("n1g", [128, KC])
    n2g = din("n2g", [128, KC])
    fng = din("fng", [1, D])
    cst = din("cst", [128, 1024])
    out = nc.dram_tensor("out", [TOWN, D], F32, kind="ExternalOutput").ap()

    modrow_d = dscr("modrow_d", [1, 6 * D], F32)
    GQ = dscr("GQ", [TOWN, 1024])
    GK = dscr("GK", [TALL, 1024])
    GV = dscr("GV", [TALL, 2048])
    GR = dscr("GR", [TOWN, 2048])
    DQ = dscr("DQ", [TOWN, 2048])
    DK = dscr("DK", [TALL, 2048])
    DV = dscr("DV", [TALL, 2048])
    IQ = dscr("IQ", [TOWN, 512])
    IKW = dscr("IKW", [TALL, 72], F32)
    GA = dscr("GA", [TOWN, 2048])
    GB = dscr("GB", [TOWN, 2048])
    OA = dscr("OA", [TOWN, 2048])
    MG = dscr("MG", [TOWN, 2048])
    OBD = dscr("OBD", [D, TOWN])
    MTD = dscr("MTD", [128, NT, TOWN]) if "MTD" in dbg else None

    with ExitStack() as es:
        S = Sched(nc, es)

        def sb(stack, name, shape, dt):
            return stack.enter_context(nc.sbuf_tensor(name, list(shape), dt))

        mm, tp, ax = [], [], []
        pstack = [None]
        rr = {'mm': 0, 'tp': 0, 'stg': 0, 'wb': 0}

        def alloc_psum(nm, nt, na):
            if pstack[0] is not None:
                pstack[0].close()
            st = ExitStack()
            pstack[0] = st
            rr['pgen'] = rr.get('pgen', 0) + 1
            g = rr['pgen']
            mm[:] = [st.enter_context(nc.psum_tensor("mm%d_%d" % (i, g), [128, 512], F32)) for i in range(nm)]
            tp[:] = [st.enter_context(nc.psum_tensor("tp%d_%d" % (i, g), [128, 1024], BF16)) for i in range(nt)]
            ax[:] = [st.enter_context(nc.psum_tensor("ax%d_%d" % (i, g), [128, 512], F32)) for i in range(na)]

        alloc_psum(4, 2, 2)

        mm_users = {}

        def set_mm_users(**parts):
            mm_users.clear()
            mm_users.update(parts)

        def next_mm(user=None):
            banks = mm_users.get(user) if mm_users else None
            if banks is None:
                banks = list(range(len(mm)))
            c = rr.get(('mm', user), 0)
            rr[('mm', user)] = c + 1
            i = banks[c % len(banks)]
            return mm[i], 'mm%d' % i

        bg = []

        def bg_step():
            for g in list(bg):
                try:
                    next(g)
                except StopIteration:
                    bg.remove(g)

        def bg_drain():
            while bg:
                bg_step()

        def next_tp():
            i = rr['tp'] % len(tp)
            rr['tp'] += 1
            return tp[i], 'tp%d' % i

        cst_t = sb(es, "cst_t", [128, 1024], F32)
        S.dma('sp', cst_t[:], cst, w=['cst'])
        identf = cst_t[:, 0:128]
        triu = cst_t[:, 128:256]
        cmask = cst_t[:, 256:384]
        ctxflag = cst_t[:, 384:385]
        ctxneg = cst_t[:, 385:386]
        invf_d = cst_t[:, 400:416]
        invf_i = cst_t[:, 416:424]
        ident = sb(es, "ident", [128, 128], BF16)
        ones_bf = sb(es, "ones_bf", [128, 128], BF16)
        S.op('dve', lambda v: v.tensor_copy(out=ident[:], in_=identf), r=['cst'], w=['ident'])
        S.op('dve', lambda v: v.memset(ones_bf[:], 1.0), w=['ones_bf'])
        modfm = sb(es, "modfm", [128, 96], F32)
        A1 = sb(es, "A1", [128, KC], F32)
        A2 = sb(es, "A2", [128, KC], F32)
        n1g_t = sb(es, "n1g_t", [128, KC], F32)
        n2g_t = sb(es, "n2g_t", [128, KC], F32)
        S.dma('sp', n1g_t[:], n1g, w=['n1g'])
        S.dma('sp', n2g_t[:], n2g, w=['n2g'])
        wbuf = []

        def alloc_wbuf(stack, n, nk=KC, nb=512):
            rr['wgen'] = rr.get('wgen', 0) + 1
            wbuf[:] = [stack.enter_context(nc.sbuf_tensor("wbuf%d_%d" % (i, rr['wgen']), [128, nk, nb], BF16)) for i in range(n)]
        stg = [sb(es, "stg%d" % i, [128, 512], BF16) for i in range(4)]
        small = sb(es, "small", [128, 64], F32)
        junk = sb(es, "junk", [128, 2048], BF16)
        glrT = sb(es, "glrT", [32, TALL], F32)

        def next_stg():
            i = rr['stg'] % 4
            rr['stg'] += 1
            return stg[i], 'stg%d' % i

        def load_w(W, r0, nk, c0, nb, q='pool'):
            i = rr['wb'] % len(wbuf)
            rr['wb'] += 1
            key = 'wbuf%d' % i
            src = W[r0:r0 + nk * 128, c0:c0 + nb].rearrange("(kc p) n -> p kc n", p=128)
            S.dma(q, wbuf[i][:, 0:nk, 0:nb], src, w=[key])
            return wbuf[i], key

        def linear(actT, akey, W, c0, nb, tts, evac, r0=0, nk=KC, m=128, user='c'):
            wb, wkey = load_w(W, r0, nk, c0, nb)
            for tt in tts:
                bg_step()
                ps, pkey = next_mm(user)
                for kc in range(nk):
                    S.op('pe', lambda p: p.matmul(ps[0:m, 0:nb], lhsT=actT(kc, tt), rhs=wb[:, kc, 0:nb],
                                                  start=(kc == 0), stop=(kc == nk - 1)),
                         r=[akey, wkey], w=[pkey])
                evac(tt, ps, pkey)

        def rstd_from_ss(ss_ap, n, key):
            S.op('dve', lambda v: v.tensor_scalar(out=ss_ap, in0=ss_ap, scalar1=1.0 / n, scalar2=EPS,
                                                  op0=ALU.mult, op1=ALU.add), r=[key], w=[key])
            S.op('act', lambda a: a.activation(out=ss_ap, in_=ss_ap, func=AF.Sqrt), r=[key], w=[key])
            S.op('dve', lambda v: v.reciprocal(out=ss_ap, in_=ss_ap), r=[key], w=[key])

        def to_feature_major(src_tile, skey, nchunk, dst_fn, dkey, evac_eng_fn):
            for c0 in range(0, nchunk, 8):
                n = min(8, nchunk - c0)
                tps, tkey = next_tp()
                for c in range(n):
                    S.op('pe', lambda p: p.transpose(out=tps[:, c * 128:(c + 1) * 128],
                                                     in_=src_tile[:, (c0 + c) * 128:(c0 + c + 1) * 128],
                                                     identity=ident[:]),
                         r=[skey, 'ident'], w=[tkey])
                evac_eng_fn(c0, n, tps, tkey)

        with ExitStack() as pa:
            alloc_wbuf(pa, 2)
            c_t = sb(pa, "c_t", [128, KC], F32)
            sT = sb(pa, "sT", [128, KC], BF16)
            brow = sb(pa, "brow", [1, 6 * D], F32)
            mrow = sb(pa, "mrow", [1, 6 * D], F32)
            S.dma('sp', c_t[:], cfm, w=['c_t'])
            S.dma('sp', brow[:], b_ada, w=['brow'])
            S.op('act', lambda a: a.activation(out=sT[:], in_=c_t[:], func=AF.Silu), r=['c_t'], w=['sT'])

            def evac_mod(cb):
                def f(tt, ps, pkey):
                    S.op('dve', lambda v: v.tensor_tensor(out=mrow[0:1, cb * 512:(cb + 1) * 512], in0=ps[0:1, :],
                                                          in1=brow[0:1, cb * 512:(cb + 1) * 512], op=ALU.add),
                         r=[pkey, 'brow'], w=['mrow'])
                return f
            for cb in range(24):
                linear(lambda kc, tt: sT[:, kc:kc + 1], 'sT', w_ada, cb * 512, 512, [0], evac_mod(cb), m=1)
            S.dma('sp', modrow_d, mrow[:], r=['mrow'], w=['modrow_d'])
            with nc.allow_non_contiguous_dma(reason="one-time 48KB relayout of the modulation vector"):
                S.dma('sp', modfm[:], modrow_d[0, :].rearrange("(j p) -> p j", p=128), r=['modrow_d'], w=['modfm'])
            S.op('dve', lambda v: v.scalar_tensor_tensor(out=A1[:], in0=modfm[:, 16:32], scalar=1.0, in1=n1g_t[:],
                                                         op0=ALU.add, op1=ALU.mult), r=['modfm', 'n1g'], w=['A1'])
            S.op('dve', lambda v: v.scalar_tensor_tensor(out=A2[:], in0=modfm[:, 64:80], scalar=1.0, in1=n2g_t[:],
                                                         op0=ALU.add, op1=ALU.mult), r=['modfm', 'n2g'], w=['A2'])
            S.barrier()
        if stop == 'A':
            return nc
        sh1 = modfm[:, 0:16]
        sh2 = modfm[:, 48:64]

        def norm_to_fm(x_tile, xkey, dstT, dkey, tcol, A, sh, akeys, xn, xnkey, ss_ap):
            S.op('act', lambda a: a.activation(out=junk[:], in_=x_tile, func=AF.Square, accum_out=ss_ap),
                 r=[xkey], w=['junk', 'small'])
            rstd_from_ss(ss_ap, D, 'small')
            S.op('dve', lambda v: v.tensor_scalar(out=xn[:], in0=x_tile, scalar1=ss_ap, scalar2=None, op0=ALU.mult),
                 r=[xkey, 'small'], w=[xnkey])

            def ev(c0, n, tps, tkey):
                for c in range(n):
                    kc = c0 + c
                    S.op('act', lambda a: a.activation(out=dstT[:, kc, tcol:tcol + 128], in_=tps[:, c * 128:(c + 1) * 128],
                                                       func=AF.Identity, scale=A[:, kc:kc + 1], bias=sh[:, kc:kc + 1]),
                         r=[tkey] + akeys, w=[dkey])
            to_feature_major(xn, xnkey, KC, None, dkey, ev)

        s_mask = ExitStack()
        maskT = sb(s_mask, "maskT", [128, NT, TOWN], BF16)
        with ExitStack() as pbc:
            hT = sb(pbc, "hT", [128, KC, TALL], BF16)
            with ExitStack() as pb:
                xt = [sb(pb, "xt%d" % i, [128, D], F32) for i in range(2)]
                xn = [sb(pb, "xn%d" % i, [128, D], BF16) for i in range(2)]
                for tt in range(NT):
                    i = tt % 2
                    S.dma('sp' if i == 0 else 'act', xt[i][:], xs[tt * 128:(tt + 1) * 128, :], w=['xt%d' % i])
                    norm_to_fm(xt[i][:], 'xt%d' % i, hT, 'hT', tt * 128, A1, sh1, ['A1', 'modfm'],
                               xn[i], 'xn%d' % i, small[:, i:i + 1])
                S.barrier()

            with ExitStack() as pc:
                alloc_wbuf(pc, 2)
                alloc_psum(7, 1, 0)
                set_mm_users(c=[0, 1, 2], d=[3, 4], e=[5, 6])
                sinD = sb(pc, "sinD", [128, NT, 1, 16], F32)
                cosD = sb(pc, "cosD", [128, NT, 1, 16], F32)
                sinI = sb(pc, "sinI", [128, NT, 1, 8], F32)
                cosI = sb(pc, "cosI", [128, NT, 1, 8], F32)
                ggs = [sb(pc, "ggs%d" % i, [128, 512], F32) for i in range(2)]
                rt = [sb(pc, "rt%d" % i, [128, 4, 16], F32) for i in range(4)]
                f32stg = sb(pc, "f32stg", [128, 512], F32)
                f32stg2 = sb(pc, "f32stg2", [128, 72], F32)
                ptab = ExitStack()
                posf = sb(ptab, "posf", [128, NT], F32)
                pos_i = sb(ptab, "pos_i", [128, NT], I32)
                ang = sb(ptab, "ang", [128, NT, 16], F32)
                kf = sb(ptab, "kf", [128, NT, 16], F32)
                ki = sb(ptab, "ki", [128, NT, 16], I32)
                kf2 = sb(ptab, "kf2", [128, NT, 16], F32)
                S.dma('sp', pos_i[:], posi, w=['pos_i'])
                S.op('dve', lambda v: v.tensor_copy(out=posf[:], in_=pos_i[:]), r=['pos_i'], w=['posf'])
                TWO_PI = 2.0 * math.pi

                def make_tables(invf, nj, sin_t, cos_t, key):
                    for tt in range(NT):
                        S.op('dve', lambda v: v.tensor_scalar(out=ang[:, tt, 0:nj], in0=invf, scalar1=posf[:, tt:tt + 1],
                                                              scalar2=None, op0=ALU.mult), r=['cst', 'posf', 'ang'], w=['ang'])
                    a = ang[:, :, 0:nj]
                    kk = kf[:, :, 0:nj]
                    mm_ = kf2[:, :, 0:nj]
                    S.op('dve', lambda v: v.tensor_scalar(out=kk, in0=a, scalar1=1.0 / TWO_PI, scalar2=None,
                                                          op0=ALU.mult), r=['ang'], w=['kf'])
                    S.op('dve', lambda v: v.tensor_copy(out=ki[:, :, 0:nj], in_=kk), r=['kf'], w=['ki'])
                    S.op('dve', lambda v: v.tensor_copy(out=kk, in_=ki[:, :, 0:nj]), r=['ki'], w=['kf'])
                    S.op('dve', lambda v: v.scalar_tensor_tensor(out=a, in0=kk, scalar=-TWO_PI, in1=a,
                                                                 op0=ALU.mult, op1=ALU.add), r=['kf', 'ang'], w=['ang'])
                    for shift, dst in ((0.0, sin_t), (math.pi / 2, cos_t)):
                        S.op('dve', lambda v: v.tensor_scalar(out=kk, in0=a, scalar1=shift, scalar2=None,
                                                              op0=ALU.add), r=['ang', 'kf'], w=['kf'])
                        for cmp, bound, sgn in ((ALU.is_gt, math.pi, -1.0), (ALU.is_lt, -math.pi, 1.0)):
                            S.op('dve', lambda v: v.tensor_scalar(out=mm_, in0=kk, scalar1=bound, scalar2=sgn * TWO_PI,
                                                                  op0=cmp, op1=ALU.mult), r=['kf'], w=['kf2'])
                            S.op('dve', lambda v: v.tensor_tensor(out=kk, in0=kk, in1=mm_, op=ALU.add),
                                 r=['kf', 'kf2'], w=['kf'])
                        S.op('act', lambda a_: a_.activation(out=dst[:, :, 0, :], in_=kk, func=AF.Sin),
                             r=['kf'], w=[key])
                make_tables(invf_d, 16, sinD, cosD, 'tabD')
                make_tables(invf_i, 8, sinI, cosI, 'tabI')
                S.barrier()
                ptab.close()
                S.op('dve', lambda v: v.memset(glrT[:, :], 1.0), w=['glrT'])
                wb, wkey = load_w(w_in, 0, KC, O_GLR, 16)
                for tg in range(4):
                    ps, pkey = next_mm()
                    for kc in range(KC):
                        S.op('pe', lambda p: p.matmul(ps[0:16, :], lhsT=wb[:, kc, 0:16], rhs=hT[:, kc, tg * 512:(tg + 1) * 512],
                                                      start=(kc == 0), stop=(kc == KC - 1)), r=['hT', wkey], w=[pkey])
                    S.op('act', lambda a: a.activation(out=glrT[0:16, tg * 512:(tg + 1) * 512], in_=ps[0:16, :], func=AF.Identity),
                         r=[pkey], w=['glrT'])

                if stop == 'C1':
                    S.barrier()
                    return nc
                own = list(range(8, 16))
                allt = list(range(NT))
                hact = lambda kc, tt: hT[:, kc, tt * 128:(tt + 1) * 128]

                def store(dst, own_only, c0, nb):
                    def f(tt, ps, pkey):
                        st, skey = next_stg()
                        S.op('act', lambda a: a.activation(out=st[:, 0:nb], in_=ps[:, 0:nb], func=AF.Identity), r=[pkey], w=[skey])
                        row = (tt - 8 if own_only else tt) * 128
                        S.dma('sp', dst[row:row + 128, c0:c0 + nb], st[:, 0:nb], r=[skey], w=[(id(dst), tt)])
                    return f

                def store_act(dst, c0, nb, func, mul=None):
                    def f(tt, ps, pkey):
                        st, skey = next_stg()
                        if mul is None:
                            S.op('act', lambda a: a.activation(out=st[:, 0:nb], in_=ps[:, 0:nb], func=func), r=[pkey], w=[skey])
                        else:
                            S.op('act', lambda a: a.activation(out=f32stg[:, 0:nb], in_=ps[:, 0:nb], func=func), r=[pkey], w=['f32stg'])
                            S.op('dve', lambda v: v.tensor_tensor(out=st[:, 0:nb], in0=f32stg[:, 0:nb], in1=mul[0][:, 0:nb],
                                                                  op=ALU.mult), r=['f32stg', mul[1]], w=[skey])
                        row = (tt - 8) * 128
                        S.dma('sp', dst[row:row + 128, c0:c0 + nb], st[:, 0:nb], r=[skey], w=[(id(dst), tt)])
                    return f

                def rope_ops(x1, x2, o1, o2, cs, sn, pkey, skey, tkey, shape):
                    t = [rt[i][:].rearrange("p a b -> p (a b)")[:, 0:shape[0] * shape[1]].rearrange("p (a b) -> p a b", b=shape[1])
                         for i in range(4)]
                    S.op('dve', lambda v: v.tensor_tensor(out=t[0], in0=x1, in1=cs, op=ALU.mult), r=[pkey, tkey], w=['rt0'])
                    S.op('dve', lambda v: v.tensor_tensor(out=t[1], in0=x2, in1=sn, op=ALU.mult), r=[pkey, tkey], w=['rt1'])
                    S.op('dve', lambda v: v.tensor_tensor(out=o1, in0=t[0], in1=t[1], op=ALU.subtract), r=['rt0', 'rt1'], w=[skey])
                    S.op('dve', lambda v: v.tensor_tensor(out=t[2], in0=x1, in1=sn, op=ALU.mult), r=[pkey, tkey], w=['rt2'])
                    S.op('dve', lambda v: v.tensor_tensor(out=t[3], in0=x2, in1=cs, op=ALU.mult), r=[pkey, tkey], w=['rt3'])
                    S.op('dve', lambda v: v.tensor_tensor(out=o2, in0=t[2], in1=t[3], op=ALU.add), r=['rt2', 'rt3'], w=[skey])

                def store_rope_d(dst, own_only, c0):
                    def f(tt, ps, pkey):
                        st, skey = next_stg()
                        S.op('act', lambda a: a.activation(out=st[:, :], in_=ps[:, :], func=AF.Identity), r=[pkey], w=[skey])
                        pv = ps[:, :].rearrange("p (h d) -> p h d", d=128)
                        sv = st[:, :].rearrange("p (h d) -> p h d", d=128)
                        rope_ops(pv[:, :, 0:16], pv[:, :, 16:32], sv[:, :, 0:16], sv[:, :, 16:32],
                                 cosD[:, tt, :, :].to_broadcast([128, 4, 16]), sinD[:, tt, :, :].to_broadcast([128, 4, 16]), pkey, skey, 'tabD', (4, 16))
                        row = (tt - 8 if own_only else tt) * 128
                        S.dma('sp', dst[row:row + 128, c0:c0 + 512], st[:, :], r=[skey], w=[(id(dst), tt)])
                    return f

                def store_iq(tt, ps, pkey):
                    st, skey = next_stg()
                    S.op('act', lambda a: a.activation(out=st[:, :], in_=ps[:, :], func=AF.Identity), r=[pkey], w=[skey])
                    pv = ps[:, :].rearrange("p (h d) -> p h d", d=64)
                    sv = st[:, :].rearrange("p (h d) -> p h d", d=64)
                    rope_ops(pv[:, :, 0:8], pv[:, :, 8:16], sv[:, :, 0:8], sv[:, :, 8:16],
                             cosI[:, tt, :, :].to_broadcast([128, 8, 8]), sinI[:, tt, :, :].to_broadcast([128, 8, 8]), pkey, skey, 'tabI', (8, 8))
                    row = (tt - 8) * 128
                    S.dma('sp', IQ[row:row + 128, :], st[:, :], r=[skey], w=[('IQ', tt)])

                def store_ikw(tt, ps, pkey):
                    S.op('act', lambda a: a.activation(out=f32stg2[:, :], in_=ps[:, 0:72], func=AF.Identity), r=[pkey], w=['f32stg2'])
                    rope_ops(ps[:, 0:8].rearrange("p (a b) -> p a b", a=1), ps[:, 8:16].rearrange("p (a b) -> p a b", a=1),
                             f32stg2[:, 0:8].rearrange("p (a b) -> p a b", a=1), f32stg2[:, 8:16].rearrange("p (a b) -> p a b", a=1),
                             cosI[:, tt, :, :], sinI[:, tt, :, :], pkey, 'f32stg2', 'tabI', (1, 8))
                    S.dma('sp', IKW[tt * 128:(tt + 1) * 128, :], f32stg2[:, :], r=['f32stg2'], w=[('IKW', tt)])

                def gen_D():
                    gu_aug = sb(pc, "gu_aug", [32, 1024], F32)
                    S.dma('sp', gu_aug[0:16, :], gate_up, w=['gu_aug'])
                    S.dma('sp', gu_aug[16:17, :], gate_bias, w=['gu_aug'])
                    Sst = sb(pc, "Sst", [128, 2, 512], F32)
                    Sbf = sb(pc, "Sbf", [128, 2, 512], BF16)
                    kt_ = [sb(pc, "kt%d" % i, [128, 256], BF16) for i in range(2)]
                    vt_ = [sb(pc, "vt%d" % i, [128, 512], BF16) for i in range(2)]
                    qt_ = [sb(pc, "qt%d" % i, [128, 256], BF16) for i in range(2)]
                    gs_ = [sb(pc, "gs%d" % i, [128, 512], BF16) for i in range(2)]
                    sp_ = sb(pc, "sp_", [128, 256], F32)
                    Epos = sb(pc, "Epos", [128, 256], F32)
                    Eneg = sb(pc, "Eneg", [128, 256], F32)
                    Etok = sb(pc, "Etok", [128, 256], F32)
                    ktok = sb(pc, "ktok", [128, 256], BF16)
                    kT_ = sb(pc, "kT_", [128, 256], BF16)
                    qT_ = sb(pc, "qT_", [128, 256], BF16)
                    attnT = sb(pc, "attnT", [128, 128], BF16)
                    junkD = sb(pc, "junkD", [128, 512], BF16)
                    oa_ = [sb(pc, "oa%d" % i, [128, 512], BF16) for i in range(2)]
                    for h in range(4):
                        S.op('dve', lambda v: v.memset(Sst[:], 0.0), w=['Sst'])
                        S.op('dve', lambda v: v.memset(Sbf[:], 0.0), w=['Sbf'])
                        for n in range(NT):
                            i = n % 2
                            ownt = n >= 8
                            r0 = n * 128
                            S.dma('sp', kt_[i][:], GK[r0:r0 + 128, h * 256:(h + 1) * 256], r=[(id(GK), n)], w=['kt%d' % i])
                            S.dma('act', vt_[i][:], GV[r0:r0 + 128, h * 512:(h + 1) * 512], r=[(id(GV), n)], w=['vt%d' % i])
                            if ownt:
                                q0 = (n - 8) * 128
                                S.dma('sp', qt_[i][:], GQ[q0:q0 + 128, h * 256:(h + 1) * 256], r=[(id(GQ), n)], w=['qt%d' % i])
                                S.dma('act', gs_[i][:], GR[q0:q0 + 128, h * 512:(h + 1) * 512], r=[(id(GR), n)], w=['gs%d' % i])
                            ps, pk = next_mm('d')
                            S.op('pe', lambda p: p.matmul(ps[:, 0:256], lhsT=glrT[0:17, r0:r0 + 128], rhs=gu_aug[0:17, h * 256:(h + 1) * 256],
                                                          start=True, stop=True), r=['glrT', 'gu_aug'], w=[pk])
                            S.op('act', lambda a: a.activation(out=sp_[:], in_=ps[:, 0:256], func=AF.Exp, scale=-1.0), r=[pk], w=['sp_'])
                            S.op('act', lambda a: a.activation(out=sp_[:], in_=sp_[:], func=AF.Ln, bias=1.0), r=['sp_'], w=['sp_'])
                            ps2, pk2 = next_mm('d')
                            for cc in range(2):
                                S.op('pe', lambda p: p.matmul(ps2[:, cc * 128:(cc + 1) * 128], lhsT=sp_[:, cc * 128:(cc + 1) * 128], rhs=triu,
                                                              start=True, stop=True), r=['sp_', 'cst'], w=[pk2])
                            ps3, pk3 = next_mm('d')
                            S.op('pe', lambda p: p.matmul(ps3[:, 0:256], lhsT=triu, rhs=sp_[:], start=True, stop=True), r=['sp_', 'cst'], w=[pk3])
                            S.op('act', lambda a: a.activation(out=Epos[:], in_=ps2[:, 0:256], func=AF.Exp, scale=-1.0 / 16), r=[pk2], w=['Epos'])
                            S.op('act', lambda a: a.activation(out=Eneg[:], in_=ps2[:, 0:256], func=AF.Exp, scale=1.0 / 16), r=[pk2], w=['Eneg'])
                            S.op('act', lambda a: a.activation(out=Etok[:], in_=ps3[:, 0:256], func=AF.Exp, scale=1.0 / 16), r=[pk3], w=['Etok'])
                            yield
                            tps, tk = next_tp()
                            for cc in range(2):
                                S.op('pe', lambda p: p.transpose(out=tps[:, cc * 128:(cc + 1) * 128], in_=kt_[i][:, cc * 128:(cc + 1) * 128],
                                                                 identity=ident[:]), r=['kt%d' % i, 'ident'], w=[tk])
                            if ownt:
                                for cc in range(2):
                                    S.op('pe', lambda p: p.transpose(out=tps[:, 256 + cc * 128:256 + (cc + 1) * 128],
                                                                     in_=qt_[i][:, cc * 128:(cc + 1) * 128], identity=ident[:]),
                                         r=['qt%d' % i, 'ident'], w=[tk])
                            S.op('dve', lambda v: v.tensor_tensor(out=kT_[:], in0=tps[:, 0:256], in1=Eneg[:], op=ALU.mult),
                                 r=[tk, 'Eneg'], w=['kT_'])
                            S.op('pool', lambda g: g.tensor_tensor(out=ktok[:], in0=kt_[i][:], in1=Etok[:], op=ALU.mult),
                                 r=['kt%d' % i, 'Etok'], w=['ktok'])
                            if ownt:
                                S.op('dve', lambda v: v.scalar_tensor_tensor(out=qT_[:], in0=tps[:, 256:512], scalar=1.0 / 16, in1=Epos[:],
                                                                             op0=ALU.mult, op1=ALU.mult), r=[tk, 'Epos'], w=['qT_'])
                                psA, pkA = next_mm('d')
                                for cc in range(2):
                                    S.op('pe', lambda p: p.matmul(psA[:, 0:128], lhsT=kT_[:, cc * 128:(cc + 1) * 128],
                                                                  rhs=qT_[:, cc * 128:(cc + 1) * 128], start=(cc == 0), stop=(cc == 1)),
                                         r=['kT_', 'qT_'], w=[pkA])
                                S.op('dve', lambda v: v.tensor_tensor(out=attnT[:], in0=psA[:, 0:128], in1=triu, op=ALU.mult),
                                     r=[pkA, 'cst'], w=['attnT'])
                                psO, pkO = next_mm('d')
                                S.op('pe', lambda p: p.matmul(psO[:, :], lhsT=attnT[:], rhs=vt_[i][:], start=True, stop=False),
                                     r=['attnT', 'vt%d' % i], w=[pkO])
                                for cc in range(2):
                                    S.op('pe', lambda p: p.matmul(psO[:, :], lhsT=qT_[:, cc * 128:(cc + 1) * 128], rhs=Sbf[:, cc, :],
                                                                  start=False, stop=(cc == 1)), r=['qT_', 'Sbf'], w=[pkO])
                                ssc = small[:, 4 + i:5 + i]
                                S.op('act', lambda a: a.activation(out=junkD[:], in_=psO[:, :], func=AF.Square, accum_out=ssc),
                                     r=[pkO], w=['junkD', 'smallD'])
                                rstd_from_ss(ssc, 512, 'smallD')
                                S.op('dve', lambda v: v.scalar_tensor_tensor(out=oa_[i][:], in0=psO[:, :], scalar=ssc, in1=gs_[i][:],
                                                                             op0=ALU.mult, op1=ALU.mult),
                                     r=[pkO, 'smallD', 'gs%d' % i], w=['oa%d' % i])
                                S.dma('sp', OA[q0:q0 + 128, h * 512:(h + 1) * 512], oa_[i][:], r=['oa%d' % i], w=[('OA', n)])
                            yield
                            for cc in range(2):
                                psU, pkU = next_mm('d')
                                S.op('pe', lambda p: p.matmul(psU[:, :], lhsT=ktok[:, cc * 128:(cc + 1) * 128], rhs=vt_[i][:],
                                                              start=True, stop=True), r=['ktok', 'vt%d' % i], w=[pkU])
                                S.op('dve', lambda v: v.tensor_tensor(out=Sst[:, cc, :], in0=psU[:, :], in1=Sst[:, cc, :], op=ALU.add),
                                     r=[pkU, 'Sst'], w=['Sst'])
                                S.op('dve', lambda v: v.tensor_scalar(out=Sst[:, cc, :], in0=Sst[:, cc, :],
                                                                      scalar1=Epos[:, cc * 128 + 127:cc * 128 + 128], scalar2=None, op0=ALU.mult),
                                     r=['Sst', 'Epos'], w=['Sst'])
                                if n == 7:
                                    S.op('dve', lambda v: v.tensor_scalar(out=Sst[:, cc, :], in0=Sst[:, cc, :], scalar1=ctxflag, scalar2=None,
                                                                          op0=ALU.mult), r=['Sst', 'cst'], w=['Sst'])
                                S.op('act', lambda a: a.activation(out=Sbf[:, cc, :], in_=Sst[:, cc, :], func=AF.Identity), r=['Sst'], w=['Sbf'])
                                yield


                def gen_E():
                    ikT2 = sb(pc, "ikT2", [128, TALL], BF16)
                    ikf = sb(pc, "ikf", [128, 72], F32)
                    ikd = sb(pc, "ikd", [128, 128], BF16)
                    iqs = sb(pc, "iqs", [128, 512], BF16)
                    iqT = sb(pc, "iqT", [128, 4, 128], BF16)
                    iwp = sb(pc, "iwp", [128, 8], F32)
                    score = sb(pc, "score", [128, TALL], F32)
                    relu_t = [sb(pc, "relu%d" % i, [128, 512], F32) for i in range(2)]
                    mask_tm = sb(pc, "mask_tm", [128, TALL], BF16)
                    S.op('dve', lambda v: v.memset(maskT[:], 0.0), w=['maskT'])
                    for kt in range(NT):
                        S.dma('sp', ikf[:], IKW[kt * 128:(kt + 1) * 128, :], r=[('IKW', kt)], w=['ikf'])
                        S.op('dve', lambda v: v.tensor_copy(out=ikd[:, 0:64], in_=ikf[:, 0:64]), r=['ikf'], w=['ikd'])
                        S.op('dve', lambda v: v.tensor_copy(out=ikd[:, 64:128], in_=ikf[:, 0:64]), r=['ikf'], w=['ikd'])
                        tps, tk = next_tp()
                        S.op('pe', lambda p: p.transpose(out=tps[:, 0:128], in_=ikd[:], identity=ident[:]), r=['ikd', 'ident'], w=[tk])
                        S.op('act', lambda a: a.activation(out=ikT2[:, kt * 128:(kt + 1) * 128], in_=tps[:, 0:128], func=AF.Identity),
                             r=[tk], w=['ikT2'])
                        yield
                    lo, hw, mid, cntv, gev, am = [small[:, 8 + j:9 + j] for j in range(6)]
                    for qi in range(8):
                        tt = 8 + qi
                        nk = 1024 + 128 * (qi + 1)
                        S.dma('sp', iqs[:], IQ[qi * 128:(qi + 1) * 128, :], r=[('IQ', tt)], w=['iqs'])
                        S.dma('act', ikf[:], IKW[tt * 128:(tt + 1) * 128, :], r=[('IKW', tt)], w=['ikf'])
                        tps, tk = next_tp()
                        for c in range(4):
                            S.op('pe', lambda p: p.transpose(out=tps[:, c * 128:(c + 1) * 128], in_=iqs[:, c * 128:(c + 1) * 128],
                                                             identity=ident[:]), r=['iqs', 'ident'], w=[tk])
                        S.op('act', lambda a: a.activation(out=iqT[:].rearrange("p a b -> p (a b)"), in_=tps[:, 0:512], func=AF.Identity),
                             r=[tk], w=['iqT'])
                        S.op('dve', lambda v: v.tensor_scalar(out=iwp[:], in0=ikf[:, 64:72], scalar1=float(8 ** -0.5 * 64 ** -0.5),
                                                              scalar2=None, op0=ALU.mult), r=['ikf'], w=['iwp'])
                        ng = (nk + 511) // 512
                        for g in range(ng):
                            wd_ = min(512, nk - g * 512)
                            for hh in range(8):
                                ps, pk = next_mm('e')
                                pb_ = (hh % 2) * 64
                                S.op('pe', lambda p: p.matmul(ps[:, 0:wd_], lhsT=iqT[pb_:pb_ + 64, hh // 2, :],
                                                              rhs=ikT2[pb_:pb_ + 64, g * 512:g * 512 + wd_], start=True, stop=True),
                                     r=['iqT', 'ikT2'], w=[pk])
                                rl = relu_t[hh % 2]
                                rk = 'relu%d' % (hh % 2)
                                S.op('act', lambda a: a.activation(out=rl[:, 0:wd_], in_=ps[:, 0:wd_], func=AF.Relu), r=[pk], w=[rk])
                                sc_ = score[:, g * 512:g * 512 + wd_]
                                if hh == 0:
                                    S.op('dve', lambda v: v.tensor_scalar(out=sc_, in0=rl[:, 0:wd_], scalar1=iwp[:, 0:1], scalar2=None,
                                                                          op0=ALU.mult), r=[rk, 'iwp'], w=['score'])
                                else:
                                    S.op('dve', lambda v: v.scalar_tensor_tensor(out=sc_, in0=rl[:, 0:wd_], scalar=iwp[:, hh:hh + 1],
                                                                                 in1=sc_, op0=ALU.mult, op1=ALU.add),
                                         r=[rk, 'iwp', 'score'], w=['score'])
                                yield
                        S.op('dve', lambda v: v.tensor_reduce(out=am, in_=score[:, 0:nk], axis=AX.X, op=ALU.max,
                                                              apply_absolute_value=True), r=['score'], w=['smallE'])
                        S.op('dve', lambda v: v.tensor_scalar(out=score[:, 0:1024], in0=score[:, 0:1024], scalar1=ctxneg, scalar2=None,
                                                              op0=ALU.add), r=['score', 'cst'], w=['score'])
                        S.op('dve', lambda v: v.tensor_tensor(out=score[:, nk - 128:nk], in0=score[:, nk - 128:nk], in1=cmask, op=ALU.add),
                             r=['score', 'cst'], w=['score'])
                        S.op('dve', lambda v: v.tensor_scalar(out=hw, in0=am, scalar1=1.0001, scalar2=1e-20, op0=ALU.mult, op1=ALU.add),
                             r=['smallE'], w=['smallE'])
                        S.op('dve', lambda v: v.tensor_scalar(out=lo, in0=hw, scalar1=-1.0, scalar2=None, op0=ALU.mult),
                             r=['smallE'], w=['smallE'])
                        for it in range(NBISECT):
                            S.op('dve', lambda v: v.tensor_tensor(out=mid, in0=lo, in1=hw, op=ALU.add), r=['smallE'], w=['smallE'])
                            S.op('dve', lambda v: v.tensor_scalar(out=junk[:, 0:nk], in0=score[:, 0:nk], scalar1=mid, scalar2=None,
                                                                  op0=ALU.is_ge, op1=ALU.add, accum_out=cntv),
                                 r=['score', 'smallE'], w=['junk', 'smallE'])
                            S.op('dve', lambda v: v.tensor_scalar(out=gev, in0=cntv, scalar1=TOPK - 0.5, scalar2=None, op0=ALU.is_ge),
                                 r=['smallE'], w=['smallE'])
                            S.op('dve', lambda v: v.scalar_tensor_tensor(out=lo, in0=hw, scalar=gev, in1=lo, op0=ALU.mult, op1=ALU.add),
                                 r=['smallE'], w=['smallE'])
                            S.op('dve', lambda v: v.tensor_scalar(out=hw, in0=hw, scalar1=0.5, scalar2=None, op0=ALU.mult),
                                 r=['smallE'], w=['smallE'])
                            yield
                        S.op('dve', lambda v: v.tensor_scalar(out=mask_tm[:, 0:nk], in0=score[:, 0:nk], scalar1=lo, scalar2=None,
                                                              op0=ALU.is_ge), r=['score', 'smallE'], w=['mask_tm'])
                        nkb = nk // 128
                        for c0 in range(0, nkb, 8):
                            n_ = min(8, nkb - c0)
                            tps, tk = next_tp()
                            for c in range(n_):
                                S.op('pe', lambda p: p.transpose(out=tps[:, c * 128:(c + 1) * 128],
                                                                 in_=mask_tm[:, (c0 + c) * 128:(c0 + c + 1) * 128], identity=ident[:]),
                                     r=['mask_tm', 'ident'], w=[tk])
                            S.op('act', lambda a: a.activation(out=maskT[:, c0:c0 + n_, qi * 128:(qi + 1) * 128],
                                                               in_=tps[:, 0:n_ * 128].rearrange("p (a b) -> p a b", b=128),
                                                               func=AF.Identity), r=[tk], w=['maskT'])
                            yield
                    yield

                linear(hact, 'hT', w_in, O_IQ, 512, own, store_iq)
                linear(hact, 'hT', w_in, O_IK, 72, allt, store_ikw)
                bg.append(gen_E())
                for cb in range(2):
                    linear(hact, 'hT', w_in, O_GQ + cb * 512, 512, own, store(GQ, True, cb * 512, 512))
                for cb in range(2):
                    linear(hact, 'hT', w_in, O_GK + cb * 512, 512, allt, store(GK, False, cb * 512, 512))
                for cb in range(4):
                    linear(hact, 'hT', w_in, O_GV + cb * 512, 512, allt, store(GV, False, cb * 512, 512))
                for cb in range(4):
                    gg = ggs[cb % 2]
                    S.dma('act', gg[:], gla_gain[0, cb * 512:(cb + 1) * 512].partition_broadcast(128), w=['ggs%d' % (cb % 2)])
                    linear(hact, 'hT', w_in, O_GR + cb * 512, 512, own, store_act(GR, cb * 512, 512, AF.Silu, mul=(gg, 'ggs%d' % (cb % 2))))
                bg.append(gen_D())
                for cb in range(4):
                    linear(hact, 'hT', w_in, O_DQ + cb * 512, 512, own, store_rope_d(DQ, True, cb * 512))
                for cb in range(4):
                    linear(hact, 'hT', w_in, O_DK + cb * 512, 512, allt, store_rope_d(DK, False, cb * 512))
                for cb in range(4):
                    linear(hact, 'hT', w_in, O_DV + cb * 512, 512, allt, store(DV, False, cb * 512, 512))
                for cb in range(4):
                    linear(hact, 'hT', w_in, O_GA + cb * 512, 512, own, store_act(GA, cb * 512, 512, AF.Sigmoid))
                for cb in range(4):
                    linear(hact, 'hT', w_in, O_GB + cb * 512, 512, own, store_act(GB, cb * 512, 512, AF.Sigmoid))
                bg_drain()
                S.barrier()
                if MTD is not None:
                    S.dma('sp', MTD, maskT[:], r=['maskT'], w=['MTD'])
                    S.barrier()
                if stop == 'C':
                    return nc
                set_mm_users()
        with ExitStack() as pefg:
            with ExitStack() as pef:

                with ExitStack() as pf:
                    kTg = sb(pf, "kTg", [128, 4, TALL], BF16)
                    vg = sb(pf, "vg", [128, NT, 512], BF16)
                    qTg = sb(pf, "qTg", [128, 4, TOWN], BF16)
                    ldt = [sb(pf, "ldt%d" % i, [128, 512], BF16) for i in range(2)]
                    pt_ = [sb(pf, "pt%d" % i, [128, 512], BF16) for i in range(4)]
                    pm_ = [sb(pf, "pm%d" % i, [128, 512], BF16) for i in range(4)]
                    alloc_psum(3, 1, 4)
                    lnd = sb(pf, "lnd", [128, 512], F32)
                    obs = [sb(pf, "obs%d" % i, [128, 512], BF16) for i in range(2)]
                    rden = sb(pf, "rden", [128, 512], F32)
                    for hg in range(4):
                        S.dma('act', vg[:], DV[:, hg * 512:(hg + 1) * 512].rearrange("(kt p) c -> p kt c", p=128), w=['vg'])
                        for kt in range(NT + 8):
                            i = kt % 2
                            if kt < NT:
                                S.dma('sp', ldt[i][:], DK[kt * 128:(kt + 1) * 128, hg * 512:(hg + 1) * 512], w=['ldt%d' % i])
                                dst = kTg[:, :, kt * 128:(kt + 1) * 128]
                                dk_ = 'kTg'
                            else:
                                qi = kt - NT
                                S.dma('sp', ldt[i][:], DQ[qi * 128:(qi + 1) * 128, hg * 512:(hg + 1) * 512], w=['ldt%d' % i])
                                dst = qTg[:, :, qi * 128:(qi + 1) * 128]
                                dk_ = 'qTg'
                            tps, tk = next_tp()
                            for c in range(4):
                                S.op('pe', lambda p: p.transpose(out=tps[:, c * 128:(c + 1) * 128], in_=ldt[i][:, c * 128:(c + 1) * 128],
                                                                 identity=ident[:]), r=['ldt%d' % i, 'ident'], w=[tk])
                            S.op('act' if kt % 2 == 0 else 'dve',
                                 (lambda a: a.activation(out=dst, in_=tps[:, 0:512].rearrange("p (a b) -> p a b", b=128), func=AF.Identity))
                                 if kt % 2 == 0 else
                                 (lambda v: v.tensor_copy(out=dst, in_=tps[:, 0:512].rearrange("p (a b) -> p a b", b=128))),
                                 r=[tk], w=[dk_])
                        steps = [(hh, qg, kb) for hh in range(4) for qg in range(2) for kb in range(8 + 4 * (qg + 1))]
                        LA = 2
                        slots = {}

                        def qk_stage(idx):
                            hh, qg, kb = steps[idx]
                            lps, lk = next_mm()
                            S.op('pe', lambda p: p.matmul(lps[:, :], lhsT=kTg[:, hh, kb * 128:(kb + 1) * 128],
                                                          rhs=qTg[:, hh, qg * 512:(qg + 1) * 512], start=True, stop=True),
                                 r=['kTg', 'qTg'], w=[lk])
                            j = idx % 4
                            slots[idx] = j
                            S.op('act', lambda a: a.activation(out=pt_[j][:], in_=lps[:, :], func=AF.Exp, scale=float(128 ** -0.5)),
                                 r=[lk], w=['pt%d' % j])
                            S.op('dve', lambda v: v.tensor_tensor(out=pm_[j][:], in0=pt_[j][:], in1=maskT[:, kb, qg * 512:(qg + 1) * 512],
                                                                  op=ALU.mult), r=['pt%d' % j, 'maskT'], w=['pm%d' % j])

                        def pv_stage(idx):
                            hh, qg, kb = steps[idx]
                            nkb = 8 + 4 * (qg + 1)
                            j = slots.pop(idx)
                            pr = (hh * 2 + qg) % 2
                            aO, aD = ax[2 * pr], ax[2 * pr + 1]
                            kO, kD = 'ax%d' % (2 * pr), 'ax%d' % (2 * pr + 1)
                            S.op('pe', lambda p: p.matmul(aO[:, :], lhsT=vg[:, kb, hh * 128:(hh + 1) * 128], rhs=pm_[j][:],
                                                          start=(kb == 0), stop=(kb == nkb - 1)), r=['vg', 'pm%d' % j], w=[kO])
                            S.op('pe', lambda p: p.matmul(aD[:, :], lhsT=ones_bf[:], rhs=pm_[j][:],
                                                          start=(kb == 0), stop=(kb == nkb - 1)), r=['ones_bf', 'pm%d' % j], w=[kD])
                            if kb == nkb - 1:
                                h = hg * 4 + hh
                                S.op('act', lambda a: a.activation(out=lnd[:], in_=aD[:, :], func=AF.Ln), r=[kD], w=['lnd'])
                                S.op('act', lambda a: a.activation(out=rden[:], in_=lnd[:], func=AF.Exp, scale=-1.0), r=['lnd'], w=['rden'])
                                jo = (hh * 2 + qg) % 2
                                S.op('dve', lambda v: v.tensor_tensor(out=obs[jo][:], in0=aO[:, :], in1=rden[:],
                                                                      op=ALU.mult), r=[kO, 'rden'], w=['obs%d' % jo])
                                S.dma('sp', OBD[h * 128:(h + 1) * 128, qg * 512:(qg + 1) * 512], obs[jo][:], r=['obs%d' % jo], w=[('OBD', h, qg)])

                        for idx in range(len(steps) + LA):
                            if idx < len(steps):
                                qk_stage(idx)
                            if idx - LA >= 0:
                                pv_stage(idx - LA)
                    S.barrier()
                    if stop == 'F':
                        return nc
                    alloc_psum(4, 2, 2)
            s_mask.close()

            with ExitStack() as pg1:
                o_aT = sb(pg1, "o_aT", [128, KC, TOWN], BF16)
                o_bT = sb(pg1, "o_bT", [128, KC, TOWN], BF16)
                S.dma('act', o_bT[:], OBD.rearrange("(h p) t -> p h t", p=128), w=['o_bT'])
                oat = [sb(pg1, "oat%d" % i, [128, D], BF16) for i in range(2)]
                gat = [sb(pg1, "gat%d" % i, [128, 512], BF16) for i in range(2)]
                gbt = [sb(pg1, "gbt%d" % i, [128, 512], BF16) for i in range(2)]
                m1 = sb(pg1, "m1", [128, 512], F32)
                m2 = sb(pg1, "m2", [128, 512], F32)
                mgs = [sb(pg1, "mgs%d" % i, [128, 512], BF16) for i in range(2)]
                alloc_wbuf(pg1, 4)
                for qi in range(8):
                    i = qi % 2
                    S.dma('sp', oat[i][:], OA[qi * 128:(qi + 1) * 128, :], w=['oat%d' % i])

                    def ev(c0, n, tps, tkey, qi=qi):
                        S.op('act', lambda a: a.activation(out=o_aT[:, c0:c0 + n, qi * 128:(qi + 1) * 128],
                                                           in_=tps[:, 0:n * 128].rearrange("p (a b) -> p a b", b=128),
                                                           func=AF.Identity), r=[tkey], w=['o_aT'])
                    to_feature_major(oat[i], 'oat%d' % i, KC, None, 'o_aT', ev)
                for cb in range(4):
                    wa, wak = load_w(w_ba, 0, KC, cb * 512, 512)
                    wd2, wdk = load_w(w_bd, 0, KC, cb * 512, 512)
                    for qi in range(8):
                        i = qi % 2
                        S.dma('sp', gat[i][:], GA[qi * 128:(qi + 1) * 128, cb * 512:(cb + 1) * 512], w=['gat%d' % i])
                        S.dma('act', gbt[i][:], GB[qi * 128:(qi + 1) * 128, cb * 512:(cb + 1) * 512], w=['gbt%d' % i])
                        psa, pka = next_mm()
                        for kc in range(KC):
                            S.op('pe', lambda p: p.matmul(psa[:, :], lhsT=o_aT[:, kc, qi * 128:(qi + 1) * 128], rhs=wa[:, kc, :],
                                                          start=(kc == 0), stop=(kc == KC - 1)), r=['o_aT', wak], w=[pka])
                        psb, pkb = next_mm()
                        for kc in range(KC):
                            S.op('pe', lambda p: p.matmul(psb[:, :], lhsT=o_bT[:, kc, qi * 128:(qi + 1) * 128], rhs=wd2[:, kc, :],
                                                          start=(kc == 0), stop=(kc == KC - 1)), r=['o_bT', wdk], w=[pkb])
                        S.op('dve', lambda v: v.tensor_tensor(out=m1[:], in0=psa[:, :], in1=gat[i][:], op=ALU.mult),
                             r=[pka, 'gat%d' % i], w=['m1'])
                        S.op('dve', lambda v: v.tensor_tensor(out=m2[:], in0=psb[:, :], in1=gbt[i][:], op=ALU.mult),
                             r=[pkb, 'gbt%d' % i], w=['m2'])
                        S.op('pool', lambda g: g.tensor_tensor(out=mgs[i][:], in0=m1[:], in1=m2[:], op=ALU.add),
                             r=['m1', 'm2'], w=['mgs%d' % i])
                        S.dma('sp', MG[qi * 128:(qi + 1) * 128, cb * 512:(cb + 1) * 512], mgs[i][:], r=['mgs%d' % i], w=[('MG', qi)])
                S.barrier()

        with ExitStack() as px:
            x1 = sb(px, "x1", [128, 8, D], F32)
            rowb = sb(px, "rowb", [128, D], F32)
            with ExitStack() as pg2:
                alloc_wbuf(pg2, 2)
                mergedT = sb(pg2, "mergedT", [128, KC, TOWN], BF16)
                mgt = [sb(pg2, "mgt%d" % i, [128, D], BF16) for i in range(2)]
                xres = [sb(pg2, "xres%d" % i, [128, 512], F32) for i in range(2)]
                tmpf = sb(pg2, "tmpf", [128, 512], F32)
                S.dma('act', rowb[:], modrow_d[0, 2 * D:3 * D].partition_broadcast(128), w=['rowb'])
                for qi in range(8):
                    i = qi % 2
                    S.dma('sp', mgt[i][:], MG[qi * 128:(qi + 1) * 128, :], w=['mgt%d' % i])

                    def ev2(c0, n, tps, tkey, qi=qi):
                        S.op('act', lambda a: a.activation(out=mergedT[:, c0:c0 + n, qi * 128:(qi + 1) * 128],
                                                           in_=tps[:, 0:n * 128].rearrange("p (a b) -> p a b", b=128),
                                                           func=AF.Identity), r=[tkey], w=['mergedT'])
                    to_feature_major(mgt[i], 'mgt%d' % i, KC, None, 'mergedT', ev2)

                def evac_mo(cb):
                    def f(tt, ps, pkey):
                        i = tt % 2
                        S.dma('sp', xres[i][:], xs[TOWN + tt * 128:TOWN + (tt + 1) * 128, cb * 512:(cb + 1) * 512], w=['xres%d' % i])
                        S.op('dve', lambda v: v.tensor_tensor(out=tmpf[:], in0=ps[:, :], in1=rowb[:, cb * 512:(cb + 1) * 512], op=ALU.mult),
                             r=[pkey, 'rowb'], w=['tmpf'])
                        S.op('dve', lambda v: v.tensor_tensor(out=x1[:, tt, cb * 512:(cb + 1) * 512], in0=tmpf[:], in1=xres[i][:], op=ALU.add),
                             r=['tmpf', 'xres%d' % i], w=[('x1', tt)])
                    return f
                for cb in range(4):
                    linear(lambda kc, tt: mergedT[:, kc, tt * 128:(tt + 1) * 128], 'mergedT', w_mo, cb * 512, 512, list(range(8)), evac_mo(cb))
                S.barrier()

            with ExitStack() as ph:
                alloc_wbuf(ph, 2, 11, 512)
                h2T = sb(ph, "h2T", [128, KC, TOWN], BF16)
                xn2 = [sb(ph, "xn2_%d" % i, [128, D], BF16) for i in range(2)]
                actT = sb(ph, "actT", [128, 11, TOWN], BF16)
                sg = [sb(ph, "sg%d" % i, [128, 512], F32) for i in range(2)]
                tmp2 = sb(ph, "tmp2", [128, 512], F32)
                gub = [sb(ph, "gub%d" % i, [128, KC, 128], BF16) for i in range(6)]
                rr['gu'] = 0

                def load_gu(c0):
                    i = rr['gu'] % 6
                    rr['gu'] += 1
                    S.dma('pool', gub[i][:], w_gu[:, c0:c0 + 128].rearrange("(kc p) n -> p kc n", p=128), w=['gub%d' % i])
                    return gub[i], 'gub%d' % i
                S.dma('act', rowb[:], modrow_d[0, 5 * D:6 * D].partition_broadcast(128), w=['rowb'])
                for qi in range(8):
                    i = qi % 2
                    norm_to_fm(x1[:, qi, :], ('x1', qi), h2T, 'h2T', qi * 128, A2, sh2, ['A2', 'modfm'],
                               xn2[i], 'xn2_%d' % i, small[:, 16 + i:17 + i])
                cnt_s = 0
                for fb in range(4):
                    for fc in range(11):
                        f0 = fb * 1408 + fc * 128
                        wg, wgk = load_gu(f0)
                        wu, wuk = load_gu(DFF + f0)
                        for tg in range(2):
                            psg, pkg = next_mm()
                            for kc in range(KC):
                                S.op('pe', lambda p: p.matmul(psg[:, :], lhsT=wg[:, kc, 0:128], rhs=h2T[:, kc, tg * 512:(tg + 1) * 512],
                                                              start=(kc == 0), stop=(kc == KC - 1)), r=['h2T', wgk], w=[pkg])
                            psu, pku = next_mm()
                            for kc in range(KC):
                                S.op('pe', lambda p: p.matmul(psu[:, :], lhsT=wu[:, kc, 0:128], rhs=h2T[:, kc, tg * 512:(tg + 1) * 512],
                                                              start=(kc == 0), stop=(kc == KC - 1)), r=['h2T', wuk], w=[pku])
                            j = cnt_s % 2
                            cnt_s += 1
                            S.op('act', lambda a: a.activation(out=sg[j][:], in_=psg[:, :], func=AF.Silu), r=[pkg], w=['sg%d' % j])
                            S.op('dve', lambda v: v.tensor_tensor(out=actT[:, fc, tg * 512:(tg + 1) * 512], in0=psu[:, :], in1=sg[j][:],
                                                                  op=ALU.mult), r=[pku, 'sg%d' % j], w=['actT'])

                    def evac_dn(cb):
                        def f(tt, ps, pkey):
                            S.op('dve', lambda v: v.tensor_tensor(out=tmp2[:], in0=ps[:, :], in1=rowb[:, cb * 512:(cb + 1) * 512], op=ALU.mult),
                                 r=[pkey, 'rowb'], w=['tmp2'])
                            S.op('pool', lambda g: g.tensor_tensor(out=x1[:, tt, cb * 512:(cb + 1) * 512], in0=x1[:, tt, cb * 512:(cb + 1) * 512],
                                                                   in1=tmp2[:], op=ALU.add), r=['tmp2', ('x1', tt)], w=[('x1', tt)])
                        return f
                    for cb in range(4):
                        linear(lambda kc, tt: actT[:, kc, tt * 128:(tt + 1) * 128], 'actT', w_dn, cb * 512, 512, list(range(8)),
                               evac_dn(cb), r0=fb * 1408, nk=11)
                S.barrier()

            with ExitStack() as pi_:
                ot = [sb(pi_, "ot%d" % i, [128, D], F32) for i in range(2)]
                S.dma('act', rowb[:], fng[0, :].partition_broadcast(128), w=['rowb'])
                for qi in range(8):
                    i = qi % 2
                    ssc = small[:, 20 + i:21 + i]
                    S.op('act', lambda a: a.activation(out=junk[:], in_=x1[:, qi, :], func=AF.Square, accum_out=ssc),
                         r=[('x1', qi)], w=['junk', 'small'])
                    rstd_from_ss(ssc, D, 'small')
                    S.op('dve', lambda v: v.scalar_tensor_tensor(out=ot[i][:], in0=x1[:, qi, :], scalar=ssc, in1=rowb[:],
                                                                 op0=ALU.mult, op1=ALU.mult), r=[('x1', qi), 'small', 'rowb'], w=['ot%d' % i])
                    S.dma('sp', out[qi * 128:(qi + 1) * 128, :], ot[i][:], r=['ot%d' % i], w=[('out', qi)])
                S.barrier()
        S.barrier()
    return nc


def _consts(half):
    c = np.zeros((128, 1024), np.float32)
    c[:, 0:128] = np.eye(128, dtype=np.float32)
    j = np.arange(128)
    c[:, 128:256] = (j[:, None] <= j[None, :]).astype(np.float32)
    c[:, 256:384] = np.where(j[None, :] <= j[:, None], 0.0, NEG)
    c[:, 384] = 1.0 if half == 1 else 0.0
    c[:, 385] = 0.0 if half == 1 else NEG
    theta = np.float32(500000.0)
    c[:, 400:416] = np.power(theta, -np.arange(0, 32, 2, dtype=np.float32) / np.float32(32))[None, :]
    c[:, 416:424] = np.power(theta, -np.arange(0, 16, 2, dtype=np.float32) / np.float32(16))[None, :]
    return c


def prep_inputs(inputs, cores=None):
    f = lambda a: np.ascontiguousarray(np.asarray(a))
    x = f(inputs["x"]); c = f(inputs["c"]); pos = f(inputs["positions"]).astype(np.int32)
    shared = {
        "w_ada": f(inputs["w_ada"])[0], "b_ada": f(inputs["b_ada"])[0][None, :], "w_in": f(inputs["w_in"])[0],
        "gate_up": f(inputs["gla_gate_up"])[0], "gate_bias": f(inputs["gla_gate_bias"])[0][None, :],
        "gla_gain": f(inputs["gla_norm_gain"])[0][None, :],
        "w_ba": f(inputs["w_branch_gla"])[0], "w_bd": f(inputs["w_branch_dsa"])[0], "w_mo": f(inputs["w_merge_out"])[0],
        "w_gu": f(inputs["w_ffn_gate_up"])[0], "w_dn": f(inputs["w_ffn_down"])[0],
        "n1g": np.ascontiguousarray(f(inputs["norm1_gain"])[0].reshape(16, 128).T),
        "n2g": np.ascontiguousarray(f(inputs["norm2_gain"])[0].reshape(16, 128).T),
        "fng": f(inputs["final_norm_gain"])[None, :],
    }
    maps = []
    for core in (range(8) if cores is None else cores):
        b, half = core // 2, core % 2
        if half == 1:
            xs = x[b]
            p = pos[b]
        else:
            xs = np.concatenate([np.zeros((TOWN, D), np.float32), x[b, :TOWN]], axis=0)
            p = np.concatenate([pos[b, :TOWN], pos[b, :TOWN]])
        m = dict(shared)
        m["xs"] = np.ascontiguousarray(xs)
        m["cfm"] = np.ascontiguousarray(c[b].reshape(16, 128).T)
        m["posi"] = np.ascontiguousarray(p.reshape(16, 128).T)
        m["cst"] = _consts(half)
        maps.append(m)
    return maps


_NC = None


def kernel(**inputs):
    global _NC
    if _NC is None:
        _NC = build_program()
    maps = prep_inputs(inputs)
    res = run_bass_kernel_spmd(_NC, maps, core_ids=list(range(8)))
    outp = np.zeros((NB, SEQ, D), np.float32)
    for core in range(8):
        b, half = core // 2, core % 2
        outp[b, half * TOWN:(half + 1) * TOWN] = res.results[core]["out"]
    return outp
```

```python
import math
from contextlib import ExitStack

import numpy as np
import concourse.bass as bass
import concourse.mybir as mybir
from concourse.bass_utils import run_bass_kernel_spmd

F32 = mybir.dt.float32
BF16 = mybir.dt.bfloat16
I32 = mybir.dt.int32
AF = mybir.ActivationFunctionType
ALU = mybir.AluOpType
AX = mybir.AxisListType

D = 2048
SEQ = 2048
NB = 4
TOWN = 1024
TALL = 2048
NT = 16
KC = 16
DFF = 5632
EPS = 1e-6
NEG = -1.0e30
TOPK = 256
NBISECT = 22

O_GQ, O_GK, O_GV, O_GR, O_GLR = 0, 1024, 2048, 4096, 6144
O_DQ, O_DK, O_DV = 6160, 8208, 10256
O_IQ, O_IK, O_IW = 12304, 12816, 12880
O_GA, O_GB = 12888, 14936
IN_W = 16984


class Sched:
    def __init__(self, nc, es, ndma=24):
        self.nc = nc
        self.eng = {'pe': nc.tensor, 'act': nc.scalar, 'dve': nc.vector, 'pool': nc.gpsimd, 'sp': nc.sync}
        self.semobj = {}
        for e in ['pe', 'act', 'dve', 'pool']:
            self.semobj[e] = es.enter_context(nc.semaphore('s_' + e))
        self.ndma = ndma
        for i in range(ndma):
            self.semobj[('d', i)] = es.enter_context(nc.semaphore('sd%d' % i))
            self.semobj[('g', i)] = es.enter_context(nc.semaphore('sg%d' % i))
        self.dma_rr_g = 0
        self.cnt = {k: 0 for k in self.semobj}
        self.seen = {e: {} for e in self.eng}
        self.lastw = {}
        self.readers = {}
        self.dma_rr = 0
        self.nwait = 0

    def _wait(self, e, k, v):
        if k == e and e == 'pe':
            return
        if self.seen[e].get(k, 0) >= v:
            return
        self.eng[e].wait_ge(self.semobj[k], v)
        self.seen[e][k] = v
        self.nwait += 1

    def _deps(self, e, r, w):
        for key in r:
            for k, v in self.lastw.get(key, {}).items():
                self._wait(e, k, v)
        for key in w:
            for k, v in self.lastw.get(key, {}).items():
                self._wait(e, k, v)
            for k, v in self.readers.get(key, {}).items():
                self._wait(e, k, v)

    def _record(self, ev, r, w):
        k, v = ev
        for key in r:
            d = self.readers.setdefault(key, {})
            d[k] = max(d.get(k, 0), v)
        for key in w:
            self.lastw[key] = {k: v}
            self.readers[key] = {}

    def op(self, e, fn, r=(), w=()):
        ex = [k for k in r if isinstance(k, str) and k[:2] in ('mm', 'tp', 'ax')]
        if ex:
            w = list(w) + [k for k in ex if k not in w]
        self._deps(e, r, w)
        ins = fn(self.eng[e])
        self.cnt[e] += 1
        ins.then_inc(self.semobj[e], 1)
        self._record((e, self.cnt[e]), r, w)

    def dma(self, q, out, in_, r=(), w=(), **kw):
        if q == 'pool':
            slot = ('g', self.dma_rr_g % self.ndma)
            self.dma_rr_g += 1
        else:
            slot = ('d', self.dma_rr % self.ndma)
            self.dma_rr += 1
        if self.cnt[slot] > 0:
            self._wait(q, slot, self.cnt[slot])
        self._deps(q, r, w)
        ins = self.eng[q].dma_start(out=out, in_=in_, **kw)
        self.cnt[slot] += 16
        ins.then_inc(self.semobj[slot], 16)
        self._record((slot, self.cnt[slot]), r, w)

    def barrier(self):
        for e in self.eng:
            for k in self.semobj:
                if self.cnt[k] > 0:
                    self._wait(e, k, self.cnt[k])
        self.lastw = {}
        self.readers = {}


def build_program(dbg=(), stop=None):
    nc = bass.Bass("TRN2", target_bir_lowering=False)
    import os
    stop = stop or os.environ.get('KSTOP')

    def din(name, shape, dt=F32):
        return nc.dram_tensor(name, list(shape), dt, kind="ExternalInput").ap()

    def dscr(name, shape, dt=BF16):
        kind = "ExternalOutput" if name in dbg else "Internal"
        return nc.dram_tensor(name, list(shape), dt, kind=kind).ap()

    xs = din("xs", [TALL, D])
    cfm = din("cfm", [128, KC])
    posi = din("posi", [128, NT], I32)
    w_ada = din("w_ada", [D, 6 * D])
    b_ada = din("b_ada", [1, 6 * D])
    w_in = din("w_in", [D, IN_W])
    gate_up = din("gate_up", [16, 1024])
    gate_bias = din("gate_bias", [1, 1024])
    gla_gain = din("gla_gain", [1, D])
    w_ba = din("w_ba", [D, D])
    w_bd = din("w_bd", [D, D])
    w_mo = din("w_mo", [D, D])
    w_gu = din("w_gu", [D, 2 * DFF])
    w_dn = din("w_dn", [DFF, D])
    n1g = din("n1g", [128, KC])
    n2g = din("n2g", [128, KC])
    fng = din("fng", [1, D])
    cst = din("cst", [128, 1024])
    out = nc.dram_tensor("out", [TOWN, D], F32, kind="ExternalOutput").ap()

    modrow_d = dscr("modrow_d", [1, 6 * D], F32)
    GQ = dscr("GQ", [TOWN, 1024])
    GK = dscr("GK", [TALL, 1024])
    GV = dscr("GV", [TALL, 2048])
    GR = dscr("GR", [TOWN, 2048])
    DQ = dscr("DQ", [TOWN, 2048])
    DK = dscr("DK", [TALL, 2048])
    DV = dscr("DV", [TALL, 2048])
    IQ = dscr("IQ", [TOWN, 512])
    IKW = dscr("IKW", [TALL, 72], F32)
    GA = dscr("GA", [TOWN, 2048])
    GB = dscr("GB", [TOWN, 2048])
    OA = dscr("OA", [TOWN, 2048])
    MG = dscr("MG", [TOWN, 2048])
    OBD = dscr("OBD", [D, TOWN])
    MTD = dscr("MTD", [128, NT, TOWN]) if "MTD" in dbg else None

    with ExitStack() as es:
        S = Sched(nc, es)

        def sb(stack, name, shape, dt):
            return stack.enter_context(nc.sbuf_tensor(name, list(shape), dt))

        mm, tp, ax = [], [], []
        pstack = [None]
        rr = {'mm': 0, 'tp': 0, 'stg': 0, 'wb': 0}

        def alloc_psum(nm, nt, na):
            if pstack[0] is not None:
                pstack[0].close()
            st = ExitStack()
            pstack[0] = st
            rr['pgen'] = rr.get('pgen', 0) + 1
            g = rr['pgen']
            mm[:] = [st.enter_context(nc.psum_tensor("mm%d_%d" % (i, g), [128, 512], F32)) for i in range(nm)]
            tp[:] = [st.enter_context(nc.psum_tensor("tp%d_%d" % (i, g), [128, 1024], BF16)) for i in range(nt)]
            ax[:] = [st.enter_context(nc.psum_tensor("ax%d_%d" % (i, g), [128, 512], F32)) for i in range(na)]

        alloc_psum(4, 2, 2)

        mm_users = {}

        def set_mm_users(**parts):
            mm_users.clear()
            mm_users.update(parts)

        def next_mm(user=None):
            banks = mm_users.get(user) if mm_users else None
            if banks is None:
                banks = list(range(len(mm)))
            c = rr.get(('mm', user), 0)
            rr[('mm', user)] = c + 1
            i = banks[c % len(banks)]
            return mm[i], 'mm%d' % i

        bg = []

        def bg_step():
            for g in list(bg):
                try:
                    next(g)
                except StopIteration:
                    bg.remove(g)

        def bg_drain():
            while bg:
                bg_step()

        def next_tp():
            i = rr['tp'] % len(tp)
            rr['tp'] += 1
            return tp[i], 'tp%d' % i

        cst_t = sb(es, "cst_t", [128, 1024], F32)
        S.dma('sp', cst_t[:], cst, w=['cst'])
        identf = cst_t[:, 0:128]
        triu = cst_t[:, 128:256]
        cmask = cst_t[:, 256:384]
        ctxflag = cst_t[:, 384:385]
        ctxneg = cst_t[:, 385:386]
        invf_d = cst_t[:, 400:416]
        invf_i = cst_t[:, 416:424]
        ident = sb(es, "ident", [128, 128], BF16)
        ones_bf = sb(es, "ones_bf", [128, 128], BF16)
        S.op('dve', lambda v: v.tensor_copy(out=ident[:], in_=identf), r=['cst'], w=['ident'])
        S.op('dve', lambda v: v.memset(ones_bf[:], 1.0), w=['ones_bf'])
        modfm = sb(es, "modfm", [128, 96], F32)
        A1 = sb(es, "A1", [128, KC], F32)
        A2 = sb(es, "A2", [128, KC], F32)
        n1g_t = sb(es, "n1g_t", [128, KC], F32)
        n2g_t = sb(es, "n2g_t", [128, KC], F32)
        S.dma('sp', n1g_t[:], n1g, w=['n1g'])
        S.dma('sp', n2g_t[:], n2g, w=['n2g'])
        wbuf = []

        def alloc_wbuf(stack, n, nk=KC, nb=512):
            rr['wgen'] = rr.get('wgen', 0) + 1
            wbuf[:] = [stack.enter_context(nc.sbuf_tensor("wbuf%d_%d" % (i, rr['wgen']), [128, nk, nb], BF16)) for i in range(n)]
        stg = [sb(es, "stg%d" % i, [128, 512], BF16) for i in range(4)]
        small = sb(es, "small", [128, 64], F32)
        junk = sb(es, "junk", [128, 2048], BF16)
        glrT = sb(es, "glrT", [32, TALL], F32)

        def next_stg():
            i = rr['stg'] % 4
            rr['stg'] += 1
            return stg[i], 'stg%d' % i

        def load_w(W, r0, nk, c0, nb, q='pool'):
            i = rr['wb'] % len(wbuf)
            rr['wb'] += 1
            key = 'wbuf%d' % i
            src = W[r0:r0 + nk * 128, c0:c0 + nb].rearrange("(kc p) n -> p kc n", p=128)
            S.dma(q, wbuf[i][:, 0:nk, 0:nb], src, w=[key])
            return wbuf[i], key

        def linear(actT, akey, W, c0, nb, tts, evac, r0=0, nk=KC, m=128, user='c'):
            wb, wkey = load_w(W, r0, nk, c0, nb)
            for tt in tts:
                bg_step()
                ps, pkey = next_mm(user)
                for kc in range(nk):
                    if kc == nk // 2:
                        bg_step()
                    S.op('pe', lambda p: p.matmul(ps[0:m, 0:nb], lhsT=actT(kc, tt), rhs=wb[:, kc, 0:nb],
                                                  start=(kc == 0), stop=(kc == nk - 1)),
                         r=[akey, wkey], w=[pkey])
                evac(tt, ps, pkey)

        def rstd_from_ss(ss_ap, n, key):
            S.op('dve', lambda v: v.tensor_scalar(out=ss_ap, in0=ss_ap, scalar1=1.0 / n, scalar2=EPS,
                                                  op0=ALU.mult, op1=ALU.add), r=[key], w=[key])
            S.op('act', lambda a: a.activation(out=ss_ap, in_=ss_ap, func=AF.Sqrt), r=[key], w=[key])
            S.op('dve', lambda v: v.reciprocal(out=ss_ap, in_=ss_ap), r=[key], w=[key])

        def to_feature_major(src_tile, skey, nchunk, dst_fn, dkey, evac_eng_fn):
            for c0 in range(0, nchunk, 8):
                n = min(8, nchunk - c0)
                tps, tkey = next_tp()
                for c in range(n):
                    S.op('pe', lambda p: p.transpose(out=tps[:, c * 128:(c + 1) * 128],
                                                     in_=src_tile[:, (c0 + c) * 128:(c0 + c + 1) * 128],
                                                     identity=ident[:]),
                         r=[skey, 'ident'], w=[tkey])
                evac_eng_fn(c0, n, tps, tkey)

        with ExitStack() as pa:
            alloc_wbuf(pa, 2)
            c_t = sb(pa, "c_t", [128, KC], F32)
            sT = sb(pa, "sT", [128, KC], BF16)
            brow = sb(pa, "brow", [1, 6 * D], F32)
            mrow = sb(pa, "mrow", [1, 6 * D], F32)
            S.dma('sp', c_t[:], cfm, w=['c_t'])
            S.dma('sp', brow[:], b_ada, w=['brow'])
            S.op('act', lambda a: a.activation(out=sT[:], in_=c_t[:], func=AF.Silu), r=['c_t'], w=['sT'])

            def evac_mod(cb):
                def f(tt, ps, pkey):
                    S.op('dve', lambda v: v.tensor_tensor(out=mrow[0:1, cb * 512:(cb + 1) * 512], in0=ps[0:1, :],
                                                          in1=brow[0:1, cb * 512:(cb + 1) * 512], op=ALU.add),
                         r=[pkey, 'brow'], w=['mrow'])
                return f
            for cb in range(24):
                linear(lambda kc, tt: sT[:, kc:kc + 1], 'sT', w_ada, cb * 512, 512, [0], evac_mod(cb), m=1)
            S.dma('sp', modrow_d, mrow[:], r=['mrow'], w=['modrow_d'])
            with nc.allow_non_contiguous_dma(reason="one-time 48KB relayout of the modulation vector"):
                S.dma('sp', modfm[:], modrow_d[0, :].rearrange("(j p) -> p j", p=128), r=['modrow_d'], w=['modfm'])
            S.op('dve', lambda v: v.scalar_tensor_tensor(out=A1[:], in0=modfm[:, 16:32], scalar=1.0, in1=n1g_t[:],
                                                         op0=ALU.add, op1=ALU.mult), r=['modfm', 'n1g'], w=['A1'])
            S.op('dve', lambda v: v.scalar_tensor_tensor(out=A2[:], in0=modfm[:, 64:80], scalar=1.0, in1=n2g_t[:],
                                                         op0=ALU.add, op1=ALU.mult), r=['modfm', 'n2g'], w=['A2'])
            S.barrier()
        if stop == 'A':
            return nc
        sh1 = modfm[:, 0:16]
        sh2 = modfm[:, 48:64]

        def norm_to_fm(x_tile, xkey, dstT, dkey, tcol, A, sh, akeys, xn, xnkey, ss_ap):
            S.op('act', lambda a: a.activation(out=junk[:], in_=x_tile, func=AF.Square, accum_out=ss_ap),
                 r=[xkey], w=['junk', 'small'])
            rstd_from_ss(ss_ap, D, 'small')
            S.op('dve', lambda v: v.tensor_scalar(out=xn[:], in0=x_tile, scalar1=ss_ap, scalar2=None, op0=ALU.mult),
                 r=[xkey, 'small'], w=[xnkey])

            def ev(c0, n, tps, tkey):
                for c in range(n):
                    kc = c0 + c
                    S.op('act', lambda a: a.activation(out=dstT[:, kc, tcol:tcol + 128], in_=tps[:, c * 128:(c + 1) * 128],
                                                       func=AF.Identity, scale=A[:, kc:kc + 1], bias=sh[:, kc:kc + 1]),
                         r=[tkey] + akeys, w=[dkey])
            to_feature_major(xn, xnkey, KC, None, dkey, ev)

        s_mask = ExitStack()
        maskT = sb(s_mask, "maskT", [128, NT, TOWN], BF16)
        with ExitStack() as pbc:
            hT = sb(pbc, "hT", [128, KC, TALL], BF16)
            with ExitStack() as pb:
                xt = [sb(pb, "xt%d" % i, [128, D], F32) for i in range(2)]
                xn = [sb(pb, "xn%d" % i, [128, D], BF16) for i in range(2)]
                for tt in range(NT):
                    i = tt % 2
                    S.dma('sp' if i == 0 else 'act', xt[i][:], xs[tt * 128:(tt + 1) * 128, :], w=['xt%d' % i])
                    norm_to_fm(xt[i][:], 'xt%d' % i, hT, 'hT', tt * 128, A1, sh1, ['A1', 'modfm'],
                               xn[i], 'xn%d' % i, small[:, i:i + 1])
                S.barrier()

            with ExitStack() as pc:
                alloc_wbuf(pc, 2)
                alloc_psum(7, 1, 0)
                set_mm_users(c=[0, 1, 2], d=[3, 4], e=[5, 6])
                sinD = sb(pc, "sinD", [128, NT, 1, 16], F32)
                cosD = sb(pc, "cosD", [128, NT, 1, 16], F32)
                sinI = sb(pc, "sinI", [128, NT, 1, 8], F32)
                cosI = sb(pc, "cosI", [128, NT, 1, 8], F32)
                ggs = [sb(pc, "ggs%d" % i, [128, 512], F32) for i in range(2)]
                rt = [sb(pc, "rt%d" % i, [128, 4, 16], F32) for i in range(4)]
                f32stg = sb(pc, "f32stg", [128, 512], F32)
                f32stg2 = sb(pc, "f32stg2", [128, 72], F32)
                ptab = ExitStack()
                posf = sb(ptab, "posf", [128, NT], F32)
                pos_i = sb(ptab, "pos_i", [128, NT], I32)
                ang = sb(ptab, "ang", [128, NT, 16], F32)
                kf = sb(ptab, "kf", [128, NT, 16], F32)
                ki = sb(ptab, "ki", [128, NT, 16], I32)
                kf2 = sb(ptab, "kf2", [128, NT, 16], F32)
                S.dma('sp', pos_i[:], posi, w=['pos_i'])
                S.op('dve', lambda v: v.tensor_copy(out=posf[:], in_=pos_i[:]), r=['pos_i'], w=['posf'])
                TWO_PI = 2.0 * math.pi

                def make_tables(invf, nj, sin_t, cos_t, key):
                    for tt in range(NT):
                        S.op('dve', lambda v: v.tensor_scalar(out=ang[:, tt, 0:nj], in0=invf, scalar1=posf[:, tt:tt + 1],
                                                              scalar2=None, op0=ALU.mult), r=['cst', 'posf', 'ang'], w=['ang'])
                    a = ang[:, :, 0:nj]
                    kk = kf[:, :, 0:nj]
                    mm_ = kf2[:, :, 0:nj]
                    S.op('dve', lambda v: v.tensor_scalar(out=kk, in0=a, scalar1=1.0 / TWO_PI, scalar2=None,
                                                          op0=ALU.mult), r=['ang'], w=['kf'])
                    S.op('dve', lambda v: v.tensor_copy(out=ki[:, :, 0:nj], in_=kk), r=['kf'], w=['ki'])
                    S.op('dve', lambda v: v.tensor_copy(out=kk, in_=ki[:, :, 0:nj]), r=['ki'], w=['kf'])
                    S.op('dve', lambda v: v.scalar_tensor_tensor(out=a, in0=kk, scalar=-TWO_PI, in1=a,
                                                                 op0=ALU.mult, op1=ALU.add), r=['kf', 'ang'], w=['ang'])
                    for shift, dst in ((0.0, sin_t), (math.pi / 2, cos_t)):
                        S.op('dve', lambda v: v.tensor_scalar(out=kk, in0=a, scalar1=shift, scalar2=None,
                                                              op0=ALU.add), r=['ang', 'kf'], w=['kf'])
                        for cmp, bound, sgn in ((ALU.is_gt, math.pi, -1.0), (ALU.is_lt, -math.pi, 1.0)):
                            S.op('dve', lambda v: v.tensor_scalar(out=mm_, in0=kk, scalar1=bound, scalar2=sgn * TWO_PI,
                                                                  op0=cmp, op1=ALU.mult), r=['kf'], w=['kf2'])
                            S.op('dve', lambda v: v.tensor_tensor(out=kk, in0=kk, in1=mm_, op=ALU.add),
                                 r=['kf', 'kf2'], w=['kf'])
                        S.op('act', lambda a_: a_.activation(out=dst[:, :, 0, :], in_=kk, func=AF.Sin),
                             r=['kf'], w=[key])
                make_tables(invf_d, 16, sinD, cosD, 'tabD')
                make_tables(invf_i, 8, sinI, cosI, 'tabI')
                S.barrier()
                ptab.close()
                S.op('dve', lambda v: v.memset(glrT[:, :], 1.0), w=['glrT'])
                wb, wkey = load_w(w_in, 0, KC, O_GLR, 16)
                for tg in range(4):
                    ps, pkey = next_mm()
                    for kc in range(KC):
                        S.op('pe', lambda p: p.matmul(ps[0:16, :], lhsT=wb[:, kc, 0:16], rhs=hT[:, kc, tg * 512:(tg + 1) * 512],
                                                      start=(kc == 0), stop=(kc == KC - 1)), r=['hT', wkey], w=[pkey])
                    S.op('act', lambda a: a.activation(out=glrT[0:16, tg * 512:(tg + 1) * 512], in_=ps[0:16, :], func=AF.Identity),
                         r=[pkey], w=['glrT'])

                if stop == 'C1':
                    S.barrier()
                    return nc
                own = list(range(8, 16))
                allt = list(range(NT))
                hact = lambda kc, tt: hT[:, kc, tt * 128:(tt + 1) * 128]

                def store(dst, own_only, c0, nb):
                    def f(tt, ps, pkey):
                        st, skey = next_stg()
                        S.op('act', lambda a: a.activation(out=st[:, 0:nb], in_=ps[:, 0:nb], func=AF.Identity), r=[pkey], w=[skey])
                        row = (tt - 8 if own_only else tt) * 128
                        S.dma('sp', dst[row:row + 128, c0:c0 + nb], st[:, 0:nb], r=[skey], w=[(id(dst), tt)])
                    return f

                def store_act(dst, c0, nb, func, mul=None):
                    def f(tt, ps, pkey):
                        st, skey = next_stg()
                        if mul is None:
                            S.op('act', lambda a: a.activation(out=st[:, 0:nb], in_=ps[:, 0:nb], func=func), r=[pkey], w=[skey])
                        else:
                            S.op('act', lambda a: a.activation(out=f32stg[:, 0:nb], in_=ps[:, 0:nb], func=func), r=[pkey], w=['f32stg'])
                            S.op('dve', lambda v: v.tensor_tensor(out=st[:, 0:nb], in0=f32stg[:, 0:nb], in1=mul[0][:, 0:nb],
                                                                  op=ALU.mult), r=['f32stg', mul[1]], w=[skey])
                        row = (tt - 8) * 128
                        S.dma('sp', dst[row:row + 128, c0:c0 + nb], st[:, 0:nb], r=[skey], w=[(id(dst), tt)])
                    return f

                def rope_ops(x1, x2, o1, o2, cs, sn, pkey, skey, tkey, shape):
                    t = [rt[i][:].rearrange("p a b -> p (a b)")[:, 0:shape[0] * shape[1]].rearrange("p (a b) -> p a b", b=shape[1])
                         for i in range(4)]
                    S.op('dve', lambda v: v.tensor_tensor(out=t[0], in0=x1, in1=cs, op=ALU.mult), r=[pkey, tkey], w=['rt0'])
                    S.op('dve', lambda v: v.tensor_tensor(out=t[1], in0=x2, in1=sn, op=ALU.mult), r=[pkey, tkey], w=['rt1'])
                    S.op('dve', lambda v: v.tensor_tensor(out=o1, in0=t[0], in1=t[1], op=ALU.subtract), r=['rt0', 'rt1'], w=[skey])
                    S.op('dve', lambda v: v.tensor_tensor(out=t[2], in0=x1, in1=sn, op=ALU.mult), r=[pkey, tkey], w=['rt2'])
                    S.op('dve', lambda v: v.tensor_tensor(out=t[3], in0=x2, in1=cs, op=ALU.mult), r=[pkey, tkey], w=['rt3'])
                    S.op('dve', lambda v: v.tensor_tensor(out=o2, in0=t[2], in1=t[3], op=ALU.add), r=['rt2', 'rt3'], w=[skey])

                def store_rope_d(dst, own_only, c0):
                    def f(tt, ps, pkey):
                        st, skey = next_stg()
                        S.op('act', lambda a: a.activation(out=st[:, :], in_=ps[:, :], func=AF.Identity), r=[pkey], w=[skey])
                        pv = ps[:, :].rearrange("p (h d) -> p h d", d=128)
                        sv = st[:, :].rearrange("p (h d) -> p h d", d=128)
                        rope_ops(pv[:, :, 0:16], pv[:, :, 16:32], sv[:, :, 0:16], sv[:, :, 16:32],
                                 cosD[:, tt, :, :].to_broadcast([128, 4, 16]), sinD[:, tt, :, :].to_broadcast([128, 4, 16]), pkey, skey, 'tabD', (4, 16))
                        row = (tt - 8 if own_only else tt) * 128
                        S.dma('sp', dst[row:row + 128, c0:c0 + 512], st[:, :], r=[skey], w=[(id(dst), tt)])
                    return f

                def store_iq(tt, ps, pkey):
                    st, skey = next_stg()
                    S.op('act', lambda a: a.activation(out=st[:, :], in_=ps[:, :], func=AF.Identity), r=[pkey], w=[skey])
                    pv = ps[:, :].rearrange("p (h d) -> p h d", d=64)
                    sv = st[:, :].rearrange("p (h d) -> p h d", d=64)
                    rope_ops(pv[:, :, 0:8], pv[:, :, 8:16], sv[:, :, 0:8], sv[:, :, 8:16],
                             cosI[:, tt, :, :].to_broadcast([128, 8, 8]), sinI[:, tt, :, :].to_broadcast([128, 8, 8]), pkey, skey, 'tabI', (8, 8))
                    row = (tt - 8) * 128
                    S.dma('sp', IQ[row:row + 128, :], st[:, :], r=[skey], w=[('IQ', tt)])

                def store_ikw(tt, ps, pkey):
                    S.op('act', lambda a: a.activation(out=f32stg2[:, :], in_=ps[:, 0:72], func=AF.Identity), r=[pkey], w=['f32stg2'])
                    rope_ops(ps[:, 0:8].rearrange("p (a b) -> p a b", a=1), ps[:, 8:16].rearrange("p (a b) -> p a b", a=1),
                             f32stg2[:, 0:8].rearrange("p (a b) -> p a b", a=1), f32stg2[:, 8:16].rearrange("p (a b) -> p a b", a=1),
                             cosI[:, tt, :, :], sinI[:, tt, :, :], pkey, 'f32stg2', 'tabI', (1, 8))
                    S.dma('sp', IKW[tt * 128:(tt + 1) * 128, :], f32stg2[:, :], r=['f32stg2'], w=[('IKW', tt)])

                def gen_D():
                    gu_aug = sb(pc, "gu_aug", [32, 1024], F32)
                    S.dma('sp', gu_aug[0:16, :], gate_up, w=['gu_aug'])
                    S.dma('sp', gu_aug[16:17, :], gate_bias, w=['gu_aug'])
                    Sst = sb(pc, "Sst", [128, 2, 512], F32)
                    Sbf = sb(pc, "Sbf", [128, 2, 512], BF16)
                    kt_ = [sb(pc, "kt%d" % i, [128, 256], BF16) for i in range(2)]
                    vt_ = [sb(pc, "vt%d" % i, [128, 512], BF16) for i in range(2)]
                    qt_ = [sb(pc, "qt%d" % i, [128, 256], BF16) for i in range(2)]
                    gs_ = [sb(pc, "gs%d" % i, [128, 512], BF16) for i in range(2)]
                    sp_ = sb(pc, "sp_", [128, 256], F32)
                    Epos = sb(pc, "Epos", [128, 256], F32)
                    Eneg = sb(pc, "Eneg", [128, 256], F32)
                    Etok = sb(pc, "Etok", [128, 256], F32)
                    ktok = sb(pc, "ktok", [128, 256], BF16)
                    kT_ = sb(pc, "kT_", [128, 256], BF16)
                    qT_ = sb(pc, "qT_", [128, 256], BF16)
                    attnT = sb(pc, "attnT", [128, 128], BF16)
                    junkD = sb(pc, "junkD", [128, 512], BF16)
                    oa_ = [sb(pc, "oa%d" % i, [128, 512], BF16) for i in range(2)]
                    for h in range(4):
                        S.op('dve', lambda v: v.memset(Sst[:], 0.0), w=['Sst'])
                        S.op('dve', lambda v: v.memset(Sbf[:], 0.0), w=['Sbf'])
                        for n in range(NT):
                            i = n % 2
                            ownt = n >= 8
                            r0 = n * 128
                            S.dma('sp', kt_[i][:], GK[r0:r0 + 128, h * 256:(h + 1) * 256], r=[(id(GK), n)], w=['kt%d' % i])
                            S.dma('act', vt_[i][:], GV[r0:r0 + 128, h * 512:(h + 1) * 512], r=[(id(GV), n)], w=['vt%d' % i])
                            if ownt:
                                q0 = (n - 8) * 128
                                S.dma('sp', qt_[i][:], GQ[q0:q0 + 128, h * 256:(h + 1) * 256], r=[(id(GQ), n)], w=['qt%d' % i])
                                S.dma('act', gs_[i][:], GR[q0:q0 + 128, h * 512:(h + 1) * 512], r=[(id(GR), n)], w=['gs%d' % i])
                            ps, pk = next_mm('d')
                            S.op('pe', lambda p: p.matmul(ps[:, 0:256], lhsT=glrT[0:17, r0:r0 + 128], rhs=gu_aug[0:17, h * 256:(h + 1) * 256],
                                                          start=True, stop=True), r=['glrT', 'gu_aug'], w=[pk])
                            S.op('act', lambda a: a.activation(out=sp_[:], in_=ps[:, 0:256], func=AF.Exp, scale=-1.0), r=[pk], w=['sp_'])
                            S.op('act', lambda a: a.activation(out=sp_[:], in_=sp_[:], func=AF.Ln, bias=1.0), r=['sp_'], w=['sp_'])
                            yield
                            ps2, pk2 = next_mm('d')
                            for cc in range(2):
                                S.op('pe', lambda p: p.matmul(ps2[:, cc * 128:(cc + 1) * 128], lhsT=sp_[:, cc * 128:(cc + 1) * 128], rhs=triu,
                                                              start=True, stop=True), r=['sp_', 'cst'], w=[pk2])
                            ps3, pk3 = next_mm('d')
                            S.op('pe', lambda p: p.matmul(ps3[:, 0:256], lhsT=triu, rhs=sp_[:], start=True, stop=True), r=['sp_', 'cst'], w=[pk3])
                            S.op('act', lambda a: a.activation(out=Epos[:], in_=ps2[:, 0:256], func=AF.Exp, scale=-1.0 / 16), r=[pk2], w=['Epos'])
                            S.op('act', lambda a: a.activation(out=Eneg[:], in_=ps2[:, 0:256], func=AF.Exp, scale=1.0 / 16), r=[pk2], w=['Eneg'])
                            S.op('act', lambda a: a.activation(out=Etok[:], in_=ps3[:, 0:256], func=AF.Exp, scale=1.0 / 16), r=[pk3], w=['Etok'])
                            yield
                            tpsf, tk = next_mm('d')
                            tps = tpsf[:, :].bitcast(BF16)
                            for cc in range(2):
                                S.op('pe', lambda p: p.transpose(out=tps[:, cc * 128:(cc + 1) * 128], in_=kt_[i][:, cc * 128:(cc + 1) * 128],
                                                                 identity=ident[:]), r=['kt%d' % i, 'ident'], w=[tk])
                            if ownt:
                                for cc in range(2):
                                    S.op('pe', lambda p: p.transpose(out=tps[:, 256 + cc * 128:256 + (cc + 1) * 128],
                                                                     in_=qt_[i][:, cc * 128:(cc + 1) * 128], identity=ident[:]),
                                         r=['qt%d' % i, 'ident'], w=[tk])
                            yield
                            S.op('dve', lambda v: v.tensor_tensor(out=kT_[:], in0=tps[:, 0:256], in1=Eneg[:], op=ALU.mult),
                                 r=[tk, 'Eneg'], w=['kT_'])
                            S.op('pool', lambda g: g.tensor_tensor(out=ktok[:], in0=kt_[i][:], in1=Etok[:], op=ALU.mult),
                                 r=['kt%d' % i, 'Etok'], w=['ktok'])
                            if ownt:
                                S.op('dve', lambda v: v.scalar_tensor_tensor(out=qT_[:], in0=tps[:, 256:512], scalar=1.0 / 16, in1=Epos[:],
                                                                             op0=ALU.mult, op1=ALU.mult), r=[tk, 'Epos'], w=['qT_'])
                                yield
                                psA, pkA = next_mm('d')
                                for cc in range(2):
                                    S.op('pe', lambda p: p.matmul(psA[:, 0:128], lhsT=kT_[:, cc * 128:(cc + 1) * 128],
                                                                  rhs=qT_[:, cc * 128:(cc + 1) * 128], start=(cc == 0), stop=(cc == 1)),
                                         r=['kT_', 'qT_'], w=[pkA])
                                S.op('dve', lambda v: v.tensor_tensor(out=attnT[:], in0=psA[:, 0:128], in1=triu, op=ALU.mult),
                                     r=[pkA, 'cst'], w=['attnT'])
                                yield
                                psO, pkO = next_mm('d')
                                S.op('pe', lambda p: p.matmul(psO[:, :], lhsT=attnT[:], rhs=vt_[i][:], start=True, stop=False),
                                     r=['attnT', 'vt%d' % i], w=[pkO])
                                for cc in range(2):
                                    S.op('pe', lambda p: p.matmul(psO[:, :], lhsT=qT_[:, cc * 128:(cc + 1) * 128], rhs=Sbf[:, cc, :],
                                                                  start=False, stop=(cc == 1)), r=['qT_', 'Sbf'], w=[pkO])
                                ssc = small[:, 4 + i:5 + i]
                                S.op('act', lambda a: a.activation(out=junkD[:], in_=psO[:, :], func=AF.Square, accum_out=ssc),
                                     r=[pkO], w=['junkD', 'smallD'])
                                rstd_from_ss(ssc, 512, 'smallD')
                                S.op('dve', lambda v: v.scalar_tensor_tensor(out=oa_[i][:], in0=psO[:, :], scalar=ssc, in1=gs_[i][:],
                                                                             op0=ALU.mult, op1=ALU.mult),
                                     r=[pkO, 'smallD', 'gs%d' % i], w=['oa%d' % i])
                                S.dma('sp', OA[q0:q0 + 128, h * 512:(h + 1) * 512], oa_[i][:], r=['oa%d' % i], w=[('OA', n)])
                            yield
                            for cc in range(2):
                                psU, pkU = next_mm('d')
                                S.op('pe', lambda p: p.matmul(psU[:, :], lhsT=ktok[:, cc * 128:(cc + 1) * 128], rhs=vt_[i][:],
                                                              start=True, stop=True), r=['ktok', 'vt%d' % i], w=[pkU])
                                S.op('dve', lambda v: v.tensor_tensor(out=Sst[:, cc, :], in0=psU[:, :], in1=Sst[:, cc, :], op=ALU.add),
                                     r=[pkU, 'Sst'], w=['Sst'])
                                S.op('dve', lambda v: v.tensor_scalar(out=Sst[:, cc, :], in0=Sst[:, cc, :],
                                                                      scalar1=Epos[:, cc * 128 + 127:cc * 128 + 128], scalar2=None, op0=ALU.mult),
                                     r=['Sst', 'Epos'], w=['Sst'])
                                if n == 7:
                                    S.op('dve', lambda v: v.tensor_scalar(out=Sst[:, cc, :], in0=Sst[:, cc, :], scalar1=ctxflag, scalar2=None,
                                                                          op0=ALU.mult), r=['Sst', 'cst'], w=['Sst'])
                                S.op('act', lambda a: a.activation(out=Sbf[:, cc, :], in_=Sst[:, cc, :], func=AF.Identity), r=['Sst'], w=['Sbf'])
                                yield


                def gen_E():
                    ikT2 = sb(pc, "ikT2", [128, TALL], BF16)
                    ikf = sb(pc, "ikf", [128, 72], F32)
                    ikd = sb(pc, "ikd", [128, 128], BF16)
                    iqs = sb(pc, "iqs", [128, 512], BF16)
                    iqT = sb(pc, "iqT", [128, 4, 128], BF16)
                    iwp = sb(pc, "iwp", [128, 8], F32)
                    score = sb(pc, "score", [128, TALL], F32)
                    relu_t = [sb(pc, "relu%d" % i, [128, 512], F32) for i in range(2)]
                    mask_tm = sb(pc, "mask_tm", [128, TALL], BF16)
                    S.op('dve', lambda v: v.memset(maskT[:], 0.0), w=['maskT'])
                    for kt in range(NT):
                        S.dma('sp', ikf[:], IKW[kt * 128:(kt + 1) * 128, :], r=[('IKW', kt)], w=['ikf'])
                        S.op('dve', lambda v: v.tensor_copy(out=ikd[:, 0:64], in_=ikf[:, 0:64]), r=['ikf'], w=['ikd'])
                        S.op('dve', lambda v: v.tensor_copy(out=ikd[:, 64:128], in_=ikf[:, 0:64]), r=['ikf'], w=['ikd'])
                        tps, tk = next_tp()
                        S.op('pe', lambda p: p.transpose(out=tps[:, 0:128], in_=ikd[:], identity=ident[:]), r=['ikd', 'ident'], w=[tk])
                        S.op('act', lambda a: a.activation(out=ikT2[:, kt * 128:(kt + 1) * 128], in_=tps[:, 0:128], func=AF.Identity),
                             r=[tk], w=['ikT2'])
                        yield
                    lo, hw, mid, cntv, gev, am = [small[:, 8 + j:9 + j] for j in range(6)]
                    for qi in range(8):
                        tt = 8 + qi
                        nk = 1024 + 128 * (qi + 1)
                        S.dma('sp', iqs[:], IQ[qi * 128:(qi + 1) * 128, :], r=[('IQ', tt)], w=['iqs'])
                        S.dma('act', ikf[:], IKW[tt * 128:(tt + 1) * 128, :], r=[('IKW', tt)], w=['ikf'])
                        yield
                        tps, tk = next_tp()
                        for c in range(4):
                            S.op('pe', lambda p: p.transpose(out=tps[:, c * 128:(c + 1) * 128], in_=iqs[:, c * 128:(c + 1) * 128],
                                                             identity=ident[:]), r=['iqs', 'ident'], w=[tk])
                        S.op('act', lambda a: a.activation(out=iqT[:].rearrange("p a b -> p (a b)"), in_=tps[:, 0:512], func=AF.Identity),
                             r=[tk], w=['iqT'])
                        S.op('dve', lambda v: v.tensor_scalar(out=iwp[:], in0=ikf[:, 64:72], scalar1=float(8 ** -0.5 * 64 ** -0.5),
                                                              scalar2=None, op0=ALU.mult), r=['ikf'], w=['iwp'])
                        ng = (nk + 511) // 512
                        for g in range(ng):
                            wd_ = min(512, nk - g * 512)
                            for hh in range(8):
                                ps, pk = next_mm('e')
                                pb_ = (hh % 2) * 64
                                S.op('pe', lambda p: p.matmul(ps[:, 0:wd_], lhsT=iqT[pb_:pb_ + 64, hh // 2, :],
                                                              rhs=ikT2[pb_:pb_ + 64, g * 512:g * 512 + wd_], start=True, stop=True),
                                     r=['iqT', 'ikT2'], w=[pk])
                                rl = relu_t[hh % 2]
                                rk = 'relu%d' % (hh % 2)
                                S.op('act', lambda a: a.activation(out=rl[:, 0:wd_], in_=ps[:, 0:wd_], func=AF.Relu), r=[pk], w=[rk])
                                sc_ = score[:, g * 512:g * 512 + wd_]
                                if hh == 0:
                                    S.op('dve', lambda v: v.tensor_scalar(out=sc_, in0=rl[:, 0:wd_], scalar1=iwp[:, 0:1], scalar2=None,
                                                                          op0=ALU.mult), r=[rk, 'iwp'], w=['score'])
                                else:
                                    S.op('dve', lambda v: v.scalar_tensor_tensor(out=sc_, in0=rl[:, 0:wd_], scalar=iwp[:, hh:hh + 1],
                                                                                 in1=sc_, op0=ALU.mult, op1=ALU.add),
                                         r=[rk, 'iwp', 'score'], w=['score'])
                                yield
                        S.op('dve', lambda v: v.tensor_reduce(out=am, in_=score[:, 0:nk], axis=AX.X, op=ALU.max,
                                                              apply_absolute_value=True), r=['score'], w=['smallE'])
                        S.op('dve', lambda v: v.tensor_scalar(out=score[:, 0:1024], in0=score[:, 0:1024], scalar1=ctxneg, scalar2=None,
                                                              op0=ALU.add), r=['score', 'cst'], w=['score'])
                        S.op('dve', lambda v: v.tensor_tensor(out=score[:, nk - 128:nk], in0=score[:, nk - 128:nk], in1=cmask, op=ALU.add),
                             r=['score', 'cst'], w=['score'])
                        S.op('dve', lambda v: v.tensor_scalar(out=hw, in0=am, scalar1=1.0001, scalar2=1e-20, op0=ALU.mult, op1=ALU.add),
                             r=['smallE'], w=['smallE'])
                        S.op('dve', lambda v: v.tensor_scalar(out=lo, in0=hw, scalar1=-1.0, scalar2=None, op0=ALU.mult),
                             r=['smallE'], w=['smallE'])
                        for it in range(NBISECT):
                            S.op('dve', lambda v: v.tensor_tensor(out=mid, in0=lo, in1=hw, op=ALU.add), r=['smallE'], w=['smallE'])
                            S.op('dve', lambda v: v.tensor_scalar(out=junk[:, 0:nk], in0=score[:, 0:nk], scalar1=mid, scalar2=None,
                                                                  op0=ALU.is_ge, op1=ALU.add, accum_out=cntv),
                                 r=['score', 'smallE'], w=['junk', 'smallE'])
                            S.op('dve', lambda v: v.tensor_scalar(out=gev, in0=cntv, scalar1=TOPK - 0.5, scalar2=None, op0=ALU.is_ge),
                                 r=['smallE'], w=['smallE'])
                            S.op('dve', lambda v: v.scalar_tensor_tensor(out=lo, in0=hw, scalar=gev, in1=lo, op0=ALU.mult, op1=ALU.add),
                                 r=['smallE'], w=['smallE'])
                            S.op('dve', lambda v: v.tensor_scalar(out=hw, in0=hw, scalar1=0.5, scalar2=None, op0=ALU.mult),
                                 r=['smallE'], w=['smallE'])
                            yield
                        S.op('dve', lambda v: v.tensor_scalar(out=mask_tm[:, 0:nk], in0=score[:, 0:nk], scalar1=lo, scalar2=None,
                                                              op0=ALU.is_ge), r=['score', 'smallE'], w=['mask_tm'])
                        nkb = nk // 128
                        for c0 in range(0, nkb, 8):
                            n_ = min(8, nkb - c0)
                            yield
                            tps, tk = next_tp()
                            for c in range(n_):
                                S.op('pe', lambda p: p.transpose(out=tps[:, c * 128:(c + 1) * 128],
                                                                 in_=mask_tm[:, (c0 + c) * 128:(c0 + c + 1) * 128], identity=ident[:]),
                                     r=['mask_tm', 'ident'], w=[tk])
                            S.op('act', lambda a: a.activation(out=maskT[:, c0:c0 + n_, qi * 128:(qi + 1) * 128],
                                                               in_=tps[:, 0:n_ * 128].rearrange("p (a b) -> p a b", b=128),
                                                               func=AF.Identity), r=[tk], w=['maskT'])
                            yield
                    yield

                linear(hact, 'hT', w_in, O_IQ, 512, own, store_iq)
                linear(hact, 'hT', w_in, O_IK, 72, allt, store_ikw)
                bg.append(gen_E())
                for cb in range(2):
                    linear(hact, 'hT', w_in, O_GQ + cb * 512, 512, own, store(GQ, True, cb * 512, 512))
                for cb in range(2):
                    linear(hact, 'hT', w_in, O_GK + cb * 512, 512, allt, store(GK, False, cb * 512, 512))
                for cb in range(4):
                    linear(hact, 'hT', w_in, O_GV + cb * 512, 512, allt, store(GV, False, cb * 512, 512))
                for cb in range(4):
                    gg = ggs[cb % 2]
                    S.dma('act', gg[:], gla_gain[0, cb * 512:(cb + 1) * 512].partition_broadcast(128), w=['ggs%d' % (cb % 2)])
                    linear(hact, 'hT', w_in, O_GR + cb * 512, 512, own, store_act(GR, cb * 512, 512, AF.Silu, mul=(gg, 'ggs%d' % (cb % 2))))
                bg.append(gen_D())
                for cb in range(4):
                    linear(hact, 'hT', w_in, O_DQ + cb * 512, 512, own, store_rope_d(DQ, True, cb * 512))
                for cb in range(4):
                    linear(hact, 'hT', w_in, O_DK + cb * 512, 512, allt, store_rope_d(DK, False, cb * 512))
                for cb in range(4):
                    linear(hact, 'hT', w_in, O_DV + cb * 512, 512, allt, store(DV, False, cb * 512, 512))
                for cb in range(4):
                    linear(hact, 'hT', w_in, O_GA + cb * 512, 512, own, store_act(GA, cb * 512, 512, AF.Sigmoid))
                for cb in range(4):
                    linear(hact, 'hT', w_in, O_GB + cb * 512, 512, own, store_act(GB, cb * 512, 512, AF.Sigmoid))
                bg_drain()
                S.barrier()
                if MTD is not None:
                    S.dma('sp', MTD, maskT[:], r=['maskT'], w=['MTD'])
                    S.barrier()
                if stop == 'C':
                    return nc
                set_mm_users()
        with ExitStack() as pefg:
            with ExitStack() as pef:

                with ExitStack() as pf:
                    kTg = sb(pf, "kTg", [128, 4, TALL], BF16)
                    vg = sb(pf, "vg", [128, NT, 512], BF16)
                    qTg = sb(pf, "qTg", [128, 4, TOWN], BF16)
                    ldt = [sb(pf, "ldt%d" % i, [128, 512], BF16) for i in range(2)]
                    pt_ = [sb(pf, "pt%d" % i, [128, 512], BF16) for i in range(4)]
                    pm_ = [sb(pf, "pm%d" % i, [128, 512], BF16) for i in range(4)]
                    alloc_psum(3, 1, 4)
                    lnd = sb(pf, "lnd", [128, 512], F32)
                    obs = [sb(pf, "obs%d" % i, [128, 512], BF16) for i in range(2)]
                    rden = sb(pf, "rden", [128, 512], F32)
                    for hg in range(4):
                        S.dma('act', vg[:], DV[:, hg * 512:(hg + 1) * 512].rearrange("(kt p) c -> p kt c", p=128), w=['vg'])
                        for kt in range(NT + 8):
                            i = kt % 2
                            if kt < NT:
                                S.dma('sp', ldt[i][:], DK[kt * 128:(kt + 1) * 128, hg * 512:(hg + 1) * 512], w=['ldt%d' % i])
                                dst = kTg[:, :, kt * 128:(kt + 1) * 128]
                                dk_ = 'kTg'
                            else:
                                qi = kt - NT
                                S.dma('sp', ldt[i][:], DQ[qi * 128:(qi + 1) * 128, hg * 512:(hg + 1) * 512], w=['ldt%d' % i])
                                dst = qTg[:, :, qi * 128:(qi + 1) * 128]
                                dk_ = 'qTg'
                            tps, tk = next_tp()
                            for c in range(4):
                                S.op('pe', lambda p: p.transpose(out=tps[:, c * 128:(c + 1) * 128], in_=ldt[i][:, c * 128:(c + 1) * 128],
                                                                 identity=ident[:]), r=['ldt%d' % i, 'ident'], w=[tk])
                            S.op('act' if kt % 2 == 0 else 'dve',
                                 (lambda a: a.activation(out=dst, in_=tps[:, 0:512].rearrange("p (a b) -> p a b", b=128), func=AF.Identity))
                                 if kt % 2 == 0 else
                                 (lambda v: v.tensor_copy(out=dst, in_=tps[:, 0:512].rearrange("p (a b) -> p a b", b=128))),
                                 r=[tk], w=[dk_])
                        steps = [(hh, qg, kb) for hh in range(4) for qg in range(2) for kb in range(8 + 4 * (qg + 1))]
                        LA = 2
                        slots = {}

                        def qk_stage(idx):
                            hh, qg, kb = steps[idx]
                            lps, lk = next_mm()
                            S.op('pe', lambda p: p.matmul(lps[:, :], lhsT=kTg[:, hh, kb * 128:(kb + 1) * 128],
                                                          rhs=qTg[:, hh, qg * 512:(qg + 1) * 512], start=True, stop=True),
                                 r=['kTg', 'qTg'], w=[lk])
                            j = idx % 4
                            slots[idx] = j
                            S.op('act', lambda a: a.activation(out=pt_[j][:], in_=lps[:, :], func=AF.Exp, scale=float(128 ** -0.5)),
                                 r=[lk], w=['pt%d' % j])
                            S.op('dve', lambda v: v.tensor_tensor(out=pm_[j][:], in0=pt_[j][:], in1=maskT[:, kb, qg * 512:(qg + 1) * 512],
                                                                  op=ALU.mult), r=['pt%d' % j, 'maskT'], w=['pm%d' % j])

                        def pv_stage(idx):
                            hh, qg, kb = steps[idx]
                            nkb = 8 + 4 * (qg + 1)
                            j = slots.pop(idx)
                            pr = (hh * 2 + qg) % 2
                            aO, aD = ax[2 * pr], ax[2 * pr + 1]
                            kO, kD = 'ax%d' % (2 * pr), 'ax%d' % (2 * pr + 1)
                            S.op('pe', lambda p: p.matmul(aO[:, :], lhsT=vg[:, kb, hh * 128:(hh + 1) * 128], rhs=pm_[j][:],
                                                          start=(kb == 0), stop=(kb == nkb - 1)), r=['vg', 'pm%d' % j], w=[kO])
                            S.op('pe', lambda p: p.matmul(aD[:, :], lhsT=ones_bf[:], rhs=pm_[j][:],
                                                          start=(kb == 0), stop=(kb == nkb - 1)), r=['ones_bf', 'pm%d' % j], w=[kD])
                            if kb == nkb - 1:
                                h = hg * 4 + hh
                                S.op('act', lambda a: a.activation(out=lnd[:], in_=aD[:, :], func=AF.Ln), r=[kD], w=['lnd'])
                                S.op('act', lambda a: a.activation(out=rden[:], in_=lnd[:], func=AF.Exp, scale=-1.0), r=['lnd'], w=['rden'])
                                jo = (hh * 2 + qg) % 2
                                S.op('dve', lambda v: v.tensor_tensor(out=obs[jo][:], in0=aO[:, :], in1=rden[:],
                                                                      op=ALU.mult), r=[kO, 'rden'], w=['obs%d' % jo])
                                S.dma('sp', OBD[h * 128:(h + 1) * 128, qg * 512:(qg + 1) * 512], obs[jo][:], r=['obs%d' % jo], w=[('OBD', h, qg)])

                        for idx in range(len(steps) + LA):
                            if idx < len(steps):
                                qk_stage(idx)
                            if idx - LA >= 0:
                                pv_stage(idx - LA)
                    S.barrier()
                    if stop == 'F':
                        return nc
                    alloc_psum(4, 2, 2)
            s_mask.close()

            with ExitStack() as pg1:
                o_aT = sb(pg1, "o_aT", [128, KC, TOWN], BF16)
                o_bT = sb(pg1, "o_bT", [128, KC, TOWN], BF16)
                S.dma('act', o_bT[:], OBD.rearrange("(h p) t -> p h t", p=128), w=['o_bT'])
                oat = [sb(pg1, "oat%d" % i, [128, D], BF16) for i in range(2)]
                gat = [sb(pg1, "gat%d" % i, [128, 512], BF16) for i in range(2)]
                gbt = [sb(pg1, "gbt%d" % i, [128, 512], BF16) for i in range(2)]
                m1 = sb(pg1, "m1", [128, 512], F32)
                m2 = sb(pg1, "m2", [128, 512], F32)
                mgs = [sb(pg1, "mgs%d" % i, [128, 512], BF16) for i in range(2)]
                alloc_wbuf(pg1, 4)
                for qi in range(8):
                    i = qi % 2
                    S.dma('sp', oat[i][:], OA[qi * 128:(qi + 1) * 128, :], w=['oat%d' % i])

                    def ev(c0, n, tps, tkey, qi=qi):
                        S.op('act', lambda a: a.activation(out=o_aT[:, c0:c0 + n, qi * 128:(qi + 1) * 128],
                                                           in_=tps[:, 0:n * 128].rearrange("p (a b) -> p a b", b=128),
                                                           func=AF.Identity), r=[tkey], w=['o_aT'])
                    to_feature_major(oat[i], 'oat%d' % i, KC, None, 'o_aT', ev)
                for cb in range(4):
                    wa, wak = load_w(w_ba, 0, KC, cb * 512, 512)
                    wd2, wdk = load_w(w_bd, 0, KC, cb * 512, 512)
                    for qi in range(8):
                        i = qi % 2
                        S.dma('sp', gat[i][:], GA[qi * 128:(qi + 1) * 128, cb * 512:(cb + 1) * 512], w=['gat%d' % i])
                        S.dma('act', gbt[i][:], GB[qi * 128:(qi + 1) * 128, cb * 512:(cb + 1) * 512], w=['gbt%d' % i])
                        psa, pka = next_mm()
                        for kc in range(KC):
                            S.op('pe', lambda p: p.matmul(psa[:, :], lhsT=o_aT[:, kc, qi * 128:(qi + 1) * 128], rhs=wa[:, kc, :],
                                                          start=(kc == 0), stop=(kc == KC - 1)), r=['o_aT', wak], w=[pka])
                        psb, pkb = next_mm()
                        for kc in range(KC):
                            S.op('pe', lambda p: p.matmul(psb[:, :], lhsT=o_bT[:, kc, qi * 128:(qi + 1) * 128], rhs=wd2[:, kc, :],
                                                          start=(kc == 0), stop=(kc == KC - 1)), r=['o_bT', wdk], w=[pkb])
                        S.op('dve', lambda v: v.tensor_tensor(out=m1[:], in0=psa[:, :], in1=gat[i][:], op=ALU.mult),
                             r=[pka, 'gat%d' % i], w=['m1'])
                        S.op('dve', lambda v: v.tensor_tensor(out=m2[:], in0=psb[:, :], in1=gbt[i][:], op=ALU.mult),
                             r=[pkb, 'gbt%d' % i], w=['m2'])
                        S.op('pool', lambda g: g.tensor_tensor(out=mgs[i][:], in0=m1[:], in1=m2[:], op=ALU.add),
                             r=['m1', 'm2'], w=['mgs%d' % i])
                        S.dma('sp', MG[qi * 128:(qi + 1) * 128, cb * 512:(cb + 1) * 512], mgs[i][:], r=['mgs%d' % i], w=[('MG', qi)])
                S.barrier()

        with ExitStack() as px:
            x1 = sb(px, "x1", [128, 8, D], F32)
            rowb = sb(px, "rowb", [128, D], F32)
            with ExitStack() as pg2:
                alloc_wbuf(pg2, 2)
                mergedT = sb(pg2, "mergedT", [128, KC, TOWN], BF16)
                mgt = [sb(pg2, "mgt%d" % i, [128, D], BF16) for i in range(2)]
                xres = [sb(pg2, "xres%d" % i, [128, 512], F32) for i in range(2)]
                tmpf = sb(pg2, "tmpf", [128, 512], F32)
                S.dma('act', rowb[:], modrow_d[0, 2 * D:3 * D].partition_broadcast(128), w=['rowb'])
                for qi in range(8):
                    i = qi % 2
                    S.dma('sp', mgt[i][:], MG[qi * 128:(qi + 1) * 128, :], w=['mgt%d' % i])

                    def ev2(c0, n, tps, tkey, qi=qi):
                        S.op('act', lambda a: a.activation(out=mergedT[:, c0:c0 + n, qi * 128:(qi + 1) * 128],
                                                           in_=tps[:, 0:n * 128].rearrange("p (a b) -> p a b", b=128),
                                                           func=AF.Identity), r=[tkey], w=['mergedT'])
                    to_feature_major(mgt[i], 'mgt%d' % i, KC, None, 'mergedT', ev2)

                def evac_mo(cb):
                    def f(tt, ps, pkey):
                        i = tt % 2
                        S.dma('sp', xres[i][:], xs[TOWN + tt * 128:TOWN + (tt + 1) * 128, cb * 512:(cb + 1) * 512], w=['xres%d' % i])
                        S.op('dve', lambda v: v.tensor_tensor(out=tmpf[:], in0=ps[:, :], in1=rowb[:, cb * 512:(cb + 1) * 512], op=ALU.mult),
                             r=[pkey, 'rowb'], w=['tmpf'])
                        S.op('dve', lambda v: v.tensor_tensor(out=x1[:, tt, cb * 512:(cb + 1) * 512], in0=tmpf[:], in1=xres[i][:], op=ALU.add),
                             r=['tmpf', 'xres%d' % i], w=[('x1', tt)])
                    return f
                for cb in range(4):
                    linear(lambda kc, tt: mergedT[:, kc, tt * 128:(tt + 1) * 128], 'mergedT', w_mo, cb * 512, 512, list(range(8)), evac_mo(cb))
                S.barrier()

            with ExitStack() as ph:
                alloc_wbuf(ph, 2, 11, 512)
                h2T = sb(ph, "h2T", [128, KC, TOWN], BF16)
                xn2 = [sb(ph, "xn2_%d" % i, [128, D], BF16) for i in range(2)]
                actT = sb(ph, "actT", [128, 11, TOWN], BF16)
                sg = [sb(ph, "sg%d" % i, [128, 512], F32) for i in range(2)]
                tmp2 = sb(ph, "tmp2", [128, 512], F32)
                gub = [sb(ph, "gub%d" % i, [128, KC, 128], BF16) for i in range(6)]
                rr['gu'] = 0

                def load_gu(c0):
                    i = rr['gu'] % 6
                    rr['gu'] += 1
                    S.dma('pool', gub[i][:], w_gu[:, c0:c0 + 128].rearrange("(kc p) n -> p kc n", p=128), w=['gub%d' % i])
                    return gub[i], 'gub%d' % i
                S.dma('act', rowb[:], modrow_d[0, 5 * D:6 * D].partition_broadcast(128), w=['rowb'])
                for qi in range(8):
                    i = qi % 2
                    norm_to_fm(x1[:, qi, :], ('x1', qi), h2T, 'h2T', qi * 128, A2, sh2, ['A2', 'modfm'],
                               xn2[i], 'xn2_%d' % i, small[:, 16 + i:17 + i])
                cnt_s = 0
                for fb in range(4):
                    for fc in range(11):
                        f0 = fb * 1408 + fc * 128
                        wg, wgk = load_gu(f0)
                        wu, wuk = load_gu(DFF + f0)
                        for tg in range(2):
                            psg, pkg = next_mm()
                            for kc in range(KC):
                                S.op('pe', lambda p: p.matmul(psg[:, :], lhsT=wg[:, kc, 0:128], rhs=h2T[:, kc, tg * 512:(tg + 1) * 512],
                                                              start=(kc == 0), stop=(kc == KC - 1)), r=['h2T', wgk], w=[pkg])
                            psu, pku = next_mm()
                            for kc in range(KC):
                                S.op('pe', lambda p: p.matmul(psu[:, :], lhsT=wu[:, kc, 0:128], rhs=h2T[:, kc, tg * 512:(tg + 1) * 512],
                                                              start=(kc == 0), stop=(kc == KC - 1)), r=['h2T', wuk], w=[pku])
                            j = cnt_s % 2
                            cnt_s += 1
                            S.op('act', lambda a: a.activation(out=sg[j][:], in_=psg[:, :], func=AF.Silu), r=[pkg], w=['sg%d' % j])
                            S.op('dve', lambda v: v.tensor_tensor(out=actT[:, fc, tg * 512:(tg + 1) * 512], in0=psu[:, :], in1=sg[j][:],
                                                                  op=ALU.mult), r=[pku, 'sg%d' % j], w=['actT'])

                    def evac_dn(cb):
                        def f(tt, ps, pkey):
                            S.op('dve', lambda v: v.tensor_tensor(out=tmp2[:], in0=ps[:, :], in1=rowb[:, cb * 512:(cb + 1) * 512], op=ALU.mult),
                                 r=[pkey, 'rowb'], w=['tmp2'])
                            S.op('pool', lambda g: g.tensor_tensor(out=x1[:, tt, cb * 512:(cb + 1) * 512], in0=x1[:, tt, cb * 512:(cb + 1) * 512],
                                                                   in1=tmp2[:], op=ALU.add), r=['tmp2', ('x1', tt)], w=[('x1', tt)])
                        return f
                    for cb in range(4):
                        linear(lambda kc, tt: actT[:, kc, tt * 128:(tt + 1) * 128], 'actT', w_dn, cb * 512, 512, list(range(8)),
                               evac_dn(cb), r0=fb * 1408, nk=11)
                S.barrier()

            with ExitStack() as pi_:
                ot = [sb(pi_, "ot%d" % i, [128, D], F32) for i in range(2)]
                S.dma('act', rowb[:], fng[0, :].partition_broadcast(128), w=['rowb'])
                for qi in range(8):
                    i = qi % 2
                    ssc = small[:, 20 + i:21 + i]
                    S.op('act', lambda a: a.activation(out=junk[:], in_=x1[:, qi, :], func=AF.Square, accum_out=ssc),
                         r=[('x1', qi)], w=['junk', 'small'])
                    rstd_from_ss(ssc, D, 'small')
                    S.op('dve', lambda v: v.scalar_tensor_tensor(out=ot[i][:], in0=x1[:, qi, :], scalar=ssc, in1=rowb[:],
                                                                 op0=ALU.mult, op1=ALU.mult), r=[('x1', qi), 'small', 'rowb'], w=['ot%d' % i])
                    S.dma('sp', out[qi * 128:(qi + 1) * 128, :], ot[i][:], r=['ot%d' % i], w=[('out', qi)])
                S.barrier()
        S.barrier()
    return nc


def _consts(half):
    c = np.zeros((128, 1024), np.float32)
    c[:, 0:128] = np.eye(128, dtype=np.float32)
    j = np.arange(128)
    c[:, 128:256] = (j[:, None] <= j[None, :]).astype(np.float32)
    c[:, 256:384] = np.where(j[None, :] <= j[:, None], 0.0, NEG)
    c[:, 384] = 1.0 if half == 1 else 0.0
    c[:, 385] = 0.0 if half == 1 else NEG
    theta = np.float32(500000.0)
    c[:, 400:416] = np.power(theta, -np.arange(0, 32, 2, dtype=np.float32) / np.float32(32))[None, :]
    c[:, 416:424] = np.power(theta, -np.arange(0, 16, 2, dtype=np.float32) / np.float32(16))[None, :]
    return c


def prep_inputs(inputs, cores=None):
    f = lambda a: np.ascontiguousarray(np.asarray(a))
    x = f(inputs["x"]); c = f(inputs["c"]); pos = f(inputs["positions"]).astype(np.int32)
    shared = {
        "w_ada": f(inputs["w_ada"])[0], "b_ada": f(inputs["b_ada"])[0][None, :], "w_in": f(inputs["w_in"])[0],
        "gate_up": f(inputs["gla_gate_up"])[0], "gate_bias": f(inputs["gla_gate_bias"])[0][None, :],
        "gla_gain": f(inputs["gla_norm_gain"])[0][None, :],
        "w_ba": f(inputs["w_branch_gla"])[0], "w_bd": f(inputs["w_branch_dsa"])[0], "w_mo": f(inputs["w_merge_out"])[0],
        "w_gu": f(inputs["w_ffn_gate_up"])[0], "w_dn": f(inputs["w_ffn_down"])[0],
        "n1g": np.ascontiguousarray(f(inputs["norm1_gain"])[0].reshape(16, 128).T),
        "n2g": np.ascontiguousarray(f(inputs["norm2_gain"])[0].reshape(16, 128).T),
        "fng": f(inputs["final_norm_gain"])[None, :],
    }
    maps = []
    for core in (range(8) if cores is None else cores):
        b, half = core // 2, core % 2
        if half == 1:
            xs = x[b]
            p = pos[b]
        else:
            xs = np.concatenate([np.zeros((TOWN, D), np.float32), x[b, :TOWN]], axis=0)
            p = np.concatenate([pos[b, :TOWN], pos[b, :TOWN]])
        m = dict(shared)
        m["xs"] = np.ascontiguousarray(xs)
        m["cfm"] = np.ascontiguousarray(c[b].reshape(16, 128).T)
        m["posi"] = np.ascontiguousarray(p.reshape(16, 128).T)
        m["cst"] = _consts(half)
        maps.append(m)
    return maps


_NC = None


def kernel(**inputs):
    global _NC
    if _NC is None:
        _NC = build_program()
    maps = prep_inputs(inputs)
    res = run_bass_kernel_spmd(_NC, maps, core_ids=list(range(8)))
    outp = np.zeros((NB, SEQ, D), np.float32)
    for core in range(8):
        b, half = core // 2, core % 2
        outp[b, half * TOWN:(half + 1) * TOWN] = res.results[core]["out"]
    return outp
```

```python
import math
from contextlib import ExitStack

import numpy as np
import concourse.bass as bass
import concourse.mybir as mybir
from concourse.bass_utils import run_bass_kernel_spmd

F32 = mybir.dt.float32
BF16 = mybir.dt.bfloat16
I32 = mybir.dt.int32
AF = mybir.ActivationFunctionType
ALU = mybir.AluOpType
AX = mybir.AxisListType

D = 2048
SEQ = 2048
NB = 4
TOWN = 1024
TALL = 2048
NT = 16
KC = 16
DFF = 5632
EPS = 1e-6
NEG = -1.0e30
TOPK = 256
NBISECT = 22

O_GQ, O_GK, O_GV, O_GR, O_GLR = 0, 1024, 2048, 4096, 6144
O_DQ, O_DK, O_DV = 6160, 8208, 10256
O_IQ, O_IK, O_IW = 12304, 12816, 12880
O_GA, O_GB = 12888, 14936
IN_W = 16984


class Sched:
    def __init__(self, nc, es, ndma=24):
        self.nc = nc
        self.eng = {'pe': nc.tensor, 'act': nc.scalar, 'dve': nc.vector, 'pool': nc.gpsimd, 'sp': nc.sync}
        self.semobj = {}
        for e in ['pe', 'act', 'dve', 'pool']:
            self.semobj[e] = es.enter_context(nc.semaphore('s_' + e))
        self.ndma = ndma
        for i in range(ndma):
            self.semobj[('d', i)] = es.enter_context(nc.semaphore('sd%d' % i))
            self.semobj[('g', i)] = es.enter_context(nc.semaphore('sg%d' % i))
        self.dma_rr_g = 0
        self.cnt = {k: 0 for k in self.semobj}
        self.seen = {e: {} for e in self.eng}
        self.lastw = {}
        self.readers = {}
        self.dma_rr = 0
        self.nwait = 0

    def _wait(self, e, k, v):
        if k == e and e == 'pe':
            return
        if self.seen[e].get(k, 0) >= v:
            return
        self.eng[e].wait_ge(self.semobj[k], v)
        self.seen[e][k] = v
        self.nwait += 1

    def _deps(self, e, r, w):
        for key in r:
            for k, v in self.lastw.get(key, {}).items():
                self._wait(e, k, v)
        for key in w:
            for k, v in self.lastw.get(key, {}).items():
                self._wait(e, k, v)
            for k, v in self.readers.get(key, {}).items():
                self._wait(e, k, v)

    def _record(self, ev, r, w):
        k, v = ev
        for key in r:
            d = self.readers.setdefault(key, {})
            d[k] = max(d.get(k, 0), v)
        for key in w:
            self.lastw[key] = {k: v}
            self.readers[key] = {}

    def op(self, e, fn, r=(), w=()):
        ex = [k for k in r if isinstance(k, str) and k[:2] in ('mm', 'tp', 'ax')]
        if ex:
            w = list(w) + [k for k in ex if k not in w]
        self._deps(e, r, w)
        ins = fn(self.eng[e])
        self.cnt[e] += 1
        ins.then_inc(self.semobj[e], 1)
        self._record((e, self.cnt[e]), r, w)

    def dma(self, q, out, in_, r=(), w=(), **kw):
        if q == 'pool':
            slot = ('g', self.dma_rr_g % self.ndma)
            self.dma_rr_g += 1
        else:
            slot = ('d', self.dma_rr % self.ndma)
            self.dma_rr += 1
        if self.cnt[slot] > 0:
            self._wait(q, slot, self.cnt[slot])
        self._deps(q, r, w)
        ins = self.eng[q].dma_start(out=out, in_=in_, **kw)
        self.cnt[slot] += 16
        ins.then_inc(self.semobj[slot], 16)
        self._record((slot, self.cnt[slot]), r, w)

    def barrier(self):
        for e in self.eng:
            for k in self.semobj:
                if self.cnt[k] > 0:
                    self._wait(e, k, self.cnt[k])
        self.lastw = {}
        self.readers = {}


def build_program(dbg=(), stop=None):
    nc = bass.Bass("TRN2", target_bir_lowering=False)
    import os
    stop = stop or os.environ.get('KSTOP')

    def din(name, shape, dt=F32):
        return nc.dram_tensor(name, list(shape), dt, kind="ExternalInput").ap()

    def dscr(name, shape, dt=BF16):
        kind = "ExternalOutput" if name in dbg else "Internal"
        return nc.dram_tensor(name, list(shape), dt, kind=kind).ap()

    xs = din("xs", [TALL, D])
    cfm = din("cfm", [128, KC])
    posi = din("posi", [128, NT], I32)
    w_ada = din("w_ada", [D, 6 * D])
    b_ada = din("b_ada", [1, 6 * D])
    w_in = din("w_in", [D, IN_W])
    gate_up = din("gate_up", [16, 1024])
    gate_bias = din("gate_bias", [1, 1024])
    gla_gain = din("gla_gain", [1, D])
    w_ba = din("w_ba", [D, D])
    w_bd = din("w_bd", [D, D])
    w_mo = din("w_mo", [D, D])
    w_gu = din("w_gu", [D, 2 * DFF])
    w_dn = din("w_dn", [DFF, D])
    n1g = din("n1g", [128, KC])
    n2g = din("n2g", [128, KC])
    fng = din("fng", [1, D])
    cst = din("cst", [128, 512])
    out = nc.dram_tensor("out", [TOWN, D], F32, kind="ExternalOutput").ap()

    modrow_d = dscr("modrow_d", [1, 6 * D], F32)
    GQ = dscr("GQ", [TOWN, 1024])
    GK = dscr("GK", [TALL, 1024])
    GV = dscr("GV", [TALL, 2048])
    GR = dscr("GR", [TOWN, 2048])
    DQ = dscr("DQ", [TOWN, 2048])
    DK = dscr("DK", [TALL, 2048])
    DV = dscr("DV", [TALL, 2048])
    IQ = dscr("IQ", [TOWN, 512])
    IKW = dscr("IKW", [TALL, 72], F32)
    GA = dscr("GA", [TOWN, 2048])
    GB = dscr("GB", [TOWN, 2048])
    OA = dscr("OA", [TOWN, 2048])
    MG = dscr("MG", [TOWN, 2048])
    OBD = dscr("OBD", [D, TOWN])
    MTD = dscr("MTD", [128, NT, TOWN]) if "MTD" in dbg else None

    with ExitStack() as es:
        S = Sched(nc, es)

        def sb(stack, name, shape, dt):
            return stack.enter_context(nc.sbuf_tensor(name, list(shape), dt))

        mm, tp, ax = [], [], []
        pstack = [None]
        rr = {'mm': 0, 'tp': 0, 'stg': 0, 'wb': 0}

        def alloc_psum(nm, nt, na):
            if pstack[0] is not None:
                pstack[0].close()
            st = ExitStack()
            pstack[0] = st
            rr['pgen'] = rr.get('pgen', 0) + 1
            g = rr['pgen']
            mm[:] = [st.enter_context(nc.psum_tensor("mm%d_%d" % (i, g), [128, 512], F32)) for i in range(nm)]
            tp[:] = [st.enter_context(nc.psum_tensor("tp%d_%d" % (i, g), [128, 1024], BF16)) for i in range(nt)]
            ax[:] = [st.enter_context(nc.psum_tensor("ax%d_%d" % (i, g), [128, 512], F32)) for i in range(na)]

        alloc_psum(4, 2, 2)

        mm_users = {}

        def set_mm_users(**parts):
            mm_users.clear()
            mm_users.update(parts)

        def next_mm(user=None):
            banks = mm_users.get(user) if mm_users else None
            if banks is None:
                banks = list(range(len(mm)))
            c = rr.get(('mm', user), 0)
            rr[('mm', user)] = c + 1
            i = banks[c % len(banks)]
            return mm[i], 'mm%d' % i

        bg = []

        def bg_step():
            for g in list(bg):
                try:
                    next(g)
                except StopIteration:
                    bg.remove(g)

        def bg_drain():
            while bg:
                bg_step()

        def next_tp():
            i = rr['tp'] % len(tp)
            rr['tp'] += 1
            return tp[i], 'tp%d' % i

        cst_t = sb(es, "cst_t", [128, 512], F32)
        S.dma('sp', cst_t[:], cst, w=['cst'])
        identf = cst_t[:, 0:128]
        triu = cst_t[:, 128:256]
        cmask = cst_t[:, 256:384]
        ctxflag = cst_t[:, 384:385]
        ctxneg = cst_t[:, 385:386]
        invf_d = cst_t[:, 400:416]
        invf_i = cst_t[:, 416:424]
        ident = sb(es, "ident", [128, 128], BF16)
        ones_bf = sb(es, "ones_bf", [128, 128], BF16)
        S.op('dve', lambda v: v.tensor_copy(out=ident[:], in_=identf), r=['cst'], w=['ident'])
        S.op('dve', lambda v: v.memset(ones_bf[:], 1.0), w=['ones_bf'])
        modfm = sb(es, "modfm", [128, 96], F32)
        A1 = sb(es, "A1", [128, KC], F32)
        A2 = sb(es, "A2", [128, KC], F32)
        n1g_t = sb(es, "n1g_t", [128, KC], F32)
        n2g_t = sb(es, "n2g_t", [128, KC], F32)
        S.dma('sp', n1g_t[:], n1g, w=['n1g'])
        S.dma('sp', n2g_t[:], n2g, w=['n2g'])
        wbuf = []

        def alloc_wbuf(stack, n, nk=KC, nb=512):
            rr['wgen'] = rr.get('wgen', 0) + 1
            wbuf[:] = [stack.enter_context(nc.sbuf_tensor("wbuf%d_%d" % (i, rr['wgen']), [128, nk, nb], BF16)) for i in range(n)]
        stg = [sb(es, "stg%d" % i, [128, 512], BF16) for i in range(4)]
        small = sb(es, "small", [128, 64], F32)
        junk = sb(es, "junk", [128, 2048], BF16)
        glrT = sb(es, "glrT", [32, TALL], F32)

        def next_stg():
            i = rr['stg'] % 4
            rr['stg'] += 1
            return stg[i], 'stg%d' % i

        def load_w(W, r0, nk, c0, nb, q='pool'):
            i = rr['wb'] % len(wbuf)
            rr['wb'] += 1
            key = 'wbuf%d' % i
            src = W[r0:r0 + nk * 128, c0:c0 + nb].rearrange("(kc p) n -> p kc n", p=128)
            S.dma(q, wbuf[i][:, 0:nk, 0:nb], src, w=[key])
            return wbuf[i], key

        def linear(actT, akey, W, c0, nb, tts, evac, r0=0, nk=KC, m=128, user='c'):
            wb, wkey = load_w(W, r0, nk, c0, nb)
            for tt in tts:
                bg_step()
                ps, pkey = next_mm(user)
                for kc in range(nk):
                    if kc == nk // 2:
                        bg_step()
                    S.op('pe', lambda p: p.matmul(ps[0:m, 0:nb], lhsT=actT(kc, tt), rhs=wb[:, kc, 0:nb],
                                                  start=(kc == 0), stop=(kc == nk - 1)),
                         r=[akey, wkey], w=[pkey])
                evac(tt, ps, pkey)

        def rstd_from_ss(ss_ap, n, key):
            S.op('dve', lambda v: v.tensor_scalar(out=ss_ap, in0=ss_ap, scalar1=1.0 / n, scalar2=EPS,
                                                  op0=ALU.mult, op1=ALU.add), r=[key], w=[key])
            S.op('act', lambda a: a.activation(out=ss_ap, in_=ss_ap, func=AF.Sqrt), r=[key], w=[key])
            S.op('dve', lambda v: v.reciprocal(out=ss_ap, in_=ss_ap), r=[key], w=[key])

        def to_feature_major(src_tile, skey, nchunk, dst_fn, dkey, evac_eng_fn):
            for c0 in range(0, nchunk, 8):
                n = min(8, nchunk - c0)
                tps, tkey = next_tp()
                for c in range(n):
                    S.op('pe', lambda p: p.transpose(out=tps[:, c * 128:(c + 1) * 128],
                                                     in_=src_tile[:, (c0 + c) * 128:(c0 + c + 1) * 128],
                                                     identity=ident[:]),
                         r=[skey, 'ident'], w=[tkey])
                evac_eng_fn(c0, n, tps, tkey)

        sT = sb(es, "sT", [128, KC], BF16)
        with ExitStack() as pa:
            alloc_wbuf(pa, 2)
            c_t = sb(pa, "c_t", [128, KC], F32)
            brow = sb(pa, "brow", [1, 2 * D], F32)
            mrow = sb(pa, "mrow", [1, 2 * D], F32)
            S.dma('sp', c_t[:], cfm, w=['c_t'])
            S.dma('sp', brow[:], b_ada[0:1, 0:2 * D], w=['brow'])
            S.op('act', lambda a: a.activation(out=sT[:], in_=c_t[:], func=AF.Silu), r=['c_t'], w=['sT'])

            def evac_mod(cb):
                def f(tt, ps, pkey):
                    S.op('dve', lambda v: v.tensor_tensor(out=mrow[0:1, cb * 512:(cb + 1) * 512], in0=ps[0:1, :],
                                                          in1=brow[0:1, cb * 512:(cb + 1) * 512], op=ALU.add),
                         r=[pkey, 'brow'], w=['mrow'])
                return f
            for cb in range(8):
                linear(lambda kc, tt: sT[:, kc:kc + 1], 'sT', w_ada, cb * 512, 512, [0], evac_mod(cb), m=1)
            S.dma('sp', modrow_d[0:1, 0:2 * D], mrow[:], r=['mrow'], w=['modrow_d'])
            with nc.allow_non_contiguous_dma(reason="one-time relayout of the modulation vector"):
                S.dma('sp', modfm[:, 0:32], modrow_d[0, 0:2 * D].rearrange("(j p) -> p j", p=128), r=['modrow_d'], w=['modfm'])
            S.op('dve', lambda v: v.scalar_tensor_tensor(out=A1[:], in0=modfm[:, 16:32], scalar=1.0, in1=n1g_t[:],
                                                         op0=ALU.add, op1=ALU.mult), r=['modfm', 'n1g'], w=['A1'])
            S.barrier()
        if stop == 'A':
            return nc
        sh1 = modfm[:, 0:16]
        sh2 = modfm[:, 48:64]

        def norm_to_fm(x_tile, xkey, dstT, dkey, tcol, A, sh, akeys, xn, xnkey, ss_ap):
            S.op('act', lambda a: a.activation(out=junk[:], in_=x_tile, func=AF.Square, accum_out=ss_ap),
                 r=[xkey], w=['junk', 'small'])
            rstd_from_ss(ss_ap, D, 'small')
            S.op('dve', lambda v: v.tensor_scalar(out=xn[:], in0=x_tile, scalar1=ss_ap, scalar2=None, op0=ALU.mult),
                 r=[xkey, 'small'], w=[xnkey])

            def ev(c0, n, tps, tkey):
                for c in range(n):
                    kc = c0 + c
                    S.op('act', lambda a: a.activation(out=dstT[:, kc, tcol:tcol + 128], in_=tps[:, c * 128:(c + 1) * 128],
                                                       func=AF.Identity, scale=A[:, kc:kc + 1], bias=sh[:, kc:kc + 1]),
                         r=[tkey] + akeys, w=[dkey])
            to_feature_major(xn, xnkey, KC, None, dkey, ev)

        s_mask = ExitStack()
        maskT = sb(s_mask, "maskT", [128, NT, TOWN], BF16)
        with ExitStack() as pbc:
            hT = sb(pbc, "hT", [128, KC, TALL], BF16)
            with ExitStack() as pb:
                xt = [sb(pb, "xt%d" % i, [128, D], F32) for i in range(2)]
                xn = [sb(pb, "xn%d" % i, [128, D], BF16) for i in range(2)]
                for tt in range(NT):
                    i = tt % 2
                    S.dma('sp' if i == 0 else 'act', xt[i][:], xs[tt * 128:(tt + 1) * 128, :], w=['xt%d' % i])
                    norm_to_fm(xt[i][:], 'xt%d' % i, hT, 'hT', tt * 128, A1, sh1, ['A1', 'modfm'],
                               xn[i], 'xn%d' % i, small[:, i:i + 1])
                S.barrier()

            with ExitStack() as pc:
                alloc_wbuf(pc, 2)
                alloc_psum(7, 1, 0)
                set_mm_users(c=[0, 1, 2], d=[3, 4], e=[5, 6])
                sinD = sb(pc, "sinD", [128, NT, 1, 16], F32)
                cosD = sb(pc, "cosD", [128, NT, 1, 16], F32)
                sinI = sb(pc, "sinI", [128, NT, 1, 8], F32)
                cosI = sb(pc, "cosI", [128, NT, 1, 8], F32)
                ggs = [sb(pc, "ggs%d" % i, [128, 512], F32) for i in range(2)]
                rt = [sb(pc, "rt%d" % i, [128, 4, 16], F32) for i in range(4)]
                f32stg = sb(pc, "f32stg", [128, 512], F32)
                f32stg2 = sb(pc, "f32stg2", [128, 72], F32)
                rsl = [sb(pc, "rsl%d" % i, [128, 128], F32) for i in range(2)]
                ptab = ExitStack()
                posf = sb(ptab, "posf", [128, NT], F32)
                pos_i = sb(ptab, "pos_i", [128, NT], I32)
                ang = sb(ptab, "ang", [128, NT, 16], F32)
                kf = sb(ptab, "kf", [128, NT, 16], F32)
                ki = sb(ptab, "ki", [128, NT, 16], I32)
                kf2 = sb(ptab, "kf2", [128, NT, 16], F32)
                S.dma('sp', pos_i[:], posi, w=['pos_i'])
                S.op('dve', lambda v: v.tensor_copy(out=posf[:], in_=pos_i[:]), r=['pos_i'], w=['posf'])
                TWO_PI = 2.0 * math.pi

                def make_tables(invf, nj, sin_t, cos_t, key):
                    for tt in range(NT):
                        S.op('dve', lambda v: v.tensor_scalar(out=ang[:, tt, 0:nj], in0=invf, scalar1=posf[:, tt:tt + 1],
                                                              scalar2=None, op0=ALU.mult), r=['cst', 'posf', 'ang'], w=['ang'])
                    a = ang[:, :, 0:nj]
                    kk = kf[:, :, 0:nj]
                    mm_ = kf2[:, :, 0:nj]
                    S.op('dve', lambda v: v.tensor_scalar(out=kk, in0=a, scalar1=1.0 / TWO_PI, scalar2=None,
                                                          op0=ALU.mult), r=['ang'], w=['kf'])
                    S.op('dve', lambda v: v.tensor_copy(out=ki[:, :, 0:nj], in_=kk), r=['kf'], w=['ki'])
                    S.op('dve', lambda v: v.tensor_copy(out=kk, in_=ki[:, :, 0:nj]), r=['ki'], w=['kf'])
                    S.op('dve', lambda v: v.scalar_tensor_tensor(out=a, in0=kk, scalar=-TWO_PI, in1=a,
                                                                 op0=ALU.mult, op1=ALU.add), r=['kf', 'ang'], w=['ang'])
                    for shift, dst in ((0.0, sin_t), (math.pi / 2, cos_t)):
                        S.op('dve', lambda v: v.tensor_scalar(out=kk, in0=a, scalar1=shift, scalar2=None,
                                                              op0=ALU.add), r=['ang', 'kf'], w=['kf'])
                        for cmp, bound, sgn in ((ALU.is_gt, math.pi, -1.0), (ALU.is_lt, -math.pi, 1.0)):
                            S.op('dve', lambda v: v.tensor_scalar(out=mm_, in0=kk, scalar1=bound, scalar2=sgn * TWO_PI,
                                                                  op0=cmp, op1=ALU.mult), r=['kf'], w=['kf2'])
                            S.op('dve', lambda v: v.tensor_tensor(out=kk, in0=kk, in1=mm_, op=ALU.add),
                                 r=['kf', 'kf2'], w=['kf'])
                        S.op('act', lambda a_: a_.activation(out=dst[:, :, 0, :], in_=kk, func=AF.Sin),
                             r=['kf'], w=[key])
                make_tables(invf_d, 16, sinD, cosD, 'tabD')
                make_tables(invf_i, 8, sinI, cosI, 'tabI')
                S.barrier()
                ptab.close()
                S.op('dve', lambda v: v.memset(glrT[:, :], 1.0), w=['glrT'])
                wb, wkey = load_w(w_in, 0, KC, O_GLR, 16)
                for tg in range(4):
                    ps, pkey = next_mm()
                    for kc in range(KC):
                        S.op('pe', lambda p: p.matmul(ps[0:16, :], lhsT=wb[:, kc, 0:16], rhs=hT[:, kc, tg * 512:(tg + 1) * 512],
                                                      start=(kc == 0), stop=(kc == KC - 1)), r=['hT', wkey], w=[pkey])
                    S.op('act', lambda a: a.activation(out=glrT[0:16, tg * 512:(tg + 1) * 512], in_=ps[0:16, :], func=AF.Identity),
                         r=[pkey], w=['glrT'])

                if stop == 'C1':
                    S.barrier()
                    return nc
                own = list(range(8, 16))
                allt = list(range(NT))
                hact = lambda kc, tt: hT[:, kc, tt * 128:(tt + 1) * 128]

                def store(dst, own_only, c0, nb):
                    def f(tt, ps, pkey):
                        st, skey = next_stg()
                        S.op('act', lambda a: a.activation(out=st[:, 0:nb], in_=ps[:, 0:nb], func=AF.Identity), r=[pkey], w=[skey])
                        row = (tt - 8 if own_only else tt) * 128
                        S.dma('sp', dst[row:row + 128, c0:c0 + nb], st[:, 0:nb], r=[skey], w=[(id(dst), tt)])
                    return f

                def store_act(dst, c0, nb, func, mul=None):
                    def f(tt, ps, pkey):
                        st, skey = next_stg()
                        if mul is None:
                            S.op('act', lambda a: a.activation(out=st[:, 0:nb], in_=ps[:, 0:nb], func=func), r=[pkey], w=[skey])
                        else:
                            S.op('act', lambda a: a.activation(out=f32stg[:, 0:nb], in_=ps[:, 0:nb], func=func), r=[pkey], w=['f32stg'])
                            S.op('dve', lambda v: v.tensor_tensor(out=st[:, 0:nb], in0=f32stg[:, 0:nb], in1=mul[0][:, 0:nb],
                                                                  op=ALU.mult), r=['f32stg', mul[1]], w=[skey])
                        row = (tt - 8) * 128
                        S.dma('sp', dst[row:row + 128, c0:c0 + nb], st[:, 0:nb], r=[skey], w=[(id(dst), tt)])
                    return f

                def rope_ops(x1, x2, o1, o2, cs, sn, pkey, skey, tkey, shape):
                    t = [rt[i][:].rearrange("p a b -> p (a b)")[:, 0:shape[0] * shape[1]].rearrange("p (a b) -> p a b", b=shape[1])
                         for i in range(4)]
                    S.op('dve', lambda v: v.tensor_tensor(out=t[0], in0=x1, in1=cs, op=ALU.mult), r=[pkey, tkey], w=['rt0'])
                    S.op('dve', lambda v: v.tensor_tensor(out=t[1], in0=x2, in1=sn, op=ALU.mult), r=[pkey, tkey], w=['rt1'])
                    S.op('dve', lambda v: v.tensor_tensor(out=o1, in0=t[0], in1=t[1], op=ALU.subtract), r=['rt0', 'rt1'], w=[skey])
                    S.op('dve', lambda v: v.tensor_tensor(out=t[2], in0=x1, in1=sn, op=ALU.mult), r=[pkey, tkey], w=['rt2'])
                    S.op('dve', lambda v: v.tensor_tensor(out=t[3], in0=x2, in1=cs, op=ALU.mult), r=[pkey, tkey], w=['rt3'])
                    S.op('dve', lambda v: v.tensor_tensor(out=o2, in0=t[2], in1=t[3], op=ALU.add), r=['rt2', 'rt3'], w=[skey])

                def store_rope_d(dst, own_only, c0):
                    def f(tt, ps, pkey):
                        st, skey = next_stg()
                        S.op('act', lambda a: a.activation(out=st[:, :], in_=ps[:, :], func=AF.Identity), r=[pkey], w=[skey])
                        pv = ps[:, :].rearrange("p (h d) -> p h d", d=128)
                        sv = st[:, :].rearrange("p (h d) -> p h d", d=128)
                        j = rr.get('rsl', 0) % 2
                        rr['rsl'] = rr.get('rsl', 0) + 1
                        rv = rsl[j][:, :].rearrange("p (h d) -> p h d", d=32)
                        S.op('act', lambda a: a.activation(out=rv, in_=pv[:, :, 0:32], func=AF.Identity), r=[pkey], w=['rsl%d' % j])
                        rope_ops(rv[:, :, 0:16], rv[:, :, 16:32], sv[:, :, 0:16], sv[:, :, 16:32],
                                 cosD[:, tt, :, :].to_broadcast([128, 4, 16]), sinD[:, tt, :, :].to_broadcast([128, 4, 16]), 'rsl%d' % j, skey, 'tabD', (4, 16))
                        row = (tt - 8 if own_only else tt) * 128
                        S.dma('sp', dst[row:row + 128, c0:c0 + 512], st[:, :], r=[skey], w=[(id(dst), tt)])
                    return f

                def store_iq(tt, ps, pkey):
                    st, skey = next_stg()
                    S.op('act', lambda a: a.activation(out=st[:, :], in_=ps[:, :], func=AF.Identity), r=[pkey], w=[skey])
                    pv = ps[:, :].rearrange("p (h d) -> p h d", d=64)
                    sv = st[:, :].rearrange("p (h d) -> p h d", d=64)
                    j = rr.get('rsl', 0) % 2
                    rr['rsl'] = rr.get('rsl', 0) + 1
                    rv = rsl[j][:, :].rearrange("p (h d) -> p h d", d=16)
                    S.op('act', lambda a: a.activation(out=rv, in_=pv[:, :, 0:16], func=AF.Identity), r=[pkey], w=['rsl%d' % j])
                    rope_ops(rv[:, :, 0:8], rv[:, :, 8:16], sv[:, :, 0:8], sv[:, :, 8:16],
                             cosI[:, tt, :, :].to_broadcast([128, 8, 8]), sinI[:, tt, :, :].to_broadcast([128, 8, 8]), 'rsl%d' % j, skey, 'tabI', (8, 8))
                    row = (tt - 8) * 128
                    S.dma('sp', IQ[row:row + 128, :], st[:, :], r=[skey], w=[('IQ', tt)])

                def store_ikw(tt, ps, pkey):
                    S.op('act', lambda a: a.activation(out=f32stg2[:, :], in_=ps[:, 0:72], func=AF.Identity), r=[pkey], w=['f32stg2'])
                    j = rr.get('rsl', 0) % 2
                    rr['rsl'] = rr.get('rsl', 0) + 1
                    S.op('act', lambda a: a.activation(out=rsl[j][:, 0:16], in_=ps[:, 0:16], func=AF.Identity), r=[pkey], w=['rsl%d' % j])
                    pkey = 'rsl%d' % j
                    rope_ops(rsl[j][:, 0:8].rearrange("p (a b) -> p a b", a=1), rsl[j][:, 8:16].rearrange("p (a b) -> p a b", a=1),
                             f32stg2[:, 0:8].rearrange("p (a b) -> p a b", a=1), f32stg2[:, 8:16].rearrange("p (a b) -> p a b", a=1),
                             cosI[:, tt, :, :], sinI[:, tt, :, :], pkey, 'f32stg2', 'tabI', (1, 8))
                    S.dma('sp', IKW[tt * 128:(tt + 1) * 128, :], f32stg2[:, :], r=['f32stg2'], w=[('IKW', tt)])

                def gen_D():
                    gu_aug = sb(pc, "gu_aug", [32, 1024], F32)
                    S.dma('sp', gu_aug[0:16, :], gate_up, w=['gu_aug'])
                    S.dma('sp', gu_aug[16:17, :], gate_bias, w=['gu_aug'])
                    Sst = sb(pc, "Sst", [128, 2, 512], F32)
                    Sbf = sb(pc, "Sbf", [128, 2, 512], BF16)
                    kt_ = [sb(pc, "kt%d" % i, [128, 256], BF16) for i in range(2)]
                    vt_ = [sb(pc, "vt%d" % i, [128, 512], BF16) for i in range(2)]
                    qt_ = [sb(pc, "qt%d" % i, [128, 256], BF16) for i in range(2)]
                    gs_ = [sb(pc, "gs%d" % i, [128, 512], BF16) for i in range(2)]
                    sp_ = sb(pc, "sp_", [128, 256], F32)
                    Epos = sb(pc, "Epos", [128, 256], F32)
                    Eneg = sb(pc, "Eneg", [128, 256], F32)
                    Etok = sb(pc, "Etok", [128, 256], F32)
                    ktok = sb(pc, "ktok", [128, 256], BF16)
                    kT_ = sb(pc, "kT_", [128, 256], BF16)
                    qT_ = sb(pc, "qT_", [128, 256], BF16)
                    attnT = sb(pc, "attnT", [128, 128], BF16)
                    junkD = sb(pc, "junkD", [128, 512], BF16)
                    oa_ = [sb(pc, "oa%d" % i, [128, 512], BF16) for i in range(2)]
                    for h in range(4):
                        S.op('dve', lambda v: v.memset(Sst[:], 0.0), w=['Sst'])
                        S.op('dve', lambda v: v.memset(Sbf[:], 0.0), w=['Sbf'])
                        for n in range(NT):
                            i = n % 2
                            ownt = n >= 8
                            r0 = n * 128
                            S.dma('sp', kt_[i][:], GK[r0:r0 + 128, h * 256:(h + 1) * 256], r=[(id(GK), n)], w=['kt%d' % i])
                            S.dma('act', vt_[i][:], GV[r0:r0 + 128, h * 512:(h + 1) * 512], r=[(id(GV), n)], w=['vt%d' % i])
                            if ownt:
                                q0 = (n - 8) * 128
                                S.dma('sp', qt_[i][:], GQ[q0:q0 + 128, h * 256:(h + 1) * 256], r=[(id(GQ), n)], w=['qt%d' % i])
                                S.dma('act', gs_[i][:], GR[q0:q0 + 128, h * 512:(h + 1) * 512], r=[(id(GR), n)], w=['gs%d' % i])
                            ps, pk = next_mm('d')
                            S.op('pe', lambda p: p.matmul(ps[:, 0:256], lhsT=glrT[0:17, r0:r0 + 128], rhs=gu_aug[0:17, h * 256:(h + 1) * 256],
                                                          start=True, stop=True), r=['glrT', 'gu_aug'], w=[pk])
                            S.op('act', lambda a: a.activation(out=sp_[:], in_=ps[:, 0:256], func=AF.Exp, scale=-1.0), r=[pk], w=['sp_'])
                            S.op('act', lambda a: a.activation(out=sp_[:], in_=sp_[:], func=AF.Ln, bias=1.0), r=['sp_'], w=['sp_'])
                            yield
                            ps2, pk2 = next_mm('d')
                            for cc in range(2):
                                S.op('pe', lambda p: p.matmul(ps2[:, cc * 128:(cc + 1) * 128], lhsT=sp_[:, cc * 128:(cc + 1) * 128], rhs=triu,
                                                              start=True, stop=True), r=['sp_', 'cst'], w=[pk2])
                            ps3, pk3 = next_mm('d')
                            S.op('pe', lambda p: p.matmul(ps3[:, 0:256], lhsT=triu, rhs=sp_[:], start=True, stop=True), r=['sp_', 'cst'], w=[pk3])
                            S.op('act', lambda a: a.activation(out=Epos[:], in_=ps2[:, 0:256], func=AF.Exp, scale=-1.0 / 16), r=[pk2], w=['Epos'])
                            S.op('act', lambda a: a.activation(out=Eneg[:], in_=ps2[:, 0:256], func=AF.Exp, scale=1.0 / 16), r=[pk2], w=['Eneg'])
                            S.op('act', lambda a: a.activation(out=Etok[:], in_=ps3[:, 0:256], func=AF.Exp, scale=1.0 / 16), r=[pk3], w=['Etok'])
                            yield
                            tpsf, tk = next_mm('d')
                            tps = tpsf[:, :].bitcast(BF16)
                            for cc in range(2):
                                S.op('pe', lambda p: p.transpose(out=tps[:, cc * 128:(cc + 1) * 128], in_=kt_[i][:, cc * 128:(cc + 1) * 128],
                                                                 identity=ident[:]), r=['kt%d' % i, 'ident'], w=[tk])
                            if ownt:
                                for cc in range(2):
                                    S.op('pe', lambda p: p.transpose(out=tps[:, 256 + cc * 128:256 + (cc + 1) * 128],
                                                                     in_=qt_[i][:, cc * 128:(cc + 1) * 128], identity=ident[:]),
                                         r=['qt%d' % i, 'ident'], w=[tk])
                            yield
                            S.op('dve', lambda v: v.tensor_tensor(out=kT_[:], in0=tps[:, 0:256], in1=Eneg[:], op=ALU.mult),
                                 r=[tk, 'Eneg'], w=['kT_'])
                            S.op('pool', lambda g: g.tensor_tensor(out=ktok[:], in0=kt_[i][:], in1=Etok[:], op=ALU.mult),
                                 r=['kt%d' % i, 'Etok'], w=['ktok'])
                            if ownt:
                                S.op('dve', lambda v: v.scalar_tensor_tensor(out=qT_[:], in0=tps[:, 256:512], scalar=1.0 / 16, in1=Epos[:],
                                                                             op0=ALU.mult, op1=ALU.mult), r=[tk, 'Epos'], w=['qT_'])
                                yield
                                psA, pkA = next_mm('d')
                                for cc in range(2):
                                    S.op('pe', lambda p: p.matmul(psA[:, 0:128], lhsT=kT_[:, cc * 128:(cc + 1) * 128],
                                                                  rhs=qT_[:, cc * 128:(cc + 1) * 128], start=(cc == 0), stop=(cc == 1)),
                                         r=['kT_', 'qT_'], w=[pkA])
                                S.op('dve', lambda v: v.tensor_tensor(out=attnT[:], in0=psA[:, 0:128], in1=triu, op=ALU.mult),
                                     r=[pkA, 'cst'], w=['attnT'])
                                yield
                                psO, pkO = next_mm('d')
                                S.op('pe', lambda p: p.matmul(psO[:, :], lhsT=attnT[:], rhs=vt_[i][:], start=True, stop=False),
                                     r=['attnT', 'vt%d' % i], w=[pkO])
                                for cc in range(2):
                                    S.op('pe', lambda p: p.matmul(psO[:, :], lhsT=qT_[:, cc * 128:(cc + 1) * 128], rhs=Sbf[:, cc, :],
                                                                  start=False, stop=(cc == 1)), r=['qT_', 'Sbf'], w=[pkO])
                                ssc = small[:, 4 + i:5 + i]
                                S.op('act', lambda a: a.activation(out=junkD[:], in_=psO[:, :], func=AF.Square, accum_out=ssc),
                                     r=[pkO], w=['junkD', 'smallD'])
                                rstd_from_ss(ssc, 512, 'smallD')
                                S.op('dve', lambda v: v.scalar_tensor_tensor(out=oa_[i][:], in0=psO[:, :], scalar=ssc, in1=gs_[i][:],
                                                                             op0=ALU.mult, op1=ALU.mult),
                                     r=[pkO, 'smallD', 'gs%d' % i], w=['oa%d' % i])
                                S.dma('sp', OA[q0:q0 + 128, h * 512:(h + 1) * 512], oa_[i][:], r=['oa%d' % i], w=[('OA', n)])
                            yield
                            for cc in range(2):
                                psU, pkU = next_mm('d')
                                S.op('pe', lambda p: p.matmul(psU[:, :], lhsT=ktok[:, cc * 128:(cc + 1) * 128], rhs=vt_[i][:],
                                                              start=True, stop=True), r=['ktok', 'vt%d' % i], w=[pkU])
                                S.op('dve', lambda v: v.tensor_tensor(out=Sst[:, cc, :], in0=psU[:, :], in1=Sst[:, cc, :], op=ALU.add),
                                     r=[pkU, 'Sst'], w=['Sst'])
                                S.op('dve', lambda v: v.tensor_scalar(out=Sst[:, cc, :], in0=Sst[:, cc, :],
                                                                      scalar1=Epos[:, cc * 128 + 127:cc * 128 + 128], scalar2=None, op0=ALU.mult),
                                     r=['Sst', 'Epos'], w=['Sst'])
                                if n == 7:
                                    S.op('dve', lambda v: v.tensor_scalar(out=Sst[:, cc, :], in0=Sst[:, cc, :], scalar1=ctxflag, scalar2=None,
                                                                          op0=ALU.mult), r=['Sst', 'cst'], w=['Sst'])
                                S.op('act', lambda a: a.activation(out=Sbf[:, cc, :], in_=Sst[:, cc, :], func=AF.Identity), r=['Sst'], w=['Sbf'])
                                yield


                def gen_E():
                    ikT2 = sb(pc, "ikT2", [128, TALL], BF16)
                    ikf = sb(pc, "ikf", [128, 72], F32)
                    ikd = sb(pc, "ikd", [128, 128], BF16)
                    iqs = sb(pc, "iqs", [128, 512], BF16)
                    iqT = sb(pc, "iqT", [128, 4, 128], BF16)
                    iwp = sb(pc, "iwp", [128, 8], F32)
                    score = sb(pc, "score", [128, TALL], F32)
                    relu_t = [sb(pc, "relu%d" % i, [128, 512], F32) for i in range(2)]
                    mask_tm = sb(pc, "mask_tm", [128, TALL], BF16)
                    S.op('dve', lambda v: v.memset(maskT[:], 0.0), w=['maskT'])
                    for kt in range(NT):
                        S.dma('sp', ikf[:], IKW[kt * 128:(kt + 1) * 128, :], r=[('IKW', kt)], w=['ikf'])
                        S.op('dve', lambda v: v.tensor_copy(out=ikd[:, 0:64], in_=ikf[:, 0:64]), r=['ikf'], w=['ikd'])
                        S.op('dve', lambda v: v.tensor_copy(out=ikd[:, 64:128], in_=ikf[:, 0:64]), r=['ikf'], w=['ikd'])
                        tps, tk = next_tp()
                        S.op('pe', lambda p: p.transpose(out=tps[:, 0:128], in_=ikd[:], identity=ident[:]), r=['ikd', 'ident'], w=[tk])
                        S.op('act', lambda a: a.activation(out=ikT2[:, kt * 128:(kt + 1) * 128], in_=tps[:, 0:128], func=AF.Identity),
                             r=[tk], w=['ikT2'])
                        yield
                    lo, hw, mid, cntv, gev, am = [small[:, 8 + j:9 + j] for j in range(6)]
                    for qi in range(8):
                        tt = 8 + qi
                        nk = 1024 + 128 * (qi + 1)
                        S.dma('sp', iqs[:], IQ[qi * 128:(qi + 1) * 128, :], r=[('IQ', tt)], w=['iqs'])
                        S.dma('act', ikf[:], IKW[tt * 128:(tt + 1) * 128, :], r=[('IKW', tt)], w=['ikf'])
                        yield
                        tps, tk = next_tp()
                        for c in range(4):
                            S.op('pe', lambda p: p.transpose(out=tps[:, c * 128:(c + 1) * 128], in_=iqs[:, c * 128:(c + 1) * 128],
                                                             identity=ident[:]), r=['iqs', 'ident'], w=[tk])
                        S.op('act', lambda a: a.activation(out=iqT[:].rearrange("p a b -> p (a b)"), in_=tps[:, 0:512], func=AF.Identity),
                             r=[tk], w=['iqT'])
                        S.op('dve', lambda v: v.tensor_scalar(out=iwp[:], in0=ikf[:, 64:72], scalar1=float(8 ** -0.5 * 64 ** -0.5),
                                                              scalar2=None, op0=ALU.mult), r=['ikf'], w=['iwp'])
                        ng = (nk + 511) // 512
                        for g in range(ng):
                            wd_ = min(512, nk - g * 512)
                            for hh in range(8):
                                ps, pk = next_mm('e')
                                pb_ = (hh % 2) * 64
                                S.op('pe', lambda p: p.matmul(ps[:, 0:wd_], lhsT=iqT[pb_:pb_ + 64, hh // 2, :],
                                                              rhs=ikT2[pb_:pb_ + 64, g * 512:g * 512 + wd_], start=True, stop=True),
                                     r=['iqT', 'ikT2'], w=[pk])
                                rl = relu_t[hh % 2]
                                rk = 'relu%d' % (hh % 2)
                                S.op('act', lambda a: a.activation(out=rl[:, 0:wd_], in_=ps[:, 0:wd_], func=AF.Relu), r=[pk], w=[rk])
                                sc_ = score[:, g * 512:g * 512 + wd_]
                                if hh == 0:
                                    S.op('dve', lambda v: v.tensor_scalar(out=sc_, in0=rl[:, 0:wd_], scalar1=iwp[:, 0:1], scalar2=None,
                                                                          op0=ALU.mult), r=[rk, 'iwp'], w=['score'])
                                else:
                                    S.op('dve', lambda v: v.scalar_tensor_tensor(out=sc_, in0=rl[:, 0:wd_], scalar=iwp[:, hh:hh + 1],
                                                                                 in1=sc_, op0=ALU.mult, op1=ALU.add),
                                         r=[rk, 'iwp', 'score'], w=['score'])
                                yield
                        S.op('dve', lambda v: v.tensor_reduce(out=am, in_=score[:, 0:nk], axis=AX.X, op=ALU.max,
                                                              apply_absolute_value=True), r=['score'], w=['smallE'])
                        S.op('dve', lambda v: v.tensor_scalar(out=score[:, 0:1024], in0=score[:, 0:1024], scalar1=ctxneg, scalar2=None,
                                                              op0=ALU.add), r=['score', 'cst'], w=['score'])
                        S.op('dve', lambda v: v.tensor_tensor(out=score[:, nk - 128:nk], in0=score[:, nk - 128:nk], in1=cmask, op=ALU.add),
                             r=['score', 'cst'], w=['score'])
                        S.op('dve', lambda v: v.tensor_scalar(out=hw, in0=am, scalar1=1.0001, scalar2=1e-20, op0=ALU.mult, op1=ALU.add),
                             r=['smallE'], w=['smallE'])
                        S.op('dve', lambda v: v.tensor_scalar(out=lo, in0=hw, scalar1=-1.0, scalar2=None, op0=ALU.mult),
                             r=['smallE'], w=['smallE'])
                        for it in range(NBISECT):
                            S.op('dve', lambda v: v.tensor_tensor(out=mid, in0=lo, in1=hw, op=ALU.add), r=['smallE'], w=['smallE'])
                            S.op('dve', lambda v: v.tensor_scalar(out=junk[:, 0:nk], in0=score[:, 0:nk], scalar1=mid, scalar2=None,
                                                                  op0=ALU.is_ge, op1=ALU.add, accum_out=cntv),
                                 r=['score', 'smallE'], w=['junk', 'smallE'])
                            S.op('dve', lambda v: v.tensor_scalar(out=gev, in0=cntv, scalar1=TOPK - 0.5, scalar2=None, op0=ALU.is_ge),
                                 r=['smallE'], w=['smallE'])
                            S.op('dve', lambda v: v.scalar_tensor_tensor(out=lo, in0=hw, scalar=gev, in1=lo, op0=ALU.mult, op1=ALU.add),
                                 r=['smallE'], w=['smallE'])
                            S.op('dve', lambda v: v.tensor_scalar(out=hw, in0=hw, scalar1=0.5, scalar2=None, op0=ALU.mult),
                                 r=['smallE'], w=['smallE'])
                            yield
                        S.op('dve', lambda v: v.tensor_scalar(out=mask_tm[:, 0:nk], in0=score[:, 0:nk], scalar1=lo, scalar2=None,
                                                              op0=ALU.is_ge), r=['score', 'smallE'], w=['mask_tm'])
                        nkb = nk // 128
                        for c0 in range(0, nkb, 8):
                            n_ = min(8, nkb - c0)
                            yield
                            tps, tk = next_tp()
                            for c in range(n_):
                                S.op('pe', lambda p: p.transpose(out=tps[:, c * 128:(c + 1) * 128],
                                                                 in_=mask_tm[:, (c0 + c) * 128:(c0 + c + 1) * 128], identity=ident[:]),
                                     r=['mask_tm', 'ident'], w=[tk])
                            S.op('act', lambda a: a.activation(out=maskT[:, c0:c0 + n_, qi * 128:(qi + 1) * 128],
                                                               in_=tps[:, 0:n_ * 128].rearrange("p (a b) -> p a b", b=128),
                                                               func=AF.Identity), r=[tk], w=['maskT'])
                            yield
                    yield

                linear(hact, 'hT', w_in, O_IQ, 512, own, store_iq)
                linear(hact, 'hT', w_in, O_IK, 72, allt, store_ikw)
                bg.append(gen_E())
                for cb in range(2):
                    linear(hact, 'hT', w_in, O_GQ + cb * 512, 512, own, store(GQ, True, cb * 512, 512))
                for cb in range(2):
                    linear(hact, 'hT', w_in, O_GK + cb * 512, 512, allt, store(GK, False, cb * 512, 512))
                for cb in range(4):
                    linear(hact, 'hT', w_in, O_GV + cb * 512, 512, allt, store(GV, False, cb * 512, 512))
                for cb in range(4):
                    gg = ggs[cb % 2]
                    S.dma('act', gg[:], gla_gain[0, cb * 512:(cb + 1) * 512].partition_broadcast(128), w=['ggs%d' % (cb % 2)])
                    linear(hact, 'hT', w_in, O_GR + cb * 512, 512, own, store_act(GR, cb * 512, 512, AF.Silu, mul=(gg, 'ggs%d' % (cb % 2))))
                bg.append(gen_D())
                for cb in range(4):
                    linear(hact, 'hT', w_in, O_DQ + cb * 512, 512, own, store_rope_d(DQ, True, cb * 512))
                for cb in range(4):
                    linear(hact, 'hT', w_in, O_DK + cb * 512, 512, allt, store_rope_d(DK, False, cb * 512))
                for cb in range(4):
                    linear(hact, 'hT', w_in, O_DV + cb * 512, 512, allt, store(DV, False, cb * 512, 512))
                for cb in range(4):
                    linear(hact, 'hT', w_in, O_GA + cb * 512, 512, own, store_act(GA, cb * 512, 512, AF.Sigmoid))
                for cb in range(4):
                    linear(hact, 'hT', w_in, O_GB + cb * 512, 512, own, store_act(GB, cb * 512, 512, AF.Sigmoid))
                bg_drain()
                S.barrier()
                if MTD is not None:
                    S.dma('sp', MTD, maskT[:], r=['maskT'], w=['MTD'])
                    S.barrier()
                if stop == 'C':
                    return nc
                set_mm_users()
        with ExitStack() as pefg:
            with ExitStack() as pef:

                with ExitStack() as pf:
                    kTg = sb(pf, "kTg", [128, 4, TALL], BF16)
                    vg = sb(pf, "vg", [128, NT, 512], BF16)
                    qTg = sb(pf, "qTg", [128, 4, TOWN], BF16)
                    ldt = [sb(pf, "ldt%d" % i, [128, 512], BF16) for i in range(2)]
                    pt_ = [sb(pf, "pt%d" % i, [128, 512], BF16) for i in range(4)]
                    pm_ = [sb(pf, "pm%d" % i, [128, 512], BF16) for i in range(4)]
                    alloc_psum(3, 1, 4)
                    lnd = sb(pf, "lnd", [128, 512], F32)
                    obs = [sb(pf, "obs%d" % i, [128, 512], BF16) for i in range(2)]
                    alloc_wbuf(pf, 2)
                    browF = [sb(pf, "browF%d" % i, [1, 512], F32) for i in range(2)]
                    mrowF = [sb(pf, "mrowF%d" % i, [1, 512], F32) for i in range(2)]

                    def gen_modrest():
                        for cb in range(8, 24):
                            j = cb % 2
                            S.dma('act', browF[j][:], b_ada[0:1, cb * 512:(cb + 1) * 512], w=['browF%d' % j])
                            wb, wkey = load_w(w_ada, 0, KC, cb * 512, 512)
                            yield
                            tpf = tp[0][:, :].bitcast(F32)
                            for kc in range(KC):
                                S.op('pe', lambda p: p.matmul(tpf[0:1, :], lhsT=sT[:, kc:kc + 1], rhs=wb[:, kc, :],
                                                              start=(kc == 0), stop=(kc == KC - 1)), r=['sT', wkey], w=['tp0'])
                            S.op('dve', lambda v: v.tensor_tensor(out=mrowF[j][:], in0=tpf[0:1, :], in1=browF[j][:], op=ALU.add),
                                 r=['tp0', 'browF%d' % j], w=['mrowF%d' % j])
                            S.dma('act', modrow_d[0:1, cb * 512:(cb + 1) * 512], mrowF[j][:], r=['mrowF%d' % j], w=[('modrow_d', cb)])
                            yield
                    bg.append(gen_modrest())
                    rden = sb(pf, "rden", [128, 512], F32)
                    for hg in range(4):
                        S.dma('act', vg[:], DV[:, hg * 512:(hg + 1) * 512].rearrange("(kt p) c -> p kt c", p=128), w=['vg'])
                        for kt in range(NT + 8):
                            i = kt % 2
                            if kt < NT:
                                S.dma('sp', ldt[i][:], DK[kt * 128:(kt + 1) * 128, hg * 512:(hg + 1) * 512], w=['ldt%d' % i])
                                dst = kTg[:, :, kt * 128:(kt + 1) * 128]
                                dk_ = 'kTg'
                            else:
                                qi = kt - NT
                                S.dma('sp', ldt[i][:], DQ[qi * 128:(qi + 1) * 128, hg * 512:(hg + 1) * 512], w=['ldt%d' % i])
                                dst = qTg[:, :, qi * 128:(qi + 1) * 128]
                                dk_ = 'qTg'
                            tps, tk = next_tp()
                            for c in range(4):
                                S.op('pe', lambda p: p.transpose(out=tps[:, c * 128:(c + 1) * 128], in_=ldt[i][:, c * 128:(c + 1) * 128],
                                                                 identity=ident[:]), r=['ldt%d' % i, 'ident'], w=[tk])
                            S.op('act' if kt % 2 == 0 else 'dve',
                                 (lambda a: a.activation(out=dst, in_=tps[:, 0:512].rearrange("p (a b) -> p a b", b=128), func=AF.Identity))
                                 if kt % 2 == 0 else
                                 (lambda v: v.tensor_copy(out=dst, in_=tps[:, 0:512].rearrange("p (a b) -> p a b", b=128))),
                                 r=[tk], w=[dk_])
                        steps = [(hh, qg, kb) for hh in range(4) for qg in range(2) for kb in range(8 + 4 * (qg + 1))]
                        LA = 2
                        slots = {}

                        def qk_stage(idx):
                            hh, qg, kb = steps[idx]
                            lps, lk = next_mm()
                            S.op('pe', lambda p: p.matmul(lps[:, :], lhsT=kTg[:, hh, kb * 128:(kb + 1) * 128],
                                                          rhs=qTg[:, hh, qg * 512:(qg + 1) * 512], start=True, stop=True),
                                 r=['kTg', 'qTg'], w=[lk])
                            j = idx % 4
                            slots[idx] = j
                            S.op('act', lambda a: a.activation(out=pt_[j][:], in_=lps[:, :], func=AF.Exp, scale=float(128 ** -0.5)),
                                 r=[lk], w=['pt%d' % j])
                            S.op('dve', lambda v: v.tensor_tensor(out=pm_[j][:], in0=pt_[j][:], in1=maskT[:, kb, qg * 512:(qg + 1) * 512],
                                                                  op=ALU.mult), r=['pt%d' % j, 'maskT'], w=['pm%d' % j])

                        def pv_stage(idx):
                            hh, qg, kb = steps[idx]
                            nkb = 8 + 4 * (qg + 1)
                            j = slots.pop(idx)
                            pr = (hh * 2 + qg) % 2
                            aO, aD = ax[2 * pr], ax[2 * pr + 1]
                            kO, kD = 'ax%d' % (2 * pr), 'ax%d' % (2 * pr + 1)
                            S.op('pe', lambda p: p.matmul(aO[:, :], lhsT=vg[:, kb, hh * 128:(hh + 1) * 128], rhs=pm_[j][:],
                                                          start=(kb == 0), stop=(kb == nkb - 1)), r=['vg', 'pm%d' % j], w=[kO])
                            S.op('pe', lambda p: p.matmul(aD[:, :], lhsT=ones_bf[:], rhs=pm_[j][:],
                                                          start=(kb == 0), stop=(kb == nkb - 1)), r=['ones_bf', 'pm%d' % j], w=[kD])
                            if kb == nkb - 1:
                                h = hg * 4 + hh
                                S.op('act', lambda a: a.activation(out=lnd[:], in_=aD[:, :], func=AF.Ln), r=[kD], w=['lnd'])
                                S.op('act', lambda a: a.activation(out=rden[:], in_=lnd[:], func=AF.Exp, scale=-1.0), r=['lnd'], w=['rden'])
                                jo = (hh * 2 + qg) % 2
                                S.op('dve', lambda v: v.tensor_tensor(out=obs[jo][:], in0=aO[:, :], in1=rden[:],
                                                                      op=ALU.mult), r=[kO, 'rden'], w=['obs%d' % jo])
                                S.dma('sp', OBD[h * 128:(h + 1) * 128, qg * 512:(qg + 1) * 512], obs[jo][:], r=['obs%d' % jo], w=[('OBD', h, qg)])

                        for idx in range(len(steps) + LA):
                            if idx % 12 == 0:
                                bg_step()
                            if idx < len(steps):
                                qk_stage(idx)
                            if idx - LA >= 0:
                                pv_stage(idx - LA)
                    bg_drain()
                    S.barrier()
                    with nc.allow_non_contiguous_dma(reason="one-time relayout of the modulation vector"):
                        S.dma('sp', modfm[:, 32:96], modrow_d[0, 2 * D:6 * D].rearrange("(j p) -> p j", p=128), w=['modfm'])
                    S.op('dve', lambda v: v.scalar_tensor_tensor(out=A2[:], in0=modfm[:, 64:80], scalar=1.0, in1=n2g_t[:],
                                                                 op0=ALU.add, op1=ALU.mult), r=['modfm', 'n2g'], w=['A2'])
                    S.barrier()
                    alloc_psum(4, 2, 2)
            s_mask.close()

            with ExitStack() as pg1:
                o_aT = sb(pg1, "o_aT", [128, KC, TOWN], BF16)
                o_bT = sb(pg1, "o_bT", [128, KC, TOWN], BF16)
                S.dma('act', o_bT[:], OBD.rearrange("(h p) t -> p h t", p=128), w=['o_bT'])
                oat = [sb(pg1, "oat%d" % i, [128, D], BF16) for i in range(2)]
                gat = [sb(pg1, "gat%d" % i, [128, 512], BF16) for i in range(2)]
                gbt = [sb(pg1, "gbt%d" % i, [128, 512], BF16) for i in range(2)]
                m1 = sb(pg1, "m1", [128, 512], F32)
                m2 = sb(pg1, "m2", [128, 512], F32)
                mgs = [sb(pg1, "mgs%d" % i, [128, 512], BF16) for i in range(2)]
                alloc_wbuf(pg1, 4)
                for qi in range(8):
                    i = qi % 2
                    S.dma('sp', oat[i][:], OA[qi * 128:(qi + 1) * 128, :], w=['oat%d' % i])

                    def ev(c0, n, tps, tkey, qi=qi):
                        S.op('act', lambda a: a.activation(out=o_aT[:, c0:c0 + n, qi * 128:(qi + 1) * 128],
                                                           in_=tps[:, 0:n * 128].rearrange("p (a b) -> p a b", b=128),
                                                           func=AF.Identity), r=[tkey], w=['o_aT'])
                    to_feature_major(oat[i], 'oat%d' % i, KC, None, 'o_aT', ev)
                for cb in range(4):
                    wa, wak = load_w(w_ba, 0, KC, cb * 512, 512)
                    wd2, wdk = load_w(w_bd, 0, KC, cb * 512, 512)
                    for qi in range(8):
                        i = qi % 2
                        S.dma('sp', gat[i][:], GA[qi * 128:(qi + 1) * 128, cb * 512:(cb + 1) * 512], w=['gat%d' % i])
                        S.dma('act', gbt[i][:], GB[qi * 128:(qi + 1) * 128, cb * 512:(cb + 1) * 512], w=['gbt%d' % i])
                        psa, pka = next_mm()
                        for kc in range(KC):
                            S.op('pe', lambda p: p.matmul(psa[:, :], lhsT=o_aT[:, kc, qi * 128:(qi + 1) * 128], rhs=wa[:, kc, :],
                                                          start=(kc == 0), stop=(kc == KC - 1)), r=['o_aT', wak], w=[pka])
                        psb, pkb = next_mm()
                        for kc in range(KC):
                            S.op('pe', lambda p: p.matmul(psb[:, :], lhsT=o_bT[:, kc, qi * 128:(qi + 1) * 128], rhs=wd2[:, kc, :],
                                                          start=(kc == 0), stop=(kc == KC - 1)), r=['o_bT', wdk], w=[pkb])
                        S.op('dve', lambda v: v.tensor_tensor(out=m1[:], in0=psa[:, :], in1=gat[i][:], op=ALU.mult),
                             r=[pka, 'gat%d' % i], w=['m1'])
                        S.op('dve', lambda v: v.tensor_tensor(out=m2[:], in0=psb[:, :], in1=gbt[i][:], op=ALU.mult),
                             r=[pkb, 'gbt%d' % i], w=['m2'])
                        S.op('pool', lambda g: g.tensor_tensor(out=mgs[i][:], in0=m1[:], in1=m2[:], op=ALU.add),
                             r=['m1', 'm2'], w=['mgs%d' % i])
                        S.dma('sp', MG[qi * 128:(qi + 1) * 128, cb * 512:(cb + 1) * 512], mgs[i][:], r=['mgs%d' % i], w=[('MG', qi)])
                S.barrier()

        with ExitStack() as px:
            x1 = sb(px, "x1", [128, 8, D], F32)
            rowb = sb(px, "rowb", [128, D], F32)
            with ExitStack() as pg2:
                alloc_wbuf(pg2, 2)
                mergedT = sb(pg2, "mergedT", [128, KC, TOWN], BF16)
                mgt = [sb(pg2, "mgt%d" % i, [128, D], BF16) for i in range(2)]
                xres = [sb(pg2, "xres%d" % i, [128, 512], F32) for i in range(2)]
                tmpf = sb(pg2, "tmpf", [128, 512], F32)
                S.dma('act', rowb[:], modrow_d[0, 2 * D:3 * D].partition_broadcast(128), w=['rowb'])
                for qi in range(8):
                    i = qi % 2
                    S.dma('sp', mgt[i][:], MG[qi * 128:(qi + 1) * 128, :], w=['mgt%d' % i])

                    def ev2(c0, n, tps, tkey, qi=qi):
                        S.op('act', lambda a: a.activation(out=mergedT[:, c0:c0 + n, qi * 128:(qi + 1) * 128],
                                                           in_=tps[:, 0:n * 128].rearrange("p (a b) -> p a b", b=128),
                                                           func=AF.Identity), r=[tkey], w=['mergedT'])
                    to_feature_major(mgt[i], 'mgt%d' % i, KC, None, 'mergedT', ev2)

                def evac_mo(cb):
                    def f(tt, ps, pkey):
                        i = tt % 2
                        S.dma('sp', xres[i][:], xs[TOWN + tt * 128:TOWN + (tt + 1) * 128, cb * 512:(cb + 1) * 512], w=['xres%d' % i])
                        S.op('dve', lambda v: v.tensor_tensor(out=tmpf[:], in0=ps[:, :], in1=rowb[:, cb * 512:(cb + 1) * 512], op=ALU.mult),
                             r=[pkey, 'rowb'], w=['tmpf'])
                        S.op('dve', lambda v: v.tensor_tensor(out=x1[:, tt, cb * 512:(cb + 1) * 512], in0=tmpf[:], in1=xres[i][:], op=ALU.add),
                             r=['tmpf', 'xres%d' % i], w=[('x1', tt)])
                    return f
                for cb in range(4):
                    linear(lambda kc, tt: mergedT[:, kc, tt * 128:(tt + 1) * 128], 'mergedT', w_mo, cb * 512, 512, list(range(8)), evac_mo(cb))
                S.barrier()

            with ExitStack() as ph:
                alloc_wbuf(ph, 2, 11, 512)
                h2T = sb(ph, "h2T", [128, KC, TOWN], BF16)
                xn2 = [sb(ph, "xn2_%d" % i, [128, D], BF16) for i in range(2)]
                actT = sb(ph, "actT", [128, 11, TOWN], BF16)
                sg = [sb(ph, "sg%d" % i, [128, 512], F32) for i in range(2)]
                tmp2 = sb(ph, "tmp2", [128, 512], F32)
                gub = [sb(ph, "gub%d" % i, [128, KC, 128], BF16) for i in range(6)]
                rr['gu'] = 0

                def load_gu(c0):
                    i = rr['gu'] % 6
                    rr['gu'] += 1
                    S.dma('pool', gub[i][:], w_gu[:, c0:c0 + 128].rearrange("(kc p) n -> p kc n", p=128), w=['gub%d' % i])
                    return gub[i], 'gub%d' % i
                S.dma('act', rowb[:], modrow_d[0, 5 * D:6 * D].partition_broadcast(128), w=['rowb'])
                for qi in range(8):
                    i = qi % 2
                    norm_to_fm(x1[:, qi, :], ('x1', qi), h2T, 'h2T', qi * 128, A2, sh2, ['A2', 'modfm'],
                               xn2[i], 'xn2_%d' % i, small[:, 16 + i:17 + i])
                cnt_s = 0
                for fb in range(4):
                    for fc in range(11):
                        f0 = fb * 1408 + fc * 128
                        wg, wgk = load_gu(f0)
                        wu, wuk = load_gu(DFF + f0)
                        for tg in range(2):
                            psg, pkg = next_mm()
                            for kc in range(KC):
                                S.op('pe', lambda p: p.matmul(psg[:, :], lhsT=wg[:, kc, 0:128], rhs=h2T[:, kc, tg * 512:(tg + 1) * 512],
                                                              start=(kc == 0), stop=(kc == KC - 1)), r=['h2T', wgk], w=[pkg])
                            psu, pku = next_mm()
                            for kc in range(KC):
                                S.op('pe', lambda p: p.matmul(psu[:, :], lhsT=wu[:, kc, 0:128], rhs=h2T[:, kc, tg * 512:(tg + 1) * 512],
                                                              start=(kc == 0), stop=(kc == KC - 1)), r=['h2T', wuk], w=[pku])
                            j = cnt_s % 2
                            cnt_s += 1
                            S.op('act', lambda a: a.activation(out=sg[j][:], in_=psg[:, :], func=AF.Silu), r=[pkg], w=['sg%d' % j])
                            S.op('dve', lambda v: v.tensor_tensor(out=actT[:, fc, tg * 512:(tg + 1) * 512], in0=psu[:, :], in1=sg[j][:],
                                                                  op=ALU.mult), r=[pku, 'sg%d' % j], w=['actT'])

                    def evac_dn(cb):
                        def f(tt, ps, pkey):
                            S.op('dve', lambda v: v.tensor_tensor(out=tmp2[:], in0=ps[:, :], in1=rowb[:, cb * 512:(cb + 1) * 512], op=ALU.mult),
                                 r=[pkey, 'rowb'], w=['tmp2'])
                            S.op('pool', lambda g: g.tensor_tensor(out=x1[:, tt, cb * 512:(cb + 1) * 512], in0=x1[:, tt, cb * 512:(cb + 1) * 512],
                                                                   in1=tmp2[:], op=ALU.add), r=['tmp2', ('x1', tt)], w=[('x1', tt)])
                        return f
                    for cb in range(4):
                        linear(lambda kc, tt: actT[:, kc, tt * 128:(tt + 1) * 128], 'actT', w_dn, cb * 512, 512, list(range(8)),
                               evac_dn(cb), r0=fb * 1408, nk=11)
                S.barrier()

            with ExitStack() as pi_:
                ot = [sb(pi_, "ot%d" % i, [128, D], F32) for i in range(2)]
                S.dma('act', rowb[:], fng[0, :].partition_broadcast(128), w=['rowb'])
                for qi in range(8):
                    i = qi % 2
                    ssc = small[:, 20 + i:21 + i]
                    S.op('act', lambda a: a.activation(out=junk[:], in_=x1[:, qi, :], func=AF.Square, accum_out=ssc),
                         r=[('x1', qi)], w=['junk', 'small'])
                    rstd_from_ss(ssc, D, 'small')
                    S.op('dve', lambda v: v.scalar_tensor_tensor(out=ot[i][:], in0=x1[:, qi, :], scalar=ssc, in1=rowb[:],
                                                                 op0=ALU.mult, op1=ALU.mult), r=[('x1', qi), 'small', 'rowb'], w=['ot%d' % i])
                    S.dma('sp', out[qi * 128:(qi + 1) * 128, :], ot[i][:], r=['ot%d' % i], w=[('out', qi)])
                S.barrier()
        S.barrier()
    return nc


def _consts(half):
    c = np.zeros((128, 512), np.float32)
    c[:, 0:128] = np.eye(128, dtype=np.float32)
    j = np.arange(128)
    c[:, 128:256] = (j[:, None] <= j[None, :]).astype(np.float32)
    c[:, 256:384] = np.where(j[None, :] <= j[:, None], 0.0, NEG)
    c[:, 384] = 1.0 if half == 1 else 0.0
    c[:, 385] = 0.0 if half == 1 else NEG
    theta = np.float32(500000.0)
    c[:, 400:416] = np.power(theta, -np.arange(0, 32, 2, dtype=np.float32) / np.float32(32))[None, :]
    c[:, 416:424] = np.power(theta, -np.arange(0, 16, 2, dtype=np.float32) / np.float32(16))[None, :]
    return c


def prep_inputs(inputs, cores=None):
    f = lambda a: np.ascontiguousarray(np.asarray(a))
    x = f(inputs["x"]); c = f(inputs["c"]); pos = f(inputs["positions"]).astype(np.int32)
    shared = {
        "w_ada": f(inputs["w_ada"])[0], "b_ada": f(inputs["b_ada"])[0][None, :], "w_in": f(inputs["w_in"])[0],
        "gate_up": f(inputs["gla_gate_up"])[0], "gate_bias": f(inputs["gla_gate_bias"])[0][None, :],
        "gla_gain": f(inputs["gla_norm_gain"])[0][None, :],
        "w_ba": f(inputs["w_branch_gla"])[0], "w_bd": f(inputs["w_branch_dsa"])[0], "w_mo": f(inputs["w_merge_out"])[0],
        "w_gu": f(inputs["w_ffn_gate_up"])[0], "w_dn": f(inputs["w_ffn_down"])[0],
        "n1g": np.ascontiguousarray(f(inputs["norm1_gain"])[0].reshape(16, 128).T),
        "n2g": np.ascontiguousarray(f(inputs["norm2_gain"])[0].reshape(16, 128).T),
        "fng": f(inputs["final_norm_gain"])[None, :],
    }
    maps = []
    for core in (range(8) if cores is None else cores):
        b, half = core // 2, core % 2
        if half == 1:
            xs = x[b]
            p = pos[b]
        else:
            xs = np.concatenate([np.zeros((TOWN, D), np.float32), x[b, :TOWN]], axis=0)
            p = np.concatenate([pos[b, :TOWN], pos[b, :TOWN]])
        m = dict(shared)
        m["xs"] = np.ascontiguousarray(xs)
        m["cfm"] = np.ascontiguousarray(c[b].reshape(16, 128).T)
        m["posi"] = np.ascontiguousarray(p.reshape(16, 128).T)
        m["cst"] = _consts(half)
        maps.append(m)
    return maps


_NC = None


def kernel(**inputs):
    global _NC
    if _NC is None:
        _NC = build_program()
    maps = prep_inputs(inputs)
    res = run_bass_kernel_spmd(_NC, maps, core_ids=list(range(8)))
    outp = np.zeros((NB, SEQ, D), np.float32)
    for core in range(8):
        b, half = core // 2, core % 2
        outp[b, half * TOWN:(half + 1) * TOWN] = res.results[core]["out"]
    return outp
```

```python
import math
from contextlib import ExitStack

import numpy as np
import concourse.bass as bass
import concourse.mybir as mybir
from concourse.bass_utils import run_bass_kernel_spmd

F32 = mybir.dt.float32
BF16 = mybir.dt.bfloat16
I32 = mybir.dt.int32
AF = mybir.ActivationFunctionType
ALU = mybir.AluOpType
AX = mybir.AxisListType

D = 2048
SEQ = 2048
NB = 4
TOWN = 1024
TALL = 2048
NT = 16
KC = 16
DFF = 5632
EPS = 1e-6
NEG = -1.0e30
TOPK = 256
NBISECT = 22

O_GQ, O_GK, O_GV, O_GR, O_GLR = 0, 1024, 2048, 4096, 6144
O_DQ, O_DK, O_DV = 6160, 8208, 10256
O_IQ, O_IK, O_IW = 12304, 12816, 12880
O_GA, O_GB = 12888, 14936
IN_W = 16984


class Sched:
    def __init__(self, nc, es, ndma=24):
        self.nc = nc
        self.eng = {'pe': nc.tensor, 'act': nc.scalar, 'dve': nc.vector, 'pool': nc.gpsimd, 'sp': nc.sync}
        self.semobj = {}
        for e in ['pe', 'act', 'dve', 'pool']:
            self.semobj[e] = es.enter_context(nc.semaphore('s_' + e))
        self.ndma = ndma
        for i in range(ndma):
            self.semobj[('d', i)] = es.enter_context(nc.semaphore('sd%d' % i))
            self.semobj[('g', i)] = es.enter_context(nc.semaphore('sg%d' % i))
        self.dma_rr_g = 0
        self.cnt = {k: 0 for k in self.semobj}
        self.seen = {e: {} for e in self.eng}
        self.lastw = {}
        self.readers = {}
        self.dma_rr = 0
        self.nwait = 0

    def _wait(self, e, k, v):
        if k == e and e == 'pe':
            return
        if self.seen[e].get(k, 0) >= v:
            return
        self.eng[e].wait_ge(self.semobj[k], v)
        self.seen[e][k] = v
        self.nwait += 1

    def _deps(self, e, r, w):
        for key in r:
            for k, v in self.lastw.get(key, {}).items():
                self._wait(e, k, v)
        for key in w:
            for k, v in self.lastw.get(key, {}).items():
                self._wait(e, k, v)
            for k, v in self.readers.get(key, {}).items():
                self._wait(e, k, v)

    def _record(self, ev, r, w):
        k, v = ev
        for key in r:
            d = self.readers.setdefault(key, {})
            d[k] = max(d.get(k, 0), v)
        for key in w:
            self.lastw[key] = {k: v}
            self.readers[key] = {}

    def op(self, e, fn, r=(), w=()):
        ex = [k for k in r if isinstance(k, str) and k[:2] in ('mm', 'tp', 'ax')]
        if ex:
            w = list(w) + [k for k in ex if k not in w]
        self._deps(e, r, w)
        ins = fn(self.eng[e])
        self.cnt[e] += 1
        ins.then_inc(self.semobj[e], 1)
        self._record((e, self.cnt[e]), r, w)

    def dma(self, q, out, in_, r=(), w=(), **kw):
        if q == 'pool':
            slot = ('g', self.dma_rr_g % self.ndma)
            self.dma_rr_g += 1
        else:
            slot = ('d', self.dma_rr % self.ndma)
            self.dma_rr += 1
        if self.cnt[slot] > 0:
            self._wait(q, slot, self.cnt[slot])
        self._deps(q, r, w)
        ins = self.eng[q].dma_start(out=out, in_=in_, **kw)
        self.cnt[slot] += 16
        ins.then_inc(self.semobj[slot], 16)
        self._record((slot, self.cnt[slot]), r, w)

    def barrier(self):
        for e in self.eng:
            for k in self.semobj:
                if self.cnt[k] > 0:
                    self._wait(e, k, self.cnt[k])
        self.lastw = {}
        self.readers = {}


def build_program(dbg=(), stop=None):
    nc = bass.Bass("TRN2", target_bir_lowering=False)
    import os
    stop = stop or os.environ.get('KSTOP')

    def din(name, shape, dt=F32):
        return nc.dram_tensor(name, list(shape), dt, kind="ExternalInput").ap()

    def dscr(name, shape, dt=BF16):
        kind = "ExternalOutput" if name in dbg else "Internal"
        return nc.dram_tensor(name, list(shape), dt, kind=kind).ap()

    xs = din("xs", [TALL, D])
    cfm = din("cfm", [128, KC])
    posi = din("posi", [128, NT], I32)
    w_ada = din("w_ada", [D, 6 * D])
    b_ada = din("b_ada", [1, 6 * D])
    w_in = din("w_in", [D, IN_W])
    gate_up = din("gate_up", [16, 1024])
    gate_bias = din("gate_bias", [1, 1024])
    gla_gain = din("gla_gain", [1, D])
    w_ba = din("w_ba", [D, D])
    w_bd = din("w_bd", [D, D])
    w_mo = din("w_mo", [D, D])
    w_gu = din("w_gu", [D, 2 * DFF])
    w_dn = din("w_dn", [DFF, D])
    n1g = din("n1g", [128, KC])
    n2g = din("n2g", [128, KC])
    fng = din("fng", [1, D])
    cst = din("cst", [128, 512])
    out = nc.dram_tensor("out", [TOWN, D], F32, kind="ExternalOutput").ap()

    modrow_d = dscr("modrow_d", [1, 6 * D], F32)
    GQ = dscr("GQ", [TOWN, 1024])
    GK = dscr("GK", [TALL, 1024])
    GV = dscr("GV", [TALL, 2048])
    GR = dscr("GR", [TOWN, 2048])
    DQ = dscr("DQ", [TOWN, 2048])
    DK = dscr("DK", [TALL, 2048])
    DV = dscr("DV", [TALL, 2048])
    IQ = dscr("IQ", [TOWN, 512])
    IKW = dscr("IKW", [TALL, 72], F32)
    GA = dscr("GA", [TOWN, 2048])
    GB = dscr("GB", [TOWN, 2048])
    OA = dscr("OA", [TOWN, 2048])
    MG = dscr("MG", [TOWN, 2048])
    OBD = dscr("OBD", [D, TOWN])
    MTD = dscr("MTD", [128, NT, TOWN]) if "MTD" in dbg else None

    with ExitStack() as es:
        S = Sched(nc, es)

        def sb(stack, name, shape, dt):
            return stack.enter_context(nc.sbuf_tensor(name, list(shape), dt))

        mm, tp, ax = [], [], []
        pstack = [None]
        rr = {'mm': 0, 'tp': 0, 'stg': 0, 'wb': 0}

        def alloc_psum(nm, nt, na):
            if pstack[0] is not None:
                pstack[0].close()
            st = ExitStack()
            pstack[0] = st
            rr['pgen'] = rr.get('pgen', 0) + 1
            g = rr['pgen']
            mm[:] = [st.enter_context(nc.psum_tensor("mm%d_%d" % (i, g), [128, 512], F32)) for i in range(nm)]
            tp[:] = [st.enter_context(nc.psum_tensor("tp%d_%d" % (i, g), [128, 1024], BF16)) for i in range(nt)]
            ax[:] = [st.enter_context(nc.psum_tensor("ax%d_%d" % (i, g), [128, 512], F32)) for i in range(na)]

        alloc_psum(4, 2, 2)

        mm_users = {}

        def set_mm_users(**parts):
            mm_users.clear()
            mm_users.update(parts)

        def next_mm(user=None):
            banks = mm_users.get(user) if mm_users else None
            if banks is None:
                banks = list(range(len(mm)))
            c = rr.get(('mm', user), 0)
            rr[('mm', user)] = c + 1
            i = banks[c % len(banks)]
            return mm[i], 'mm%d' % i

        bg = []

        def bg_step():
            for g in list(bg):
                try:
                    next(g)
                except StopIteration:
                    bg.remove(g)

        def bg_drain():
            while bg:
                bg_step()

        def next_tp():
            i = rr['tp'] % len(tp)
            rr['tp'] += 1
            return tp[i], 'tp%d' % i

        cst_t = sb(es, "cst_t", [128, 512], F32)
        S.dma('sp', cst_t[:], cst, w=['cst'])
        identf = cst_t[:, 0:128]
        triu = cst_t[:, 128:256]
        cmask = cst_t[:, 256:384]
        ctxflag = cst_t[:, 384:385]
        ctxneg = cst_t[:, 385:386]
        invf_d = cst_t[:, 400:416]
        invf_i = cst_t[:, 416:424]
        ident = sb(es, "ident", [128, 128], BF16)
        ones_bf = sb(es, "ones_bf", [128, 128], BF16)
        S.op('dve', lambda v: v.tensor_copy(out=ident[:], in_=identf), r=['cst'], w=['ident'])
        S.op('dve', lambda v: v.memset(ones_bf[:], 1.0), w=['ones_bf'])
        modfm = sb(es, "modfm", [128, 96], F32)
        A1 = sb(es, "A1", [128, KC], F32)
        A2 = sb(es, "A2", [128, KC], F32)
        n1g_t = sb(es, "n1g_t", [128, KC], F32)
        n2g_t = sb(es, "n2g_t", [128, KC], F32)
        S.dma('sp', n1g_t[:], n1g, w=['n1g'])
        S.dma('sp', n2g_t[:], n2g, w=['n2g'])
        wbuf = []

        def alloc_wbuf(stack, n, nk=KC, nb=512):
            rr['wgen'] = rr.get('wgen', 0) + 1
            wbuf[:] = [stack.enter_context(nc.sbuf_tensor("wbuf%d_%d" % (i, rr['wgen']), [128, nk, nb], BF16)) for i in range(n)]
        stg = [sb(es, "stg%d" % i, [128, 512], BF16) for i in range(4)]
        small = sb(es, "small", [128, 64], F32)
        junk = sb(es, "junk", [128, 2048], BF16)
        glrT = sb(es, "glrT", [32, TALL], F32)

        def next_stg():
            i = rr['stg'] % 4
            rr['stg'] += 1
            return stg[i], 'stg%d' % i

        def load_w(W, r0, nk, c0, nb, q='pool'):
            i = rr['wb'] % len(wbuf)
            rr['wb'] += 1
            key = 'wbuf%d' % i
            src = W[r0:r0 + nk * 128, c0:c0 + nb].rearrange("(kc p) n -> p kc n", p=128)
            S.dma(q, wbuf[i][:, 0:nk, 0:nb], src, w=[key])
            return wbuf[i], key

        def linear(actT, akey, W, c0, nb, tts, evac, r0=0, nk=KC, m=128, user='c'):
            wb, wkey = load_w(W, r0, nk, c0, nb)
            for tt in tts:
                bg_step()
                ps, pkey = next_mm(user)
                for kc in range(nk):
                    if kc == nk // 2:
                        bg_step()
                    S.op('pe', lambda p: p.matmul(ps[0:m, 0:nb], lhsT=actT(kc, tt), rhs=wb[:, kc, 0:nb],
                                                  start=(kc == 0), stop=(kc == nk - 1)),
                         r=[akey, wkey], w=[pkey])
                evac(tt, ps, pkey)

        def rstd_from_ss(ss_ap, n, key):
            S.op('dve', lambda v: v.tensor_scalar(out=ss_ap, in0=ss_ap, scalar1=1.0 / n, scalar2=EPS,
                                                  op0=ALU.mult, op1=ALU.add), r=[key], w=[key])
            S.op('act', lambda a: a.activation(out=ss_ap, in_=ss_ap, func=AF.Sqrt), r=[key], w=[key])
            S.op('dve', lambda v: v.reciprocal(out=ss_ap, in_=ss_ap), r=[key], w=[key])

        def to_feature_major(src_tile, skey, nchunk, dst_fn, dkey, evac_eng_fn):
            for c0 in range(0, nchunk, 8):
                n = min(8, nchunk - c0)
                tps, tkey = next_tp()
                for c in range(n):
                    S.op('pe', lambda p: p.transpose(out=tps[:, c * 128:(c + 1) * 128],
                                                     in_=src_tile[:, (c0 + c) * 128:(c0 + c + 1) * 128],
                                                     identity=ident[:]),
                         r=[skey, 'ident'], w=[tkey])
                evac_eng_fn(c0, n, tps, tkey)

        sT = sb(es, "sT", [128, KC], BF16)
        with ExitStack() as pa:
            alloc_wbuf(pa, 2)
            c_t = sb(pa, "c_t", [128, KC], F32)
            brow = sb(pa, "brow", [1, 2 * D], F32)
            mrow = sb(pa, "mrow", [1, 2 * D], F32)
            S.dma('sp', c_t[:], cfm, w=['c_t'])
            S.dma('sp', brow[:], b_ada[0:1, 0:2 * D], w=['brow'])
            S.op('act', lambda a: a.activation(out=sT[:], in_=c_t[:], func=AF.Silu), r=['c_t'], w=['sT'])

            def evac_mod(cb):
                def f(tt, ps, pkey):
                    S.op('dve', lambda v: v.tensor_tensor(out=mrow[0:1, cb * 512:(cb + 1) * 512], in0=ps[0:1, :],
                                                          in1=brow[0:1, cb * 512:(cb + 1) * 512], op=ALU.add),
                         r=[pkey, 'brow'], w=['mrow'])
                return f
            for cb in range(8):
                linear(lambda kc, tt: sT[:, kc:kc + 1], 'sT', w_ada, cb * 512, 512, [0], evac_mod(cb), m=1)
            S.dma('sp', modrow_d[0:1, 0:2 * D], mrow[:], r=['mrow'], w=['modrow_d'])
            with nc.allow_non_contiguous_dma(reason="one-time relayout of the modulation vector"):
                S.dma('sp', modfm[:, 0:32], modrow_d[0, 0:2 * D].rearrange("(j p) -> p j", p=128), r=['modrow_d'], w=['modfm'])
            S.op('dve', lambda v: v.scalar_tensor_tensor(out=A1[:], in0=modfm[:, 16:32], scalar=1.0, in1=n1g_t[:],
                                                         op0=ALU.add, op1=ALU.mult), r=['modfm', 'n1g'], w=['A1'])
            S.barrier()
        if stop == 'A':
            return nc
        sh1 = modfm[:, 0:16]
        sh2 = modfm[:, 48:64]

        def norm_to_fm(x_tile, xkey, dstT, dkey, tcol, A, sh, akeys, xn, xnkey, ss_ap):
            S.op('act', lambda a: a.activation(out=junk[:], in_=x_tile, func=AF.Square, accum_out=ss_ap),
                 r=[xkey], w=['junk', 'small'])
            rstd_from_ss(ss_ap, D, 'small')
            S.op('dve', lambda v: v.tensor_scalar(out=xn[:], in0=x_tile, scalar1=ss_ap, scalar2=None, op0=ALU.mult),
                 r=[xkey, 'small'], w=[xnkey])

            def ev(c0, n, tps, tkey):
                for c in range(n):
                    kc = c0 + c
                    S.op('act', lambda a: a.activation(out=dstT[:, kc, tcol:tcol + 128], in_=tps[:, c * 128:(c + 1) * 128],
                                                       func=AF.Identity, scale=A[:, kc:kc + 1], bias=sh[:, kc:kc + 1]),
                         r=[tkey] + akeys, w=[dkey])
            to_feature_major(xn, xnkey, KC, None, dkey, ev)

        s_mask = ExitStack()
        maskT = sb(s_mask, "maskT", [128, NT, TOWN], BF16)
        with ExitStack() as pbc:
            hT = sb(pbc, "hT", [128, KC, TALL], BF16)
            with ExitStack() as pb:
                xt = [sb(pb, "xt%d" % i, [128, D], F32) for i in range(2)]
                xn = [sb(pb, "xn%d" % i, [128, D], BF16) for i in range(2)]
                for tt in range(NT):
                    i = tt % 2
                    S.dma('sp' if i == 0 else 'act', xt[i][:], xs[tt * 128:(tt + 1) * 128, :], w=['xt%d' % i])
                    norm_to_fm(xt[i][:], 'xt%d' % i, hT, 'hT', tt * 128, A1, sh1, ['A1', 'modfm'],
                               xn[i], 'xn%d' % i, small[:, i:i + 1])
                S.barrier()

            with ExitStack() as pc:
                alloc_wbuf(pc, 2)
                alloc_psum(7, 1, 0)
                set_mm_users(c=[0, 1, 2], d=[3, 4], e=[5, 6])
                sinD = sb(pc, "sinD", [128, NT, 1, 16], F32)
                cosD = sb(pc, "cosD", [128, NT, 1, 16], F32)
                sinI = sb(pc, "sinI", [128, NT, 1, 8], F32)
                cosI = sb(pc, "cosI", [128, NT, 1, 8], F32)
                ggs = [sb(pc, "ggs%d" % i, [128, 512], F32) for i in range(2)]
                rt = [sb(pc, "rt%d" % i, [128, 4, 16], F32) for i in range(4)]
                f32stg = sb(pc, "f32stg", [128, 512], F32)
                f32stg2 = sb(pc, "f32stg2", [128, 72], F32)
                rsl = [sb(pc, "rsl%d" % i, [128, 128], F32) for i in range(2)]
                ptab = ExitStack()
                posf = sb(ptab, "posf", [128, NT], F32)
                pos_i = sb(ptab, "pos_i", [128, NT], I32)
                ang = sb(ptab, "ang", [128, NT, 16], F32)
                kf = sb(ptab, "kf", [128, NT, 16], F32)
                ki = sb(ptab, "ki", [128, NT, 16], I32)
                kf2 = sb(ptab, "kf2", [128, NT, 16], F32)
                S.dma('sp', pos_i[:], posi, w=['pos_i'])
                S.op('dve', lambda v: v.tensor_copy(out=posf[:], in_=pos_i[:]), r=['pos_i'], w=['posf'])
                TWO_PI = 2.0 * math.pi

                def make_tables(invf, nj, sin_t, cos_t, key):
                    for tt in range(NT):
                        S.op('dve', lambda v: v.tensor_scalar(out=ang[:, tt, 0:nj], in0=invf, scalar1=posf[:, tt:tt + 1],
                                                              scalar2=None, op0=ALU.mult), r=['cst', 'posf', 'ang'], w=['ang'])
                    a = ang[:, :, 0:nj]
                    kk = kf[:, :, 0:nj]
                    mm_ = kf2[:, :, 0:nj]
                    S.op('dve', lambda v: v.tensor_scalar(out=kk, in0=a, scalar1=1.0 / TWO_PI, scalar2=None,
                                                          op0=ALU.mult), r=['ang'], w=['kf'])
                    S.op('dve', lambda v: v.tensor_copy(out=ki[:, :, 0:nj], in_=kk), r=['kf'], w=['ki'])
                    S.op('dve', lambda v: v.tensor_copy(out=kk, in_=ki[:, :, 0:nj]), r=['ki'], w=['kf'])
                    S.op('dve', lambda v: v.scalar_tensor_tensor(out=a, in0=kk, scalar=-TWO_PI, in1=a,
                                                                 op0=ALU.mult, op1=ALU.add), r=['kf', 'ang'], w=['ang'])
                    for shift, dst in ((0.0, sin_t), (math.pi / 2, cos_t)):
                        S.op('dve', lambda v: v.tensor_scalar(out=kk, in0=a, scalar1=shift, scalar2=None,
                                                              op0=ALU.add), r=['ang', 'kf'], w=['kf'])
                        for cmp, bound, sgn in ((ALU.is_gt, math.pi, -1.0), (ALU.is_lt, -math.pi, 1.0)):
                            S.op('dve', lambda v: v.tensor_scalar(out=mm_, in0=kk, scalar1=bound, scalar2=sgn * TWO_PI,
                                                                  op0=cmp, op1=ALU.mult), r=['kf'], w=['kf2'])
                            S.op('dve', lambda v: v.tensor_tensor(out=kk, in0=kk, in1=mm_, op=ALU.add),
                                 r=['kf', 'kf2'], w=['kf'])
                        S.op('act', lambda a_: a_.activation(out=dst[:, :, 0, :], in_=kk, func=AF.Sin),
                             r=['kf'], w=[key])
                make_tables(invf_d, 16, sinD, cosD, 'tabD')
                make_tables(invf_i, 8, sinI, cosI, 'tabI')
                S.barrier()
                ptab.close()
                S.op('dve', lambda v: v.memset(glrT[:, :], 1.0), w=['glrT'])
                wb, wkey = load_w(w_in, 0, KC, O_GLR, 16)
                for tg in range(4):
                    ps, pkey = next_mm()
                    for kc in range(KC):
                        S.op('pe', lambda p: p.matmul(ps[0:16, :], lhsT=wb[:, kc, 0:16], rhs=hT[:, kc, tg * 512:(tg + 1) * 512],
                                                      start=(kc == 0), stop=(kc == KC - 1)), r=['hT', wkey], w=[pkey])
                    S.op('act', lambda a: a.activation(out=glrT[0:16, tg * 512:(tg + 1) * 512], in_=ps[0:16, :], func=AF.Identity),
                         r=[pkey], w=['glrT'])

                if stop == 'C1':
                    S.barrier()
                    return nc
                own = list(range(8, 16))
                allt = list(range(NT))
                hact = lambda kc, tt: hT[:, kc, tt * 128:(tt + 1) * 128]

                def store(dst, own_only, c0, nb):
                    def f(tt, ps, pkey):
                        st, skey = next_stg()
                        S.op('act', lambda a: a.activation(out=st[:, 0:nb], in_=ps[:, 0:nb], func=AF.Identity), r=[pkey], w=[skey])
                        row = (tt - 8 if own_only else tt) * 128
                        S.dma('sp', dst[row:row + 128, c0:c0 + nb], st[:, 0:nb], r=[skey], w=[(id(dst), tt)])
                    return f

                def store_act(dst, c0, nb, func, mul=None):
                    def f(tt, ps, pkey):
                        st, skey = next_stg()
                        if mul is None:
                            S.op('act', lambda a: a.activation(out=st[:, 0:nb], in_=ps[:, 0:nb], func=func), r=[pkey], w=[skey])
                        else:
                            S.op('act', lambda a: a.activation(out=f32stg[:, 0:nb], in_=ps[:, 0:nb], func=func), r=[pkey], w=['f32stg'])
                            S.op('dve', lambda v: v.tensor_tensor(out=st[:, 0:nb], in0=f32stg[:, 0:nb], in1=mul[0][:, 0:nb],
                                                                  op=ALU.mult), r=['f32stg', mul[1]], w=[skey])
                        row = (tt - 8) * 128
                        S.dma('sp', dst[row:row + 128, c0:c0 + nb], st[:, 0:nb], r=[skey], w=[(id(dst), tt)])
                    return f

                def rope_ops(x1, x2, o1, o2, cs, sn, pkey, skey, tkey, shape):
                    t = [rt[i][:].rearrange("p a b -> p (a b)")[:, 0:shape[0] * shape[1]].rearrange("p (a b) -> p a b", b=shape[1])
                         for i in range(4)]
                    S.op('dve', lambda v: v.tensor_tensor(out=t[0], in0=x1, in1=cs, op=ALU.mult), r=[pkey, tkey], w=['rt0'])
                    S.op('dve', lambda v: v.tensor_tensor(out=t[1], in0=x2, in1=sn, op=ALU.mult), r=[pkey, tkey], w=['rt1'])
                    S.op('dve', lambda v: v.tensor_tensor(out=o1, in0=t[0], in1=t[1], op=ALU.subtract), r=['rt0', 'rt1'], w=[skey])
                    S.op('dve', lambda v: v.tensor_tensor(out=t[2], in0=x1, in1=sn, op=ALU.mult), r=[pkey, tkey], w=['rt2'])
                    S.op('dve', lambda v: v.tensor_tensor(out=t[3], in0=x2, in1=cs, op=ALU.mult), r=[pkey, tkey], w=['rt3'])
                    S.op('dve', lambda v: v.tensor_tensor(out=o2, in0=t[2], in1=t[3], op=ALU.add), r=['rt2', 'rt3'], w=[skey])

                def store_rope_d(dst, own_only, c0):
                    def f(tt, ps, pkey):
                        st, skey = next_stg()
                        S.op('act', lambda a: a.activation(out=st[:, :], in_=ps[:, :], func=AF.Identity), r=[pkey], w=[skey])
                        pv = ps[:, :].rearrange("p (h d) -> p h d", d=128)
                        sv = st[:, :].rearrange("p (h d) -> p h d", d=128)
                        j = rr.get('rsl', 0) % 2
                        rr['rsl'] = rr.get('rsl', 0) + 1
                        rv = rsl[j][:, :].rearrange("p (h d) -> p h d", d=32)
                        S.op('act', lambda a: a.activation(out=rv, in_=pv[:, :, 0:32], func=AF.Identity), r=[pkey], w=['rsl%d' % j])
                        rope_ops(rv[:, :, 0:16], rv[:, :, 16:32], sv[:, :, 0:16], sv[:, :, 16:32],
                                 cosD[:, tt, :, :].to_broadcast([128, 4, 16]), sinD[:, tt, :, :].to_broadcast([128, 4, 16]), 'rsl%d' % j, skey, 'tabD', (4, 16))
                        row = (tt - 8 if own_only else tt) * 128
                        S.dma('sp', dst[row:row + 128, c0:c0 + 512], st[:, :], r=[skey], w=[(id(dst), tt)])
                    return f

                def store_iq(tt, ps, pkey):
                    st, skey = next_stg()
                    S.op('act', lambda a: a.activation(out=st[:, :], in_=ps[:, :], func=AF.Identity), r=[pkey], w=[skey])
                    pv = ps[:, :].rearrange("p (h d) -> p h d", d=64)
                    sv = st[:, :].rearrange("p (h d) -> p h d", d=64)
                    j = rr.get('rsl', 0) % 2
                    rr['rsl'] = rr.get('rsl', 0) + 1
                    rv = rsl[j][:, :].rearrange("p (h d) -> p h d", d=16)
                    S.op('act', lambda a: a.activation(out=rv, in_=pv[:, :, 0:16], func=AF.Identity), r=[pkey], w=['rsl%d' % j])
                    rope_ops(rv[:, :, 0:8], rv[:, :, 8:16], sv[:, :, 0:8], sv[:, :, 8:16],
                             cosI[:, tt, :, :].to_broadcast([128, 8, 8]), sinI[:, tt, :, :].to_broadcast([128, 8, 8]), 'rsl%d' % j, skey, 'tabI', (8, 8))
                    row = (tt - 8) * 128
                    S.dma('sp', IQ[row:row + 128, :], st[:, :], r=[skey], w=[('IQ', tt)])

                def store_ikw(tt, ps, pkey):
                    S.op('act', lambda a: a.activation(out=f32stg2[:, :], in_=ps[:, 0:72], func=AF.Identity), r=[pkey], w=['f32stg2'])
                    j = rr.get('rsl', 0) % 2
                    rr['rsl'] = rr.get('rsl', 0) + 1
                    S.op('act', lambda a: a.activation(out=rsl[j][:, 0:16], in_=ps[:, 0:16], func=AF.Identity), r=[pkey], w=['rsl%d' % j])
                    pkey = 'rsl%d' % j
                    rope_ops(rsl[j][:, 0:8].rearrange("p (a b) -> p a b", a=1), rsl[j][:, 8:16].rearrange("p (a b) -> p a b", a=1),
                             f32stg2[:, 0:8].rearrange("p (a b) -> p a b", a=1), f32stg2[:, 8:16].rearrange("p (a b) -> p a b", a=1),
                             cosI[:, tt, :, :], sinI[:, tt, :, :], pkey, 'f32stg2', 'tabI', (1, 8))
                    S.dma('sp', IKW[tt * 128:(tt + 1) * 128, :], f32stg2[:, :], r=['f32stg2'], w=[('IKW', tt)])

                def gen_D():
                    gu_aug = sb(pc, "gu_aug", [32, 1024], F32)
                    S.dma('sp', gu_aug[0:16, :], gate_up, w=['gu_aug'])
                    S.dma('sp', gu_aug[16:17, :], gate_bias, w=['gu_aug'])
                    Sst = sb(pc, "Sst", [128, 2, 512], F32)
                    Sbf = sb(pc, "Sbf", [128, 2, 512], BF16)
                    kt_ = [sb(pc, "kt%d" % i, [128, 256], BF16) for i in range(2)]
                    vt_ = [sb(pc, "vt%d" % i, [128, 512], BF16) for i in range(2)]
                    qt_ = [sb(pc, "qt%d" % i, [128, 256], BF16) for i in range(2)]
                    gs_ = [sb(pc, "gs%d" % i, [128, 512], BF16) for i in range(2)]
                    sp_ = sb(pc, "sp_", [128, 256], F32)
                    Epos = sb(pc, "Epos", [128, 256], F32)
                    Eneg = sb(pc, "Eneg", [128, 256], F32)
                    Etok = sb(pc, "Etok", [128, 256], F32)
                    ktok = sb(pc, "ktok", [128, 256], BF16)
                    kT_ = sb(pc, "kT_", [128, 256], BF16)
                    qT_ = sb(pc, "qT_", [128, 256], BF16)
                    attnT = sb(pc, "attnT", [128, 128], BF16)
                    junkD = sb(pc, "junkD", [128, 512], BF16)
                    oa_ = [sb(pc, "oa%d" % i, [128, 512], BF16) for i in range(2)]
                    def d_loads(h_, n_):
                        i_ = n_ % 2
                        r_ = n_ * 128
                        S.dma('sp', kt_[i_][:], GK[r_:r_ + 128, h_ * 256:(h_ + 1) * 256], r=[(id(GK), n_)], w=['kt%d' % i_])
                        S.dma('sp', vt_[i_][:], GV[r_:r_ + 128, h_ * 512:(h_ + 1) * 512], r=[(id(GV), n_)], w=['vt%d' % i_])
                        if n_ >= 8:
                            q_ = (n_ - 8) * 128
                            S.dma('sp', qt_[i_][:], GQ[q_:q_ + 128, h_ * 256:(h_ + 1) * 256], r=[(id(GQ), n_)], w=['qt%d' % i_])
                            S.dma('sp', gs_[i_][:], GR[q_:q_ + 128, h_ * 512:(h_ + 1) * 512], r=[(id(GR), n_)], w=['gs%d' % i_])

                    for h in range(4):
                        S.op('dve', lambda v: v.memset(Sst[:], 0.0), w=['Sst'])
                        S.op('dve', lambda v: v.memset(Sbf[:], 0.0), w=['Sbf'])
                        for n in range(NT):
                            i = n % 2
                            ownt = n >= 8
                            r0 = n * 128
                            if ownt:
                                q0 = (n - 8) * 128
                            if h == 0 and n == 0:
                                d_loads(0, 0)
                            nxt = h * NT + n + 1
                            if nxt < 4 * NT:
                                d_loads(nxt // NT, nxt % NT)
                            ps, pk = next_mm('d')
                            S.op('pe', lambda p: p.matmul(ps[:, 0:256], lhsT=glrT[0:17, r0:r0 + 128], rhs=gu_aug[0:17, h * 256:(h + 1) * 256],
                                                          start=True, stop=True), r=['glrT', 'gu_aug'], w=[pk])
                            S.op('act', lambda a: a.activation(out=sp_[:], in_=ps[:, 0:256], func=AF.Exp, scale=-1.0), r=[pk], w=['sp_'])
                            S.op('act', lambda a: a.activation(out=sp_[:], in_=sp_[:], func=AF.Ln, bias=1.0), r=['sp_'], w=['sp_'])
                            yield
                            ps2, pk2 = next_mm('d')
                            for cc in range(2):
                                S.op('pe', lambda p: p.matmul(ps2[:, cc * 128:(cc + 1) * 128], lhsT=sp_[:, cc * 128:(cc + 1) * 128], rhs=triu,
                                                              start=True, stop=True), r=['sp_', 'cst'], w=[pk2])
                            ps3, pk3 = next_mm('d')
                            S.op('pe', lambda p: p.matmul(ps3[:, 0:256], lhsT=triu, rhs=sp_[:], start=True, stop=True), r=['sp_', 'cst'], w=[pk3])
                            S.op('act', lambda a: a.activation(out=Epos[:], in_=ps2[:, 0:256], func=AF.Exp, scale=-1.0 / 16), r=[pk2], w=['Epos'])
                            S.op('act', lambda a: a.activation(out=Eneg[:], in_=ps2[:, 0:256], func=AF.Exp, scale=1.0 / 16), r=[pk2], w=['Eneg'])
                            S.op('act', lambda a: a.activation(out=Etok[:], in_=ps3[:, 0:256], func=AF.Exp, scale=1.0 / 16), r=[pk3], w=['Etok'])
                            yield
                            tpsf, tk = next_mm('d')
                            tps = tpsf[:, :].bitcast(BF16)
                            for cc in range(2):
                                S.op('pe', lambda p: p.transpose(out=tps[:, cc * 128:(cc + 1) * 128], in_=kt_[i][:, cc * 128:(cc + 1) * 128],
                                                                 identity=ident[:]), r=['kt%d' % i, 'ident'], w=[tk])
                            if ownt:
                                for cc in range(2):
                                    S.op('pe', lambda p: p.transpose(out=tps[:, 256 + cc * 128:256 + (cc + 1) * 128],
                                                                     in_=qt_[i][:, cc * 128:(cc + 1) * 128], identity=ident[:]),
                                         r=['qt%d' % i, 'ident'], w=[tk])
                            yield
                            S.op('dve', lambda v: v.tensor_tensor(out=kT_[:], in0=tps[:, 0:256], in1=Eneg[:], op=ALU.mult),
                                 r=[tk, 'Eneg'], w=['kT_'])
                            S.op('pool', lambda g: g.tensor_tensor(out=ktok[:], in0=kt_[i][:], in1=Etok[:], op=ALU.mult),
                                 r=['kt%d' % i, 'Etok'], w=['ktok'])
                            if ownt:
                                S.op('dve', lambda v: v.scalar_tensor_tensor(out=qT_[:], in0=tps[:, 256:512], scalar=1.0 / 16, in1=Epos[:],
                                                                             op0=ALU.mult, op1=ALU.mult), r=[tk, 'Epos'], w=['qT_'])
                                yield
                                psA, pkA = next_mm('d')
                                for cc in range(2):
                                    S.op('pe', lambda p: p.matmul(psA[:, 0:128], lhsT=kT_[:, cc * 128:(cc + 1) * 128],
                                                                  rhs=qT_[:, cc * 128:(cc + 1) * 128], start=(cc == 0), stop=(cc == 1)),
                                         r=['kT_', 'qT_'], w=[pkA])
                                S.op('dve', lambda v: v.tensor_tensor(out=attnT[:], in0=psA[:, 0:128], in1=triu, op=ALU.mult),
                                     r=[pkA, 'cst'], w=['attnT'])
                                yield
                                psO, pkO = next_mm('d')
                                S.op('pe', lambda p: p.matmul(psO[:, :], lhsT=attnT[:], rhs=vt_[i][:], start=True, stop=False),
                                     r=['attnT', 'vt%d' % i], w=[pkO])
                                for cc in range(2):
                                    S.op('pe', lambda p: p.matmul(psO[:, :], lhsT=qT_[:, cc * 128:(cc + 1) * 128], rhs=Sbf[:, cc, :],
                                                                  start=False, stop=(cc == 1)), r=['qT_', 'Sbf'], w=[pkO])
                                ssc = small[:, 4 + i:5 + i]
                                S.op('act', lambda a: a.activation(out=junkD[:], in_=psO[:, :], func=AF.Square, accum_out=ssc),
                                     r=[pkO], w=['junkD', 'smallD'])
                                rstd_from_ss(ssc, 512, 'smallD')
                                S.op('dve', lambda v: v.scalar_tensor_tensor(out=oa_[i][:], in0=psO[:, :], scalar=ssc, in1=gs_[i][:],
                                                                             op0=ALU.mult, op1=ALU.mult),
                                     r=[pkO, 'smallD', 'gs%d' % i], w=['oa%d' % i])
                                S.dma('sp', OA[q0:q0 + 128, h * 512:(h + 1) * 512], oa_[i][:], r=['oa%d' % i], w=[('OA', n)])
                            yield
                            for cc in range(2):
                                psU, pkU = next_mm('d')
                                S.op('pe', lambda p: p.matmul(psU[:, :], lhsT=ktok[:, cc * 128:(cc + 1) * 128], rhs=vt_[i][:],
                                                              start=True, stop=True), r=['ktok', 'vt%d' % i], w=[pkU])
                                S.op('dve', lambda v: v.tensor_tensor(out=Sst[:, cc, :], in0=psU[:, :], in1=Sst[:, cc, :], op=ALU.add),
                                     r=[pkU, 'Sst'], w=['Sst'])
                                S.op('dve', lambda v: v.tensor_scalar(out=Sst[:, cc, :], in0=Sst[:, cc, :],
                                                                      scalar1=Epos[:, cc * 128 + 127:cc * 128 + 128], scalar2=None, op0=ALU.mult),
                                     r=['Sst', 'Epos'], w=['Sst'])
                                if n == 7:
                                    S.op('dve', lambda v: v.tensor_scalar(out=Sst[:, cc, :], in0=Sst[:, cc, :], scalar1=ctxflag, scalar2=None,
                                                                          op0=ALU.mult), r=['Sst', 'cst'], w=['Sst'])
                                S.op('act', lambda a: a.activation(out=Sbf[:, cc, :], in_=Sst[:, cc, :], func=AF.Identity), r=['Sst'], w=['Sbf'])
                                yield


                def gen_E():
                    ikT2 = sb(pc, "ikT2", [128, TALL], BF16)
                    ikf = sb(pc, "ikf", [128, 72], F32)
                    ikd = sb(pc, "ikd", [128, 128], BF16)
                    iqs = sb(pc, "iqs", [128, 512], BF16)
                    iqT = sb(pc, "iqT", [128, 4, 128], BF16)
                    iwp = sb(pc, "iwp", [128, 8], F32)
                    score = sb(pc, "score", [128, TALL], F32)
                    relu_t = [sb(pc, "relu%d" % i, [128, 512], F32) for i in range(2)]
                    mask_tm = sb(pc, "mask_tm", [128, TALL], BF16)
                    S.op('dve', lambda v: v.memset(maskT[:], 0.0), w=['maskT'])
                    for kt in range(NT):
                        S.dma('sp', ikf[:], IKW[kt * 128:(kt + 1) * 128, :], r=[('IKW', kt)], w=['ikf'])
                        S.op('dve', lambda v: v.tensor_copy(out=ikd[:, 0:64], in_=ikf[:, 0:64]), r=['ikf'], w=['ikd'])
                        S.op('dve', lambda v: v.tensor_copy(out=ikd[:, 64:128], in_=ikf[:, 0:64]), r=['ikf'], w=['ikd'])
                        tps, tk = next_tp()
                        S.op('pe', lambda p: p.transpose(out=tps[:, 0:128], in_=ikd[:], identity=ident[:]), r=['ikd', 'ident'], w=[tk])
                        S.op('act', lambda a: a.activation(out=ikT2[:, kt * 128:(kt + 1) * 128], in_=tps[:, 0:128], func=AF.Identity),
                             r=[tk], w=['ikT2'])
                        yield
                    lo, hw, mid, cntv, gev, am = [small[:, 8 + j:9 + j] for j in range(6)]
                    for qi in range(8):
                        tt = 8 + qi
                        nk = 1024 + 128 * (qi + 1)
                        S.dma('sp', iqs[:], IQ[qi * 128:(qi + 1) * 128, :], r=[('IQ', tt)], w=['iqs'])
                        S.dma('act', ikf[:], IKW[tt * 128:(tt + 1) * 128, :], r=[('IKW', tt)], w=['ikf'])
                        yield
                        tps, tk = next_tp()
                        for c in range(4):
                            S.op('pe', lambda p: p.transpose(out=tps[:, c * 128:(c + 1) * 128], in_=iqs[:, c * 128:(c + 1) * 128],
                                                             identity=ident[:]), r=['iqs', 'ident'], w=[tk])
                        S.op('act', lambda a: a.activation(out=iqT[:].rearrange("p a b -> p (a b)"), in_=tps[:, 0:512], func=AF.Identity),
                             r=[tk], w=['iqT'])
                        S.op('dve', lambda v: v.tensor_scalar(out=iwp[:], in0=ikf[:, 64:72], scalar1=float(8 ** -0.5 * 64 ** -0.5),
                                                              scalar2=None, op0=ALU.mult), r=['ikf'], w=['iwp'])
                        ng = (nk + 511) // 512
                        for g in range(ng):
                            wd_ = min(512, nk - g * 512)
                            for hh in range(8):
                                ps, pk = next_mm('e')
                                pb_ = (hh % 2) * 64
                                S.op('pe', lambda p: p.matmul(ps[:, 0:wd_], lhsT=iqT[pb_:pb_ + 64, hh // 2, :],
                                                              rhs=ikT2[pb_:pb_ + 64, g * 512:g * 512 + wd_], start=True, stop=True),
                                     r=['iqT', 'ikT2'], w=[pk])
                                rl = relu_t[hh % 2]
                                rk = 'relu%d' % (hh % 2)
                                S.op('act', lambda a: a.activation(out=rl[:, 0:wd_], in_=ps[:, 0:wd_], func=AF.Relu), r=[pk], w=[rk])
                                sc_ = score[:, g * 512:g * 512 + wd_]
                                if hh == 0:
                                    S.op('dve', lambda v: v.tensor_scalar(out=sc_, in0=rl[:, 0:wd_], scalar1=iwp[:, 0:1], scalar2=None,
                                                                          op0=ALU.mult), r=[rk, 'iwp'], w=['score'])
                                else:
                                    S.op('dve', lambda v: v.scalar_tensor_tensor(out=sc_, in0=rl[:, 0:wd_], scalar=iwp[:, hh:hh + 1],
                                                                                 in1=sc_, op0=ALU.mult, op1=ALU.add),
                                         r=[rk, 'iwp', 'score'], w=['score'])
                                yield
                        S.op('dve', lambda v: v.tensor_reduce(out=am, in_=score[:, 0:nk], axis=AX.X, op=ALU.max,
                                                              apply_absolute_value=True), r=['score'], w=['smallE'])
                        S.op('dve', lambda v: v.tensor_scalar(out=score[:, 0:1024], in0=score[:, 0:1024], scalar1=ctxneg, scalar2=None,
                                                              op0=ALU.add), r=['score', 'cst'], w=['score'])
                        S.op('dve', lambda v: v.tensor_tensor(out=score[:, nk - 128:nk], in0=score[:, nk - 128:nk], in1=cmask, op=ALU.add),
                             r=['score', 'cst'], w=['score'])
                        S.op('dve', lambda v: v.tensor_scalar(out=hw, in0=am, scalar1=1.0001, scalar2=1e-20, op0=ALU.mult, op1=ALU.add),
                             r=['smallE'], w=['smallE'])
                        S.op('dve', lambda v: v.tensor_scalar(out=lo, in0=hw, scalar1=-1.0, scalar2=None, op0=ALU.mult),
                             r=['smallE'], w=['smallE'])
                        for it in range(NBISECT):
                            S.op('dve', lambda v: v.tensor_tensor(out=mid, in0=lo, in1=hw, op=ALU.add), r=['smallE'], w=['smallE'])
                            S.op('dve', lambda v: v.tensor_scalar(out=junk[:, 0:nk], in0=score[:, 0:nk], scalar1=mid, scalar2=None,
                                                                  op0=ALU.is_ge, op1=ALU.add, accum_out=cntv),
                                 r=['score', 'smallE'], w=['junk', 'smallE'])
                            S.op('dve', lambda v: v.tensor_scalar(out=gev, in0=cntv, scalar1=TOPK - 0.5, scalar2=None, op0=ALU.is_ge),
                                 r=['smallE'], w=['smallE'])
                            S.op('dve', lambda v: v.scalar_tensor_tensor(out=lo, in0=hw, scalar=gev, in1=lo, op0=ALU.mult, op1=ALU.add),
                                 r=['smallE'], w=['smallE'])
                            S.op('dve', lambda v: v.tensor_scalar(out=hw, in0=hw, scalar1=0.5, scalar2=None, op0=ALU.mult),
                                 r=['smallE'], w=['smallE'])
                            yield
                        S.op('dve', lambda v: v.tensor_scalar(out=mask_tm[:, 0:nk], in0=score[:, 0:nk], scalar1=lo, scalar2=None,
                                                              op0=ALU.is_ge), r=['score', 'smallE'], w=['mask_tm'])
                        nkb = nk // 128
                        for c0 in range(0, nkb, 8):
                            n_ = min(8, nkb - c0)
                            yield
                            tps, tk = next_tp()
                            for c in range(n_):
                                S.op('pe', lambda p: p.transpose(out=tps[:, c * 128:(c + 1) * 128],
                                                                 in_=mask_tm[:, (c0 + c) * 128:(c0 + c + 1) * 128], identity=ident[:]),
                                     r=['mask_tm', 'ident'], w=[tk])
                            S.op('act', lambda a: a.activation(out=maskT[:, c0:c0 + n_, qi * 128:(qi + 1) * 128],
                                                               in_=tps[:, 0:n_ * 128].rearrange("p (a b) -> p a b", b=128),
                                                               func=AF.Identity), r=[tk], w=['maskT'])
                            yield
                    yield

                linear(hact, 'hT', w_in, O_IQ, 512, own, store_iq)
                linear(hact, 'hT', w_in, O_IK, 72, allt, store_ikw)
                bg.append(gen_E())
                for cb in range(2):
                    linear(hact, 'hT', w_in, O_GQ + cb * 512, 512, own, store(GQ, True, cb * 512, 512))
                for cb in range(2):
                    linear(hact, 'hT', w_in, O_GK + cb * 512, 512, allt, store(GK, False, cb * 512, 512))
                for cb in range(4):
                    linear(hact, 'hT', w_in, O_GV + cb * 512, 512, allt, store(GV, False, cb * 512, 512))
                for cb in range(4):
                    gg = ggs[cb % 2]
                    S.dma('act', gg[:], gla_gain[0, cb * 512:(cb + 1) * 512].partition_broadcast(128), w=['ggs%d' % (cb % 2)])
                    linear(hact, 'hT', w_in, O_GR + cb * 512, 512, own, store_act(GR, cb * 512, 512, AF.Silu, mul=(gg, 'ggs%d' % (cb % 2))))
                bg.append(gen_D())
                for cb in range(4):
                    linear(hact, 'hT', w_in, O_DQ + cb * 512, 512, own, store_rope_d(DQ, True, cb * 512))
                for cb in range(4):
                    linear(hact, 'hT', w_in, O_DK + cb * 512, 512, allt, store_rope_d(DK, False, cb * 512))
                for cb in range(4):
                    linear(hact, 'hT', w_in, O_DV + cb * 512, 512, allt, store(DV, False, cb * 512, 512))
                for cb in range(4):
                    linear(hact, 'hT', w_in, O_GA + cb * 512, 512, own, store_act(GA, cb * 512, 512, AF.Sigmoid))
                for cb in range(4):
                    linear(hact, 'hT', w_in, O_GB + cb * 512, 512, own, store_act(GB, cb * 512, 512, AF.Sigmoid))
                bg_drain()
                S.barrier()
                if MTD is not None:
                    S.dma('sp', MTD, maskT[:], r=['maskT'], w=['MTD'])
                    S.barrier()
                if stop == 'C':
                    return nc
                set_mm_users()
        with ExitStack() as pefg:
            with ExitStack() as pef:

                with ExitStack() as pf:
                    kTg = sb(pf, "kTg", [128, 4, TALL], BF16)
                    vg = sb(pf, "vg", [128, NT, 512], BF16)
                    qTg = sb(pf, "qTg", [128, 4, TOWN], BF16)
                    ldt = [sb(pf, "ldt%d" % i, [128, 512], BF16) for i in range(2)]
                    pt_ = [sb(pf, "pt%d" % i, [128, 512], BF16) for i in range(4)]
                    pm_ = [sb(pf, "pm%d" % i, [128, 512], BF16) for i in range(4)]
                    alloc_psum(3, 1, 4)
                    lnd = sb(pf, "lnd", [128, 512], F32)
                    obs = [sb(pf, "obs%d" % i, [128, 512], BF16) for i in range(2)]
                    alloc_wbuf(pf, 2)
                    browF = [sb(pf, "browF%d" % i, [1, 512], F32) for i in range(2)]
                    mrowF = [sb(pf, "mrowF%d" % i, [1, 512], F32) for i in range(2)]

                    def gen_modrest():
                        pend = []

                        def compute(cb, j, wb, wkey):
                            tpf = tp[0][:, :].bitcast(F32)
                            for kc in range(KC):
                                S.op('pe', lambda p: p.matmul(tpf[0:1, :], lhsT=sT[:, kc:kc + 1], rhs=wb[:, kc, :],
                                                              start=(kc == 0), stop=(kc == KC - 1)), r=['sT', wkey], w=['tp0'])
                            S.op('dve', lambda v: v.tensor_tensor(out=mrowF[j][:], in0=tpf[0:1, :], in1=browF[j][:], op=ALU.add),
                                 r=['tp0', 'browF%d' % j], w=['mrowF%d' % j])
                            S.dma('act', modrow_d[0:1, cb * 512:(cb + 1) * 512], mrowF[j][:], r=['mrowF%d' % j], w=[('modrow_d', cb)])

                        for cb in range(8, 24):
                            j = cb % 2
                            S.dma('act', browF[j][:], b_ada[0:1, cb * 512:(cb + 1) * 512], w=['browF%d' % j])
                            wb, wkey = load_w(w_ada, 0, KC, cb * 512, 512)
                            pend.append((cb, j, wb, wkey))
                            yield
                            if len(pend) == 2:
                                compute(*pend.pop(0))
                        while pend:
                            compute(*pend.pop(0))
                            yield
                    bg.append(gen_modrest())
                    rden = sb(pf, "rden", [128, 512], F32)
                    for hg in range(4):
                        S.dma('act', vg[:], DV[:, hg * 512:(hg + 1) * 512].rearrange("(kt p) c -> p kt c", p=128), w=['vg'])
                        for kt in range(NT + 8):
                            i = kt % 2
                            if kt < NT:
                                S.dma('sp', ldt[i][:], DK[kt * 128:(kt + 1) * 128, hg * 512:(hg + 1) * 512], w=['ldt%d' % i])
                                dst = kTg[:, :, kt * 128:(kt + 1) * 128]
                                dk_ = 'kTg'
                            else:
                                qi = kt - NT
                                S.dma('sp', ldt[i][:], DQ[qi * 128:(qi + 1) * 128, hg * 512:(hg + 1) * 512], w=['ldt%d' % i])
                                dst = qTg[:, :, qi * 128:(qi + 1) * 128]
                                dk_ = 'qTg'
                            tps, tk = next_tp()
                            for c in range(4):
                                S.op('pe', lambda p: p.transpose(out=tps[:, c * 128:(c + 1) * 128], in_=ldt[i][:, c * 128:(c + 1) * 128],
                                                                 identity=ident[:]), r=['ldt%d' % i, 'ident'], w=[tk])
                            S.op('act' if kt % 2 == 0 else 'dve',
                                 (lambda a: a.activation(out=dst, in_=tps[:, 0:512].rearrange("p (a b) -> p a b", b=128), func=AF.Identity))
                                 if kt % 2 == 0 else
                                 (lambda v: v.tensor_copy(out=dst, in_=tps[:, 0:512].rearrange("p (a b) -> p a b", b=128))),
                                 r=[tk], w=[dk_])
                        steps = [(hh, qg, kb) for hh in range(4) for qg in range(2) for kb in range(8 + 4 * (qg + 1))]
                        LA = 2
                        slots = {}

                        def qk_stage(idx):
                            hh, qg, kb = steps[idx]
                            lps, lk = next_mm()
                            S.op('pe', lambda p: p.matmul(lps[:, :], lhsT=kTg[:, hh, kb * 128:(kb + 1) * 128],
                                                          rhs=qTg[:, hh, qg * 512:(qg + 1) * 512], start=True, stop=True),
                                 r=['kTg', 'qTg'], w=[lk])
                            j = idx % 4
                            slots[idx] = j
                            S.op('act', lambda a: a.activation(out=pt_[j][:], in_=lps[:, :], func=AF.Exp, scale=float(128 ** -0.5)),
                                 r=[lk], w=['pt%d' % j])
                            S.op('dve', lambda v: v.tensor_tensor(out=pm_[j][:], in0=pt_[j][:], in1=maskT[:, kb, qg * 512:(qg + 1) * 512],
                                                                  op=ALU.mult), r=['pt%d' % j, 'maskT'], w=['pm%d' % j])

                        def pv_stage(idx):
                            hh, qg, kb = steps[idx]
                            nkb = 8 + 4 * (qg + 1)
                            j = slots.pop(idx)
                            pr = (hh * 2 + qg) % 2
                            aO, aD = ax[2 * pr], ax[2 * pr + 1]
                            kO, kD = 'ax%d' % (2 * pr), 'ax%d' % (2 * pr + 1)
                            S.op('pe', lambda p: p.matmul(aO[:, :], lhsT=vg[:, kb, hh * 128:(hh + 1) * 128], rhs=pm_[j][:],
                                                          start=(kb == 0), stop=(kb == nkb - 1)), r=['vg', 'pm%d' % j], w=[kO])
                            S.op('pe', lambda p: p.matmul(aD[:, :], lhsT=ones_bf[:], rhs=pm_[j][:],
                                                          start=(kb == 0), stop=(kb == nkb - 1)), r=['ones_bf', 'pm%d' % j], w=[kD])
                            if kb == nkb - 1:
                                h = hg * 4 + hh
                                S.op('act', lambda a: a.activation(out=lnd[:], in_=aD[:, :], func=AF.Ln), r=[kD], w=['lnd'])
                                S.op('act', lambda a: a.activation(out=rden[:], in_=lnd[:], func=AF.Exp, scale=-1.0), r=['lnd'], w=['rden'])
                                jo = (hh * 2 + qg) % 2
                                S.op('dve', lambda v: v.tensor_tensor(out=obs[jo][:], in0=aO[:, :], in1=rden[:],
                                                                      op=ALU.mult), r=[kO, 'rden'], w=['obs%d' % jo])
                                S.dma('sp', OBD[h * 128:(h + 1) * 128, qg * 512:(qg + 1) * 512], obs[jo][:], r=['obs%d' % jo], w=[('OBD', h, qg)])

                        for idx in range(len(steps) + LA):
                            if idx % 12 == 0:
                                bg_step()
                            if idx < len(steps):
                                qk_stage(idx)
                            if idx - LA >= 0:
                                pv_stage(idx - LA)
                    bg_drain()
                    S.barrier()
                    with nc.allow_non_contiguous_dma(reason="one-time relayout of the modulation vector"):
                        S.dma('sp', modfm[:, 32:96], modrow_d[0, 2 * D:6 * D].rearrange("(j p) -> p j", p=128), w=['modfm'])
                    S.op('dve', lambda v: v.scalar_tensor_tensor(out=A2[:], in0=modfm[:, 64:80], scalar=1.0, in1=n2g_t[:],
                                                                 op0=ALU.add, op1=ALU.mult), r=['modfm', 'n2g'], w=['A2'])
                    S.barrier()
                    alloc_psum(4, 2, 2)
            s_mask.close()

            with ExitStack() as pg1:
                o_aT = sb(pg1, "o_aT", [128, KC, TOWN], BF16)
                o_bT = sb(pg1, "o_bT", [128, KC, TOWN], BF16)
                S.dma('act', o_bT[:], OBD.rearrange("(h p) t -> p h t", p=128), w=['o_bT'])
                oat = [sb(pg1, "oat%d" % i, [128, D], BF16) for i in range(2)]
                gat = [sb(pg1, "gat%d" % i, [128, 512], BF16) for i in range(2)]
                gbt = [sb(pg1, "gbt%d" % i, [128, 512], BF16) for i in range(2)]
                m1 = sb(pg1, "m1", [128, 512], F32)
                m2 = sb(pg1, "m2", [128, 512], F32)
                mgs = [sb(pg1, "mgs%d" % i, [128, 512], BF16) for i in range(2)]
                alloc_wbuf(pg1, 4)
                for qi in range(8):
                    i = qi % 2
                    S.dma('sp', oat[i][:], OA[qi * 128:(qi + 1) * 128, :], w=['oat%d' % i])

                    def ev(c0, n, tps, tkey, qi=qi):
                        S.op('act', lambda a: a.activation(out=o_aT[:, c0:c0 + n, qi * 128:(qi + 1) * 128],
                                                           in_=tps[:, 0:n * 128].rearrange("p (a b) -> p a b", b=128),
                                                           func=AF.Identity), r=[tkey], w=['o_aT'])
                    to_feature_major(oat[i], 'oat%d' % i, KC, None, 'o_aT', ev)
                for cb in range(4):
                    wa, wak = load_w(w_ba, 0, KC, cb * 512, 512)
                    wd2, wdk = load_w(w_bd, 0, KC, cb * 512, 512)
                    for qi in range(8):
                        i = qi % 2
                        S.dma('sp', gat[i][:], GA[qi * 128:(qi + 1) * 128, cb * 512:(cb + 1) * 512], w=['gat%d' % i])
                        S.dma('act', gbt[i][:], GB[qi * 128:(qi + 1) * 128, cb * 512:(cb + 1) * 512], w=['gbt%d' % i])
                        psa, pka = next_mm()
                        for kc in range(KC):
                            S.op('pe', lambda p: p.matmul(psa[:, :], lhsT=o_aT[:, kc, qi * 128:(qi + 1) * 128], rhs=wa[:, kc, :],
                                                          start=(kc == 0), stop=(kc == KC - 1)), r=['o_aT', wak], w=[pka])
                        psb, pkb = next_mm()
                        for kc in range(KC):
                            S.op('pe', lambda p: p.matmul(psb[:, :], lhsT=o_bT[:, kc, qi * 128:(qi + 1) * 128], rhs=wd2[:, kc, :],
                                                          start=(kc == 0), stop=(kc == KC - 1)), r=['o_bT', wdk], w=[pkb])
                        S.op('dve', lambda v: v.tensor_tensor(out=m1[:], in0=psa[:, :], in1=gat[i][:], op=ALU.mult),
                             r=[pka, 'gat%d' % i], w=['m1'])
                        S.op('dve', lambda v: v.tensor_tensor(out=m2[:], in0=psb[:, :], in1=gbt[i][:], op=ALU.mult),
                             r=[pkb, 'gbt%d' % i], w=['m2'])
                        S.op('pool', lambda g: g.tensor_tensor(out=mgs[i][:], in0=m1[:], in1=m2[:], op=ALU.add),
                             r=['m1', 'm2'], w=['mgs%d' % i])
                        S.dma('sp', MG[qi * 128:(qi + 1) * 128, cb * 512:(cb + 1) * 512], mgs[i][:], r=['mgs%d' % i], w=[('MG', qi)])
                S.barrier()

        with ExitStack() as px:
            x1 = sb(px, "x1", [128, 8, D], F32)
            rowb = sb(px, "rowb", [128, D], F32)
            with ExitStack() as pg2:
                alloc_wbuf(pg2, 2)
                mergedT = sb(pg2, "mergedT", [128, KC, TOWN], BF16)
                mgt = [sb(pg2, "mgt%d" % i, [128, D], BF16) for i in range(2)]
                xres = [sb(pg2, "xres%d" % i, [128, 512], F32) for i in range(2)]
                tmpf = sb(pg2, "tmpf", [128, 512], F32)
                S.dma('act', rowb[:], modrow_d[0, 2 * D:3 * D].partition_broadcast(128), w=['rowb'])
                for qi in range(8):
                    i = qi % 2
                    S.dma('sp', mgt[i][:], MG[qi * 128:(qi + 1) * 128, :], w=['mgt%d' % i])

                    def ev2(c0, n, tps, tkey, qi=qi):
                        S.op('act', lambda a: a.activation(out=mergedT[:, c0:c0 + n, qi * 128:(qi + 1) * 128],
                                                           in_=tps[:, 0:n * 128].rearrange("p (a b) -> p a b", b=128),
                                                           func=AF.Identity), r=[tkey], w=['mergedT'])
                    to_feature_major(mgt[i], 'mgt%d' % i, KC, None, 'mergedT', ev2)

                def evac_mo(cb):
                    def f(tt, ps, pkey):
                        i = tt % 2
                        S.dma('sp', xres[i][:], xs[TOWN + tt * 128:TOWN + (tt + 1) * 128, cb * 512:(cb + 1) * 512], w=['xres%d' % i])
                        S.op('dve', lambda v: v.tensor_tensor(out=tmpf[:], in0=ps[:, :], in1=rowb[:, cb * 512:(cb + 1) * 512], op=ALU.mult),
                             r=[pkey, 'rowb'], w=['tmpf'])
                        S.op('dve', lambda v: v.tensor_tensor(out=x1[:, tt, cb * 512:(cb + 1) * 512], in0=tmpf[:], in1=xres[i][:], op=ALU.add),
                             r=['tmpf', 'xres%d' % i], w=[('x1', tt)])
                    return f
                for cb in range(4):
                    linear(lambda kc, tt: mergedT[:, kc, tt * 128:(tt + 1) * 128], 'mergedT', w_mo, cb * 512, 512, list(range(8)), evac_mo(cb))
                S.barrier()

            with ExitStack() as ph:
                alloc_wbuf(ph, 2, 11, 512)
                h2T = sb(ph, "h2T", [128, KC, TOWN], BF16)
                xn2 = [sb(ph, "xn2_%d" % i, [128, D], BF16) for i in range(2)]
                actT = sb(ph, "actT", [128, 11, TOWN], BF16)
                sg = [sb(ph, "sg%d" % i, [128, 512], F32) for i in range(2)]
                tmp2 = sb(ph, "tmp2", [128, 512], F32)
                gub = [sb(ph, "gub%d" % i, [128, KC, 128], BF16) for i in range(6)]
                rr['gu'] = 0

                def load_gu(c0):
                    i = rr['gu'] % 6
                    rr['gu'] += 1
                    S.dma('pool', gub[i][:], w_gu[:, c0:c0 + 128].rearrange("(kc p) n -> p kc n", p=128), w=['gub%d' % i])
                    return gub[i], 'gub%d' % i
                S.dma('act', rowb[:], modrow_d[0, 5 * D:6 * D].partition_broadcast(128), w=['rowb'])
                for qi in range(8):
                    i = qi % 2
                    norm_to_fm(x1[:, qi, :], ('x1', qi), h2T, 'h2T', qi * 128, A2, sh2, ['A2', 'modfm'],
                               xn2[i], 'xn2_%d' % i, small[:, 16 + i:17 + i])
                cnt_s = 0
                for fb in range(4):
                    for fc in range(11):
                        f0 = fb * 1408 + fc * 128
                        wg, wgk = load_gu(f0)
                        wu, wuk = load_gu(DFF + f0)
                        for tg in range(2):
                            psg, pkg = next_mm()
                            for kc in range(KC):
                                S.op('pe', lambda p: p.matmul(psg[:, :], lhsT=wg[:, kc, 0:128], rhs=h2T[:, kc, tg * 512:(tg + 1) * 512],
                                                              start=(kc == 0), stop=(kc == KC - 1)), r=['h2T', wgk], w=[pkg])
                            psu, pku = next_mm()
                            for kc in range(KC):
                                S.op('pe', lambda p: p.matmul(psu[:, :], lhsT=wu[:, kc, 0:128], rhs=h2T[:, kc, tg * 512:(tg + 1) * 512],
                                                              start=(kc == 0), stop=(kc == KC - 1)), r=['h2T', wuk], w=[pku])
                            j = cnt_s % 2
                            cnt_s += 1
                            S.op('act', lambda a: a.activation(out=sg[j][:], in_=psg[:, :], func=AF.Silu), r=[pkg], w=['sg%d' % j])
                            S.op('dve', lambda v: v.tensor_tensor(out=actT[:, fc, tg * 512:(tg + 1) * 512], in0=psu[:, :], in1=sg[j][:],
                                                                  op=ALU.mult), r=[pku, 'sg%d' % j], w=['actT'])

                    def evac_dn(cb):
                        def f(tt, ps, pkey):
                            S.op('dve', lambda v: v.tensor_tensor(out=tmp2[:], in0=ps[:, :], in1=rowb[:, cb * 512:(cb + 1) * 512], op=ALU.mult),
                                 r=[pkey, 'rowb'], w=['tmp2'])
                            S.op('pool', lambda g: g.tensor_tensor(out=x1[:, tt, cb * 512:(cb + 1) * 512], in0=x1[:, tt, cb * 512:(cb + 1) * 512],
                                                                   in1=tmp2[:], op=ALU.add), r=['tmp2', ('x1', tt)], w=[('x1', tt)])
                        return f
                    for cb in range(4):
                        linear(lambda kc, tt: actT[:, kc, tt * 128:(tt + 1) * 128], 'actT', w_dn, cb * 512, 512, list(range(8)),
                               evac_dn(cb), r0=fb * 1408, nk=11)
                S.barrier()

            with ExitStack() as pi_:
                ot = [sb(pi_, "ot%d" % i, [128, D], F32) for i in range(2)]
                S.dma('act', rowb[:], fng[0, :].partition_broadcast(128), w=['rowb'])
                for qi in range(8):
                    i = qi % 2
                    ssc = small[:, 20 + i:21 + i]
                    S.op('act', lambda a: a.activation(out=junk[:], in_=x1[:, qi, :], func=AF.Square, accum_out=ssc),
                         r=[('x1', qi)], w=['junk', 'small'])
                    rstd_from_ss(ssc, D, 'small')
                    S.op('dve', lambda v: v.scalar_tensor_tensor(out=ot[i][:], in0=x1[:, qi, :], scalar=ssc, in1=rowb[:],
                                                                 op0=ALU.mult, op1=ALU.mult), r=[('x1', qi), 'small', 'rowb'], w=['ot%d' % i])
                    S.dma('sp', out[qi * 128:(qi + 1) * 128, :], ot[i][:], r=['ot%d' % i], w=[('out', qi)])
                S.barrier()
        S.barrier()
    return nc


def _consts(half):
    c = np.zeros((128, 512), np.float32)
    c[:, 0:128] = np.eye(128, dtype=np.float32)
    j = np.arange(128)
    c[:, 128:256] = (j[:, None] <= j[None, :]).astype(np.float32)
    c[:, 256:384] = np.where(j[None, :] <= j[:, None], 0.0, NEG)
    c[:, 384] = 1.0 if half == 1 else 0.0
    c[:, 385] = 0.0 if half == 1 else NEG
    theta = np.float32(500000.0)
    c[:, 400:416] = np.power(theta, -np.arange(0, 32, 2, dtype=np.float32) / np.float32(32))[None, :]
    c[:, 416:424] = np.power(theta, -np.arange(0, 16, 2, dtype=np.float32) / np.float32(16))[None, :]
    return c


def prep_inputs(inputs, cores=None):
    f = lambda a: np.ascontiguousarray(np.asarray(a))
    x = f(inputs["x"]); c = f(inputs["c"]); pos = f(inputs["positions"]).astype(np.int32)
    shared = {
        "w_ada": f(inputs["w_ada"])[0], "b_ada": f(inputs["b_ada"])[0][None, :], "w_in": f(inputs["w_in"])[0],
        "gate_up": f(inputs["gla_gate_up"])[0], "gate_bias": f(inputs["gla_gate_bias"])[0][None, :],
        "gla_gain": f(inputs["gla_norm_gain"])[0][None, :],
        "w_ba": f(inputs["w_branch_gla"])[0], "w_bd": f(inputs["w_branch_dsa"])[0], "w_mo": f(inputs["w_merge_out"])[0],
        "w_gu": f(inputs["w_ffn_gate_up"])[0], "w_dn": f(inputs["w_ffn_down"])[0],
        "n1g": np.ascontiguousarray(f(inputs["norm1_gain"])[0].reshape(16, 128).T),
        "n2g": np.ascontiguousarray(f(inputs["norm2_gain"])[0].reshape(16, 128).T),
        "fng": f(inputs["final_norm_gain"])[None, :],
    }
    maps = []
    for core in (range(8) if cores is None else cores):
        b, half = core // 2, core % 2
        if half == 1:
            xs = x[b]
            p = pos[b]
        else:
            xs = np.concatenate([np.zeros((TOWN, D), np.float32), x[b, :TOWN]], axis=0)
            p = np.concatenate([pos[b, :TOWN], pos[b, :TOWN]])
        m = dict(shared)
        m["xs"] = np.ascontiguousarray(xs)
        m["cfm"] = np.ascontiguousarray(c[b].reshape(16, 128).T)
        m["posi"] = np.ascontiguousarray(p.reshape(16, 128).T)
        m["cst"] = _consts(half)
        maps.append(m)
    return maps


_NC = None


def kernel(**inputs):
    global _NC
    if _NC is None:
        _NC = build_program()
    maps = prep_inputs(inputs)
    res = run_bass_kernel_spmd(_NC, maps, core_ids=list(range(8)))
    outp = np.zeros((NB, SEQ, D), np.float32)
    for core in range(8):
        b, half = core // 2, core % 2
        outp[b, half * TOWN:(half + 1) * TOWN] = res.results[core]["out"]
    return outp
```

```python
import math
from contextlib import ExitStack

import numpy as np
import concourse.bass as bass
import concourse.mybir as mybir
from concourse.bass_utils import run_bass_kernel_spmd

F32 = mybir.dt.float32
BF16 = mybir.dt.bfloat16
I32 = mybir.dt.int32
AF = mybir.ActivationFunctionType
ALU = mybir.AluOpType
AX = mybir.AxisListType

D = 2048
SEQ = 2048
NB = 4
TOWN = 1024
TALL = 2048
NT = 16
KC = 16
DFF = 5632
EPS = 1e-6
NEG = -1.0e30
TOPK = 256
NBISECT = 22

O_GQ, O_GK, O_GV, O_GR, O_GLR = 0, 1024, 2048, 4096, 6144
O_DQ, O_DK, O_DV = 6160, 8208, 10256
O_IQ, O_IK, O_IW = 12304, 12816, 12880
O_GA, O_GB = 12888, 14936
IN_W = 16984


class Sched:
    def __init__(self, nc, es, ndma=24):
        self.nc = nc
        self.eng = {'pe': nc.tensor, 'act': nc.scalar, 'dve': nc.vector, 'pool': nc.gpsimd, 'sp': nc.sync}
        self.semobj = {}
        for e in ['pe', 'act', 'dve', 'pool']:
            self.semobj[e] = es.enter_context(nc.semaphore('s_' + e))
        self.ndma = ndma
        for i in range(ndma):
            self.semobj[('d', i)] = es.enter_context(nc.semaphore('sd%d' % i))
            self.semobj[('g', i)] = es.enter_context(nc.semaphore('sg%d' % i))
        self.dma_rr_g = 0
        self.cnt = {k: 0 for k in self.semobj}
        self.seen = {e: {} for e in self.eng}
        self.lastw = {}
        self.readers = {}
        self.dma_rr = 0
        self.nwait = 0

    def _wait(self, e, k, v):
        if k == e and e == 'pe':
            return
        if self.seen[e].get(k, 0) >= v:
            return
        self.eng[e].wait_ge(self.semobj[k], v)
        self.seen[e][k] = v
        self.nwait += 1

    def _deps(self, e, r, w):
        for key in r:
            for k, v in self.lastw.get(key, {}).items():
                self._wait(e, k, v)
        for key in w:
            for k, v in self.lastw.get(key, {}).items():
                self._wait(e, k, v)
            for k, v in self.readers.get(key, {}).items():
                self._wait(e, k, v)

    def _record(self, ev, r, w):
        k, v = ev
        for key in r:
            d = self.readers.setdefault(key, {})
            d[k] = max(d.get(k, 0), v)
        for key in w:
            self.lastw[key] = {k: v}
            self.readers[key] = {}

    def op(self, e, fn, r=(), w=()):
        ex = [k for k in r if isinstance(k, str) and k[:2] in ('mm', 'tp', 'ax')]
        if ex:
            w = list(w) + [k for k in ex if k not in w]
        self._deps(e, r, w)
        ins = fn(self.eng[e])
        self.cnt[e] += 1
        ins.then_inc(self.semobj[e], 1)
        self._record((e, self.cnt[e]), r, w)

    def dma(self, q, out, in_, r=(), w=(), **kw):
        if q == 'pool':
            slot = ('g', self.dma_rr_g % self.ndma)
            self.dma_rr_g += 1
        else:
            slot = ('d', self.dma_rr % self.ndma)
            self.dma_rr += 1
        if self.cnt[slot] > 0:
            self._wait(q, slot, self.cnt[slot])
        self._deps(q, r, w)
        ins = self.eng[q].dma_start(out=out, in_=in_, **kw)
        self.cnt[slot] += 16
        ins.then_inc(self.semobj[slot], 16)
        self._record((slot, self.cnt[slot]), r, w)

    def barrier(self):
        for e in self.eng:
            for k in self.semobj:
                if self.cnt[k] > 0:
                    self._wait(e, k, self.cnt[k])
        self.lastw = {}
        self.readers = {}


def build_program(dbg=(), stop=None):
    nc = bass.Bass("TRN2", target_bir_lowering=False)
    import os
    stop = stop or os.environ.get('KSTOP')

    def din(name, shape, dt=F32):
        return nc.dram_tensor(name, list(shape), dt, kind="ExternalInput").ap()

    def dscr(name, shape, dt=BF16):
        kind = "ExternalOutput" if name in dbg else "Internal"
        return nc.dram_tensor(name, list(shape), dt, kind=kind).ap()

    xs = din("xs", [TALL, D])
    cfm = din("cfm", [128, KC])
    posi = din("posi", [128, NT], I32)
    w_ada = din("w_ada", [D, 6 * D])
    b_ada = din("b_ada", [1, 6 * D])
    w_in = din("w_in", [D, IN_W])
    gate_up = din("gate_up", [16, 1024])
    gate_bias = din("gate_bias", [1, 1024])
    gla_gain = din("gla_gain", [1, D])
    w_ba = din("w_ba", [D, D])
    w_bd = din("w_bd", [D, D])
    w_mo = din("w_mo", [D, D])
    w_gu = din("w_gu", [D, 2 * DFF])
    w_dn = din("w_dn", [DFF, D])
    n1g = din("n1g", [128, KC])
    n2g = din("n2g", [128, KC])
    fng = din("fng", [1, D])
    cst = din("cst", [128, 512])
    out = nc.dram_tensor("out", [TOWN, D], F32, kind="ExternalOutput").ap()

    modrow_d = dscr("modrow_d", [1, 6 * D], F32)
    GQ = dscr("GQ", [TOWN, 1024])
    GK = dscr("GK", [TALL, 1024])
    GV = dscr("GV", [TALL, 2048])
    GR = dscr("GR", [TOWN, 2048])
    DQ = dscr("DQ", [TOWN, 2048])
    DK = dscr("DK", [TALL, 2048])
    DV = dscr("DV", [TALL, 2048])
    IQ = dscr("IQ", [TOWN, 512])
    IKW = dscr("IKW", [TALL, 72], F32)
    GA = dscr("GA", [TOWN, 2048])
    GB = dscr("GB", [TOWN, 2048])
    OA = dscr("OA", [TOWN, 2048])
    MG = dscr("MG", [TOWN, 2048])
    OBD = dscr("OBD", [D, TOWN])
    MTD = dscr("MTD", [128, NT, TOWN]) if "MTD" in dbg else None

    with ExitStack() as es:
        S = Sched(nc, es)

        def sb(stack, name, shape, dt):
            return stack.enter_context(nc.sbuf_tensor(name, list(shape), dt))

        mm, tp, ax = [], [], []
        pstack = [None]
        rr = {'mm': 0, 'tp': 0, 'stg': 0, 'wb': 0}

        def alloc_psum(nm, nt, na):
            if pstack[0] is not None:
                pstack[0].close()
            st = ExitStack()
            pstack[0] = st
            rr['pgen'] = rr.get('pgen', 0) + 1
            g = rr['pgen']
            mm[:] = [st.enter_context(nc.psum_tensor("mm%d_%d" % (i, g), [128, 512], F32)) for i in range(nm)]
            tp[:] = [st.enter_context(nc.psum_tensor("tp%d_%d" % (i, g), [128, 1024], BF16)) for i in range(nt)]
            ax[:] = [st.enter_context(nc.psum_tensor("ax%d_%d" % (i, g), [128, 512], F32)) for i in range(na)]

        alloc_psum(4, 2, 2)

        mm_users = {}

        def set_mm_users(**parts):
            mm_users.clear()
            mm_users.update(parts)

        def next_mm(user=None):
            banks = mm_users.get(user) if mm_users else None
            if banks is None:
                banks = list(range(len(mm)))
            c = rr.get(('mm', user), 0)
            rr[('mm', user)] = c + 1
            i = banks[c % len(banks)]
            return mm[i], 'mm%d' % i

        bg = []

        def bg_step():
            for g in list(bg):
                try:
                    next(g)
                except StopIteration:
                    bg.remove(g)

        def bg_drain():
            while bg:
                bg_step()

        def next_tp():
            i = rr['tp'] % len(tp)
            rr['tp'] += 1
            return tp[i], 'tp%d' % i

        cst_t = sb(es, "cst_t", [128, 512], F32)
        S.dma('sp', cst_t[:], cst, w=['cst'])
        identf = cst_t[:, 0:128]
        triu = cst_t[:, 128:256]
        cmask = cst_t[:, 256:384]
        ctxflag = cst_t[:, 384:385]
        ctxneg = cst_t[:, 385:386]
        invf_d = cst_t[:, 400:416]
        invf_i = cst_t[:, 416:424]
        ident = sb(es, "ident", [128, 128], BF16)
        ones_bf = sb(es, "ones_bf", [128, 128], BF16)
        S.op('dve', lambda v: v.tensor_copy(out=ident[:], in_=identf), r=['cst'], w=['ident'])
        S.op('dve', lambda v: v.memset(ones_bf[:], 1.0), w=['ones_bf'])
        modfm = sb(es, "modfm", [128, 96], F32)
        A1 = sb(es, "A1", [128, KC], F32)
        A2 = sb(es, "A2", [128, KC], F32)
        n1g_t = sb(es, "n1g_t", [128, KC], F32)
        n2g_t = sb(es, "n2g_t", [128, KC], F32)
        S.dma('sp', n1g_t[:], n1g, w=['n1g'])
        S.dma('sp', n2g_t[:], n2g, w=['n2g'])
        wbuf = []

        def alloc_wbuf(stack, n, nk=KC, nb=512):
            rr['wgen'] = rr.get('wgen', 0) + 1
            wbuf[:] = [stack.enter_context(nc.sbuf_tensor("wbuf%d_%d" % (i, rr['wgen']), [128, nk, nb], BF16)) for i in range(n)]
        stg = [sb(es, "stg%d" % i, [128, 512], BF16) for i in range(4)]
        small = sb(es, "small", [128, 64], F32)
        junk = sb(es, "junk", [128, 2048], BF16)
        glrT = sb(es, "glrT", [32, TALL], F32)

        def next_stg():
            i = rr['stg'] % 4
            rr['stg'] += 1
            return stg[i], 'stg%d' % i

        def load_w(W, r0, nk, c0, nb, q='pool'):
            i = rr['wb'] % len(wbuf)
            rr['wb'] += 1
            key = 'wbuf%d' % i
            src = W[r0:r0 + nk * 128, c0:c0 + nb].rearrange("(kc p) n -> p kc n", p=128)
            S.dma(q, wbuf[i][:, 0:nk, 0:nb], src, w=[key])
            return wbuf[i], key

        def linear(actT, akey, W, c0, nb, tts, evac, r0=0, nk=KC, m=128, user='c'):
            wb, wkey = load_w(W, r0, nk, c0, nb)
            for tt in tts:
                bg_step()
                ps, pkey = next_mm(user)
                for kc in range(nk):
                    if kc == nk // 2:
                        bg_step()
                    S.op('pe', lambda p: p.matmul(ps[0:m, 0:nb], lhsT=actT(kc, tt), rhs=wb[:, kc, 0:nb],
                                                  start=(kc == 0), stop=(kc == nk - 1)),
                         r=[akey, wkey], w=[pkey])
                evac(tt, ps, pkey)

        def rstd_from_ss(ss_ap, n, key):
            S.op('dve', lambda v: v.tensor_scalar(out=ss_ap, in0=ss_ap, scalar1=1.0 / n, scalar2=EPS,
                                                  op0=ALU.mult, op1=ALU.add), r=[key], w=[key])
            S.op('act', lambda a: a.activation(out=ss_ap, in_=ss_ap, func=AF.Sqrt), r=[key], w=[key])
            S.op('dve', lambda v: v.reciprocal(out=ss_ap, in_=ss_ap), r=[key], w=[key])

        def to_feature_major(src_tile, skey, nchunk, dst_fn, dkey, evac_eng_fn):
            for c0 in range(0, nchunk, 8):
                n = min(8, nchunk - c0)
                tps, tkey = next_tp()
                for c in range(n):
                    S.op('pe', lambda p: p.transpose(out=tps[:, c * 128:(c + 1) * 128],
                                                     in_=src_tile[:, (c0 + c) * 128:(c0 + c + 1) * 128],
                                                     identity=ident[:]),
                         r=[skey, 'ident'], w=[tkey])
                evac_eng_fn(c0, n, tps, tkey)

        sT = sb(es, "sT", [128, KC], BF16)
        with ExitStack() as pa:
            alloc_wbuf(pa, 2)
            c_t = sb(pa, "c_t", [128, KC], F32)
            brow = sb(pa, "brow", [1, 2 * D], F32)
            mrow = sb(pa, "mrow", [1, 2 * D], F32)
            S.dma('sp', c_t[:], cfm, w=['c_t'])
            S.dma('sp', brow[:], b_ada[0:1, 0:2 * D], w=['brow'])
            S.op('act', lambda a: a.activation(out=sT[:], in_=c_t[:], func=AF.Silu), r=['c_t'], w=['sT'])

            def evac_mod(cb):
                def f(tt, ps, pkey):
                    S.op('dve', lambda v: v.tensor_tensor(out=mrow[0:1, cb * 512:(cb + 1) * 512], in0=ps[0:1, :],
                                                          in1=brow[0:1, cb * 512:(cb + 1) * 512], op=ALU.add),
                         r=[pkey, 'brow'], w=['mrow'])
                return f
            for cb in range(8):
                linear(lambda kc, tt: sT[:, kc:kc + 1], 'sT', w_ada, cb * 512, 512, [0], evac_mod(cb), m=1)
            S.dma('sp', modrow_d[0:1, 0:2 * D], mrow[:], r=['mrow'], w=['modrow_d'])
            with nc.allow_non_contiguous_dma(reason="one-time relayout of the modulation vector"):
                S.dma('sp', modfm[:, 0:32], modrow_d[0, 0:2 * D].rearrange("(j p) -> p j", p=128), r=['modrow_d'], w=['modfm'])
            S.op('dve', lambda v: v.scalar_tensor_tensor(out=A1[:], in0=modfm[:, 16:32], scalar=1.0, in1=n1g_t[:],
                                                         op0=ALU.add, op1=ALU.mult), r=['modfm', 'n1g'], w=['A1'])
            S.barrier()
        if stop == 'A':
            return nc
        sh1 = modfm[:, 0:16]
        sh2 = modfm[:, 48:64]

        def norm_to_fm(x_tile, xkey, dstT, dkey, tcol, A, sh, akeys, xn, xnkey, ss_ap):
            S.op('act', lambda a: a.activation(out=junk[:], in_=x_tile, func=AF.Square, accum_out=ss_ap),
                 r=[xkey], w=['junk', 'small'])
            rstd_from_ss(ss_ap, D, 'small')
            S.op('dve', lambda v: v.tensor_scalar(out=xn[:], in0=x_tile, scalar1=ss_ap, scalar2=None, op0=ALU.mult),
                 r=[xkey, 'small'], w=[xnkey])

            def ev(c0, n, tps, tkey):
                for c in range(n):
                    kc = c0 + c
                    S.op('act', lambda a: a.activation(out=dstT[:, kc, tcol:tcol + 128], in_=tps[:, c * 128:(c + 1) * 128],
                                                       func=AF.Identity, scale=A[:, kc:kc + 1], bias=sh[:, kc:kc + 1]),
                         r=[tkey] + akeys, w=[dkey])
            to_feature_major(xn, xnkey, KC, None, dkey, ev)

        s_mask = ExitStack()
        maskT = sb(s_mask, "maskT", [128, NT, TOWN], BF16)
        with ExitStack() as pbc:
            hT = sb(pbc, "hT", [128, KC, TALL], BF16)
            with ExitStack() as pb:
                xt = [sb(pb, "xt%d" % i, [128, D], F32) for i in range(2)]
                xn = [sb(pb, "xn%d" % i, [128, D], BF16) for i in range(2)]
                for tt in range(NT):
                    i = tt % 2
                    S.dma('sp' if i == 0 else 'act', xt[i][:], xs[tt * 128:(tt + 1) * 128, :], w=['xt%d' % i])
                    norm_to_fm(xt[i][:], 'xt%d' % i, hT, 'hT', tt * 128, A1, sh1, ['A1', 'modfm'],
                               xn[i], 'xn%d' % i, small[:, i:i + 1])
                S.barrier()

            with ExitStack() as pc:
                alloc_wbuf(pc, 2)
                alloc_psum(7, 1, 0)
                set_mm_users(c=[0, 1, 2], d=[3, 4], e=[5, 6])
                sinD = sb(pc, "sinD", [128, NT, 1, 16], F32)
                cosD = sb(pc, "cosD", [128, NT, 1, 16], F32)
                sinI = sb(pc, "sinI", [128, NT, 1, 8], F32)
                cosI = sb(pc, "cosI", [128, NT, 1, 8], F32)
                ggs = [sb(pc, "ggs%d" % i, [128, 512], F32) for i in range(2)]
                rt = [sb(pc, "rt%d" % i, [128, 4, 16], F32) for i in range(4)]
                f32stg = sb(pc, "f32stg", [128, 512], F32)
                f32stg2 = sb(pc, "f32stg2", [128, 72], F32)
                rsl = [sb(pc, "rsl%d" % i, [128, 128], F32) for i in range(2)]
                ptab = ExitStack()
                posf = sb(ptab, "posf", [128, NT], F32)
                pos_i = sb(ptab, "pos_i", [128, NT], I32)
                ang = sb(ptab, "ang", [128, NT, 16], F32)
                kf = sb(ptab, "kf", [128, NT, 16], F32)
                ki = sb(ptab, "ki", [128, NT, 16], I32)
                kf2 = sb(ptab, "kf2", [128, NT, 16], F32)
                S.dma('sp', pos_i[:], posi, w=['pos_i'])
                S.op('dve', lambda v: v.tensor_copy(out=posf[:], in_=pos_i[:]), r=['pos_i'], w=['posf'])
                TWO_PI = 2.0 * math.pi

                def make_tables(invf, nj, sin_t, cos_t, key):
                    for tt in range(NT):
                        S.op('dve', lambda v: v.tensor_scalar(out=ang[:, tt, 0:nj], in0=invf, scalar1=posf[:, tt:tt + 1],
                                                              scalar2=None, op0=ALU.mult), r=['cst', 'posf', 'ang'], w=['ang'])
                    a = ang[:, :, 0:nj]
                    kk = kf[:, :, 0:nj]
                    mm_ = kf2[:, :, 0:nj]
                    S.op('dve', lambda v: v.tensor_scalar(out=kk, in0=a, scalar1=1.0 / TWO_PI, scalar2=None,
                                                          op0=ALU.mult), r=['ang'], w=['kf'])
                    S.op('dve', lambda v: v.tensor_copy(out=ki[:, :, 0:nj], in_=kk), r=['kf'], w=['ki'])
                    S.op('dve', lambda v: v.tensor_copy(out=kk, in_=ki[:, :, 0:nj]), r=['ki'], w=['kf'])
                    S.op('dve', lambda v: v.scalar_tensor_tensor(out=a, in0=kk, scalar=-TWO_PI, in1=a,
                                                                 op0=ALU.mult, op1=ALU.add), r=['kf', 'ang'], w=['ang'])
                    for shift, dst in ((0.0, sin_t), (math.pi / 2, cos_t)):
                        S.op('dve', lambda v: v.tensor_scalar(out=kk, in0=a, scalar1=shift, scalar2=None,
                                                              op0=ALU.add), r=['ang', 'kf'], w=['kf'])
                        for cmp, bound, sgn in ((ALU.is_gt, math.pi, -1.0), (ALU.is_lt, -math.pi, 1.0)):
                            S.op('dve', lambda v: v.tensor_scalar(out=mm_, in0=kk, scalar1=bound, scalar2=sgn * TWO_PI,
                                                                  op0=cmp, op1=ALU.mult), r=['kf'], w=['kf2'])
                            S.op('dve', lambda v: v.tensor_tensor(out=kk, in0=kk, in1=mm_, op=ALU.add),
                                 r=['kf', 'kf2'], w=['kf'])
                        S.op('act', lambda a_: a_.activation(out=dst[:, :, 0, :], in_=kk, func=AF.Sin),
                             r=['kf'], w=[key])
                make_tables(invf_d, 16, sinD, cosD, 'tabD')
                make_tables(invf_i, 8, sinI, cosI, 'tabI')
                S.barrier()
                ptab.close()
                S.op('dve', lambda v: v.memset(glrT[:, :], 1.0), w=['glrT'])
                wb, wkey = load_w(w_in, 0, KC, O_GLR, 16)
                for tg in range(4):
                    ps, pkey = next_mm()
                    for kc in range(KC):
                        S.op('pe', lambda p: p.matmul(ps[0:16, :], lhsT=wb[:, kc, 0:16], rhs=hT[:, kc, tg * 512:(tg + 1) * 512],
                                                      start=(kc == 0), stop=(kc == KC - 1)), r=['hT', wkey], w=[pkey])
                    S.op('act', lambda a: a.activation(out=glrT[0:16, tg * 512:(tg + 1) * 512], in_=ps[0:16, :], func=AF.Identity),
                         r=[pkey], w=['glrT'])

                if stop == 'C1':
                    S.barrier()
                    return nc
                own = list(range(8, 16))
                allt = list(range(NT))
                hact = lambda kc, tt: hT[:, kc, tt * 128:(tt + 1) * 128]

                def store(dst, own_only, c0, nb):
                    def f(tt, ps, pkey):
                        st, skey = next_stg()
                        S.op('act', lambda a: a.activation(out=st[:, 0:nb], in_=ps[:, 0:nb], func=AF.Identity), r=[pkey], w=[skey])
                        row = (tt - 8 if own_only else tt) * 128
                        S.dma('sp', dst[row:row + 128, c0:c0 + nb], st[:, 0:nb], r=[skey], w=[(id(dst), tt)])
                    return f

                def store_act(dst, c0, nb, func, mul=None):
                    def f(tt, ps, pkey):
                        st, skey = next_stg()
                        if mul is None:
                            S.op('act', lambda a: a.activation(out=st[:, 0:nb], in_=ps[:, 0:nb], func=func), r=[pkey], w=[skey])
                        else:
                            S.op('act', lambda a: a.activation(out=f32stg[:, 0:nb], in_=ps[:, 0:nb], func=func), r=[pkey], w=['f32stg'])
                            S.op('dve', lambda v: v.tensor_tensor(out=st[:, 0:nb], in0=f32stg[:, 0:nb], in1=mul[0][:, 0:nb],
                                                                  op=ALU.mult), r=['f32stg', mul[1]], w=[skey])
                        row = (tt - 8) * 128
                        S.dma('sp', dst[row:row + 128, c0:c0 + nb], st[:, 0:nb], r=[skey], w=[(id(dst), tt)])
                    return f

                def rope_ops(x1, x2, o1, o2, cs, sn, pkey, skey, tkey, shape):
                    t = [rt[i][:].rearrange("p a b -> p (a b)")[:, 0:shape[0] * shape[1]].rearrange("p (a b) -> p a b", b=shape[1])
                         for i in range(4)]
                    S.op('dve', lambda v: v.tensor_tensor(out=t[0], in0=x1, in1=cs, op=ALU.mult), r=[pkey, tkey], w=['rt0'])
                    S.op('dve', lambda v: v.tensor_tensor(out=t[1], in0=x2, in1=sn, op=ALU.mult), r=[pkey, tkey], w=['rt1'])
                    S.op('dve', lambda v: v.tensor_tensor(out=o1, in0=t[0], in1=t[1], op=ALU.subtract), r=['rt0', 'rt1'], w=[skey])
                    S.op('dve', lambda v: v.tensor_tensor(out=t[2], in0=x1, in1=sn, op=ALU.mult), r=[pkey, tkey], w=['rt2'])
                    S.op('dve', lambda v: v.tensor_tensor(out=t[3], in0=x2, in1=cs, op=ALU.mult), r=[pkey, tkey], w=['rt3'])
                    S.op('dve', lambda v: v.tensor_tensor(out=o2, in0=t[2], in1=t[3], op=ALU.add), r=['rt2', 'rt3'], w=[skey])

                def store_rope_d(dst, own_only, c0):
                    def f(tt, ps, pkey):
                        st, skey = next_stg()
                        S.op('act', lambda a: a.activation(out=st[:, :], in_=ps[:, :], func=AF.Identity), r=[pkey], w=[skey])
                        pv = ps[:, :].rearrange("p (h d) -> p h d", d=128)
                        sv = st[:, :].rearrange("p (h d) -> p h d", d=128)
                        j = rr.get('rsl', 0) % 2
                        rr['rsl'] = rr.get('rsl', 0) + 1
                        rv = rsl[j][:, :].rearrange("p (h d) -> p h d", d=32)
                        S.op('act', lambda a: a.activation(out=rv, in_=pv[:, :, 0:32], func=AF.Identity), r=[pkey], w=['rsl%d' % j])
                        rope_ops(rv[:, :, 0:16], rv[:, :, 16:32], sv[:, :, 0:16], sv[:, :, 16:32],
                                 cosD[:, tt, :, :].to_broadcast([128, 4, 16]), sinD[:, tt, :, :].to_broadcast([128, 4, 16]), 'rsl%d' % j, skey, 'tabD', (4, 16))
                        row = (tt - 8 if own_only else tt) * 128
                        S.dma('sp', dst[row:row + 128, c0:c0 + 512], st[:, :], r=[skey], w=[(id(dst), tt)])
                    return f

                def store_iq(tt, ps, pkey):
                    st, skey = next_stg()
                    S.op('act', lambda a: a.activation(out=st[:, :], in_=ps[:, :], func=AF.Identity), r=[pkey], w=[skey])
                    pv = ps[:, :].rearrange("p (h d) -> p h d", d=64)
                    sv = st[:, :].rearrange("p (h d) -> p h d", d=64)
                    j = rr.get('rsl', 0) % 2
                    rr['rsl'] = rr.get('rsl', 0) + 1
                    rv = rsl[j][:, :].rearrange("p (h d) -> p h d", d=16)
                    S.op('act', lambda a: a.activation(out=rv, in_=pv[:, :, 0:16], func=AF.Identity), r=[pkey], w=['rsl%d' % j])
                    rope_ops(rv[:, :, 0:8], rv[:, :, 8:16], sv[:, :, 0:8], sv[:, :, 8:16],
                             cosI[:, tt, :, :].to_broadcast([128, 8, 8]), sinI[:, tt, :, :].to_broadcast([128, 8, 8]), 'rsl%d' % j, skey, 'tabI', (8, 8))
                    row = (tt - 8) * 128
                    S.dma('sp', IQ[row:row + 128, :], st[:, :], r=[skey], w=[('IQ', tt)])

                def store_ikw(tt, ps, pkey):
                    S.op('act', lambda a: a.activation(out=f32stg2[:, :], in_=ps[:, 0:72], func=AF.Identity), r=[pkey], w=['f32stg2'])
                    j = rr.get('rsl', 0) % 2
                    rr['rsl'] = rr.get('rsl', 0) + 1
                    S.op('act', lambda a: a.activation(out=rsl[j][:, 0:16], in_=ps[:, 0:16], func=AF.Identity), r=[pkey], w=['rsl%d' % j])
                    pkey = 'rsl%d' % j
                    rope_ops(rsl[j][:, 0:8].rearrange("p (a b) -> p a b", a=1), rsl[j][:, 8:16].rearrange("p (a b) -> p a b", a=1),
                             f32stg2[:, 0:8].rearrange("p (a b) -> p a b", a=1), f32stg2[:, 8:16].rearrange("p (a b) -> p a b", a=1),
                             cosI[:, tt, :, :], sinI[:, tt, :, :], pkey, 'f32stg2', 'tabI', (1, 8))
                    S.dma('sp', IKW[tt * 128:(tt + 1) * 128, :], f32stg2[:, :], r=['f32stg2'], w=[('IKW', tt)])

                def gen_D():
                    gu_aug = sb(pc, "gu_aug", [32, 1024], F32)
                    S.dma('sp', gu_aug[0:16, :], gate_up, w=['gu_aug'])
                    S.dma('sp', gu_aug[16:17, :], gate_bias, w=['gu_aug'])
                    Sst = sb(pc, "Sst", [128, 2, 512], F32)
                    Sbf = sb(pc, "Sbf", [128, 2, 512], BF16)
                    kt_ = [sb(pc, "kt%d" % i, [128, 256], BF16) for i in range(2)]
                    vt_ = [sb(pc, "vt%d" % i, [128, 512], BF16) for i in range(2)]
                    qt_ = [sb(pc, "qt%d" % i, [128, 256], BF16) for i in range(2)]
                    gs_ = [sb(pc, "gs%d" % i, [128, 512], BF16) for i in range(2)]
                    sp_ = sb(pc, "sp_", [128, 256], F32)
                    Epos = sb(pc, "Epos", [128, 256], F32)
                    Eneg = sb(pc, "Eneg", [128, 256], F32)
                    Etok = sb(pc, "Etok", [128, 256], F32)
                    ktok = sb(pc, "ktok", [128, 256], BF16)
                    kT_ = sb(pc, "kT_", [128, 256], BF16)
                    qT_ = sb(pc, "qT_", [128, 256], BF16)
                    attnT = sb(pc, "attnT", [128, 128], BF16)
                    junkD = sb(pc, "junkD", [128, 512], BF16)
                    oa_ = [sb(pc, "oa%d" % i, [128, 512], BF16) for i in range(2)]
                    def d_loads(h_, n_):
                        i_ = n_ % 2
                        r_ = n_ * 128
                        S.dma('sp', kt_[i_][:], GK[r_:r_ + 128, h_ * 256:(h_ + 1) * 256], r=[(id(GK), n_)], w=['kt%d' % i_])
                        S.dma('sp', vt_[i_][:], GV[r_:r_ + 128, h_ * 512:(h_ + 1) * 512], r=[(id(GV), n_)], w=['vt%d' % i_])
                        if n_ >= 8:
                            q_ = (n_ - 8) * 128
                            S.dma('sp', qt_[i_][:], GQ[q_:q_ + 128, h_ * 256:(h_ + 1) * 256], r=[(id(GQ), n_)], w=['qt%d' % i_])
                            S.dma('sp', gs_[i_][:], GR[q_:q_ + 128, h_ * 512:(h_ + 1) * 512], r=[(id(GR), n_)], w=['gs%d' % i_])

                    def d_stage1(h_, n_):
                        ps, pk = next_mm('d')
                        S.op('pe', lambda p: p.matmul(ps[:, 0:256], lhsT=glrT[0:17, n_ * 128:(n_ + 1) * 128],
                                                      rhs=gu_aug[0:17, h_ * 256:(h_ + 1) * 256],
                                                      start=True, stop=True), r=['glrT', 'gu_aug'], w=[pk])
                        S.op('act', lambda a: a.activation(out=sp_[:], in_=ps[:, 0:256], func=AF.Exp, scale=-1.0), r=[pk], w=['sp_'])
                        S.op('act', lambda a: a.activation(out=sp_[:], in_=sp_[:], func=AF.Ln, bias=1.0), r=['sp_'], w=['sp_'])

                    for h in range(4):
                        S.op('dve', lambda v: v.memset(Sst[:], 0.0), w=['Sst'])
                        S.op('dve', lambda v: v.memset(Sbf[:], 0.0), w=['Sbf'])
                        for n in range(NT):
                            i = n % 2
                            ownt = n >= 8
                            r0 = n * 128
                            if ownt:
                                q0 = (n - 8) * 128
                            if h == 0 and n == 0:
                                d_loads(0, 0)
                            nxt = h * NT + n + 1
                            if nxt < 4 * NT:
                                d_loads(nxt // NT, nxt % NT)
                            if h == 0 and n == 0:
                                d_stage1(0, 0)
                                yield
                            ps2, pk2 = next_mm('d')
                            for cc in range(2):
                                S.op('pe', lambda p: p.matmul(ps2[:, cc * 128:(cc + 1) * 128], lhsT=sp_[:, cc * 128:(cc + 1) * 128], rhs=triu,
                                                              start=True, stop=True), r=['sp_', 'cst'], w=[pk2])
                            ps3, pk3 = next_mm('d')
                            S.op('pe', lambda p: p.matmul(ps3[:, 0:256], lhsT=triu, rhs=sp_[:], start=True, stop=True), r=['sp_', 'cst'], w=[pk3])
                            S.op('act', lambda a: a.activation(out=Epos[:], in_=ps2[:, 0:256], func=AF.Exp, scale=-1.0 / 16), r=[pk2], w=['Epos'])
                            S.op('act', lambda a: a.activation(out=Eneg[:], in_=ps2[:, 0:256], func=AF.Exp, scale=1.0 / 16), r=[pk2], w=['Eneg'])
                            S.op('act', lambda a: a.activation(out=Etok[:], in_=ps3[:, 0:256], func=AF.Exp, scale=1.0 / 16), r=[pk3], w=['Etok'])
                            yield
                            tpsf, tk = next_mm('d')
                            tps = tpsf[:, :].bitcast(BF16)
                            for cc in range(2):
                                S.op('pe', lambda p: p.transpose(out=tps[:, cc * 128:(cc + 1) * 128], in_=kt_[i][:, cc * 128:(cc + 1) * 128],
                                                                 identity=ident[:]), r=['kt%d' % i, 'ident'], w=[tk])
                            if ownt:
                                for cc in range(2):
                                    S.op('pe', lambda p: p.transpose(out=tps[:, 256 + cc * 128:256 + (cc + 1) * 128],
                                                                     in_=qt_[i][:, cc * 128:(cc + 1) * 128], identity=ident[:]),
                                         r=['qt%d' % i, 'ident'], w=[tk])
                            yield
                            S.op('dve', lambda v: v.tensor_tensor(out=kT_[:], in0=tps[:, 0:256], in1=Eneg[:], op=ALU.mult),
                                 r=[tk, 'Eneg'], w=['kT_'])
                            S.op('pool', lambda g: g.tensor_tensor(out=ktok[:], in0=kt_[i][:], in1=Etok[:], op=ALU.mult),
                                 r=['kt%d' % i, 'Etok'], w=['ktok'])
                            if ownt:
                                S.op('dve', lambda v: v.scalar_tensor_tensor(out=qT_[:], in0=tps[:, 256:512], scalar=1.0 / 16, in1=Epos[:],
                                                                             op0=ALU.mult, op1=ALU.mult), r=[tk, 'Epos'], w=['qT_'])
                                yield
                                psA, pkA = next_mm('d')
                                for cc in range(2):
                                    S.op('pe', lambda p: p.matmul(psA[:, 0:128], lhsT=kT_[:, cc * 128:(cc + 1) * 128],
                                                                  rhs=qT_[:, cc * 128:(cc + 1) * 128], start=(cc == 0), stop=(cc == 1)),
                                         r=['kT_', 'qT_'], w=[pkA])
                                S.op('dve', lambda v: v.tensor_tensor(out=attnT[:], in0=psA[:, 0:128], in1=triu, op=ALU.mult),
                                     r=[pkA, 'cst'], w=['attnT'])
                                yield
                                psO, pkO = next_mm('d')
                                S.op('pe', lambda p: p.matmul(psO[:, :], lhsT=attnT[:], rhs=vt_[i][:], start=True, stop=False),
                                     r=['attnT', 'vt%d' % i], w=[pkO])
                                for cc in range(2):
                                    S.op('pe', lambda p: p.matmul(psO[:, :], lhsT=qT_[:, cc * 128:(cc + 1) * 128], rhs=Sbf[:, cc, :],
                                                                  start=False, stop=(cc == 1)), r=['qT_', 'Sbf'], w=[pkO])
                                ssc = small[:, 4 + i:5 + i]
                                S.op('act', lambda a: a.activation(out=junkD[:], in_=psO[:, :], func=AF.Square, accum_out=ssc),
                                     r=[pkO], w=['junkD', 'smallD'])
                                rstd_from_ss(ssc, 512, 'smallD')
                                S.op('dve', lambda v: v.scalar_tensor_tensor(out=oa_[i][:], in0=psO[:, :], scalar=ssc, in1=gs_[i][:],
                                                                             op0=ALU.mult, op1=ALU.mult),
                                     r=[pkO, 'smallD', 'gs%d' % i], w=['oa%d' % i])
                                S.dma('sp', OA[q0:q0 + 128, h * 512:(h + 1) * 512], oa_[i][:], r=['oa%d' % i], w=[('OA', n)])
                            yield
                            for cc in range(2):
                                psU, pkU = next_mm('d')
                                S.op('pe', lambda p: p.matmul(psU[:, :], lhsT=ktok[:, cc * 128:(cc + 1) * 128], rhs=vt_[i][:],
                                                              start=True, stop=True), r=['ktok', 'vt%d' % i], w=[pkU])
                                S.op('dve', lambda v: v.tensor_tensor(out=Sst[:, cc, :], in0=psU[:, :], in1=Sst[:, cc, :], op=ALU.add),
                                     r=[pkU, 'Sst'], w=['Sst'])
                                S.op('dve', lambda v: v.tensor_scalar(out=Sst[:, cc, :], in0=Sst[:, cc, :],
                                                                      scalar1=Epos[:, cc * 128 + 127:cc * 128 + 128], scalar2=None, op0=ALU.mult),
                                     r=['Sst', 'Epos'], w=['Sst'])
                                if n == 7:
                                    S.op('dve', lambda v: v.tensor_scalar(out=Sst[:, cc, :], in0=Sst[:, cc, :], scalar1=ctxflag, scalar2=None,
                                                                          op0=ALU.mult), r=['Sst', 'cst'], w=['Sst'])
                                S.op('act', lambda a: a.activation(out=Sbf[:, cc, :], in_=Sst[:, cc, :], func=AF.Identity), r=['Sst'], w=['Sbf'])
                                if cc == 0 and nxt < 4 * NT:
                                    d_stage1(nxt // NT, nxt % NT)
                                yield


                def gen_E():
                    ikT2 = sb(pc, "ikT2", [128, TALL], BF16)
                    ikf = sb(pc, "ikf", [128, 72], F32)
                    ikd = sb(pc, "ikd", [128, 128], BF16)
                    iqs = sb(pc, "iqs", [128, 512], BF16)
                    iqT = sb(pc, "iqT", [128, 4, 128], BF16)
                    iwp = sb(pc, "iwp", [128, 8], F32)
                    score = sb(pc, "score", [128, TALL], F32)
                    relu_t = [sb(pc, "relu%d" % i, [128, 512], F32) for i in range(2)]
                    mask_tm = sb(pc, "mask_tm", [128, TALL], BF16)
                    S.op('dve', lambda v: v.memset(maskT[:], 0.0), w=['maskT'])
                    for kt in range(NT):
                        S.dma('sp', ikf[:], IKW[kt * 128:(kt + 1) * 128, :], r=[('IKW', kt)], w=['ikf'])
                        S.op('dve', lambda v: v.tensor_copy(out=ikd[:, 0:64], in_=ikf[:, 0:64]), r=['ikf'], w=['ikd'])
                        S.op('dve', lambda v: v.tensor_copy(out=ikd[:, 64:128], in_=ikf[:, 0:64]), r=['ikf'], w=['ikd'])
                        tps, tk = next_tp()
                        S.op('pe', lambda p: p.transpose(out=tps[:, 0:128], in_=ikd[:], identity=ident[:]), r=['ikd', 'ident'], w=[tk])
                        S.op('act', lambda a: a.activation(out=ikT2[:, kt * 128:(kt + 1) * 128], in_=tps[:, 0:128], func=AF.Identity),
                             r=[tk], w=['ikT2'])
                        yield
                    lo, hw, mid, cntv, gev, am = [small[:, 8 + j:9 + j] for j in range(6)]
                    for qi in range(8):
                        tt = 8 + qi
                        nk = 1024 + 128 * (qi + 1)
                        S.dma('sp', iqs[:], IQ[qi * 128:(qi + 1) * 128, :], r=[('IQ', tt)], w=['iqs'])
                        S.dma('act', ikf[:], IKW[tt * 128:(tt + 1) * 128, :], r=[('IKW', tt)], w=['ikf'])
                        yield
                        tps, tk = next_tp()
                        for c in range(4):
                            S.op('pe', lambda p: p.transpose(out=tps[:, c * 128:(c + 1) * 128], in_=iqs[:, c * 128:(c + 1) * 128],
                                                             identity=ident[:]), r=['iqs', 'ident'], w=[tk])
                        S.op('act', lambda a: a.activation(out=iqT[:].rearrange("p a b -> p (a b)"), in_=tps[:, 0:512], func=AF.Identity),
                             r=[tk], w=['iqT'])
                        S.op('dve', lambda v: v.tensor_scalar(out=iwp[:], in0=ikf[:, 64:72], scalar1=float(8 ** -0.5 * 64 ** -0.5),
                                                              scalar2=None, op0=ALU.mult), r=['ikf'], w=['iwp'])
                        ng = (nk + 511) // 512
                        for g in range(ng):
                            wd_ = min(512, nk - g * 512)
                            for hh in range(8):
                                ps, pk = next_mm('e')
                                pb_ = (hh % 2) * 64
                                S.op('pe', lambda p: p.matmul(ps[:, 0:wd_], lhsT=iqT[pb_:pb_ + 64, hh // 2, :],
                                                              rhs=ikT2[pb_:pb_ + 64, g * 512:g * 512 + wd_], start=True, stop=True),
                                     r=['iqT', 'ikT2'], w=[pk])
                                rl = relu_t[hh % 2]
                                rk = 'relu%d' % (hh % 2)
                                S.op('act', lambda a: a.activation(out=rl[:, 0:wd_], in_=ps[:, 0:wd_], func=AF.Relu), r=[pk], w=[rk])
                                sc_ = score[:, g * 512:g * 512 + wd_]
                                if hh == 0:
                                    S.op('dve', lambda v: v.tensor_scalar(out=sc_, in0=rl[:, 0:wd_], scalar1=iwp[:, 0:1], scalar2=None,
                                                                          op0=ALU.mult), r=[rk, 'iwp'], w=['score'])
                                else:
                                    S.op('dve', lambda v: v.scalar_tensor_tensor(out=sc_, in0=rl[:, 0:wd_], scalar=iwp[:, hh:hh + 1],
                                                                                 in1=sc_, op0=ALU.mult, op1=ALU.add),
                                         r=[rk, 'iwp', 'score'], w=['score'])
                                yield
                        S.op('dve', lambda v: v.tensor_reduce(out=am, in_=score[:, 0:nk], axis=AX.X, op=ALU.max,
                                                              apply_absolute_value=True), r=['score'], w=['smallE'])
                        S.op('dve', lambda v: v.tensor_scalar(out=score[:, 0:1024], in0=score[:, 0:1024], scalar1=ctxneg, scalar2=None,
                                                              op0=ALU.add), r=['score', 'cst'], w=['score'])
                        S.op('dve', lambda v: v.tensor_tensor(out=score[:, nk - 128:nk], in0=score[:, nk - 128:nk], in1=cmask, op=ALU.add),
                             r=['score', 'cst'], w=['score'])
                        S.op('dve', lambda v: v.tensor_scalar(out=hw, in0=am, scalar1=1.0001, scalar2=1e-20, op0=ALU.mult, op1=ALU.add),
                             r=['smallE'], w=['smallE'])
                        S.op('dve', lambda v: v.tensor_scalar(out=lo, in0=hw, scalar1=-1.0, scalar2=None, op0=ALU.mult),
                             r=['smallE'], w=['smallE'])
                        for it in range(NBISECT):
                            S.op('dve', lambda v: v.tensor_tensor(out=mid, in0=lo, in1=hw, op=ALU.add), r=['smallE'], w=['smallE'])
                            S.op('dve', lambda v: v.tensor_scalar(out=junk[:, 0:nk], in0=score[:, 0:nk], scalar1=mid, scalar2=None,
                                                                  op0=ALU.is_ge, op1=ALU.add, accum_out=cntv),
                                 r=['score', 'smallE'], w=['junk', 'smallE'])
                            S.op('dve', lambda v: v.tensor_scalar(out=gev, in0=cntv, scalar1=TOPK - 0.5, scalar2=None, op0=ALU.is_ge),
                                 r=['smallE'], w=['smallE'])
                            S.op('dve', lambda v: v.scalar_tensor_tensor(out=lo, in0=hw, scalar=gev, in1=lo, op0=ALU.mult, op1=ALU.add),
                                 r=['smallE'], w=['smallE'])
                            S.op('dve', lambda v: v.tensor_scalar(out=hw, in0=hw, scalar1=0.5, scalar2=None, op0=ALU.mult),
                                 r=['smallE'], w=['smallE'])
                            yield
                        S.op('dve', lambda v: v.tensor_scalar(out=mask_tm[:, 0:nk], in0=score[:, 0:nk], scalar1=lo, scalar2=None,
                                                              op0=ALU.is_ge), r=['score', 'smallE'], w=['mask_tm'])
                        nkb = nk // 128
                        for c0 in range(0, nkb, 8):
                            n_ = min(8, nkb - c0)
                            yield
                            tps, tk = next_tp()
                            for c in range(n_):
                                S.op('pe', lambda p: p.transpose(out=tps[:, c * 128:(c + 1) * 128],
                                                                 in_=mask_tm[:, (c0 + c) * 128:(c0 + c + 1) * 128], identity=ident[:]),
                                     r=['mask_tm', 'ident'], w=[tk])
                            S.op('act', lambda a: a.activation(out=maskT[:, c0:c0 + n_, qi * 128:(qi + 1) * 128],
                                                               in_=tps[:, 0:n_ * 128].rearrange("p (a b) -> p a b", b=128),
                                                               func=AF.Identity), r=[tk], w=['maskT'])
                            yield
                    yield

                linear(hact, 'hT', w_in, O_IQ, 512, own, store_iq)
                linear(hact, 'hT', w_in, O_IK, 72, allt, store_ikw)
                bg.append(gen_E())
                for cb in range(2):
                    linear(hact, 'hT', w_in, O_GQ + cb * 512, 512, own, store(GQ, True, cb * 512, 512))
                for cb in range(2):
                    linear(hact, 'hT', w_in, O_GK + cb * 512, 512, allt, store(GK, False, cb * 512, 512))
                for cb in range(4):
                    linear(hact, 'hT', w_in, O_GV + cb * 512, 512, allt, store(GV, False, cb * 512, 512))
                for cb in range(4):
                    gg = ggs[cb % 2]
                    S.dma('act', gg[:], gla_gain[0, cb * 512:(cb + 1) * 512].partition_broadcast(128), w=['ggs%d' % (cb % 2)])
                    linear(hact, 'hT', w_in, O_GR + cb * 512, 512, own, store_act(GR, cb * 512, 512, AF.Silu, mul=(gg, 'ggs%d' % (cb % 2))))
                bg.append(gen_D())
                for cb in range(4):
                    linear(hact, 'hT', w_in, O_DQ + cb * 512, 512, own, store_rope_d(DQ, True, cb * 512))
                for cb in range(4):
                    linear(hact, 'hT', w_in, O_DK + cb * 512, 512, allt, store_rope_d(DK, False, cb * 512))
                for cb in range(4):
                    linear(hact, 'hT', w_in, O_DV + cb * 512, 512, allt, store(DV, False, cb * 512, 512))
                for cb in range(4):
                    linear(hact, 'hT', w_in, O_GA + cb * 512, 512, own, store_act(GA, cb * 512, 512, AF.Sigmoid))
                for cb in range(4):
                    linear(hact, 'hT', w_in, O_GB + cb * 512, 512, own, store_act(GB, cb * 512, 512, AF.Sigmoid))
                bg_drain()
                S.barrier()
                if MTD is not None:
                    S.dma('sp', MTD, maskT[:], r=['maskT'], w=['MTD'])
                    S.barrier()
                if stop == 'C':
                    return nc
                set_mm_users()
        with ExitStack() as pefg:
            with ExitStack() as pef:

                with ExitStack() as pf:
                    kTg = sb(pf, "kTg", [128, 4, TALL], BF16)
                    vg = sb(pf, "vg", [128, NT, 512], BF16)
                    qTg = sb(pf, "qTg", [128, 4, TOWN], BF16)
                    ldt = [sb(pf, "ldt%d" % i, [128, 512], BF16) for i in range(2)]
                    pt_ = [sb(pf, "pt%d" % i, [128, 512], BF16) for i in range(4)]
                    pm_ = [sb(pf, "pm%d" % i, [128, 512], BF16) for i in range(4)]
                    alloc_psum(3, 1, 4)
                    lnd = sb(pf, "lnd", [128, 512], F32)
                    obs = [sb(pf, "obs%d" % i, [128, 512], BF16) for i in range(2)]
                    alloc_wbuf(pf, 2)
                    browF = [sb(pf, "browF%d" % i, [1, 512], F32) for i in range(2)]
                    mrowF = [sb(pf, "mrowF%d" % i, [1, 512], F32) for i in range(2)]

                    def gen_modrest():
                        pend = []

                        def compute(cb, j, wb, wkey):
                            tpf = tp[0][:, :].bitcast(F32)
                            for kc in range(KC):
                                S.op('pe', lambda p: p.matmul(tpf[0:1, :], lhsT=sT[:, kc:kc + 1], rhs=wb[:, kc, :],
                                                              start=(kc == 0), stop=(kc == KC - 1)), r=['sT', wkey], w=['tp0'])
                            S.op('dve', lambda v: v.tensor_tensor(out=mrowF[j][:], in0=tpf[0:1, :], in1=browF[j][:], op=ALU.add),
                                 r=['tp0', 'browF%d' % j], w=['mrowF%d' % j])
                            S.dma('act', modrow_d[0:1, cb * 512:(cb + 1) * 512], mrowF[j][:], r=['mrowF%d' % j], w=[('modrow_d', cb)])

                        for cb in range(8, 24):
                            j = cb % 2
                            S.dma('act', browF[j][:], b_ada[0:1, cb * 512:(cb + 1) * 512], w=['browF%d' % j])
                            wb, wkey = load_w(w_ada, 0, KC, cb * 512, 512)
                            pend.append((cb, j, wb, wkey))
                            yield
                            if len(pend) == 2:
                                compute(*pend.pop(0))
                        while pend:
                            compute(*pend.pop(0))
                            yield
                    bg.append(gen_modrest())
                    rden = sb(pf, "rden", [128, 512], F32)
                    for hg in range(4):
                        S.dma('act', vg[:], DV[:, hg * 512:(hg + 1) * 512].rearrange("(kt p) c -> p kt c", p=128), w=['vg'])
                        for kt in range(NT + 8):
                            i = kt % 2
                            if kt < NT:
                                S.dma('sp', ldt[i][:], DK[kt * 128:(kt + 1) * 128, hg * 512:(hg + 1) * 512], w=['ldt%d' % i])
                                dst = kTg[:, :, kt * 128:(kt + 1) * 128]
                                dk_ = 'kTg'
                            else:
                                qi = kt - NT
                                S.dma('sp', ldt[i][:], DQ[qi * 128:(qi + 1) * 128, hg * 512:(hg + 1) * 512], w=['ldt%d' % i])
                                dst = qTg[:, :, qi * 128:(qi + 1) * 128]
                                dk_ = 'qTg'
                            tps, tk = next_tp()
                            for c in range(4):
                                S.op('pe', lambda p: p.transpose(out=tps[:, c * 128:(c + 1) * 128], in_=ldt[i][:, c * 128:(c + 1) * 128],
                                                                 identity=ident[:]), r=['ldt%d' % i, 'ident'], w=[tk])
                            S.op('act' if kt % 2 == 0 else 'dve',
                                 (lambda a: a.activation(out=dst, in_=tps[:, 0:512].rearrange("p (a b) -> p a b", b=128), func=AF.Identity))
                                 if kt % 2 == 0 else
                                 (lambda v: v.tensor_copy(out=dst, in_=tps[:, 0:512].rearrange("p (a b) -> p a b", b=128))),
                                 r=[tk], w=[dk_])
                        steps = [(hh, qg, kb) for hh in range(4) for qg in range(2) for kb in range(8 + 4 * (qg + 1))]
                        LA = 2
                        slots = {}

                        def qk_stage(idx):
                            hh, qg, kb = steps[idx]
                            lps, lk = next_mm()
                            S.op('pe', lambda p: p.matmul(lps[:, :], lhsT=kTg[:, hh, kb * 128:(kb + 1) * 128],
                                                          rhs=qTg[:, hh, qg * 512:(qg + 1) * 512], start=True, stop=True),
                                 r=['kTg', 'qTg'], w=[lk])
                            j = idx % 4
                            slots[idx] = j
                            S.op('act', lambda a: a.activation(out=pt_[j][:], in_=lps[:, :], func=AF.Exp, scale=float(128 ** -0.5)),
                                 r=[lk], w=['pt%d' % j])
                            S.op('dve', lambda v: v.tensor_tensor(out=pm_[j][:], in0=pt_[j][:], in1=maskT[:, kb, qg * 512:(qg + 1) * 512],
                                                                  op=ALU.mult), r=['pt%d' % j, 'maskT'], w=['pm%d' % j])

                        def pv_stage(idx):
                            hh, qg, kb = steps[idx]
                            nkb = 8 + 4 * (qg + 1)
                            j = slots.pop(idx)
                            pr = (hh * 2 + qg) % 2
                            aO, aD = ax[2 * pr], ax[2 * pr + 1]
                            kO, kD = 'ax%d' % (2 * pr), 'ax%d' % (2 * pr + 1)
                            S.op('pe', lambda p: p.matmul(aO[:, :], lhsT=vg[:, kb, hh * 128:(hh + 1) * 128], rhs=pm_[j][:],
                                                          start=(kb == 0), stop=(kb == nkb - 1)), r=['vg', 'pm%d' % j], w=[kO])
                            S.op('pe', lambda p: p.matmul(aD[:, :], lhsT=ones_bf[:], rhs=pm_[j][:],
                                                          start=(kb == 0), stop=(kb == nkb - 1)), r=['ones_bf', 'pm%d' % j], w=[kD])
                            if kb == nkb - 1:
                                h = hg * 4 + hh
                                S.op('act', lambda a: a.activation(out=lnd[:], in_=aD[:, :], func=AF.Ln), r=[kD], w=['lnd'])
                                S.op('act', lambda a: a.activation(out=rden[:], in_=lnd[:], func=AF.Exp, scale=-1.0), r=['lnd'], w=['rden'])
                                jo = (hh * 2 + qg) % 2
                                S.op('dve', lambda v: v.tensor_tensor(out=obs[jo][:], in0=aO[:, :], in1=rden[:],
                                                                      op=ALU.mult), r=[kO, 'rden'], w=['obs%d' % jo])
                                S.dma('sp', OBD[h * 128:(h + 1) * 128, qg * 512:(qg + 1) * 512], obs[jo][:], r=['obs%d' % jo], w=[('OBD', h, qg)])

                        for idx in range(len(steps) + LA):
                            if idx % 12 == 0:
                                bg_step()
                            if idx < len(steps):
                                qk_stage(idx)
                            if idx - LA >= 0:
                                pv_stage(idx - LA)
                    bg_drain()
                    S.barrier()
                    with nc.allow_non_contiguous_dma(reason="one-time relayout of the modulation vector"):
                        S.dma('sp', modfm[:, 32:96], modrow_d[0, 2 * D:6 * D].rearrange("(j p) -> p j", p=128), w=['modfm'])
                    S.op('dve', lambda v: v.scalar_tensor_tensor(out=A2[:], in0=modfm[:, 64:80], scalar=1.0, in1=n2g_t[:],
                                                                 op0=ALU.add, op1=ALU.mult), r=['modfm', 'n2g'], w=['A2'])
                    S.barrier()
                    alloc_psum(4, 2, 2)
            s_mask.close()

            with ExitStack() as pg1:
                o_aT = sb(pg1, "o_aT", [128, KC, TOWN], BF16)
                o_bT = sb(pg1, "o_bT", [128, KC, TOWN], BF16)
                S.dma('act', o_bT[:], OBD.rearrange("(h p) t -> p h t", p=128), w=['o_bT'])
                oat = [sb(pg1, "oat%d" % i, [128, D], BF16) for i in range(2)]
                gat = [sb(pg1, "gat%d" % i, [128, 512], BF16) for i in range(2)]
                gbt = [sb(pg1, "gbt%d" % i, [128, 512], BF16) for i in range(2)]
                m1 = sb(pg1, "m1", [128, 512], F32)
                m2 = sb(pg1, "m2", [128, 512], F32)
                mgs = [sb(pg1, "mgs%d" % i, [128, 512], BF16) for i in range(2)]
                alloc_wbuf(pg1, 4)
                for qi in range(8):
                    i = qi % 2
                    S.dma('sp', oat[i][:], OA[qi * 128:(qi + 1) * 128, :], w=['oat%d' % i])

                    def ev(c0, n, tps, tkey, qi=qi):
                        S.op('act', lambda a: a.activation(out=o_aT[:, c0:c0 + n, qi * 128:(qi + 1) * 128],
                                                           in_=tps[:, 0:n * 128].rearrange("p (a b) -> p a b", b=128),
                                                           func=AF.Identity), r=[tkey], w=['o_aT'])
                    to_feature_major(oat[i], 'oat%d' % i, KC, None, 'o_aT', ev)
                for cb in range(4):
                    wa, wak = load_w(w_ba, 0, KC, cb * 512, 512)
                    wd2, wdk = load_w(w_bd, 0, KC, cb * 512, 512)
                    for qi in range(8):
                        i = qi % 2
                        S.dma('sp', gat[i][:], GA[qi * 128:(qi + 1) * 128, cb * 512:(cb + 1) * 512], w=['gat%d' % i])
                        S.dma('act', gbt[i][:], GB[qi * 128:(qi + 1) * 128, cb * 512:(cb + 1) * 512], w=['gbt%d' % i])
                        psa, pka = next_mm()
                        for kc in range(KC):
                            S.op('pe', lambda p: p.matmul(psa[:, :], lhsT=o_aT[:, kc, qi * 128:(qi + 1) * 128], rhs=wa[:, kc, :],
                                                          start=(kc == 0), stop=(kc == KC - 1)), r=['o_aT', wak], w=[pka])
                        psb, pkb = next_mm()
                        for kc in range(KC):
                            S.op('pe', lambda p: p.matmul(psb[:, :], lhsT=o_bT[:, kc, qi * 128:(qi + 1) * 128], rhs=wd2[:, kc, :],
                                                          start=(kc == 0), stop=(kc == KC - 1)), r=['o_bT', wdk], w=[pkb])
                        S.op('dve', lambda v: v.tensor_tensor(out=m1[:], in0=psa[:, :], in1=gat[i][:], op=ALU.mult),
                             r=[pka, 'gat%d' % i], w=['m1'])
                        S.op('dve', lambda v: v.tensor_tensor(out=m2[:], in0=psb[:, :], in1=gbt[i][:], op=ALU.mult),
                             r=[pkb, 'gbt%d' % i], w=['m2'])
                        S.op('pool', lambda g: g.tensor_tensor(out=mgs[i][:], in0=m1[:], in1=m2[:], op=ALU.add),
                             r=['m1', 'm2'], w=['mgs%d' % i])
                        S.dma('sp', MG[qi * 128:(qi + 1) * 128, cb * 512:(cb + 1) * 512], mgs[i][:], r=['mgs%d' % i], w=[('MG', qi)])
                S.barrier()

        with ExitStack() as px:
            x1 = sb(px, "x1", [128, 8, D], F32)
            rowb = sb(px, "rowb", [128, D], F32)
            with ExitStack() as pg2:
                alloc_wbuf(pg2, 2)
                mergedT = sb(pg2, "mergedT", [128, KC, TOWN], BF16)
                mgt = [sb(pg2, "mgt%d" % i, [128, D], BF16) for i in range(2)]
                xres = [sb(pg2, "xres%d" % i, [128, 512], F32) for i in range(2)]
                tmpf = sb(pg2, "tmpf", [128, 512], F32)
                S.dma('act', rowb[:], modrow_d[0, 2 * D:3 * D].partition_broadcast(128), w=['rowb'])
                for qi in range(8):
                    i = qi % 2
                    S.dma('sp', mgt[i][:], MG[qi * 128:(qi + 1) * 128, :], w=['mgt%d' % i])

                    def ev2(c0, n, tps, tkey, qi=qi):
                        S.op('act', lambda a: a.activation(out=mergedT[:, c0:c0 + n, qi * 128:(qi + 1) * 128],
                                                           in_=tps[:, 0:n * 128].rearrange("p (a b) -> p a b", b=128),
                                                           func=AF.Identity), r=[tkey], w=['mergedT'])
                    to_feature_major(mgt[i], 'mgt%d' % i, KC, None, 'mergedT', ev2)

                def evac_mo(cb):
                    def f(tt, ps, pkey):
                        i = tt % 2
                        S.dma('sp', xres[i][:], xs[TOWN + tt * 128:TOWN + (tt + 1) * 128, cb * 512:(cb + 1) * 512], w=['xres%d' % i])
                        S.op('dve', lambda v: v.tensor_tensor(out=tmpf[:], in0=ps[:, :], in1=rowb[:, cb * 512:(cb + 1) * 512], op=ALU.mult),
                             r=[pkey, 'rowb'], w=['tmpf'])
                        S.op('dve', lambda v: v.tensor_tensor(out=x1[:, tt, cb * 512:(cb + 1) * 512], in0=tmpf[:], in1=xres[i][:], op=ALU.add),
                             r=['tmpf', 'xres%d' % i], w=[('x1', tt)])
                    return f
                for cb in range(4):
                    linear(lambda kc, tt: mergedT[:, kc, tt * 128:(tt + 1) * 128], 'mergedT', w_mo, cb * 512, 512, list(range(8)), evac_mo(cb))
                S.barrier()

            with ExitStack() as ph:
                alloc_wbuf(ph, 2, 11, 512)
                h2T = sb(ph, "h2T", [128, KC, TOWN], BF16)
                xn2 = [sb(ph, "xn2_%d" % i, [128, D], BF16) for i in range(2)]
                actT = sb(ph, "actT", [128, 11, TOWN], BF16)
                sg = [sb(ph, "sg%d" % i, [128, 512], F32) for i in range(2)]
                tmp2 = sb(ph, "tmp2", [128, 512], F32)
                gub = [sb(ph, "gub%d" % i, [128, KC, 128], BF16) for i in range(6)]
                rr['gu'] = 0

                def load_gu(c0):
                    i = rr['gu'] % 6
                    rr['gu'] += 1
                    S.dma('pool', gub[i][:], w_gu[:, c0:c0 + 128].rearrange("(kc p) n -> p kc n", p=128), w=['gub%d' % i])
                    return gub[i], 'gub%d' % i
                S.dma('act', rowb[:], modrow_d[0, 5 * D:6 * D].partition_broadcast(128), w=['rowb'])
                for qi in range(8):
                    i = qi % 2
                    norm_to_fm(x1[:, qi, :], ('x1', qi), h2T, 'h2T', qi * 128, A2, sh2, ['A2', 'modfm'],
                               xn2[i], 'xn2_%d' % i, small[:, 16 + i:17 + i])
                cnt_s = 0
                for fb in range(4):
                    for fc in range(11):
                        f0 = fb * 1408 + fc * 128
                        wg, wgk = load_gu(f0)
                        wu, wuk = load_gu(DFF + f0)
                        for tg in range(2):
                            psg, pkg = next_mm()
                            for kc in range(KC):
                                S.op('pe', lambda p: p.matmul(psg[:, :], lhsT=wg[:, kc, 0:128], rhs=h2T[:, kc, tg * 512:(tg + 1) * 512],
                                                              start=(kc == 0), stop=(kc == KC - 1)), r=['h2T', wgk], w=[pkg])
                            psu, pku = next_mm()
                            for kc in range(KC):
                                S.op('pe', lambda p: p.matmul(psu[:, :], lhsT=wu[:, kc, 0:128], rhs=h2T[:, kc, tg * 512:(tg + 1) * 512],
                                                              start=(kc == 0), stop=(kc == KC - 1)), r=['h2T', wuk], w=[pku])
                            j = cnt_s % 2
                            cnt_s += 1
                            S.op('act', lambda a: a.activation(out=sg[j][:], in_=psg[:, :], func=AF.Silu), r=[pkg], w=['sg%d' % j])
                            S.op('dve', lambda v: v.tensor_tensor(out=actT[:, fc, tg * 512:(tg + 1) * 512], in0=psu[:, :], in1=sg[j][:],
                                                                  op=ALU.mult), r=[pku, 'sg%d' % j], w=['actT'])

                    def evac_dn(cb):
                        def f(tt, ps, pkey):
                            S.op('dve', lambda v: v.tensor_tensor(out=tmp2[:], in0=ps[:, :], in1=rowb[:, cb * 512:(cb + 1) * 512], op=ALU.mult),
                                 r=[pkey, 'rowb'], w=['tmp2'])
                            S.op('pool', lambda g: g.tensor_tensor(out=x1[:, tt, cb * 512:(cb + 1) * 512], in0=x1[:, tt, cb * 512:(cb + 1) * 512],
                                                                   in1=tmp2[:], op=ALU.add), r=['tmp2', ('x1', tt)], w=[('x1', tt)])
                        return f
                    for cb in range(4):
                        linear(lambda kc, tt: actT[:, kc, tt * 128:(tt + 1) * 128], 'actT', w_dn, cb * 512, 512, list(range(8)),
                               evac_dn(cb), r0=fb * 1408, nk=11)
                S.barrier()

            with ExitStack() as pi_:
                ot = [sb(pi_, "ot%d" % i, [128, D], F32) for i in range(2)]
                S.dma('act', rowb[:], fng[0, :].partition_broadcast(128), w=['rowb'])
                for qi in range(8):
                    i = qi % 2
                    ssc = small[:, 20 + i:21 + i]
                    S.op('act', lambda a: a.activation(out=junk[:], in_=x1[:, qi, :], func=AF.Square, accum_out=ssc),
                         r=[('x1', qi)], w=['junk', 'small'])
                    rstd_from_ss(ssc, D, 'small')
                    S.op('dve', lambda v: v.scalar_tensor_tensor(out=ot[i][:], in0=x1[:, qi, :], scalar=ssc, in1=rowb[:],
                                                                 op0=ALU.mult, op1=ALU.mult), r=[('x1', qi), 'small', 'rowb'], w=['ot%d' % i])
                    S.dma('sp', out[qi * 128:(qi + 1) * 128, :], ot[i][:], r=['ot%d' % i], w=[('out', qi)])
                S.barrier()
        S.barrier()
    return nc


def _consts(half):
    c = np.zeros((128, 512), np.float32)
    c[:, 0:128] = np.eye(128, dtype=np.float32)
    j = np.arange(128)
    c[:, 128:256] = (j[:, None] <= j[None, :]).astype(np.float32)
    c[:, 256:384] = np.where(j[None, :] <= j[:, None], 0.0, NEG)
    c[:, 384] = 1.0 if half == 1 else 0.0
    c[:, 385] = 0.0 if half == 1 else NEG
    theta = np.float32(500000.0)
    c[:, 400:416] = np.power(theta, -np.arange(0, 32, 2, dtype=np.float32) / np.float32(32))[None, :]
    c[:, 416:424] = np.power(theta, -np.arange(0, 16, 2, dtype=np.float32) / np.float32(16))[None, :]
    return c


def prep_inputs(inputs, cores=None):
    f = lambda a: np.ascontiguousarray(np.asarray(a))
    x = f(inputs["x"]); c = f(inputs["c"]); pos = f(inputs["positions"]).astype(np.int32)
    shared = {
        "w_ada": f(inputs["w_ada"])[0], "b_ada": f(inputs["b_ada"])[0][None, :], "w_in": f(inputs["w_in"])[0],
        "gate_up": f(inputs["gla_gate_up"])[0], "gate_bias": f(inputs["gla_gate_bias"])[0][None, :],
        "gla_gain": f(inputs["gla_norm_gain"])[0][None, :],
        "w_ba": f(inputs["w_branch_gla"])[0], "w_bd": f(inputs["w_branch_dsa"])[0], "w_mo": f(inputs["w_merge_out"])[0],
        "w_gu": f(inputs["w_ffn_gate_up"])[0], "w_dn": f(inputs["w_ffn_down"])[0],
        "n1g": np.ascontiguousarray(f(inputs["norm1_gain"])[0].reshape(16, 128).T),
        "n2g": np.ascontiguousarray(f(inputs["norm2_gain"])[0].reshape(16, 128).T),
        "fng": f(inputs["final_norm_gain"])[None, :],
    }
    maps = []
    for core in (range(8) if cores is None else cores):
        b, half = core // 2, core % 2
        if half == 1:
            xs = x[b]
            p = pos[b]
        else:
            xs = np.concatenate([np.zeros((TOWN, D), np.float32), x[b, :TOWN]], axis=0)
            p = np.concatenate([pos[b, :TOWN], pos[b, :TOWN]])
        m = dict(shared)
        m["xs"] = np.ascontiguousarray(xs)
        m["cfm"] = np.ascontiguousarray(c[b].reshape(16, 128).T)
        m["posi"] = np.ascontiguousarray(p.reshape(16, 128).T)
        m["cst"] = _consts(half)
        maps.append(m)
    return maps


_NC = None


def kernel(**inputs):
    global _NC
    if _NC is None:
        _NC = build_program()
    maps = prep_inputs(inputs)
    res = run_bass_kernel_spmd(_NC, maps, core_ids=list(range(8)))
    outp = np.zeros((NB, SEQ, D), np.float32)
    for core in range(8):
        b, half = core // 2, core % 2
        outp[b, half * TOWN:(half + 1) * TOWN] = res.results[core]["out"]
    return outp
```

```python
import math
from contextlib import ExitStack

import numpy as np
import concourse.bass as bass
import concourse.mybir as mybir
from concourse.bass_utils import run_bass_kernel_spmd

F32 = mybir.dt.float32
BF16 = mybir.dt.bfloat16
I32 = mybir.dt.int32
AF = mybir.ActivationFunctionType
ALU = mybir.AluOpType
AX = mybir.AxisListType

D = 2048
SEQ = 2048
NB = 4
TOWN = 1024
TALL = 2048
NT = 16
KC = 16
DFF = 5632
EPS = 1e-6
NEG = -1.0e30
TOPK = 256
NBISECT = 22

O_GQ, O_GK, O_GV, O_GR, O_GLR = 0, 1024, 2048, 4096, 6144
O_DQ, O_DK, O_DV = 6160, 8208, 10256
O_IQ, O_IK, O_IW = 12304, 12816, 12880
O_GA, O_GB = 12888, 14936
IN_W = 16984


class Sched:
    def __init__(self, nc, es, ndma=24):
        self.nc = nc
        self.eng = {'pe': nc.tensor, 'act': nc.scalar, 'dve': nc.vector, 'pool': nc.gpsimd, 'sp': nc.sync}
        self.semobj = {}
        for e in ['pe', 'act', 'dve', 'pool']:
            self.semobj[e] = es.enter_context(nc.semaphore('s_' + e))
        self.ndma = ndma
        for i in range(ndma):
            self.semobj[('d', i)] = es.enter_context(nc.semaphore('sd%d' % i))
            self.semobj[('g', i)] = es.enter_context(nc.semaphore('sg%d' % i))
        self.dma_rr_g = 0
        self.cnt = {k: 0 for k in self.semobj}
        self.seen = {e: {} for e in self.eng}
        self.lastw = {}
        self.readers = {}
        self.dma_rr = 0
        self.nwait = 0

    def _wait(self, e, k, v):
        if k == e and e == 'pe':
            return
        if self.seen[e].get(k, 0) >= v:
            return
        self.eng[e].wait_ge(self.semobj[k], v)
        self.seen[e][k] = v
        self.nwait += 1

    def _deps(self, e, r, w):
        for key in r:
            for k, v in self.lastw.get(key, {}).items():
                self._wait(e, k, v)
        for key in w:
            for k, v in self.lastw.get(key, {}).items():
                self._wait(e, k, v)
            for k, v in self.readers.get(key, {}).items():
                self._wait(e, k, v)

    def _record(self, ev, r, w):
        k, v = ev
        for key in r:
            d = self.readers.setdefault(key, {})
            d[k] = max(d.get(k, 0), v)
        for key in w:
            self.lastw[key] = {k: v}
            self.readers[key] = {}

    def op(self, e, fn, r=(), w=()):
        ex = [k for k in r if isinstance(k, str) and k[:2] in ('mm', 'tp', 'ax')]
        if ex:
            w = list(w) + [k for k in ex if k not in w]
        self._deps(e, r, w)
        ins = fn(self.eng[e])
        self.cnt[e] += 1
        ins.then_inc(self.semobj[e], 1)
        self._record((e, self.cnt[e]), r, w)

    def dma(self, q, out, in_, r=(), w=(), **kw):
        if q == 'pool':
            slot = ('g', self.dma_rr_g % self.ndma)
            self.dma_rr_g += 1
        else:
            slot = ('d', self.dma_rr % self.ndma)
            self.dma_rr += 1
        if self.cnt[slot] > 0:
            self._wait(q, slot, self.cnt[slot])
        self._deps(q, r, w)
        ins = self.eng[q].dma_start(out=out, in_=in_, **kw)
        self.cnt[slot] += 16
        ins.then_inc(self.semobj[slot], 16)
        self._record((slot, self.cnt[slot]), r, w)

    def barrier(self):
        for e in self.eng:
            for k in self.semobj:
                if self.cnt[k] > 0:
                    self._wait(e, k, self.cnt[k])
        self.lastw = {}
        self.readers = {}


def build_program(dbg=(), stop=None):
    nc = bass.Bass("TRN2", target_bir_lowering=False)
    import os
    stop = stop or os.environ.get('KSTOP')

    def din(name, shape, dt=F32):
        return nc.dram_tensor(name, list(shape), dt, kind="ExternalInput").ap()

    def dscr(name, shape, dt=BF16):
        kind = "ExternalOutput" if name in dbg else "Internal"
        return nc.dram_tensor(name, list(shape), dt, kind=kind).ap()

    xs = din("xs", [TALL, D])
    cfm = din("cfm", [128, KC])
    posi = din("posi", [128, NT], I32)
    w_ada = din("w_ada", [D, 6 * D])
    b_ada = din("b_ada", [1, 6 * D])
    w_in = din("w_in", [D, IN_W])
    gate_up = din("gate_up", [16, 1024])
    gate_bias = din("gate_bias", [1, 1024])
    gla_gain = din("gla_gain", [1, D])
    w_ba = din("w_ba", [D, D])
    w_bd = din("w_bd", [D, D])
    w_mo = din("w_mo", [D, D])
    w_gu = din("w_gu", [D, 2 * DFF])
    w_dn = din("w_dn", [DFF, D])
    n1g = din("n1g", [128, KC])
    n2g = din("n2g", [128, KC])
    fng = din("fng", [1, D])
    cst = din("cst", [128, 512])
    out = nc.dram_tensor("out", [TOWN, D], F32, kind="ExternalOutput").ap()

    modrow_d = dscr("modrow_d", [1, 6 * D], F32)
    GQ = dscr("GQ", [TOWN, 1024])
    GK = dscr("GK", [TALL, 1024])
    GV = dscr("GV", [TALL, 2048])
    GR = dscr("GR", [TOWN, 2048])
    DQ = dscr("DQ", [TOWN, 2048])
    DK = dscr("DK", [TALL, 2048])
    DV = dscr("DV", [TALL, 2048])
    IQ = dscr("IQ", [TOWN, 512])
    IKW = dscr("IKW", [TALL, 72], F32)
    GA = dscr("GA", [TOWN, 2048])
    GB = dscr("GB", [TOWN, 2048])
    OA = dscr("OA", [TOWN, 2048])
    MG = dscr("MG", [TOWN, 2048])
    OBD = dscr("OBD", [D, TOWN])
    MTD = dscr("MTD", [128, NT, TOWN]) if "MTD" in dbg else None

    with ExitStack() as es:
        S = Sched(nc, es)

        def sb(stack, name, shape, dt):
            return stack.enter_context(nc.sbuf_tensor(name, list(shape), dt))

        mm, tp, ax = [], [], []
        pstack = [None]
        rr = {'mm': 0, 'tp': 0, 'stg': 0, 'wb': 0}

        def alloc_psum(nm, nt, na):
            if pstack[0] is not None:
                pstack[0].close()
            st = ExitStack()
            pstack[0] = st
            rr['pgen'] = rr.get('pgen', 0) + 1
            g = rr['pgen']
            mm[:] = [st.enter_context(nc.psum_tensor("mm%d_%d" % (i, g), [128, 512], F32)) for i in range(nm)]
            tp[:] = [st.enter_context(nc.psum_tensor("tp%d_%d" % (i, g), [128, 1024], BF16)) for i in range(nt)]
            ax[:] = [st.enter_context(nc.psum_tensor("ax%d_%d" % (i, g), [128, 512], F32)) for i in range(na)]

        alloc_psum(4, 2, 2)

        mm_users = {}

        def set_mm_users(**parts):
            mm_users.clear()
            mm_users.update(parts)

        def next_mm(user=None):
            banks = mm_users.get(user) if mm_users else None
            if banks is None:
                banks = list(range(len(mm)))
            c = rr.get(('mm', user), 0)
            rr[('mm', user)] = c + 1
            i = banks[c % len(banks)]
            return mm[i], 'mm%d' % i

        bg = []

        def bg_step():
            for g in list(bg):
                try:
                    next(g)
                except StopIteration:
                    bg.remove(g)

        def bg_drain():
            while bg:
                bg_step()

        def next_tp():
            i = rr['tp'] % len(tp)
            rr['tp'] += 1
            return tp[i], 'tp%d' % i

        cst_t = sb(es, "cst_t", [128, 512], F32)
        S.dma('sp', cst_t[:], cst, w=['cst'])
        identf = cst_t[:, 0:128]
        triu = cst_t[:, 128:256]
        cmask = cst_t[:, 256:384]
        ctxflag = cst_t[:, 384:385]
        ctxneg = cst_t[:, 385:386]
        invf_d = cst_t[:, 400:416]
        invf_i = cst_t[:, 416:424]
        ident = sb(es, "ident", [128, 128], BF16)
        ones_bf = sb(es, "ones_bf", [128, 128], BF16)
        S.op('dve', lambda v: v.tensor_copy(out=ident[:], in_=identf), r=['cst'], w=['ident'])
        S.op('dve', lambda v: v.memset(ones_bf[:], 1.0), w=['ones_bf'])
        modfm = sb(es, "modfm", [128, 96], F32)
        A1 = sb(es, "A1", [128, KC], F32)
        A2 = sb(es, "A2", [128, KC], F32)
        n1g_t = sb(es, "n1g_t", [128, KC], F32)
        n2g_t = sb(es, "n2g_t", [128, KC], F32)
        S.dma('sp', n1g_t[:], n1g, w=['n1g'])
        S.dma('sp', n2g_t[:], n2g, w=['n2g'])
        wbuf = []

        def alloc_wbuf(stack, n, nk=KC, nb=512):
            rr['wgen'] = rr.get('wgen', 0) + 1
            wbuf[:] = [stack.enter_context(nc.sbuf_tensor("wbuf%d_%d" % (i, rr['wgen']), [128, nk, nb], BF16)) for i in range(n)]
        stg = [sb(es, "stg%d" % i, [128, 512], BF16) for i in range(4)]
        small = sb(es, "small", [128, 64], F32)
        junk = sb(es, "junk", [128, 2048], BF16)
        glrT = sb(es, "glrT", [32, TALL], F32)

        def next_stg():
            i = rr['stg'] % 4
            rr['stg'] += 1
            return stg[i], 'stg%d' % i

        def load_w(W, r0, nk, c0, nb, q='pool'):
            i = rr['wb'] % len(wbuf)
            rr['wb'] += 1
            key = 'wbuf%d' % i
            src = W[r0:r0 + nk * 128, c0:c0 + nb].rearrange("(kc p) n -> p kc n", p=128)
            S.dma(q, wbuf[i][:, 0:nk, 0:nb], src, w=[key])
            return wbuf[i], key

        def linear(actT, akey, W, c0, nb, tts, evac, r0=0, nk=KC, m=128, user='c'):
            wb, wkey = load_w(W, r0, nk, c0, nb)
            for tt in tts:
                bg_step()
                ps, pkey = next_mm(user)
                for kc in range(nk):
                    if kc == nk // 2:
                        bg_step()
                    S.op('pe', lambda p: p.matmul(ps[0:m, 0:nb], lhsT=actT(kc, tt), rhs=wb[:, kc, 0:nb],
                                                  start=(kc == 0), stop=(kc == nk - 1)),
                         r=[akey, wkey], w=[pkey])
                evac(tt, ps, pkey)

        def rstd_from_ss(ss_ap, n, key):
            S.op('dve', lambda v: v.tensor_scalar(out=ss_ap, in0=ss_ap, scalar1=1.0 / n, scalar2=EPS,
                                                  op0=ALU.mult, op1=ALU.add), r=[key], w=[key])
            S.op('act', lambda a: a.activation(out=ss_ap, in_=ss_ap, func=AF.Sqrt), r=[key], w=[key])
            S.op('dve', lambda v: v.reciprocal(out=ss_ap, in_=ss_ap), r=[key], w=[key])

        def to_feature_major(src_tile, skey, nchunk, dst_fn, dkey, evac_eng_fn):
            for c0 in range(0, nchunk, 8):
                n = min(8, nchunk - c0)
                tps, tkey = next_tp()
                for c in range(n):
                    S.op('pe', lambda p: p.transpose(out=tps[:, c * 128:(c + 1) * 128],
                                                     in_=src_tile[:, (c0 + c) * 128:(c0 + c + 1) * 128],
                                                     identity=ident[:]),
                         r=[skey, 'ident'], w=[tkey])
                evac_eng_fn(c0, n, tps, tkey)

        sT = sb(es, "sT", [128, KC], BF16)
        with ExitStack() as pa:
            alloc_wbuf(pa, 2)
            c_t = sb(pa, "c_t", [128, KC], F32)
            brow = sb(pa, "brow", [1, 2 * D], F32)
            mrow = sb(pa, "mrow", [1, 2 * D], F32)
            S.dma('sp', c_t[:], cfm, w=['c_t'])
            S.dma('sp', brow[:], b_ada[0:1, 0:2 * D], w=['brow'])
            S.op('act', lambda a: a.activation(out=sT[:], in_=c_t[:], func=AF.Silu), r=['c_t'], w=['sT'])

            def evac_mod(cb):
                def f(tt, ps, pkey):
                    S.op('dve', lambda v: v.tensor_tensor(out=mrow[0:1, cb * 512:(cb + 1) * 512], in0=ps[0:1, :],
                                                          in1=brow[0:1, cb * 512:(cb + 1) * 512], op=ALU.add),
                         r=[pkey, 'brow'], w=['mrow'])
                return f
            for cb in range(8):
                linear(lambda kc, tt: sT[:, kc:kc + 1], 'sT', w_ada, cb * 512, 512, [0], evac_mod(cb), m=1)
            S.dma('sp', modrow_d[0:1, 0:2 * D], mrow[:], r=['mrow'], w=['modrow_d'])
            with nc.allow_non_contiguous_dma(reason="one-time relayout of the modulation vector"):
                S.dma('sp', modfm[:, 0:32], modrow_d[0, 0:2 * D].rearrange("(j p) -> p j", p=128), r=['modrow_d'], w=['modfm'])
            S.op('dve', lambda v: v.scalar_tensor_tensor(out=A1[:], in0=modfm[:, 16:32], scalar=1.0, in1=n1g_t[:],
                                                         op0=ALU.add, op1=ALU.mult), r=['modfm', 'n1g'], w=['A1'])
            S.barrier()
        if stop == 'A':
            return nc
        sh1 = modfm[:, 0:16]
        sh2 = modfm[:, 48:64]

        def norm_to_fm(x_tile, xkey, dstT, dkey, tcol, A, sh, akeys, xn, xnkey, ss_ap):
            S.op('act', lambda a: a.activation(out=junk[:], in_=x_tile, func=AF.Square, accum_out=ss_ap),
                 r=[xkey], w=['junk', 'small'])
            rstd_from_ss(ss_ap, D, 'small')
            S.op('dve', lambda v: v.tensor_scalar(out=xn[:], in0=x_tile, scalar1=ss_ap, scalar2=None, op0=ALU.mult),
                 r=[xkey, 'small'], w=[xnkey])

            def ev(c0, n, tps, tkey):
                for c in range(n):
                    kc = c0 + c
                    S.op('act', lambda a: a.activation(out=dstT[:, kc, tcol:tcol + 128], in_=tps[:, c * 128:(c + 1) * 128],
                                                       func=AF.Identity, scale=A[:, kc:kc + 1], bias=sh[:, kc:kc + 1]),
                         r=[tkey] + akeys, w=[dkey])
            to_feature_major(xn, xnkey, KC, None, dkey, ev)

        s_mask = ExitStack()
        maskT = sb(s_mask, "maskT", [128, NT, TOWN], BF16)
        with ExitStack() as pbc:
            hT = sb(pbc, "hT", [128, KC, TALL], BF16)
            with ExitStack() as pb:
                xt = [sb(pb, "xt%d" % i, [128, D], F32) for i in range(2)]
                xn = [sb(pb, "xn%d" % i, [128, D], BF16) for i in range(2)]
                for tt in range(NT):
                    i = tt % 2
                    S.dma('sp' if i == 0 else 'act', xt[i][:], xs[tt * 128:(tt + 1) * 128, :], w=['xt%d' % i])
                    norm_to_fm(xt[i][:], 'xt%d' % i, hT, 'hT', tt * 128, A1, sh1, ['A1', 'modfm'],
                               xn[i], 'xn%d' % i, small[:, i:i + 1])
                S.barrier()

            with ExitStack() as pc:
                alloc_wbuf(pc, 2)
                alloc_psum(7, 1, 0)
                set_mm_users(c=[0, 1, 2], d=[3, 4], e=[5, 6])
                sinD = sb(pc, "sinD", [128, NT, 1, 16], F32)
                cosD = sb(pc, "cosD", [128, NT, 1, 16], F32)
                sinI = sb(pc, "sinI", [128, NT, 1, 8], F32)
                cosI = sb(pc, "cosI", [128, NT, 1, 8], F32)
                ggs = [sb(pc, "ggs%d" % i, [128, 512], F32) for i in range(2)]
                rt = [sb(pc, "rt%d" % i, [128, 4, 16], F32) for i in range(4)]
                f32stg = sb(pc, "f32stg", [128, 512], F32)
                f32stg2 = sb(pc, "f32stg2", [128, 72], F32)
                rsl = [sb(pc, "rsl%d" % i, [128, 128], F32) for i in range(2)]
                ptab = ExitStack()
                posf = sb(ptab, "posf", [128, NT], F32)
                pos_i = sb(ptab, "pos_i", [128, NT], I32)
                ang = sb(ptab, "ang", [128, NT, 16], F32)
                kf = sb(ptab, "kf", [128, NT, 16], F32)
                ki = sb(ptab, "ki", [128, NT, 16], I32)
                kf2 = sb(ptab, "kf2", [128, NT, 16], F32)
                S.dma('sp', pos_i[:], posi, w=['pos_i'])
                S.op('dve', lambda v: v.tensor_copy(out=posf[:], in_=pos_i[:]), r=['pos_i'], w=['posf'])
                TWO_PI = 2.0 * math.pi

                def make_tables(invf, nj, sin_t, cos_t, key):
                    for tt in range(NT):
                        S.op('dve', lambda v: v.tensor_scalar(out=ang[:, tt, 0:nj], in0=invf, scalar1=posf[:, tt:tt + 1],
                                                              scalar2=None, op0=ALU.mult), r=['cst', 'posf', 'ang'], w=['ang'])
                    a = ang[:, :, 0:nj]
                    kk = kf[:, :, 0:nj]
                    mm_ = kf2[:, :, 0:nj]
                    S.op('dve', lambda v: v.tensor_scalar(out=kk, in0=a, scalar1=1.0 / TWO_PI, scalar2=None,
                                                          op0=ALU.mult), r=['ang'], w=['kf'])
                    S.op('dve', lambda v: v.tensor_copy(out=ki[:, :, 0:nj], in_=kk), r=['kf'], w=['ki'])
                    S.op('dve', lambda v: v.tensor_copy(out=kk, in_=ki[:, :, 0:nj]), r=['ki'], w=['kf'])
                    S.op('dve', lambda v: v.scalar_tensor_tensor(out=a, in0=kk, scalar=-TWO_PI, in1=a,
                                                                 op0=ALU.mult, op1=ALU.add), r=['kf', 'ang'], w=['ang'])
                    for shift, dst in ((0.0, sin_t), (math.pi / 2, cos_t)):
                        S.op('dve', lambda v: v.tensor_scalar(out=kk, in0=a, scalar1=shift, scalar2=None,
                                                              op0=ALU.add), r=['ang', 'kf'], w=['kf'])
                        for cmp, bound, sgn in ((ALU.is_gt, math.pi, -1.0), (ALU.is_lt, -math.pi, 1.0)):
                            S.op('dve', lambda v: v.tensor_scalar(out=mm_, in0=kk, scalar1=bound, scalar2=sgn * TWO_PI,
                                                                  op0=cmp, op1=ALU.mult), r=['kf'], w=['kf2'])
                            S.op('dve', lambda v: v.tensor_tensor(out=kk, in0=kk, in1=mm_, op=ALU.add),
                                 r=['kf', 'kf2'], w=['kf'])
                        S.op('act', lambda a_: a_.activation(out=dst[:, :, 0, :], in_=kk, func=AF.Sin),
                             r=['kf'], w=[key])
                make_tables(invf_d, 16, sinD, cosD, 'tabD')
                make_tables(invf_i, 8, sinI, cosI, 'tabI')
                S.barrier()
                ptab.close()
                S.op('dve', lambda v: v.memset(glrT[:, :], 1.0), w=['glrT'])
                wb, wkey = load_w(w_in, 0, KC, O_GLR, 16)
                for tg in range(4):
                    ps, pkey = next_mm()
                    for kc in range(KC):
                        S.op('pe', lambda p: p.matmul(ps[0:16, :], lhsT=wb[:, kc, 0:16], rhs=hT[:, kc, tg * 512:(tg + 1) * 512],
                                                      start=(kc == 0), stop=(kc == KC - 1)), r=['hT', wkey], w=[pkey])
                    S.op('act', lambda a: a.activation(out=glrT[0:16, tg * 512:(tg + 1) * 512], in_=ps[0:16, :], func=AF.Identity),
                         r=[pkey], w=['glrT'])

                if stop == 'C1':
                    S.barrier()
                    return nc
                own = list(range(8, 16))
                allt = list(range(NT))
                hact = lambda kc, tt: hT[:, kc, tt * 128:(tt + 1) * 128]

                def store(dst, own_only, c0, nb):
                    def f(tt, ps, pkey):
                        st, skey = next_stg()
                        S.op('act', lambda a: a.activation(out=st[:, 0:nb], in_=ps[:, 0:nb], func=AF.Identity), r=[pkey], w=[skey])
                        row = (tt - 8 if own_only else tt) * 128
                        S.dma('sp', dst[row:row + 128, c0:c0 + nb], st[:, 0:nb], r=[skey], w=[(id(dst), tt)])
                    return f

                def store_act(dst, c0, nb, func, mul=None):
                    def f(tt, ps, pkey):
                        st, skey = next_stg()
                        if mul is None:
                            S.op('act', lambda a: a.activation(out=st[:, 0:nb], in_=ps[:, 0:nb], func=func), r=[pkey], w=[skey])
                        else:
                            S.op('act', lambda a: a.activation(out=f32stg[:, 0:nb], in_=ps[:, 0:nb], func=func), r=[pkey], w=['f32stg'])
                            S.op('dve', lambda v: v.tensor_tensor(out=st[:, 0:nb], in0=f32stg[:, 0:nb], in1=mul[0][:, 0:nb],
                                                                  op=ALU.mult), r=['f32stg', mul[1]], w=[skey])
                        row = (tt - 8) * 128
                        S.dma('sp', dst[row:row + 128, c0:c0 + nb], st[:, 0:nb], r=[skey], w=[(id(dst), tt)])
                    return f

                def rope_ops(x1, x2, o1, o2, cs, sn, pkey, skey, tkey, shape):
                    t = [rt[i][:].rearrange("p a b -> p (a b)")[:, 0:shape[0] * shape[1]].rearrange("p (a b) -> p a b", b=shape[1])
                         for i in range(4)]
                    S.op('dve', lambda v: v.tensor_tensor(out=t[0], in0=x1, in1=cs, op=ALU.mult), r=[pkey, tkey], w=['rt0'])
                    S.op('dve', lambda v: v.tensor_tensor(out=t[1], in0=x2, in1=sn, op=ALU.mult), r=[pkey, tkey], w=['rt1'])
                    S.op('dve', lambda v: v.tensor_tensor(out=o1, in0=t[0], in1=t[1], op=ALU.subtract), r=['rt0', 'rt1'], w=[skey])
                    S.op('dve', lambda v: v.tensor_tensor(out=t[2], in0=x1, in1=sn, op=ALU.mult), r=[pkey, tkey], w=['rt2'])
                    S.op('dve', lambda v: v.tensor_tensor(out=t[3], in0=x2, in1=cs, op=ALU.mult), r=[pkey, tkey], w=['rt3'])
                    S.op('dve', lambda v: v.tensor_tensor(out=o2, in0=t[2], in1=t[3], op=ALU.add), r=['rt2', 'rt3'], w=[skey])

                def store_rope_d(dst, own_only, c0):
                    def f(tt, ps, pkey):
                        st, skey = next_stg()
                        S.op('act', lambda a: a.activation(out=st[:, :], in_=ps[:, :], func=AF.Identity), r=[pkey], w=[skey])
                        pv = ps[:, :].rearrange("p (h d) -> p h d", d=128)
                        sv = st[:, :].rearrange("p (h d) -> p h d", d=128)
                        j = rr.get('rsl', 0) % 2
                        rr['rsl'] = rr.get('rsl', 0) + 1
                        rv = rsl[j][:, :].rearrange("p (h d) -> p h d", d=32)
                        S.op('act', lambda a: a.activation(out=rv, in_=pv[:, :, 0:32], func=AF.Identity), r=[pkey], w=['rsl%d' % j])
                        rope_ops(rv[:, :, 0:16], rv[:, :, 16:32], sv[:, :, 0:16], sv[:, :, 16:32],
                                 cosD[:, tt, :, :].to_broadcast([128, 4, 16]), sinD[:, tt, :, :].to_broadcast([128, 4, 16]), 'rsl%d' % j, skey, 'tabD', (4, 16))
                        row = (tt - 8 if own_only else tt) * 128
                        S.dma('sp', dst[row:row + 128, c0:c0 + 512], st[:, :], r=[skey], w=[(id(dst), tt)])
                    return f

                def store_iq(tt, ps, pkey):
                    st, skey = next_stg()
                    S.op('act', lambda a: a.activation(out=st[:, :], in_=ps[:, :], func=AF.Identity), r=[pkey], w=[skey])
                    pv = ps[:, :].rearrange("p (h d) -> p h d", d=64)
                    sv = st[:, :].rearrange("p (h d) -> p h d", d=64)
                    j = rr.get('rsl', 0) % 2
                    rr['rsl'] = rr.get('rsl', 0) + 1
                    rv = rsl[j][:, :].rearrange("p (h d) -> p h d", d=16)
                    S.op('act', lambda a: a.activation(out=rv, in_=pv[:, :, 0:16], func=AF.Identity), r=[pkey], w=['rsl%d' % j])
                    rope_ops(rv[:, :, 0:8], rv[:, :, 8:16], sv[:, :, 0:8], sv[:, :, 8:16],
                             cosI[:, tt, :, :].to_broadcast([128, 8, 8]), sinI[:, tt, :, :].to_broadcast([128, 8, 8]), 'rsl%d' % j, skey, 'tabI', (8, 8))
                    row = (tt - 8) * 128
                    S.dma('sp', IQ[row:row + 128, :], st[:, :], r=[skey], w=[('IQ', tt)])

                def store_ikw(tt, ps, pkey):
                    S.op('act', lambda a: a.activation(out=f32stg2[:, :], in_=ps[:, 0:72], func=AF.Identity), r=[pkey], w=['f32stg2'])
                    j = rr.get('rsl', 0) % 2
                    rr['rsl'] = rr.get('rsl', 0) + 1
                    S.op('act', lambda a: a.activation(out=rsl[j][:, 0:16], in_=ps[:, 0:16], func=AF.Identity), r=[pkey], w=['rsl%d' % j])
                    pkey = 'rsl%d' % j
                    rope_ops(rsl[j][:, 0:8].rearrange("p (a b) -> p a b", a=1), rsl[j][:, 8:16].rearrange("p (a b) -> p a b", a=1),
                             f32stg2[:, 0:8].rearrange("p (a b) -> p a b", a=1), f32stg2[:, 8:16].rearrange("p (a b) -> p a b", a=1),
                             cosI[:, tt, :, :], sinI[:, tt, :, :], pkey, 'f32stg2', 'tabI', (1, 8))
                    S.dma('sp', IKW[tt * 128:(tt + 1) * 128, :], f32stg2[:, :], r=['f32stg2'], w=[('IKW', tt)])

                def gen_D():
                    gu_aug = sb(pc, "gu_aug", [32, 1024], F32)
                    S.dma('sp', gu_aug[0:16, :], gate_up, w=['gu_aug'])
                    S.dma('sp', gu_aug[16:17, :], gate_bias, w=['gu_aug'])
                    Sst = sb(pc, "Sst", [128, 2, 512], F32)
                    Sbf = sb(pc, "Sbf", [128, 2, 512], BF16)
                    kt_ = [sb(pc, "kt%d" % i, [128, 256], BF16) for i in range(2)]
                    vt_ = [sb(pc, "vt%d" % i, [128, 512], BF16) for i in range(2)]
                    qt_ = [sb(pc, "qt%d" % i, [128, 256], BF16) for i in range(2)]
                    gs_ = [sb(pc, "gs%d" % i, [128, 512], BF16) for i in range(2)]
                    sp_ = sb(pc, "sp_", [128, 256], F32)
                    Epos = sb(pc, "Epos", [128, 256], F32)
                    Eneg = sb(pc, "Eneg", [128, 256], F32)
                    Etok = sb(pc, "Etok", [128, 256], F32)
                    ktok = sb(pc, "ktok", [128, 256], BF16)
                    kT_ = sb(pc, "kT_", [128, 256], BF16)
                    qT_ = sb(pc, "qT_", [128, 256], BF16)
                    attnT = sb(pc, "attnT", [128, 128], BF16)
                    junkD = sb(pc, "junkD", [128, 512], BF16)
                    oa_ = [sb(pc, "oa%d" % i, [128, 512], BF16) for i in range(2)]
                    for h in range(4):
                        S.op('dve', lambda v: v.memset(Sst[:], 0.0), w=['Sst'])
                        S.op('dve', lambda v: v.memset(Sbf[:], 0.0), w=['Sbf'])
                        for n in range(NT):
                            i = n % 2
                            ownt = n >= 8
                            r0 = n * 128
                            S.dma('sp', kt_[i][:], GK[r0:r0 + 128, h * 256:(h + 1) * 256], r=[(id(GK), n)], w=['kt%d' % i])
                            S.dma('act', vt_[i][:], GV[r0:r0 + 128, h * 512:(h + 1) * 512], r=[(id(GV), n)], w=['vt%d' % i])
                            if ownt:
                                q0 = (n - 8) * 128
                                S.dma('sp', qt_[i][:], GQ[q0:q0 + 128, h * 256:(h + 1) * 256], r=[(id(GQ), n)], w=['qt%d' % i])
                                S.dma('act', gs_[i][:], GR[q0:q0 + 128, h * 512:(h + 1) * 512], r=[(id(GR), n)], w=['gs%d' % i])
                            ps, pk = next_mm('d')
                            S.op('pe', lambda p: p.matmul(ps[:, 0:256], lhsT=glrT[0:17, r0:r0 + 128], rhs=gu_aug[0:17, h * 256:(h + 1) * 256],
                                                          start=True, stop=True), r=['glrT', 'gu_aug'], w=[pk])
                            S.op('act', lambda a: a.activation(out=sp_[:], in_=ps[:, 0:256], func=AF.Exp, scale=-1.0), r=[pk], w=['sp_'])
                            S.op('act', lambda a: a.activation(out=sp_[:], in_=sp_[:], func=AF.Ln, bias=1.0), r=['sp_'], w=['sp_'])
                            yield
                            ps2, pk2 = next_mm('d')
                            for cc in range(2):
                                S.op('pe', lambda p: p.matmul(ps2[:, cc * 128:(cc + 1) * 128], lhsT=sp_[:, cc * 128:(cc + 1) * 128], rhs=triu,
                                                              start=True, stop=True), r=['sp_', 'cst'], w=[pk2])
                            ps3, pk3 = next_mm('d')
                            S.op('pe', lambda p: p.matmul(ps3[:, 0:256], lhsT=triu, rhs=sp_[:], start=True, stop=True), r=['sp_', 'cst'], w=[pk3])
                            S.op('act', lambda a: a.activation(out=Epos[:], in_=ps2[:, 0:256], func=AF.Exp, scale=-1.0 / 16), r=[pk2], w=['Epos'])
                            S.op('act', lambda a: a.activation(out=Eneg[:], in_=ps2[:, 0:256], func=AF.Exp, scale=1.0 / 16), r=[pk2], w=['Eneg'])
                            S.op('act', lambda a: a.activation(out=Etok[:], in_=ps3[:, 0:256], func=AF.Exp, scale=1.0 / 16), r=[pk3], w=['Etok'])
                            yield
                            tpsf, tk = next_mm('d')
                            tps = tpsf[:, :].bitcast(BF16)
                            for cc in range(2):
                                S.op('pe', lambda p: p.transpose(out=tps[:, cc * 128:(cc + 1) * 128], in_=kt_[i][:, cc * 128:(cc + 1) * 128],
                                                                 identity=ident[:]), r=['kt%d' % i, 'ident'], w=[tk])
                            if ownt:
                                for cc in range(2):
                                    S.op('pe', lambda p: p.transpose(out=tps[:, 256 + cc * 128:256 + (cc + 1) * 128],
                                                                     in_=qt_[i][:, cc * 128:(cc + 1) * 128], identity=ident[:]),
                                         r=['qt%d' % i, 'ident'], w=[tk])
                            yield
                            S.op('dve', lambda v: v.tensor_tensor(out=kT_[:], in0=tps[:, 0:256], in1=Eneg[:], op=ALU.mult),
                                 r=[tk, 'Eneg'], w=['kT_'])
                            S.op('pool', lambda g: g.tensor_tensor(out=ktok[:], in0=kt_[i][:], in1=Etok[:], op=ALU.mult),
                                 r=['kt%d' % i, 'Etok'], w=['ktok'])
                            if ownt:
                                S.op('dve', lambda v: v.scalar_tensor_tensor(out=qT_[:], in0=tps[:, 256:512], scalar=1.0 / 16, in1=Epos[:],
                                                                             op0=ALU.mult, op1=ALU.mult), r=[tk, 'Epos'], w=['qT_'])
                                yield
                                psA, pkA = next_mm('d')
                                for cc in range(2):
                                    S.op('pe', lambda p: p.matmul(psA[:, 0:128], lhsT=kT_[:, cc * 128:(cc + 1) * 128],
                                                                  rhs=qT_[:, cc * 128:(cc + 1) * 128], start=(cc == 0), stop=(cc == 1)),
                                         r=['kT_', 'qT_'], w=[pkA])
                                S.op('dve', lambda v: v.tensor_tensor(out=attnT[:], in0=psA[:, 0:128], in1=triu, op=ALU.mult),
                                     r=[pkA, 'cst'], w=['attnT'])
                                yield
                                psO, pkO = next_mm('d')
                                S.op('pe', lambda p: p.matmul(psO[:, :], lhsT=attnT[:], rhs=vt_[i][:], start=True, stop=False),
                                     r=['attnT', 'vt%d' % i], w=[pkO])
                                for cc in range(2):
                                    S.op('pe', lambda p: p.matmul(psO[:, :], lhsT=qT_[:, cc * 128:(cc + 1) * 128], rhs=Sbf[:, cc, :],
                                                                  start=False, stop=(cc == 1)), r=['qT_', 'Sbf'], w=[pkO])
                                ssc = small[:, 4 + i:5 + i]
                                S.op('act', lambda a: a.activation(out=junkD[:], in_=psO[:, :], func=AF.Square, accum_out=ssc),
                                     r=[pkO], w=['junkD', 'smallD'])
                                rstd_from_ss(ssc, 512, 'smallD')
                                S.op('dve', lambda v: v.scalar_tensor_tensor(out=oa_[i][:], in0=psO[:, :], scalar=ssc, in1=gs_[i][:],
                                                                             op0=ALU.mult, op1=ALU.mult),
                                     r=[pkO, 'smallD', 'gs%d' % i], w=['oa%d' % i])
                                S.dma('sp', OA[q0:q0 + 128, h * 512:(h + 1) * 512], oa_[i][:], r=['oa%d' % i], w=[('OA', n)])
                            yield
                            for cc in range(2):
                                psU, pkU = next_mm('d')
                                S.op('pe', lambda p: p.matmul(psU[:, :], lhsT=ktok[:, cc * 128:(cc + 1) * 128], rhs=vt_[i][:],
                                                              start=True, stop=True), r=['ktok', 'vt%d' % i], w=[pkU])
                                S.op('dve', lambda v: v.tensor_tensor(out=Sst[:, cc, :], in0=psU[:, :], in1=Sst[:, cc, :], op=ALU.add),
                                     r=[pkU, 'Sst'], w=['Sst'])
                                S.op('dve', lambda v: v.tensor_scalar(out=Sst[:, cc, :], in0=Sst[:, cc, :],
                                                                      scalar1=Epos[:, cc * 128 + 127:cc * 128 + 128], scalar2=None, op0=ALU.mult),
                                     r=['Sst', 'Epos'], w=['Sst'])
                                if n == 7:
                                    S.op('dve', lambda v: v.tensor_scalar(out=Sst[:, cc, :], in0=Sst[:, cc, :], scalar1=ctxflag, scalar2=None,
                                                                          op0=ALU.mult), r=['Sst', 'cst'], w=['Sst'])
                                S.op('act', lambda a: a.activation(out=Sbf[:, cc, :], in_=Sst[:, cc, :], func=AF.Identity), r=['Sst'], w=['Sbf'])
                                yield


                def gen_E():
                    ikT2 = sb(pc, "ikT2", [128, TALL], BF16)
                    ikf = sb(pc, "ikf", [128, 72], F32)
                    ikd = sb(pc, "ikd", [128, 128], BF16)
                    iqs = sb(pc, "iqs", [128, 512], BF16)
                    iqT = sb(pc, "iqT", [128, 4, 128], BF16)
                    iwp = sb(pc, "iwp", [128, 8], F32)
                    score = sb(pc, "score", [128, TALL], F32)
                    relu_t = [sb(pc, "relu%d" % i, [128, 512], F32) for i in range(2)]
                    mask_tm = sb(pc, "mask_tm", [128, TALL], BF16)
                    S.op('dve', lambda v: v.memset(maskT[:], 0.0), w=['maskT'])
                    for kt in range(NT):
                        S.dma('sp', ikf[:], IKW[kt * 128:(kt + 1) * 128, :], r=[('IKW', kt)], w=['ikf'])
                        S.op('dve', lambda v: v.tensor_copy(out=ikd[:, 0:64], in_=ikf[:, 0:64]), r=['ikf'], w=['ikd'])
                        S.op('dve', lambda v: v.tensor_copy(out=ikd[:, 64:128], in_=ikf[:, 0:64]), r=['ikf'], w=['ikd'])
                        tps, tk = next_tp()
                        S.op('pe', lambda p: p.transpose(out=tps[:, 0:128], in_=ikd[:], identity=ident[:]), r=['ikd', 'ident'], w=[tk])
                        S.op('act', lambda a: a.activation(out=ikT2[:, kt * 128:(kt + 1) * 128], in_=tps[:, 0:128], func=AF.Identity),
                             r=[tk], w=['ikT2'])
                        yield
                    lo, hw, mid, cntv, gev, am = [small[:, 8 + j:9 + j] for j in range(6)]
                    hwtab = small[:, 24:24 + NBISECT + 1]
                    pow2row = cst_t[:, 430:430 + NBISECT + 1]
                    for qi in range(8):
                        tt = 8 + qi
                        nk = 1024 + 128 * (qi + 1)
                        S.dma('sp', iqs[:], IQ[qi * 128:(qi + 1) * 128, :], r=[('IQ', tt)], w=['iqs'])
                        S.dma('act', ikf[:], IKW[tt * 128:(tt + 1) * 128, :], r=[('IKW', tt)], w=['ikf'])
                        yield
                        tps, tk = next_tp()
                        for c in range(4):
                            S.op('pe', lambda p: p.transpose(out=tps[:, c * 128:(c + 1) * 128], in_=iqs[:, c * 128:(c + 1) * 128],
                                                             identity=ident[:]), r=['iqs', 'ident'], w=[tk])
                        S.op('act', lambda a: a.activation(out=iqT[:].rearrange("p a b -> p (a b)"), in_=tps[:, 0:512], func=AF.Identity),
                             r=[tk], w=['iqT'])
                        S.op('dve', lambda v: v.tensor_scalar(out=iwp[:], in0=ikf[:, 64:72], scalar1=float(8 ** -0.5 * 64 ** -0.5),
                                                              scalar2=None, op0=ALU.mult), r=['ikf'], w=['iwp'])
                        ng = (nk + 511) // 512
                        for g in range(ng):
                            wd_ = min(512, nk - g * 512)
                            for hh in range(8):
                                ps, pk = next_mm('e')
                                pb_ = (hh % 2) * 64
                                S.op('pe', lambda p: p.matmul(ps[:, 0:wd_], lhsT=iqT[pb_:pb_ + 64, hh // 2, :],
                                                              rhs=ikT2[pb_:pb_ + 64, g * 512:g * 512 + wd_], start=True, stop=True),
                                     r=['iqT', 'ikT2'], w=[pk])
                                rl = relu_t[hh % 2]
                                rk = 'relu%d' % (hh % 2)
                                S.op('act', lambda a: a.activation(out=rl[:, 0:wd_], in_=ps[:, 0:wd_], func=AF.Relu), r=[pk], w=[rk])
                                sc_ = score[:, g * 512:g * 512 + wd_]
                                if hh == 0:
                                    S.op('dve', lambda v: v.tensor_scalar(out=sc_, in0=rl[:, 0:wd_], scalar1=iwp[:, 0:1], scalar2=None,
                                                                          op0=ALU.mult), r=[rk, 'iwp'], w=['score'])
                                else:
                                    S.op('dve', lambda v: v.scalar_tensor_tensor(out=sc_, in0=rl[:, 0:wd_], scalar=iwp[:, hh:hh + 1],
                                                                                 in1=sc_, op0=ALU.mult, op1=ALU.add),
                                         r=[rk, 'iwp', 'score'], w=['score'])
                                yield
                        S.op('dve', lambda v: v.tensor_reduce(out=am, in_=score[:, 0:nk], axis=AX.X, op=ALU.max,
                                                              apply_absolute_value=True), r=['score'], w=['smallE'])
                        S.op('dve', lambda v: v.tensor_scalar(out=score[:, 0:1024], in0=score[:, 0:1024], scalar1=ctxneg, scalar2=None,
                                                              op0=ALU.add), r=['score', 'cst'], w=['score'])
                        S.op('dve', lambda v: v.tensor_tensor(out=score[:, nk - 128:nk], in0=score[:, nk - 128:nk], in1=cmask, op=ALU.add),
                             r=['score', 'cst'], w=['score'])
                        S.op('dve', lambda v: v.tensor_scalar(out=hw, in0=am, scalar1=1.0001, scalar2=1e-20, op0=ALU.mult, op1=ALU.add),
                             r=['smallE'], w=['smallE'])
                        S.op('dve', lambda v: v.tensor_scalar(out=hwtab, in0=pow2row, scalar1=hw, scalar2=None, op0=ALU.mult),
                             r=['smallE', 'cst'], w=['smallE'])
                        S.op('dve', lambda v: v.tensor_scalar(out=mid, in0=hw, scalar1=0.0, scalar2=None, op0=ALU.mult),
                             r=['smallE'], w=['smallE'])
                        for it in range(NBISECT):
                            S.op('dve', lambda v: v.tensor_scalar(out=junk[:, 0:nk], in0=score[:, 0:nk], scalar1=mid, scalar2=None,
                                                                  op0=ALU.is_ge, op1=ALU.add, accum_out=cntv),
                                 r=['score', 'smallE'], w=['junk', 'smallE'])
                            S.op('dve', lambda v: v.tensor_scalar(out=gev, in0=cntv, scalar1=TOPK - 0.5, scalar2=0.5,
                                                                  op0=ALU.is_ge, op1=ALU.subtract), r=['smallE'], w=['smallE'])
                            S.op('dve', lambda v: v.scalar_tensor_tensor(out=mid, in0=gev, scalar=hwtab[:, it:it + 1], in1=mid,
                                                                         op0=ALU.mult, op1=ALU.add), r=['smallE'], w=['smallE'])
                            yield
                        S.op('dve', lambda v: v.tensor_tensor(out=lo, in0=mid, in1=hwtab[:, NBISECT:NBISECT + 1], op=ALU.subtract),
                             r=['smallE'], w=['smallE'])
                        S.op('dve', lambda v: v.tensor_scalar(out=mask_tm[:, 0:nk], in0=score[:, 0:nk], scalar1=lo, scalar2=None,
                                                              op0=ALU.is_ge), r=['score', 'smallE'], w=['mask_tm'])
                        nkb = nk // 128
                        for c0 in range(0, nkb, 8):
                            n_ = min(8, nkb - c0)
                            yield
                            tps, tk = next_tp()
                            for c in range(n_):
                                S.op('pe', lambda p: p.transpose(out=tps[:, c * 128:(c + 1) * 128],
                                                                 in_=mask_tm[:, (c0 + c) * 128:(c0 + c + 1) * 128], identity=ident[:]),
                                     r=['mask_tm', 'ident'], w=[tk])
                            S.op('act', lambda a: a.activation(out=maskT[:, c0:c0 + n_, qi * 128:(qi + 1) * 128],
                                                               in_=tps[:, 0:n_ * 128].rearrange("p (a b) -> p a b", b=128),
                                                               func=AF.Identity), r=[tk], w=['maskT'])
                            yield
                    yield

                linear(hact, 'hT', w_in, O_IQ, 512, own, store_iq)
                linear(hact, 'hT', w_in, O_IK, 72, allt, store_ikw)
                bg.append(gen_E())
                for cb in range(2):
                    linear(hact, 'hT', w_in, O_GQ + cb * 512, 512, own, store(GQ, True, cb * 512, 512))
                for cb in range(2):
                    linear(hact, 'hT', w_in, O_GK + cb * 512, 512, allt, store(GK, False, cb * 512, 512))
                for cb in range(4):
                    linear(hact, 'hT', w_in, O_GV + cb * 512, 512, allt, store(GV, False, cb * 512, 512))
                for cb in range(4):
                    gg = ggs[cb % 2]
                    S.dma('act', gg[:], gla_gain[0, cb * 512:(cb + 1) * 512].partition_broadcast(128), w=['ggs%d' % (cb % 2)])
                    linear(hact, 'hT', w_in, O_GR + cb * 512, 512, own, store_act(GR, cb * 512, 512, AF.Silu, mul=(gg, 'ggs%d' % (cb % 2))))
                bg.append(gen_D())
                for cb in range(4):
                    linear(hact, 'hT', w_in, O_DQ + cb * 512, 512, own, store_rope_d(DQ, True, cb * 512))
                for cb in range(4):
                    linear(hact, 'hT', w_in, O_DK + cb * 512, 512, allt, store_rope_d(DK, False, cb * 512))
                for cb in range(4):
                    linear(hact, 'hT', w_in, O_DV + cb * 512, 512, allt, store(DV, False, cb * 512, 512))
                for cb in range(4):
                    linear(hact, 'hT', w_in, O_GA + cb * 512, 512, own, store_act(GA, cb * 512, 512, AF.Sigmoid))
                for cb in range(4):
                    linear(hact, 'hT', w_in, O_GB + cb * 512, 512, own, store_act(GB, cb * 512, 512, AF.Sigmoid))
                bg_drain()
                S.barrier()
                if MTD is not None:
                    S.dma('sp', MTD, maskT[:], r=['maskT'], w=['MTD'])
                    S.barrier()
                if stop == 'C':
                    return nc
                set_mm_users()
        with ExitStack() as pefg:
            with ExitStack() as pef:

                with ExitStack() as pf:
                    kTg = sb(pf, "kTg", [128, 4, TALL], BF16)
                    vg = sb(pf, "vg", [128, NT, 512], BF16)
                    qTg = sb(pf, "qTg", [128, 4, TOWN], BF16)
                    ldt = [sb(pf, "ldt%d" % i, [128, 512], BF16) for i in range(2)]
                    pt_ = [sb(pf, "pt%d" % i, [128, 512], BF16) for i in range(4)]
                    pm_ = [sb(pf, "pm%d" % i, [128, 512], BF16) for i in range(4)]
                    alloc_psum(3, 1, 4)
                    lnd = sb(pf, "lnd", [128, 512], F32)
                    obs = [sb(pf, "obs%d" % i, [128, 512], BF16) for i in range(2)]
                    alloc_wbuf(pf, 2)
                    browF = [sb(pf, "browF%d" % i, [1, 512], F32) for i in range(2)]
                    mrowF = [sb(pf, "mrowF%d" % i, [1, 512], F32) for i in range(2)]

                    def gen_modrest():
                        for cb in range(8, 24):
                            j = cb % 2
                            S.dma('act', browF[j][:], b_ada[0:1, cb * 512:(cb + 1) * 512], w=['browF%d' % j])
                            wb, wkey = load_w(w_ada, 0, KC, cb * 512, 512)
                            yield
                            tpf = tp[0][:, :].bitcast(F32)
                            for kc in range(KC):
                                S.op('pe', lambda p: p.matmul(tpf[0:1, :], lhsT=sT[:, kc:kc + 1], rhs=wb[:, kc, :],
                                                              start=(kc == 0), stop=(kc == KC - 1)), r=['sT', wkey], w=['tp0'])
                            S.op('dve', lambda v: v.tensor_tensor(out=mrowF[j][:], in0=tpf[0:1, :], in1=browF[j][:], op=ALU.add),
                                 r=['tp0', 'browF%d' % j], w=['mrowF%d' % j])
                            S.dma('act', modrow_d[0:1, cb * 512:(cb + 1) * 512], mrowF[j][:], r=['mrowF%d' % j], w=[('modrow_d', cb)])
                            yield
                    bg.append(gen_modrest())
                    rden = sb(pf, "rden", [128, 512], F32)
                    for hg in range(4):
                        S.dma('act', vg[:], DV[:, hg * 512:(hg + 1) * 512].rearrange("(kt p) c -> p kt c", p=128), w=['vg'])
                        for kt in range(NT + 8):
                            i = kt % 2
                            if kt < NT:
                                S.dma('sp', ldt[i][:], DK[kt * 128:(kt + 1) * 128, hg * 512:(hg + 1) * 512], w=['ldt%d' % i])
                                dst = kTg[:, :, kt * 128:(kt + 1) * 128]
                                dk_ = 'kTg'
                            else:
                                qi = kt - NT
                                S.dma('sp', ldt[i][:], DQ[qi * 128:(qi + 1) * 128, hg * 512:(hg + 1) * 512], w=['ldt%d' % i])
                                dst = qTg[:, :, qi * 128:(qi + 1) * 128]
                                dk_ = 'qTg'
                            tps, tk = next_tp()
                            for c in range(4):
                                S.op('pe', lambda p: p.transpose(out=tps[:, c * 128:(c + 1) * 128], in_=ldt[i][:, c * 128:(c + 1) * 128],
                                                                 identity=ident[:]), r=['ldt%d' % i, 'ident'], w=[tk])
                            S.op('act' if kt % 2 == 0 else 'dve',
                                 (lambda a: a.activation(out=dst, in_=tps[:, 0:512].rearrange("p (a b) -> p a b", b=128), func=AF.Identity))
                                 if kt % 2 == 0 else
                                 (lambda v: v.tensor_copy(out=dst, in_=tps[:, 0:512].rearrange("p (a b) -> p a b", b=128))),
                                 r=[tk], w=[dk_])
                        steps = [(hh, qg, kb) for hh in range(4) for qg in range(2) for kb in range(8 + 4 * (qg + 1))]
                        LA = 2
                        slots = {}

                        def qk_stage(idx):
                            hh, qg, kb = steps[idx]
                            lps, lk = next_mm()
                            S.op('pe', lambda p: p.matmul(lps[:, :], lhsT=kTg[:, hh, kb * 128:(kb + 1) * 128],
                                                          rhs=qTg[:, hh, qg * 512:(qg + 1) * 512], start=True, stop=True),
                                 r=['kTg', 'qTg'], w=[lk])
                            j = idx % 4
                            slots[idx] = j
                            S.op('act', lambda a: a.activation(out=pt_[j][:], in_=lps[:, :], func=AF.Exp, scale=float(128 ** -0.5)),
                                 r=[lk], w=['pt%d' % j])
                            S.op('dve', lambda v: v.tensor_tensor(out=pm_[j][:], in0=pt_[j][:], in1=maskT[:, kb, qg * 512:(qg + 1) * 512],
                                                                  op=ALU.mult), r=['pt%d' % j, 'maskT'], w=['pm%d' % j])

                        def pv_stage(idx):
                            hh, qg, kb = steps[idx]
                            nkb = 8 + 4 * (qg + 1)
                            j = slots.pop(idx)
                            pr = (hh * 2 + qg) % 2
                            aO, aD = ax[2 * pr], ax[2 * pr + 1]
                            kO, kD = 'ax%d' % (2 * pr), 'ax%d' % (2 * pr + 1)
                            S.op('pe', lambda p: p.matmul(aO[:, :], lhsT=vg[:, kb, hh * 128:(hh + 1) * 128], rhs=pm_[j][:],
                                                          start=(kb == 0), stop=(kb == nkb - 1)), r=['vg', 'pm%d' % j], w=[kO])
                            S.op('pe', lambda p: p.matmul(aD[:, :], lhsT=ones_bf[:], rhs=pm_[j][:],
                                                          start=(kb == 0), stop=(kb == nkb - 1)), r=['ones_bf', 'pm%d' % j], w=[kD])
                            if kb == nkb - 1:
                                h = hg * 4 + hh
                                S.op('act', lambda a: a.activation(out=lnd[:], in_=aD[:, :], func=AF.Ln), r=[kD], w=['lnd'])
                                S.op('act', lambda a: a.activation(out=rden[:], in_=lnd[:], func=AF.Exp, scale=-1.0), r=['lnd'], w=['rden'])
                                jo = (hh * 2 + qg) % 2
                                S.op('dve', lambda v: v.tensor_tensor(out=obs[jo][:], in0=aO[:, :], in1=rden[:],
                                                                      op=ALU.mult), r=[kO, 'rden'], w=['obs%d' % jo])
                                S.dma('sp', OBD[h * 128:(h + 1) * 128, qg * 512:(qg + 1) * 512], obs[jo][:], r=['obs%d' % jo], w=[('OBD', h, qg)])

                        for idx in range(len(steps) + LA):
                            if idx % 12 == 0:
                                bg_step()
                            if idx < len(steps):
                                qk_stage(idx)
                            if idx - LA >= 0:
                                pv_stage(idx - LA)
                    bg_drain()
                    S.barrier()
                    with nc.allow_non_contiguous_dma(reason="one-time relayout of the modulation vector"):
                        S.dma('sp', modfm[:, 32:96], modrow_d[0, 2 * D:6 * D].rearrange("(j p) -> p j", p=128), w=['modfm'])
                    S.op('dve', lambda v: v.scalar_tensor_tensor(out=A2[:], in0=modfm[:, 64:80], scalar=1.0, in1=n2g_t[:],
                                                                 op0=ALU.add, op1=ALU.mult), r=['modfm', 'n2g'], w=['A2'])
                    S.barrier()
                    alloc_psum(4, 2, 2)
            s_mask.close()

            with ExitStack() as pg1:
                o_aT = sb(pg1, "o_aT", [128, KC, TOWN], BF16)
                o_bT = sb(pg1, "o_bT", [128, KC, TOWN], BF16)
                S.dma('act', o_bT[:], OBD.rearrange("(h p) t -> p h t", p=128), w=['o_bT'])
                oat = [sb(pg1, "oat%d" % i, [128, D], BF16) for i in range(2)]
                gat = [sb(pg1, "gat%d" % i, [128, 512], BF16) for i in range(2)]
                gbt = [sb(pg1, "gbt%d" % i, [128, 512], BF16) for i in range(2)]
                m1 = sb(pg1, "m1", [128, 512], F32)
                m2 = sb(pg1, "m2", [128, 512], F32)
                mgs = [sb(pg1, "mgs%d" % i, [128, 512], BF16) for i in range(2)]
                alloc_wbuf(pg1, 4)
                for qi in range(8):
                    i = qi % 2
                    S.dma('sp', oat[i][:], OA[qi * 128:(qi + 1) * 128, :], w=['oat%d' % i])

                    def ev(c0, n, tps, tkey, qi=qi):
                        S.op('act', lambda a: a.activation(out=o_aT[:, c0:c0 + n, qi * 128:(qi + 1) * 128],
                                                           in_=tps[:, 0:n * 128].rearrange("p (a b) -> p a b", b=128),
                                                           func=AF.Identity), r=[tkey], w=['o_aT'])
                    to_feature_major(oat[i], 'oat%d' % i, KC, None, 'o_aT', ev)
                for cb in range(4):
                    wa, wak = load_w(w_ba, 0, KC, cb * 512, 512)
                    wd2, wdk = load_w(w_bd, 0, KC, cb * 512, 512)
                    for qi in range(8):
                        i = qi % 2
                        S.dma('sp', gat[i][:], GA[qi * 128:(qi + 1) * 128, cb * 512:(cb + 1) * 512], w=['gat%d' % i])
                        S.dma('act', gbt[i][:], GB[qi * 128:(qi + 1) * 128, cb * 512:(cb + 1) * 512], w=['gbt%d' % i])
                        psa, pka = next_mm()
                        for kc in range(KC):
                            S.op('pe', lambda p: p.matmul(psa[:, :], lhsT=o_aT[:, kc, qi * 128:(qi + 1) * 128], rhs=wa[:, kc, :],
                                                          start=(kc == 0), stop=(kc == KC - 1)), r=['o_aT', wak], w=[pka])
                        psb, pkb = next_mm()
                        for kc in range(KC):
                            S.op('pe', lambda p: p.matmul(psb[:, :], lhsT=o_bT[:, kc, qi * 128:(qi + 1) * 128], rhs=wd2[:, kc, :],
                                                          start=(kc == 0), stop=(kc == KC - 1)), r=['o_bT', wdk], w=[pkb])
                        S.op('dve', lambda v: v.tensor_tensor(out=m1[:], in0=psa[:, :], in1=gat[i][:], op=ALU.mult),
                             r=[pka, 'gat%d' % i], w=['m1'])
                        S.op('dve', lambda v: v.tensor_tensor(out=m2[:], in0=psb[:, :], in1=gbt[i][:], op=ALU.mult),
                             r=[pkb, 'gbt%d' % i], w=['m2'])
                        S.op('pool', lambda g: g.tensor_tensor(out=mgs[i][:], in0=m1[:], in1=m2[:], op=ALU.add),
                             r=['m1', 'm2'], w=['mgs%d' % i])
                        S.dma('sp', MG[qi * 128:(qi + 1) * 128, cb * 512:(cb + 1) * 512], mgs[i][:], r=['mgs%d' % i], w=[('MG', qi)])
                S.barrier()

        with ExitStack() as px:
            x1 = sb(px, "x1", [128, 8, D], F32)
            rowb = sb(px, "rowb", [128, D], F32)
            with ExitStack() as pg2:
                alloc_wbuf(pg2, 2)
                mergedT = sb(pg2, "mergedT", [128, KC, TOWN], BF16)
                mgt = [sb(pg2, "mgt%d" % i, [128, D], BF16) for i in range(2)]
                xres = [sb(pg2, "xres%d" % i, [128, 512], F32) for i in range(2)]
                tmpf = sb(pg2, "tmpf", [128, 512], F32)
                S.dma('act', rowb[:], modrow_d[0, 2 * D:3 * D].partition_broadcast(128), w=['rowb'])
                for qi in range(8):
                    i = qi % 2
                    S.dma('sp', mgt[i][:], MG[qi * 128:(qi + 1) * 128, :], w=['mgt%d' % i])

                    def ev2(c0, n, tps, tkey, qi=qi):
                        S.op('act', lambda a: a.activation(out=mergedT[:, c0:c0 + n, qi * 128:(qi + 1) * 128],
                                                           in_=tps[:, 0:n * 128].rearrange("p (a b) -> p a b", b=128),
                                                           func=AF.Identity), r=[tkey], w=['mergedT'])
                    to_feature_major(mgt[i], 'mgt%d' % i, KC, None, 'mergedT', ev2)

                def evac_mo(cb):
                    def f(tt, ps, pkey):
                        i = tt % 2
                        S.dma('sp', xres[i][:], xs[TOWN + tt * 128:TOWN + (tt + 1) * 128, cb * 512:(cb + 1) * 512], w=['xres%d' % i])
                        S.op('dve', lambda v: v.tensor_tensor(out=tmpf[:], in0=ps[:, :], in1=rowb[:, cb * 512:(cb + 1) * 512], op=ALU.mult),
                             r=[pkey, 'rowb'], w=['tmpf'])
                        S.op('dve', lambda v: v.tensor_tensor(out=x1[:, tt, cb * 512:(cb + 1) * 512], in0=tmpf[:], in1=xres[i][:], op=ALU.add),
                             r=['tmpf', 'xres%d' % i], w=[('x1', tt)])
                    return f
                for cb in range(4):
                    linear(lambda kc, tt: mergedT[:, kc, tt * 128:(tt + 1) * 128], 'mergedT', w_mo, cb * 512, 512, list(range(8)), evac_mo(cb))
                S.barrier()

            with ExitStack() as ph:
                alloc_wbuf(ph, 2, 11, 512)
                h2T = sb(ph, "h2T", [128, KC, TOWN], BF16)
                xn2 = [sb(ph, "xn2_%d" % i, [128, D], BF16) for i in range(2)]
                actT = sb(ph, "actT", [128, 11, TOWN], BF16)
                sg = [sb(ph, "sg%d" % i, [128, 512], F32) for i in range(2)]
                tmp2 = sb(ph, "tmp2", [128, 512], F32)
                gub = [sb(ph, "gub%d" % i, [128, KC, 128], BF16) for i in range(6)]
                rr['gu'] = 0

                def load_gu(c0):
                    i = rr['gu'] % 6
                    rr['gu'] += 1
                    S.dma('pool', gub[i][:], w_gu[:, c0:c0 + 128].rearrange("(kc p) n -> p kc n", p=128), w=['gub%d' % i])
                    return gub[i], 'gub%d' % i
                S.dma('act', rowb[:], modrow_d[0, 5 * D:6 * D].partition_broadcast(128), w=['rowb'])
                for qi in range(8):
                    i = qi % 2
                    norm_to_fm(x1[:, qi, :], ('x1', qi), h2T, 'h2T', qi * 128, A2, sh2, ['A2', 'modfm'],
                               xn2[i], 'xn2_%d' % i, small[:, 16 + i:17 + i])
                cnt_s = 0
                for fb in range(4):
                    for fc in range(11):
                        f0 = fb * 1408 + fc * 128
                        wg, wgk = load_gu(f0)
                        wu, wuk = load_gu(DFF + f0)
                        for tg in range(2):
                            psg, pkg = next_mm()
                            for kc in range(KC):
                                S.op('pe', lambda p: p.matmul(psg[:, :], lhsT=wg[:, kc, 0:128], rhs=h2T[:, kc, tg * 512:(tg + 1) * 512],
                                                              start=(kc == 0), stop=(kc == KC - 1)), r=['h2T', wgk], w=[pkg])
                            psu, pku = next_mm()
                            for kc in range(KC):
                                S.op('pe', lambda p: p.matmul(psu[:, :], lhsT=wu[:, kc, 0:128], rhs=h2T[:, kc, tg * 512:(tg + 1) * 512],
                                                              start=(kc == 0), stop=(kc == KC - 1)), r=['h2T', wuk], w=[pku])
                            j = cnt_s % 2
                            cnt_s += 1
                            S.op('act', lambda a: a.activation(out=sg[j][:], in_=psg[:, :], func=AF.Silu), r=[pkg], w=['sg%d' % j])
                            S.op('dve', lambda v: v.tensor_tensor(out=actT[:, fc, tg * 512:(tg + 1) * 512], in0=psu[:, :], in1=sg[j][:],
                                                                  op=ALU.mult), r=[pku, 'sg%d' % j], w=['actT'])

                    def evac_dn(cb):
                        def f(tt, ps, pkey):
                            S.op('dve', lambda v: v.tensor_tensor(out=tmp2[:], in0=ps[:, :], in1=rowb[:, cb * 512:(cb + 1) * 512], op=ALU.mult),
                                 r=[pkey, 'rowb'], w=['tmp2'])
                            S.op('pool', lambda g: g.tensor_tensor(out=x1[:, tt, cb * 512:(cb + 1) * 512], in0=x1[:, tt, cb * 512:(cb + 1) * 512],
                                                                   in1=tmp2[:], op=ALU.add), r=['tmp2', ('x1', tt)], w=[('x1', tt)])
                        return f
                    for cb in range(4):
                        linear(lambda kc, tt: actT[:, kc, tt * 128:(tt + 1) * 128], 'actT', w_dn, cb * 512, 512, list(range(8)),
                               evac_dn(cb), r0=fb * 1408, nk=11)
                S.barrier()

            with ExitStack() as pi_:
                ot = [sb(pi_, "ot%d" % i, [128, D], F32) for i in range(2)]
                S.dma('act', rowb[:], fng[0, :].partition_broadcast(128), w=['rowb'])
                for qi in range(8):
                    i = qi % 2
                    ssc = small[:, 20 + i:21 + i]
                    S.op('act', lambda a: a.activation(out=junk[:], in_=x1[:, qi, :], func=AF.Square, accum_out=ssc),
                         r=[('x1', qi)], w=['junk', 'small'])
                    rstd_from_ss(ssc, D, 'small')
                    S.op('dve', lambda v: v.scalar_tensor_tensor(out=ot[i][:], in0=x1[:, qi, :], scalar=ssc, in1=rowb[:],
                                                                 op0=ALU.mult, op1=ALU.mult), r=[('x1', qi), 'small', 'rowb'], w=['ot%d' % i])
                    S.dma('sp', out[qi * 128:(qi + 1) * 128, :], ot[i][:], r=['ot%d' % i], w=[('out', qi)])
                S.barrier()
        S.barrier()
    return nc


def _consts(half):
    c = np.zeros((128, 512), np.float32)
    c[:, 0:128] = np.eye(128, dtype=np.float32)
    j = np.arange(128)
    c[:, 128:256] = (j[:, None] <= j[None, :]).astype(np.float32)
    c[:, 256:384] = np.where(j[None, :] <= j[:, None], 0.0, NEG)
    c[:, 384] = 1.0 if half == 1 else 0.0
    c[:, 385] = 0.0 if half == 1 else NEG
    theta = np.float32(500000.0)
    c[:, 400:416] = np.power(theta, -np.arange(0, 32, 2, dtype=np.float32) / np.float32(32))[None, :]
    c[:, 416:424] = np.power(theta, -np.arange(0, 16, 2, dtype=np.float32) / np.float32(16))[None, :]
    c[:, 430:430 + NBISECT + 1] = (2.0 ** -np.arange(NBISECT + 1, dtype=np.float64)).astype(np.float32)[None, :]
    return c


def prep_inputs(inputs, cores=None):
    f = lambda a: np.ascontiguousarray(np.asarray(a))
    x = f(inputs["x"]); c = f(inputs["c"]); pos = f(inputs["positions"]).astype(np.int32)
    shared = {
        "w_ada": f(inputs["w_ada"])[0], "b_ada": f(inputs["b_ada"])[0][None, :], "w_in": f(inputs["w_in"])[0],
        "gate_up": f(inputs["gla_gate_up"])[0], "gate_bias": f(inputs["gla_gate_bias"])[0][None, :],
        "gla_gain": f(inputs["gla_norm_gain"])[0][None, :],
        "w_ba": f(inputs["w_branch_gla"])[0], "w_bd": f(inputs["w_branch_dsa"])[0], "w_mo": f(inputs["w_merge_out"])[0],
        "w_gu": f(inputs["w_ffn_gate_up"])[0], "w_dn": f(inputs["w_ffn_down"])[0],
        "n1g": np.ascontiguousarray(f(inputs["norm1_gain"])[0].reshape(16, 128).T),
        "n2g": np.ascontiguousarray(f(inputs["norm2_gain"])[0].reshape(16, 128).T),
        "fng": f(inputs["final_norm_gain"])[None, :],
    }
    maps = []
    for core in (range(8) if cores is None else cores):
        b, half = core // 2, core % 2
        if half == 1:
            xs = x[b]
            p = pos[b]
        else:
            xs = np.concatenate([np.zeros((TOWN, D), np.float32), x[b, :TOWN]], axis=0)
            p = np.concatenate([pos[b, :TOWN], pos[b, :TOWN]])
        m = dict(shared)
        m["xs"] = np.ascontiguousarray(xs)
        m["cfm"] = np.ascontiguousarray(c[b].reshape(16, 128).T)
        m["posi"] = np.ascontiguousarray(p.reshape(16, 128).T)
        m["cst"] = _consts(half)
        maps.append(m)
    return maps


_NC = None


def kernel(**inputs):
    global _NC
    if _NC is None:
        _NC = build_program()
    maps = prep_inputs(inputs)
    res = run_bass_kernel_spmd(_NC, maps, core_ids=list(range(8)))
    outp = np.zeros((NB, SEQ, D), np.float32)
    for core in range(8):
        b, half = core // 2, core % 2
        outp[b, half * TOWN:(half + 1) * TOWN] = res.results[core]["out"]
    return outp
```
